# Optimizing a Trainium2 kernel written in Bass

```python
import jax, jax.numpy as jnp
from jax import lax
import numpy as np

D_MODEL = 1024
BATCH = 2
SEQ = 16384
DEPTH = 2

GRID_W = 64
CTX_LEN = 256
MLA_HEADS = 8
Q_LORA = 256
KV_LORA = 128
NOPE_DIM = 64
ROPE_DIM = 32
V_DIM = 64
ROPE_THETA = 10000.0
Q_BLOCK = 128
SSD_HEADS = 8
SSD_HEAD_DIM = 64
SSD_INNER = SSD_HEADS * SSD_HEAD_DIM
SSD_GROUPS = 2
SSD_STATE = 64
CONV_W = 5
CHUNK = 128
MLA_COLS = Q_LORA + KV_LORA + ROPE_DIM
XBC_DIM = SSD_INNER + 2 * SSD_GROUPS * SSD_STATE
SSD_COLS = SSD_INNER + XBC_DIM + 2 * SSD_HEADS
IN_COLS = MLA_COLS + SSD_COLS
MIX_WIDTH = MLA_HEADS * V_DIM + SSD_INNER
N_GROUPS = 4
EXPERTS_PER_GROUP = 8
N_EXPERTS = N_GROUPS * EXPERTS_PER_GROUP
TOP_K_INNER = 2
EXPERT_FF = 256
EPS = 1e-6

kernel_name = "hybrid_mla_ssd_hmoe_prefix_dit"


def rmsnorm(x, g):
    xf = x.astype(jnp.float32)
    y = xf * lax.rsqrt(jnp.mean(xf * xf, axis=-1, keepdims=True) + EPS) * g.astype(jnp.float32)
    return y.astype(x.dtype)


def modnorm(x, g, shift, scale):
    return rmsnorm(x, g) * (1 + scale) + shift


def rope2d(x, cos, sin):
    x1, x2 = jnp.split(x, 2, axis=-1)
    return jnp.concatenate([x1 * cos - x2 * sin, x2 * cos + x1 * sin], axis=-1).astype(x.dtype)


def mla_heads(p, q_norm_g, w_qb, kv_norm_g, w_kvb, cos, sin, want_q):
    b, L = p.shape[:2]
    qa, kva, kr = jnp.split(p, [Q_LORA, Q_LORA + KV_LORA], axis=-1)
    kv = (rmsnorm(kva, kv_norm_g) @ w_kvb).reshape(b, L, MLA_HEADS, NOPE_DIM + V_DIM)
    k_nope, v = jnp.split(kv, [NOPE_DIM], axis=-1)
    k_rope = kr if cos is None else rope2d(kr, cos, sin)
    if not want_q:
        return None, None, k_nope, k_rope, v
    q = (rmsnorm(qa, q_norm_g) @ w_qb).reshape(b, L, MLA_HEADS, NOPE_DIM + ROPE_DIM)
    q_nope, q_rope = jnp.split(q, [NOPE_DIM], axis=-1)
    if cos is not None:
        q_rope = rope2d(q_rope, cos[:, None, :], sin[:, None, :])
    return q_nope, q_rope, k_nope, k_rope, v


def block_attention(q_nope, q_rope, k_nope, k_rope, v):
    b, Lq, H, _ = q_nope.shape
    nb = Lq // Q_BLOCK
    scale = (NOPE_DIM + ROPE_DIM) ** -0.5
    qn = jnp.moveaxis(q_nope.reshape(b, nb, Q_BLOCK, H, NOPE_DIM), 1, 0)
    qr = jnp.moveaxis(q_rope.reshape(b, nb, Q_BLOCK, H, ROPE_DIM), 1, 0)

    def one_block(args):
        qn_b, qr_b = args
        s = jnp.einsum('bqhd,bkhd->bhqk', qn_b, k_nope) + jnp.einsum('bqhr,bkr->bhqk', qr_b, k_rope)
        p = jax.nn.softmax(s.astype(jnp.float32) * scale, axis=-1).astype(v.dtype)
        return jnp.einsum('bhqk,bkhd->bqhd', p, v)

    out = lax.map(one_block, (qn, qr))
    return jnp.moveaxis(out, 0, 1).reshape(b, Lq, H * V_DIM)


def dwconv(x, w, bias):
    y = lax.conv_general_dilated(x, w[:, None, :], window_strides=(1,),
                                 padding=[(CONV_W // 2, CONV_W // 2)],
                                 dimension_numbers=('NWC', 'WIO', 'NWC'),
                                 feature_group_count=x.shape[-1])
    return y + bias


def ssd_scan(x, dt, A, Bm, Cm, h0, want_y):
    b, L, H, P = x.shape
    G, N = Bm.shape[2], Bm.shape[3]
    nc = L // CHUNK
    Bh = jnp.repeat(Bm, H // G, axis=2).reshape(b, nc, CHUNK, H, N)
    Ch = jnp.repeat(Cm, H // G, axis=2).reshape(b, nc, CHUNK, H, N)
    dt = dt.reshape(b, nc, CHUNK, H)
    xdt = x.reshape(b, nc, CHUNK, H, P) * dt[..., None]
    acs = jnp.cumsum(dt * A, axis=2)
    a_tot = acs[:, :, -1]
    states = jnp.einsum('bcqhn,bcqh,bcqhp->bchpn', Bh, jnp.exp(a_tot[:, :, None] - acs), xdt)

    def step(h, inp):
        st, at = inp
        return jnp.exp(at)[..., None, None] * h + st, h

    h_final, h_prev = lax.scan(step, h0, (jnp.moveaxis(states, 1, 0), jnp.moveaxis(a_tot, 1, 0)))
    if not want_y:
        return None, h_final
    h_prev = jnp.moveaxis(h_prev, 0, 1)
    seg = acs[:, :, :, None, :] - acs[:, :, None, :, :]
    lower = jnp.tril(jnp.ones((CHUNK, CHUNK), bool))[None, None, :, :, None]
    decay = jnp.exp(jnp.where(lower, seg, -jnp.inf))
    scores = jnp.einsum('bcihn,bcjhn->bcijh', Ch, Bh) * decay
    y = (jnp.einsum('bcijh,bcjhp->bcihp', scores, xdt)
         + jnp.einsum('bcihn,bchpn->bcihp', Ch * jnp.exp(acs)[..., None], h_prev))
    return y.reshape(b, L, H, P), h_final


def ssd_mixer(p_lat, p_ctx, conv_w, conv_b, a_log, dt_bias, d_skip, norm_g, ctx_out):
    def prep(p):
        b, L = p.shape[:2]
        z, xbc, dtr = jnp.split(p, [SSD_INNER, SSD_INNER + XBC_DIM], axis=-1)
        xbc = jax.nn.silu(dwconv(xbc, conv_w, conv_b))
        xs, Bm, Cm = jnp.split(xbc, [SSD_INNER, SSD_INNER + SSD_GROUPS * SSD_STATE], axis=-1)
        xs = xs.reshape(b, L, SSD_HEADS, SSD_HEAD_DIM)
        Bm = Bm.reshape(b, L, SSD_GROUPS, SSD_STATE)
        Cm = Cm.reshape(b, L, SSD_GROUPS, SSD_STATE)
        dt = jax.nn.softplus(dtr.reshape(b, L, 2, SSD_HEADS).astype(jnp.float32) + dt_bias.astype(jnp.float32))
        return z, xs, Bm, Cm, dt

    A = -jnp.exp(a_log.astype(jnp.float32))
    zl, xl, Bl, Cl, dtl = prep(p_lat)
    zc, xc, Bc, Cc, dtc = prep(p_ctx)
    b = p_lat.shape[0]
    h0 = jnp.zeros((b, SSD_HEADS, SSD_HEAD_DIM, SSD_STATE), jnp.float32)
    y_lat = d_skip[:, None] * xl
    y_ctx = d_skip[:, None] * xc if ctx_out else None
    for d in range(2):
        fl = (lambda t: jnp.flip(t, axis=1)) if d == 1 else (lambda t: t)
        yc, hc = ssd_scan(fl(xc), fl(dtc[:, :, d]), A[d], fl(Bc), fl(Cc), h0, ctx_out)
        yl, _ = ssd_scan(fl(xl), fl(dtl[:, :, d]), A[d], fl(Bl), fl(Cl), hc, True)
        y_lat = y_lat + fl(yl)
        if ctx_out:
            y_ctx = y_ctx + fl(yc)

    def gated_norm(y, z):
        y = y.reshape(z.shape).astype(z.dtype) * jax.nn.silu(z)
        return rmsnorm(y, norm_g)

    return gated_norm(y_lat, zl), (gated_norm(y_ctx, zc) if ctx_out else None)


def mixer(h_lat, h_ctx, w_in, q_norm_g, w_qb, kv_norm_g, w_kvb, conv_w, conv_b, a_log, dt_bias,
          d_skip, ssd_norm_g, w_out, cos, sin, ctx_out):
    p_lat = h_lat @ w_in
    p_ctx = h_ctx @ w_in
    m_lat, s_lat = jnp.split(p_lat, [MLA_COLS], axis=-1)
    m_ctx, s_ctx = jnp.split(p_ctx, [MLA_COLS], axis=-1)
    qn_c, qr_c, kn_c, kr_c, v_c = mla_heads(m_ctx, q_norm_g, w_qb, kv_norm_g, w_kvb, None, None, ctx_out)
    qn_l, qr_l, kn_l, kr_l, v_l = mla_heads(m_lat, q_norm_g, w_qb, kv_norm_g, w_kvb, cos, sin, True)
    att_lat = block_attention(qn_l, qr_l,
                              jnp.concatenate([kn_c, kn_l], axis=1),
                              jnp.concatenate([kr_c, kr_l], axis=1),
                              jnp.concatenate([v_c, v_l], axis=1))
    ssd_lat, ssd_ctx = ssd_mixer(s_lat, s_ctx, conv_w, conv_b, a_log, dt_bias, d_skip, ssd_norm_g, ctx_out)
    o_lat = jnp.concatenate([att_lat, ssd_lat.astype(att_lat.dtype)], axis=-1) @ w_out
    if not ctx_out:
        return o_lat, None
    att_ctx = block_attention(qn_c, qr_c, kn_c, kr_c, v_c)
    o_ctx = jnp.concatenate([att_ctx, ssd_ctx.astype(att_ctx.dtype)], axis=-1) @ w_out
    return o_lat, o_ctx


def hier_moe(h, rw1, rb1, rw2, rb2, w_gate, w_up, w_down):
    n = h.shape[0]
    p1 = jax.nn.softmax((h @ rw1 + rb1).astype(jnp.float32), axis=-1)
    p_top, g_idx = lax.top_k(p1, 1)
    logit2 = (h @ rw2 + rb2).astype(jnp.float32).reshape(n, N_GROUPS, EXPERTS_PER_GROUP)
    l_sel = jnp.take_along_axis(logit2, g_idx[:, :, None], axis=1)[:, 0]
    v2, i2 = lax.top_k(jax.nn.softmax(l_sel, axis=-1), TOP_K_INNER)
    v2 = v2 / jnp.sum(v2, axis=-1, keepdims=True)
    within = jnp.sum(jax.nn.one_hot(i2, EXPERTS_PER_GROUP, dtype=jnp.float32) * v2[..., None], axis=1)
    gate = (jax.nn.one_hot(g_idx[:, 0], N_GROUPS, dtype=jnp.float32) * p_top)[:, :, None] * within[:, None, :]
    gate = gate.reshape(n, N_EXPERTS).astype(h.dtype)
    y = jnp.zeros_like(h)
    for e in range(N_EXPERTS):
        y = y + gate[:, e:e + 1] * ((jax.nn.silu(h @ w_gate[e]) * (h @ w_up[e])) @ w_down[e])
    return y


def setup_inputs(seed: int = 0) -> dict:
    key = jax.random.key(seed)
    ks = jax.random.split(key, 32)

    def nrm(k, shape, scale):
        return scale * jax.random.normal(k, shape, jnp.float32)

    dt0 = jnp.exp(jax.random.uniform(ks[17], (DEPTH, 2, SSD_HEADS), jnp.float32, np.log(1e-3), np.log(1e-1)))
    return {
        "x": nrm(ks[0], (BATCH, SEQ, D_MODEL), 1.0),
        "c": nrm(ks[1], (BATCH, D_MODEL), 1.0),
        "ctx": nrm(ks[2], (BATCH, CTX_LEN, D_MODEL), 1.0),
        "c_ctx": nrm(ks[3], (D_MODEL,), 1.0),
        "w_mod": nrm(ks[4], (DEPTH, D_MODEL, 6 * D_MODEL), 0.5 * D_MODEL ** -0.5),
        "b_mod": nrm(ks[5], (DEPTH, 6 * D_MODEL), 0.02),
        "norm1_g": 1.0 + nrm(ks[6], (DEPTH, D_MODEL), 0.02),
        "norm2_g": 1.0 + nrm(ks[7], (DEPTH, D_MODEL), 0.02),
        "w_in": nrm(ks[8], (DEPTH, D_MODEL, IN_COLS), D_MODEL ** -0.5),
        "q_norm_g": 1.0 + nrm(ks[9], (DEPTH, Q_LORA), 0.02),
        "w_qb": nrm(ks[10], (DEPTH, Q_LORA, MLA_HEADS * (NOPE_DIM + ROPE_DIM)), Q_LORA ** -0.5),
        "kv_norm_g": 1.0 + nrm(ks[11], (DEPTH, KV_LORA), 0.02),
        "w_kvb": nrm(ks[12], (DEPTH, KV_LORA, MLA_HEADS * (NOPE_DIM + V_DIM)), KV_LORA ** -0.5),
        "conv_w": nrm(ks[13], (DEPTH, CONV_W, XBC_DIM), CONV_W ** -0.5),
        "conv_b": nrm(ks[14], (DEPTH, XBC_DIM), 0.02),
        "a_log": jnp.log(jax.random.uniform(ks[15], (DEPTH, 2, SSD_HEADS), jnp.float32, 1.0, 16.0)),
        "dt_bias": dt0 + jnp.log(-jnp.expm1(-dt0)),
        "d_skip": 1.0 + nrm(ks[16], (DEPTH, SSD_HEADS), 0.1),
        "ssd_norm_g": 1.0 + nrm(ks[18], (DEPTH, SSD_INNER), 0.02),
        "w_out": nrm(ks[19], (DEPTH, MIX_WIDTH, D_MODEL), MIX_WIDTH ** -0.5),
        "router_w1": nrm(ks[20], (DEPTH, D_MODEL, N_GROUPS), D_MODEL ** -0.5),
        "router_b1": nrm(ks[21], (DEPTH, N_GROUPS), 0.01),
        "router_w2": nrm(ks[22], (DEPTH, D_MODEL, N_EXPERTS), D_MODEL ** -0.5),
        "router_b2": nrm(ks[23], (DEPTH, N_EXPERTS), 0.01),
        "w_gate": nrm(ks[24], (DEPTH, N_EXPERTS, D_MODEL, EXPERT_FF), D_MODEL ** -0.5),
        "w_up": nrm(ks[25], (DEPTH, N_EXPERTS, D_MODEL, EXPERT_FF), D_MODEL ** -0.5),
        "w_down": nrm(ks[26], (DEPTH, N_EXPERTS, EXPERT_FF, D_MODEL), EXPERT_FF ** -0.5),
        "final_g": 1.0 + nrm(ks[27], (D_MODEL,), 0.02),
    }


def reference(x, c, ctx, c_ctx, w_mod, b_mod, norm1_g, norm2_g, w_in, q_norm_g, w_qb, kv_norm_g, w_kvb,
              conv_w, conv_b, a_log, dt_bias, d_skip, ssd_norm_g, w_out, router_w1, router_b1,
              router_w2, router_b2, w_gate, w_up, w_down, final_g):
    b, L, D = x.shape
    rows = L // GRID_W
    row = jnp.repeat(jnp.arange(rows), GRID_W)
    col = jnp.tile(jnp.arange(GRID_W), rows)
    inv_freq = ROPE_THETA ** (-jnp.arange(ROPE_DIM // 4, dtype=jnp.float32) / (ROPE_DIM // 4))
    ang = jnp.concatenate([row[:, None] * inv_freq, col[:, None] * inv_freq], axis=-1)
    cos, sin = jnp.cos(ang), jnp.sin(ang)

    xl, xc = x, ctx
    n_ctx_tok = ctx.shape[0] * ctx.shape[1]
    for l in range(DEPTH):
        ctx_out = l < DEPTH - 1
        mod = jax.nn.silu(c) @ w_mod[l] + b_mod[l]
        sh1, sc1, g1, sh2, sc2, g2 = [m[:, None, :] for m in jnp.split(mod, 6, axis=-1)]
        modc = jax.nn.silu(c_ctx) @ w_mod[l] + b_mod[l]
        sh1c, sc1c, g1c, sh2c, sc2c, g2c = jnp.split(modc, 6)
        h_l = modnorm(xl, norm1_g[l], sh1, sc1)
        h_c = modnorm(xc, norm1_g[l], sh1c, sc1c)
        o_l, o_c = mixer(h_l, h_c, w_in[l], q_norm_g[l], w_qb[l], kv_norm_g[l], w_kvb[l], conv_w[l],
                         conv_b[l], a_log[l], dt_bias[l], d_skip[l], ssd_norm_g[l], w_out[l], cos, sin, ctx_out)
        xl = xl + g1 * o_l
        h2 = modnorm(xl, norm2_g[l], sh2, sc2)
        moe_args = (router_w1[l], router_b1[l], router_w2[l], router_b2[l], w_gate[l], w_up[l], w_down[l])
        if ctx_out:
            xc = xc + g1c * o_c
            h2c = modnorm(xc, norm2_g[l], sh2c, sc2c)
            y = hier_moe(jnp.concatenate([h2c.reshape(-1, D), h2.reshape(-1, D)], axis=0), *moe_args)
            xc = xc + g2c * y[:n_ctx_tok].reshape(xc.shape)
            xl = xl + g2 * y[n_ctx_tok:].reshape(xl.shape)
        else:
            xl = xl + g2 * hier_moe(h2.reshape(-1, D), *moe_args).reshape(xl.shape)
    return rmsnorm(xl, final_g)
```

```python
import contextlib
import numpy as np
import ml_dtypes
import concourse.bass as bass
import concourse.mybir as mybir
from concourse.bass_utils import run_bass_kernel_spmd

F32 = mybir.dt.float32
BF16 = mybir.dt.bfloat16
AF = mybir.ActivationFunctionType
ALU = mybir.AluOpType
AX = mybir.AxisListType
NPBF = ml_dtypes.bfloat16

D = 1024
KC = 8
CTX = 256
H = 8
QL, KVL, ROPE, NOPE, VD = 256, 128, 32, 64, 64
QK = NOPE + ROPE
SSD_IN = 512
NST = 64
IN_COLS = 1712
C_QA, C_KVA, C_KR, C_Z, C_XBC, C_DT = 0, 256, 384, 416, 928, 1696
NE, FF = 32, 256
EPS = 1e-6
GRID_W = 64
SCALE = float(QK) ** -0.5
NEG = -30000.0


class Sem:
    def __init__(self, kb, name):
        self.h = kb.es_top.enter_context(kb.nc.semaphore(name))
        self.n = 0


class Res:
    def __init__(self):
        self.w = {}
        self.r = {}


class Eng:
    def __init__(self, kb, name, be, is_pe=False):
        self.name = name
        self.be = be
        self.is_pe = is_pe
        self.sem = Sem(kb, "s_" + name)
        self.seen = {}


class TT:
    def __init__(self, kb, name, shape, dtype, space="sbuf", kind=None):
        self.kb = kb
        self.name = name
        self.res = Res()
        self.dsem = None
        self.space = space
        if kb.scope_tts and space != "dram":
            kb.scope_tts[-1].append(self)
        if space == "sbuf":
            self.t = kb.es.enter_context(kb.nc.sbuf_tensor(name, list(shape), dtype))
        elif space == "psum":
            self.t = kb.es.enter_context(kb.nc.psum_tensor(name, list(shape), dtype))
        else:
            self.t = kb.nc.dram_tensor(name, list(shape), dtype, kind=kind).ap()

    def __getitem__(self, idx):
        return self.t[idx]

    def get_dsem(self):
        if self.dsem is None:
            if self.kb.free_sems:
                self.dsem = self.kb.free_sems.pop()
            else:
                self.dsem = Sem(self.kb, "d_" + self.name)
        return self.dsem


class KB:
    def __init__(self, nc, es):
        self.nc = nc
        self.es = es
        self.es_top = es
        self.scope_tts = []
        self.free_sems = []
        self.ccsem = None
        self.pe = Eng(self, "pe", nc.tensor, True)
        self.act = Eng(self, "act", nc.scalar)
        self.dve = Eng(self, "dve", nc.vector)
        self.pool = Eng(self, "pool", nc.gpsimd)
        self.sp = Eng(self, "sp", nc.sync)
        self.uid = 0
        self.drams = []

    def name(self, p):
        self.uid += 1
        return "%s_%d" % (p, self.uid)

    def sb(self, shape, dtype, name="t"):
        return TT(self, self.name(name), shape, dtype, "sbuf")

    def ps(self, shape, dtype=F32, name="p"):
        return TT(self, self.name(name), shape, dtype, "psum")

    def dram(self, name, shape, dtype, kind):
        t = TT(self, name, shape, dtype, "dram", kind)
        self.drams.append(t)
        return t

    @contextlib.contextmanager
    def scope(self):
        outer = self.es
        inner = contextlib.ExitStack()
        self.es = inner
        self.scope_tts.append([])
        try:
            yield
        finally:
            tts = self.scope_tts.pop()
            self.barrier(tts)
            for t in tts:
                if t.dsem is not None:
                    self.free_sems.append(t.dsem)
                    t.dsem = None
            inner.close()
            self.es = outer

    def barrier(self, tts=()):
        engs = (self.pe, self.act, self.dve, self.pool, self.sp)
        sems = [e.sem for e in engs] + [t.dsem for t in tts if t.dsem is not None]
        for e in engs:
            for sm in sems:
                if sm is e.sem or sm.n <= 0 or e.seen.get(sm, 0) >= sm.n:
                    continue
                e.be.wait_ge(sm.h, sm.n)
                e.seen[sm] = sm.n

    def _waits(self, eng, reads, writes):
        need = {}
        for r in reads:
            for sm, v in r.res.w.items():
                need[sm] = max(need.get(sm, 0), v)
        for w in writes:
            for sm, v in list(w.res.w.items()) + list(w.res.r.items()):
                need[sm] = max(need.get(sm, 0), v)
        for sm, v in need.items():
            if sm is eng.sem:
                if eng.is_pe:
                    continue
                v = min(v, sm.n)
            if v <= 0 or eng.seen.get(sm, 0) >= v:
                continue
            eng.be.wait_ge(sm.h, v)
            eng.seen[sm] = v

    def op(self, eng, fn, reads=(), writes=(), inc=True):
        self._waits(eng, reads, writes)
        inst = fn(eng.be)
        if inc:
            eng.sem.n += 1
            inst.then_inc(eng.sem.h, 1)
            tick = eng.sem.n
        else:
            tick = eng.sem.n + 1
        for r in reads:
            r.res.r[eng.sem] = max(r.res.r.get(eng.sem, 0), tick)
        for w in writes:
            w.res.w[eng.sem] = max(w.res.w.get(eng.sem, 0), tick)
        return inst

    def dma(self, q, out, in_, dst, src, **kw):
        self._waits(q, [src], [dst])
        owner = dst if dst.space != "dram" else src
        ds = owner.get_dsem()
        inst = q.be.dma_start(out=out, in_=in_, **kw)
        ds.n += 16
        inst.then_inc(ds.h, 16)
        src.res.r[ds] = ds.n
        dst.res.w[ds] = ds.n
        return inst

    def collective(self, in_tt, in_ap, out_tt, out_ap, groups):
        if self.ccsem is None:
            self.ccsem = Sem(self, "ccsem")
        self._waits(self.pool, [in_tt], [out_tt])
        inst = self.pool.be.collective_compute("AllGather", ALU.bypass, replica_groups=groups, ins=[in_ap], outs=[out_ap])
        self.ccsem.n += 1
        inst.then_inc(self.ccsem.h)
        in_tt.res.r[self.ccsem] = self.ccsem.n
        out_tt.res.w[self.ccsem] = self.ccsem.n
        return inst

    def finish(self):
        need = {}
        for t in self.drams:
            for sm, v in t.res.w.items():
                need[sm] = max(need.get(sm, 0), v)
        for sm, v in need.items():
            self.sp.be.wait_ge(sm.h, v)
        for e in (self.pe, self.act, self.dve, self.pool):
            if e.sem.n > 0:
                self.sp.be.wait_ge(e.sem.h, e.sem.n)


def mm(kb, out, lhsT, rhs, start, stop, reads, writes, inc=None):
    if inc is None:
        inc = stop
    return kb.op(kb.pe, lambda e: e.matmul(out, lhsT=lhsT, rhs=rhs, start=start, stop=stop),
                 reads=reads, writes=writes, inc=inc)


def act(kb, out, in_, func, reads, writes, bias=None, scale=1.0, accum_out=None):
    kw = {}
    if bias is not None:
        kw["bias"] = bias
    if accum_out is not None:
        kw["accum_out"] = accum_out
    return kb.op(kb.act, lambda e: e.activation(out=out, in_=in_, func=func, scale=scale, **kw),
                 reads=reads, writes=writes)


def tt(kb, eng, out, in0, in1, op, reads, writes):
    return kb.op(eng, lambda e: e.tensor_tensor(out=out, in0=in0, in1=in1, op=op), reads=reads, writes=writes)


def ts(kb, eng, out, in0, s1, op0, reads, writes, s2=None, op1=None):
    if op1 is None:
        return kb.op(eng, lambda e: e.tensor_scalar(out=out, in0=in0, scalar1=s1, scalar2=None, op0=op0),
                     reads=reads, writes=writes)
    return kb.op(eng, lambda e: e.tensor_scalar(out=out, in0=in0, scalar1=s1, scalar2=s2, op0=op0, op1=op1),
                 reads=reads, writes=writes)


def stt(kb, eng, out, in0, scalar, in1, op0, op1, reads, writes):
    return kb.op(eng, lambda e: e.scalar_tensor_tensor(out=out, in0=in0, scalar=scalar, in1=in1, op0=op0, op1=op1),
                 reads=reads, writes=writes)


def cp(kb, eng, out, in_, reads, writes):
    return kb.op(eng, lambda e: e.tensor_copy(out=out, in_=in_), reads=reads, writes=writes)


def mset(kb, eng, ap, val, writes):
    return kb.op(eng, lambda e: e.memset(ap, val), reads=(), writes=writes)


class Ring:
    def __init__(self, items):
        self.items = items
        self.i = 0

    def next(self):
        t = self.items[self.i % len(self.items)]
        self.i += 1
        return t


def load_consts(kb, ins):
    c = {}
    c["ident_f"] = kb.sb([128, 128], F32, "identf")
    kb.dma(kb.sp, c["ident_f"][:], ins["ident"][:, :], c["ident_f"], ins["ident"])
    c["ident_b"] = kb.sb([128, 128], BF16, "identb")
    kb.dma(kb.pool, c["ident_b"][:], ins["ident"][:, :], c["ident_b"], ins["ident"])
    c["ones_f"] = kb.sb([128, 128], F32, "onesf")
    mset(kb, kb.dve, c["ones_f"][:], 1.0, [c["ones_f"]])
    c["ones_b"] = kb.sb([128, 128], BF16, "onesb")
    mset(kb, kb.dve, c["ones_b"][:], 1.0, [c["ones_b"]])
    c["eps"] = kb.sb([128, 1], F32, "eps")
    mset(kb, kb.dve, c["eps"][:], EPS, [c["eps"]])
    c["one1"] = kb.sb([128, 1], F32, "one1")
    mset(kb, kb.dve, c["one1"][:], 1.0, [c["one1"]])
    c["zero1"] = kb.sb([128, 1], F32, "zero1")
    mset(kb, kb.dve, c["zero1"][:], 0.0, [c["zero1"]])
    return c


def emit_mod(kb, ins, l, cst, sections):
    out = {sec: kb.sb([128, KC, 2], F32, "modT%d" % sec) for sec in sections}
    with kb.scope():
        cT = kb.sb([128, KC, 2], F32, "cT")
        kb.dma(kb.sp, cT[:], ins["cT"][:, :, :], cT, ins["cT"])
        cs = kb.sb([128, KC, 2], F32, "cs")
        act(kb, cs[:], cT[:], AF.Silu, [cT], [cs])
        bm = kb.sb([128, 6 * KC], F32, "bm")
        kb.dma(kb.sp, bm[:], ins["b_mod"][l].rearrange("(c p) -> p c", p=128), bm, ins["b_mod"],
               allow_slow_non_contiguous=True)
        wbufs = Ring([kb.sb([128, KC, 1024], F32, "wmod") for _ in range(2)])
        pm = kb.ps([128, KC, 2], F32, "pmod")
        for sec in sections:
            wt = wbufs.next()
            kb.dma(kb.sp, wt[:], ins["w_mod"][l][:, sec * 1024:(sec + 1) * 1024].rearrange("(k p) n -> p k n", p=128),
                   wt, ins["w_mod"])
            for cc in range(KC):
                for k in range(KC):
                    mm(kb, pm[:, cc, :], wt[:, k, cc * 128:(cc + 1) * 128], cs[:, k, :], k == 0, k == KC - 1,
                       [wt, cs], [pm])
            m = out[sec]
            tt(kb, kb.dve, m[:], pm[:], bm[:, sec * KC:(sec + 1) * KC].unsqueeze(2).broadcast_to([128, KC, 2]),
               ALU.add, [pm, bm], [m])
    return out


def load_col(kb, dram_tt, ap1d, n, name):
    t = kb.sb([128, n], F32, name)
    kb.dma(kb.sp, t[:], ap1d.rearrange("(c p) -> p c", p=128), t, dram_tt, allow_slow_non_contiguous=True)
    return t


def rms_bcast(kb, cst, src_sq_list, n_feat, ncols, psq, out_rstd, reads):
    n = len(src_sq_list)
    for i, (ap, kp) in enumerate(src_sq_list):
        mm(kb, psq[:, :ncols], cst["ones_b"][0:kp, :], ap, i == 0, i == n - 1, reads + [cst["ones_b"]], [psq])
    act(kb, out_rstd[:, :ncols], psq[:, :ncols], AF.Sqrt, [psq, cst["eps"]], [out_rstd],
        bias=cst["eps"][:, 0:1], scale=1.0 / n_feat)
    kb.op(kb.dve, lambda e: e.reciprocal(out=out_rstd[:, :ncols], in_=out_rstd[:, :ncols]),
          reads=[out_rstd], writes=[out_rstd])


def emit_phase_a(kb, I, T, l, with_ctx_q, cst, tri):
    with kb.scope():
        mod = emit_mod(kb, I, l, cst, [0, 1])
        g1 = load_col(kb, I["norm1_g"], I["norm1_g"][l], KC, "n1g")
        gmod = kb.sb([128, KC, 2], F32, "gmod")
        ts(kb, kb.dve, gmod[:], mod[1][:], 1.0, ALU.add, [mod[1]], [gmod])
        tt(kb, kb.dve, gmod[:], gmod[:], g1[:].unsqueeze(2).broadcast_to([128, KC, 2]), ALU.mult, [gmod, g1], [gmod])
        sh = mod[0]

        cw = kb.sb([128, 6, 5], F32, "cw")
        for k in range(5):
            kb.dma(kb.sp, cw[:, :, k], I["conv_w"][l][k].rearrange("(c p) -> p c", p=128), cw, I["conv_w"],
                   allow_slow_non_contiguous=True)
        cb = load_col(kb, I["conv_b"], I["conv_b"][l], 6, "cb")
        dtb = kb.sb([128, 16], F32, "dtb")
        kb.dma(kb.sp, dtb[:], I["dt_bias"][l:l + 1, :].broadcast_to([128, 16]), dtb, I["dt_bias"])
        Aneg = kb.sb([128, 16], F32, "Aneg")
        kb.dma(kb.sp, Aneg[:], I["a_log"][l:l + 1, :].broadcast_to([128, 16]), Aneg, I["a_log"])
        act(kb, Aneg[:], Aneg[:], AF.Exp, [Aneg], [Aneg])
        ts(kb, kb.dve, Aneg[:], Aneg[:], -1.0, ALU.mult, [Aneg], [Aneg])
        hmask = kb.sb([128, 4], F32, "hmask")
        kb.dma(kb.sp, hmask[:], I["hmask"][:, :], hmask, I["hmask"])

        xbc_l = kb.sb([128, 6, T + 4], BF16, "xbcpre_l")
        xbc_c = kb.sb([128, 6, CTX + 4], BF16, "xbcpre_c")
        mset(kb, kb.pool, xbc_c[:], 0.0, [xbc_c])
        sc_w = kb.scope(); sc_w.__enter__()
        win = kb.sb([128, KC, IN_COLS], BF16, "win")
        for k in range(KC):
            kb.dma(kb.pool, win[:, k, :], I["w_in"][l][k * 128:(k + 1) * 128, :], win, I["w_in"])
        wq = kb.sb([128, 2, H, QK], BF16, "wq")
        wqr = kb.sb([128, 2, H, QK], BF16, "wqr")
        wkn = kb.sb([128, H, NOPE], BF16, "wkn")
        wvv = kb.sb([128, H, VD], BF16, "wvv")
        sc_tmp = kb.scope(); sc_tmp.__enter__()
        wq_f = kb.sb([128, 2, H * QK], F32, "wqf")
        kb.dma(kb.sp, wq_f[:], I["w_qb"][l].rearrange("(k p) n -> p k n", p=128), wq_f, I["w_qb"])
        qg = load_col(kb, I["q_norm_g"], I["q_norm_g"][l], 2, "qg")
        mset(kb, kb.pool, wqr[:], 0.0, [wqr])
        for k in range(2):
            wv_ = wq_f[:, k, :].rearrange("p (h c) -> p h c", h=H)
            ts(kb, kb.dve, wq[:, k], wv_, qg[:, k:k + 1], ALU.mult, [wq_f, qg], [wq])
            ts(kb, kb.dve, wqr[:, k, :, 64:80], wv_[:, :, 80:96], qg[:, k:k + 1], ALU.mult, [wq_f, qg], [wqr], s2=-1.0, op1=ALU.mult)
            ts(kb, kb.dve, wqr[:, k, :, 80:96], wv_[:, :, 64:80], qg[:, k:k + 1], ALU.mult, [wq_f, qg], [wqr])
        wkv_f = kb.sb([128, H, 128], F32, "wkvf")
        kb.dma(kb.sp, wkv_f[:], I["w_kvb"][l].rearrange("p (h c) -> p h c", h=H), wkv_f, I["w_kvb"])
        kvg = load_col(kb, I["kv_norm_g"], I["kv_norm_g"][l], 1, "kvg")
        ts(kb, kb.dve, wkn[:], wkv_f[:, :, 0:NOPE], kvg[:, 0:1], ALU.mult, [wkv_f, kvg], [wkn])
        ts(kb, kb.dve, wvv[:], wkv_f[:, :, NOPE:128], kvg[:, 0:1], ALU.mult, [wkv_f, kvg], [wvv])
        sc_tmp.__exit__(None, None, None)
        wkr = kb.sb([128, KC, QK], BF16, "wkr")
        wkrr = kb.sb([128, KC, QK], BF16, "wkrr")
        mset(kb, kb.pool, wkr[:], 0.0, [wkr])
        mset(kb, kb.pool, wkrr[:], 0.0, [wkrr])
        cp(kb, kb.dve, wkr[:, :, 64:96], win[:, :, C_KR:C_KR + 32], [win], [wkr])
        ts(kb, kb.dve, wkrr[:, :, 64:80], win[:, :, C_KR + 16:C_KR + 32], -1.0, ALU.mult, [win], [wkrr])
        cp(kb, kb.dve, wkrr[:, :, 80:96], win[:, :, C_KR:C_KR + 16], [win], [wkrr])
        TB = 512
        xin = Ring([kb.sb([128, KC, TB], F32, "xin") for _ in range(2)])
        sq_r = Ring([kb.sb([128, KC, TB], BF16, "sq") for _ in range(1)])
        rstd_r = Ring([kb.sb([128, TB], F32, "rstd") for _ in range(2)])
        tmp_r = Ring([kb.sb([128, TB], F32, "tmp") for _ in range(2)])
        hT_r = Ring([kb.sb([128, KC, TB], BF16, "hT") for _ in range(2)])
        psq = kb.ps([128, TB], F32, "psq")
        pacc = Ring([kb.ps([128, TB], F32, "pacc") for _ in range(5)])
        pq2 = kb.ps([128, TB], F32, "pq2")
        qaT = kb.sb([128, 2, TB], BF16, "qaT")
        qsq = kb.sb([128, 2, TB], BF16, "qsq")
        rq = kb.sb([128, TB], F32, "rq")
        Crs = kb.sb([QK, TB], F32, "Crs")
        Srs = kb.sb([QK, TB], F32, "Srs")
        ropeC = kb.sb([QK, TB], F32, "ropeC")
        ropeS = kb.sb([QK, TB], F32, "ropeS")
        t1 = Ring([kb.sb([QK, TB], F32, "t1") for _ in range(1)])
        t2 = Ring([kb.sb([QK, TB], F32, "t2") for _ in range(1)])
        qst = Ring([kb.sb([QK, TB], BF16, "qst") for _ in range(4)])
        kst = Ring([kb.sb([NOPE, TB], BF16, "kst") for _ in range(6)])
        krp = Ring([kb.sb([QK, TB], BF16, "krp") for _ in range(2)])
        kvsq = kb.sb([128, TB], BF16, "kvsq")
        rkv = kb.sb([128, TB], F32, "rkv")
        kvn = kb.sb([128, TB], BF16, "kvn")
        vst = Ring([kb.sb([128, 4, 512], BF16, "vst") for _ in range(1)])
        zst = Ring([kb.sb([128, 4, TB], BF16, "zst") for _ in range(1)])
        dtr = kb.sb([128, 4, 16], F32, "dtr")
        dts = Ring([kb.sb([128, 4, 32], F32, "dts") for _ in range(2)])

        def run_group(n, xT_d, xoff, cidx, sfx, tabs, xbcpre, want_q, halo=None):
            nb = (n + TB - 1) // TB
            pre = [None]

            def load_x(bj):
                hl = bj == nb
                ww, oo = (4, 0) if hl else (min(TB, n - bj * TB), bj * TB)
                sr = halo if hl else xT_d
                xt_ = xin.next()
                for k in range(KC):
                    kb.dma(kb.sp, xt_[:, k, :ww], sr.t[k, :, (0 if hl else xoff + oo):(0 if hl else xoff + oo) + ww], xt_, sr)
                return xt_

            for bi in range(nb + (1 if halo is not None else 0)):
                is_halo = bi == nb
                if is_halo:
                    w_, o0 = 4, 0
                    src = halo
                else:
                    w_, o0 = min(TB, n - bi * TB), bi * TB
                    src = xT_d
                sq = sq_r.next(); rstd = rstd_r.next(); hT = hT_r.next()
                if bi == 0:
                    pre[0] = load_x(0)
                xt = pre[0]
                if bi + 1 < nb + (1 if halo is not None else 0):
                    pre[0] = load_x(bi + 1)
                act(kb, sq[:, :, :w_], xt[:, :, :w_], AF.Square, [xt], [sq])
                rms_bcast(kb, cst, [(sq[:, k, :w_], 128) for k in range(KC)], D, w_, psq, rstd, [sq])
                for k in range(KC):
                    tmp = tmp_r.next()
                    stt(kb, kb.dve, tmp[:, :w_], xt[:, k, :w_], gmod[:, k, cidx:cidx + 1], rstd[:, :w_], ALU.mult, ALU.mult,
                        [xt, gmod, rstd], [tmp])
                    act(kb, hT[:, k, :w_], tmp[:, :w_], AF.Identity, [tmp, sh], [hT], bias=sh[:, k, cidx:cidx + 1])

                def proj(pt, M, c0, wtile=None):
                    for k in range(KC):
                        lw = win[:, k, c0:c0 + M] if wtile is None else wtile[:, k, :]
                        mm(kb, pt[0:M, :w_], lw, hT[:, k, :w_], k == 0, k == KC - 1, [win if wtile is None else wtile, hT], [pt])

                for c in range(6):
                    pt = pacc.next()
                    proj(pt, 128, C_XBC + c * 128)
                    if is_halo:
                        tt(kb, kb.dve, xbcpre[:, c, 0:2], pt[:, 0:2], hmask[:, 0:2], ALU.mult, [pt, hmask], [xbcpre])
                        tt(kb, kb.dve, xbcpre[:, c, n + 2:n + 4], pt[:, 2:4], hmask[:, 2:4], ALU.mult, [pt, hmask], [xbcpre])
                    else:
                        act(kb, xbcpre[:, c, 2 + o0:2 + o0 + w_], pt[:, :w_], AF.Copy, [pt], [xbcpre])
                if is_halo:
                    continue
                zt = zst.next()
                for c in range(4):
                    pt = pacc.next()
                    proj(pt, 128, C_Z + c * 128)
                    act(kb, zt[:, c, :w_], pt[:, :w_], AF.Silu, [pt], [zt])
                for c in range(4):
                    kb.dma(kb.act, I["zs" + sfx].t[c, :, o0:o0 + w_], zt[:, c, :w_], I["zs" + sfx], zt)
                ntl = w_ // 128
                for i in range(ntl):
                    pt = pacc.next()
                    for k in range(KC):
                        mm(kb, pt[:, 0:16], hT[:, k, i * 128:(i + 1) * 128], win[:, k, C_DT:C_DT + 16], k == 0, k == KC - 1,
                           [hT, win], [pt])
                    tt(kb, kb.dve, dtr[:, i, :], pt[:, 0:16], dtb[:], ALU.add, [pt, dtb], [dtr])
                dt_ = dts.next()
                act(kb, dtr[:, :ntl, :], dtr[:, :ntl, :], AF.Exp, [dtr], [dtr])
                act(kb, dtr[:, :ntl, :], dtr[:, :ntl, :], AF.Ln, [dtr, cst["one1"]], [dtr], bias=cst["one1"][:, 0:1])
                tt(kb, kb.dve, dt_[:, :ntl, 0:16], dtr[:, :ntl, :], Aneg[:].unsqueeze(1).broadcast_to([128, ntl, 16]), ALU.mult,
                   [dtr, Aneg], [dt_])
                act(kb, dt_[:, :ntl, 16:32], dtr[:, :ntl, :], AF.Ln, [dtr], [dt_])
                kb.dma(kb.sp, I["atok" + sfx].t[o0:o0 + w_, :].rearrange("(i p) c -> p i c", p=128), dt_[:, :ntl, 0:16],
                       I["atok" + sfx], dt_)
                kb.dma(kb.sp, I["ldtok" + sfx].t[o0:o0 + w_, :].rearrange("(i p) c -> p i c", p=128), dt_[:, :ntl, 16:32],
                       I["ldtok" + sfx], dt_)
                if tabs is not None:
                    kb.dma(kb.sp, ropeC[:, :w_], I["ropeC"].t[:, o0:o0 + w_], ropeC, I["ropeC"])
                    kb.dma(kb.sp, ropeS[:, :w_], I["ropeS"].t[:, o0:o0 + w_], ropeS, I["ropeS"])
                else:
                    mset(kb, kb.pool, ropeC[:], 1.0, [ropeC])
                    mset(kb, kb.pool, ropeS[:], 0.0, [ropeS])
                pt = pacc.next()
                proj(pt, 128, C_KVA)
                act(kb, kvsq[:, :w_], pt[:, :w_], AF.Square, [pt], [kvsq])
                rms_bcast(kb, cst, [(kvsq[:, :w_], 128)], KVL, w_, psq, rkv, [kvsq])
                tt(kb, kb.dve, kvn[:, :w_], pt[:, :w_], rkv[:, :w_], ALU.mult, [pt, rkv], [kvn])
                pa = pacc.next()
                proj(pa, QK, 0, wkr)
                pb = pacc.next()
                proj(pb, QK, 0, wkrr)
                a1 = t1.next(); a2 = t2.next(); kr_ = krp.next()
                tt(kb, kb.dve, a1[64:96, :w_], pa[64:96, :w_], ropeC[64:96, :w_], ALU.mult, [pa, ropeC], [a1])
                tt(kb, kb.dve, a2[64:96, :w_], pb[64:96, :w_], ropeS[64:96, :w_], ALU.mult, [pb, ropeS], [a2])
                tt(kb, kb.pool, kr_[64:96, :w_], a1[64:96, :w_], a2[64:96, :w_], ALU.add, [a1, a2], [kr_])
                for h in range(H):
                    kb.dma(kb.pool, I["KT" + sfx].t[h, 64:96, o0:o0 + w_], kr_[64:96, :w_], I["KT" + sfx], kr_)
                for h in range(H):
                    pt = pacc.next()
                    mm(kb, pt[0:NOPE, :w_], wkn[:, h, :], kvn[:, :w_], True, True, [wkn, kvn], [pt])
                    ks_ = kst.next()
                    act(kb, ks_[:, :w_], pt[0:NOPE, :w_], AF.Copy, [pt], [ks_])
                    kb.dma(kb.act, I["KT" + sfx].t[h, 0:NOPE, o0:o0 + w_], ks_[:, :w_], I["KT" + sfx], ks_)
                vs_ = vst.next()
                for i in range(ntl):
                    pt = pacc.next()
                    mm(kb, pt[:, :], kvn[:, i * 128:(i + 1) * 128], wvv[:].rearrange("p h c -> p (h c)"), True, True,
                       [kvn, wvv], [pt])
                    act(kb, vs_[:, i, :], pt[:, :], AF.Copy, [pt], [vs_])
                kb.dma(kb.act, I["V" + sfx].t[o0:o0 + w_, :].rearrange("(i p) c -> p i c", p=128), vs_[:, :ntl, :],
                       I["V" + sfx], vs_)
                if want_q:
                    for c in range(2):
                        pt = pacc.next()
                        proj(pt, 128, C_QA + c * 128)
                        act(kb, qaT[:, c, :w_], pt[:, :w_], AF.Copy, [pt], [qaT])
                        act(kb, qsq[:, c, :w_], pt[:, :w_], AF.Square, [pt], [qsq])
                    rms_bcast(kb, cst, [(qsq[:, c, :w_], 128) for c in range(2)], QL, w_, psq, rq, [qsq])
                    tt(kb, kb.dve, Crs[:, :w_], ropeC[:, :w_], rq[0:QK, :w_], ALU.mult, [ropeC, rq], [Crs])
                    tt(kb, kb.dve, Srs[:, :w_], ropeS[:, :w_], rq[0:QK, :w_], ALU.mult, [ropeS, rq], [Srs])
                    for h in range(H):
                        pa = pacc.next()
                        for c in range(2):
                            mm(kb, pa[0:QK, :w_], wq[:, c, h, :], qaT[:, c, :w_], c == 0, c == 1, [wq, qaT], [pa])
                        for c in range(2):
                            mm(kb, pq2[0:QK, :w_], wqr[:, c, h, :], qaT[:, c, :w_], c == 0, c == 1, [wqr, qaT], [pq2])
                        a1 = t1.next(); a2 = t2.next(); q_ = qst.next()
                        tt(kb, kb.dve, a1[:, :w_], pa[0:QK, :w_], Crs[:, :w_], ALU.mult, [pa, Crs], [a1])
                        tt(kb, kb.dve, a2[:, :w_], pq2[0:QK, :w_], Srs[:, :w_], ALU.mult, [pq2, Srs], [a2])
                        tt(kb, kb.pool, q_[:, :w_], a1[:, :w_], a2[:, :w_], ALU.add, [a1, a2], [q_])
                        kb.dma(kb.pool, I["QT" + sfx].t[h, :, o0:o0 + w_], q_[:, :w_], I["QT" + sfx], q_)

        def ssd_prep(n, sfx, xbcpre):
            nch = n // 128
            cacc = kb.sb([128, n], F32, "cacc" + sfx)
            xbc = kb.sb([128, 6, n], BF16, "xbc" + sfx)
            for c in range(6):
                ts(kb, kb.dve, cacc[:], xbcpre[:, c, 0:n], cw[:, c, 0:1], ALU.mult, [xbcpre, cw], [cacc])
                for k in range(1, 5):
                    stt(kb, kb.dve, cacc[:], xbcpre[:, c, k:k + n], cw[:, c, k:k + 1], cacc[:], ALU.mult, ALU.add,
                        [xbcpre, cw, cacc], [cacc])
                act(kb, xbc[:, c, :], cacc[:], AF.Silu, [cacc, cb], [xbc], bias=cb[:, c:c + 1])
            for c in range(2):
                kb.dma(kb.sp, I["BCT" + sfx].t[c, :, :], xbc[:, 4 + c, :], I["BCT" + sfx], xbc)
            ptr = Ring([kb.ps([128, 640], BF16, "ptr" + sfx) for _ in range(2)])
            xtk = kb.sb([128, nch, 640], BF16, "xtk" + sfx)
            for ci in range(nch):
                p_ = ptr.next()
                for c in range(5):
                    kb.op(kb.pe, lambda e, c=c, p_=p_, ci=ci: e.transpose(p_[:, c * 128:(c + 1) * 128],
                                                                        xbc[:, c, ci * 128:(ci + 1) * 128], cst["ident_b"][:]),
                          reads=[xbc, cst["ident_b"]], writes=[p_], inc=(c == 4))
                cp(kb, kb.dve, xtk[:, ci, :], p_[:, :], [p_], [xtk])
            kb.dma(kb.sp, I["xtok" + sfx].t.rearrange("(i p) c -> p i c", p=128), xtk[:], I["xtok" + sfx], xtk)
            a_t = kb.sb([128, nch, 16], F32, "a_t" + sfx)
            ld_t = kb.sb([128, nch, 16], F32, "ld_t" + sfx)
            kb.dma(kb.sp, a_t[:], I["atok" + sfx].t.rearrange("(i p) c -> p i c", p=128), a_t, I["atok" + sfx])
            kb.dma(kb.sp, ld_t[:], I["ldtok" + sfx].t.rearrange("(i p) c -> p i c", p=128), ld_t, I["ldtok" + sfx])
            pcs = kb.ps([128, nch, 16], F32, "pcs" + sfx)
            ptot = kb.ps([128, nch, 16], F32, "ptot" + sfx)
            for ci in range(nch):
                for d in range(2):
                    mm(kb, pcs[:, ci, d * 8:(d + 1) * 8], tri[d][:], a_t[:, ci, d * 8:(d + 1) * 8], True, True, [tri[d], a_t], [pcs])
                mm(kb, ptot[:, ci, :], cst["ones_f"][:], a_t[:, ci, :], True, True, [cst["ones_f"], a_t], [ptot])
            tot = kb.sb([128, nch, 16], F32, "tot" + sfx)
            cp(kb, kb.dve, tot[:], ptot[:], [ptot], [tot])
            outer = kb.sb([128, nch, 16], F32, "outer" + sfx)
            mset(kb, kb.dve, outer[:], 0.0, [outer])
            for ci in range(nch - 2, -1, -1):
                tt(kb, kb.dve, outer[:, ci, 0:8], outer[:, ci + 1, 0:8], tot[:, ci + 1, 0:8], ALU.add, [outer, tot], [outer])
            for ci in range(1, nch):
                tt(kb, kb.dve, outer[:, ci, 8:16], outer[:, ci - 1, 8:16], tot[:, ci - 1, 8:16], ALU.add, [outer, tot], [outer])
            wexp = kb.sb([128, nch, 16], F32, "wexp" + sfx)
            tt(kb, kb.dve, wexp[:], tot[:], pcs[:], ALU.subtract, [tot, pcs], [wexp])
            tt(kb, kb.dve, wexp[:], wexp[:], outer[:], ALU.add, [wexp, outer], [wexp])
            tt(kb, kb.dve, wexp[:], wexp[:], ld_t[:], ALU.add, [wexp, ld_t], [wexp])
            act(kb, wexp[:], wexp[:], AF.Exp, [wexp], [wexp])
            pS = [kb.ps([128, 512], F32, "pS%d%s" % (d, sfx)) for d in range(2)]
            xw = Ring([kb.sb([128, 2, H, 64], BF16, "xw" + sfx) for _ in range(2)])
            for ci in range(nch):
                xw_ = xw.next()
                tt(kb, kb.dve, xw_[:], xtk[:, ci, 0:512].rearrange("p (h c) -> p h c", h=H).unsqueeze(1).broadcast_to([128, 2, H, 64]),
                   wexp[:, ci, :].rearrange("p (d h) -> p d h", d=2).unsqueeze(3).broadcast_to([128, 2, H, 64]), ALU.mult,
                   [xtk, wexp], [xw_])
                for d in range(2):
                    mm(kb, pS[d][:, :], xtk[:, ci, 512:640], xw_[:, d].rearrange("p h c -> p (h c)"), ci == 0, ci == nch - 1,
                       [xtk, xw_], [pS[d]], inc=True)
            Ssb = kb.sb([128, 2, 512], F32, "Ssb" + sfx)
            for d in range(2):
                cp(kb, kb.dve, Ssb[:, d, :], pS[d][:, :], [pS[d]], [Ssb])
            kb.dma(kb.sp, I["S" + sfx].t[:, :], Ssb[:].rearrange("p d c -> p (d c)"), I["S" + sfx], Ssb)
            at = kb.sb([128, 16], F32, "at" + sfx)
            tt(kb, kb.dve, at[:, 0:8], outer[:, 0, 0:8], tot[:, 0, 0:8], ALU.add, [outer, tot], [at])
            tt(kb, kb.dve, at[:, 8:16], outer[:, nch - 1, 8:16], tot[:, nch - 1, 8:16], ALU.add, [outer, tot], [at])
            kb.dma(kb.sp, I["atot" + sfx].t[0:1, :], at[0:1, :], I["atot" + sfx], at)

        run_group(CTX, I["xcT"], 0, 1, "c", None, xbc_c, with_ctx_q)
        run_group(T, I["xT"], 0, 0, "", True, xbc_l, True, halo=I["xhT"])
        sc_w.__exit__(None, None, None)
        with kb.scope():
            ssd_prep(CTX, "c", xbc_c)
        with kb.scope():
            ssd_prep(T, "", xbc_l)


def build_phase_a(T, l, with_ctx_q):
    nc = bass.Bass("TRN2", target_bir_lowering=False)
    es = contextlib.ExitStack()
    with es:
        kb = KB(nc, es)
        I = {}

        def inp(name, shape, dtype=F32):
            I[name] = kb.dram(name, shape, dtype, "ExternalInput")

        def outp(name, shape, dtype=F32):
            I[name] = kb.dram(name, shape, dtype, "ExternalOutput")

        inp("xT", [KC, 128, T]); inp("xhT", [KC, 128, 4]); inp("hmask", [128, 4]); inp("xcT", [KC, 128, CTX])
        inp("cT", [128, KC, 2]); inp("ident", [128, 128]); inp("tri", [2, 128, 128])
        inp("ropeC", [QK, T]); inp("ropeS", [QK, T])
        inp("w_mod", [2, D, 6 * D]); inp("b_mod", [2, 6 * D]); inp("norm1_g", [2, D])
        inp("w_in", [2, D, IN_COLS]); inp("q_norm_g", [2, QL]); inp("w_qb", [2, QL, H * QK])
        inp("kv_norm_g", [2, KVL]); inp("w_kvb", [2, KVL, H * 128])
        inp("conv_w", [2, 5, 768]); inp("conv_b", [2, 768]); inp("a_log", [2, 16]); inp("dt_bias", [2, 16])
        for sfx, n in (("", T), ("c", CTX)):
            outp("QT" + sfx, [H, QK, n], BF16); outp("KT" + sfx, [H, QK, n], BF16); outp("V" + sfx, [n, 512], BF16)
            outp("zs" + sfx, [4, 128, n], BF16); outp("xtok" + sfx, [n, 640], BF16); outp("BCT" + sfx, [2, 128, n], BF16)
            outp("atok" + sfx, [n, 16]); outp("ldtok" + sfx, [n, 16]); outp("S" + sfx, [128, 1024]); outp("atot" + sfx, [1, 16])

        cst = load_consts(kb, I)
        tri = [kb.sb([128, 128], F32, "tri%d" % d) for d in range(2)]
        for d in range(2):
            kb.dma(kb.sp, tri[d][:], I["tri"][d], tri[d], I["tri"])
        emit_phase_a(kb, I, T, l, with_ctx_q, cst, tri)
        kb.finish()
    return nc


def const_tables():
    j = np.arange(128)
    tri = np.stack([(j[:, None] <= j[None, :]), (j[:, None] >= j[None, :])]).astype(np.float32)
    return np.eye(128, dtype=np.float32), tri


def rope_tables_host(L):
    rows = L // GRID_W
    row = np.repeat(np.arange(rows), GRID_W)
    col = np.tile(np.arange(GRID_W), rows)
    inv = (10000.0 ** (-np.arange(8, dtype=np.float32) / 8)).astype(np.float32)
    ang = np.concatenate([row[:, None] * inv, col[:, None] * inv], -1).astype(np.float32)
    return np.cos(ang).astype(np.float32), np.sin(ang).astype(np.float32)


def fm(x2d):
    n = x2d.shape[0]
    return np.ascontiguousarray(x2d.T.reshape(KC, 128, n))


def host_inputs_a(W, l, x, ctx, c, cc, s, T, cos, sin):
    L = x.shape[0]
    ident, tri = const_tables()
    idx = [s * T - 2, s * T - 1, (s + 1) * T, (s + 1) * T + 1]
    halo = np.zeros((4, D), np.float32)
    hm = np.zeros((128, 4), np.float32)
    for i, t in enumerate(idx):
        if 0 <= t < L:
            halo[i] = x[t]
            hm[:, i] = 1.0
    ropeC = np.ones((QK, T), np.float32)
    ropeS = np.zeros((QK, T), np.float32)
    ropeC[64:80] = cos[s * T:(s + 1) * T].T
    ropeC[80:96] = cos[s * T:(s + 1) * T].T
    ropeS[64:80] = sin[s * T:(s + 1) * T].T
    ropeS[80:96] = sin[s * T:(s + 1) * T].T
    cT = np.stack([c.reshape(KC, 128).T, cc.reshape(KC, 128).T], -1).astype(np.float32)
    d = dict(xT=fm(x[s * T:(s + 1) * T]), xhT=fm(halo), hmask=hm, xcT=fm(ctx), cT=np.ascontiguousarray(cT),
             ident=ident, tri=tri, ropeC=ropeC, ropeS=ropeS)
    for k in ("w_mod", "b_mod", "norm1_g", "w_in", "q_norm_g", "w_qb", "kv_norm_g", "w_kvb", "conv_w", "conv_b"):
        d[k] = np.ascontiguousarray(W[k], dtype=np.float32)
    d["a_log"] = np.ascontiguousarray(W["a_log"], dtype=np.float32).reshape(2, 16)
    d["dt_bias"] = np.ascontiguousarray(W["dt_bias"], dtype=np.float32).reshape(2, 16)
    return d


def emit_attention(kb, cst, nq, QT_d, nk, KT_d, V_d, mix_d, sel65, loaders=None):
    NT = nk // 128
    QB = min(512, nq)
    with kb.scope():
        kT = Ring([kb.sb([QK, nk], BF16, "kT") for _ in range(2)])
        vA = Ring([kb.sb([128, NT, VD + 1], BF16, "vA") for _ in range(2)])
        for v_ in vA.items:
            mset(kb, kb.pool, v_[:], 1.0, [v_])
        qT = Ring([kb.sb([QK, nq], BF16, "qT") for _ in range(2)])
        pss = Ring([kb.ps([128, 1024], F32, "pss") for _ in range(2)])
        pso = Ring([kb.ps([128, 512], F32, "pso") for _ in range(2)])
        pden = kb.ps([64, 512], F32, "pden")
        pT = Ring([kb.sb([128, 1024], BF16, "pT") for _ in range(3)])
        osb = Ring([kb.sb([VD + 1, 512], F32, "osb") for _ in range(2)])
        rec = kb.sb([64, 512], F32, "rec")
        ost = Ring([kb.sb([64, 512], BF16, "ost") for _ in range(2)])
        def load_head(h):
            k_ = kT.next(); v_ = vA.next(); q_ = qT.next()
            if loaders is not None:
                loaders["kv"](h, k_, v_, nk)
            else:
                kb.dma(kb.sp, k_[:, :], KT_d.t[h, :, 0:nk], k_, KT_d)
                for t0 in range(0, NT, 16):
                    t1_ = min(NT, t0 + 16)
                    kb.dma(kb.sp, v_[:, t0:t1_, 0:VD],
                           V_d.t[t0 * 128:t1_ * 128, h * VD:(h + 1) * VD].rearrange("(t p) c -> p t c", p=128), v_, V_d)
            kb.dma(kb.sp, q_[:, :], QT_d.t[h, :, 0:nq], q_, QT_d)
            return k_, v_, q_

        nxt = load_head(0)
        for h in range(H):
            k_, v_, q_ = nxt
            if h + 1 < H:
                nxt = load_head(h + 1)
            for qb in range(nq // QB):
                qs = slice(qb * QB, (qb + 1) * QB)
                po = pso.next()
                npair = NT // 2
                pend = None

                def pv(pr, p_):
                    for j in range(2):
                        kt = 2 * pr + j
                        mm(kb, po[0:VD + 1, :QB], v_[:, kt, :], p_[:, j * 512:j * 512 + QB], kt == 0, kt == NT - 1,
                           [v_, p_], [po], inc=(j == 1))

                for pr in range(npair):
                    ps_ = pss.next()
                    for j in range(2):
                        kt = 2 * pr + j
                        mm(kb, ps_[:, j * 512:j * 512 + QB], k_[:, kt * 128:(kt + 1) * 128], q_[:, qs], True, True,
                           [k_, q_], [ps_], inc=(j == 1))
                    p_ = pT.next()
                    if QB == 512:
                        act(kb, p_[:, :], ps_[:, :], AF.Exp, [ps_], [p_], scale=SCALE)
                    else:
                        for j in range(2):
                            act(kb, p_[:, j * 512:j * 512 + QB], ps_[:, j * 512:j * 512 + QB], AF.Exp, [ps_], [p_], scale=SCALE)
                    if pend is not None:
                        pv(*pend)
                    pend = (pr, p_)
                pv(*pend)
                o_ = osb.next()
                cp(kb, kb.dve, o_[:, :QB], po[0:VD + 1, :QB], [po], [o_])
                mm(kb, pden[:, :QB], sel65[:, :], o_[:, :QB], True, True, [sel65, o_], [pden])
                kb.op(kb.dve, lambda e: e.reciprocal(out=rec[:, :QB], in_=pden[:, :QB]), reads=[pden], writes=[rec])
                s_ = ost.next()
                tt(kb, kb.dve, s_[:, :QB], o_[0:64, :QB], rec[:, :QB], ALU.mult, [o_, rec], [s_])
                kb.dma(kb.pool, mix_d.t[h // 2, (h % 2) * 64:(h % 2) * 64 + 64, qs], s_[:, :QB], mix_d, s_)


def emit_ssd(kb, cst, l, n, sfx, I, mix_d, yfw_d, tri, maskneg, loaders=None):
    nch = n // 128
    import os
    STOP = float(os.environ.get('SSD_STOP', '99'))
    with kb.scope():
        xtk = kb.sb([128, nch, 640], BF16, "xtk")
        kb.dma(kb.sp, xtk[:], I["xtok" + sfx].t.rearrange("(i p) c -> p i c", p=128), xtk, I["xtok" + sfx])
        bct = kb.sb([64, 2, 2, n], BF16, "bct")
        for c in range(2):
            kb.dma(kb.sp, bct[:, c, :, :], I["BCT" + sfx].t[c].rearrange("(g n) t -> n g t", g=2), bct, I["BCT" + sfx])
        a_t = kb.sb([128, nch, 16], F32, "a_t")
        ld_t = kb.sb([128, nch, 16], F32, "ld_t")
        kb.dma(kb.sp, a_t[:], I["atok" + sfx].t.rearrange("(i p) c -> p i c", p=128), a_t, I["atok" + sfx])
        kb.dma(kb.sp, ld_t[:], I["ldtok" + sfx].t.rearrange("(i p) c -> p i c", p=128), ld_t, I["ldtok" + sfx])
        dsk = kb.sb([128, H], F32, "dsk")
        kb.dma(kb.sp, dsk[:], I["d_skip"][l:l + 1, :].broadcast_to([128, H]), dsk, I["d_skip"])
        DI = kb.sb([128, H, 128], F32, "DI")
        tt(kb, kb.dve, DI[:], cst["ident_f"][:].unsqueeze(1).broadcast_to([128, H, 128]),
           dsk[:].unsqueeze(2).broadcast_to([128, H, 128]), ALU.mult, [cst["ident_f"], dsk], [DI])
        sng = kb.sb([64, H], F32, "sng")
        kb.dma(kb.sp, sng[:], I["ssd_norm_g"][l].rearrange("(h p) -> p h", p=64), sng, I["ssd_norm_g"],
               allow_slow_non_contiguous=True)
        zs_v = I["zs" + sfx].t.rearrange("c q t -> (c q) t").rearrange("(h p) t -> p h t", p=64)
        mix_v = mix_d.t[4:8].rearrange("c q t -> (c q) t").rearrange("(h p) t -> p h t", p=64)
        pct = kb.ps([128, 16], F32, "pct")
        pB = kb.ps([128, H, 128], F32, "pB")
        pG = kb.ps([128, 2, 128], F32, "pG")
        py = kb.ps([64, H, 128], F32, "py")
        pSt = kb.ps([64, 512], F32, "pSt")
        psq = kb.ps([64, 128], F32, "psqs")
        R__r = Ring([kb.sb([128, H, 128], F32, "R") for _ in range(2)])
        csl_r = Ring([kb.sb([128, H], F32, "csl") for _ in range(2)])
        dec_r = Ring([kb.sb([128, H], F32, "dec") for _ in range(2)])
        wj_r = Ring([kb.sb([128, H], F32, "wj") for _ in range(2)])
        D1_r = Ring([kb.sb([128, H, 128], F32, "D1") for _ in range(2)])
        E__r = Ring([kb.sb([128, H, 128], F32, "E") for _ in range(2)])
        Cx_r = Ring([kb.sb([128, H, 128], F32, "Cx") for _ in range(2)])
        CexpT_r = Ring([kb.sb([64, H, 128], BF16, "CexpT") for _ in range(2)])
        STf_r = Ring([kb.sb([128, H, 128], F32, "STf") for _ in range(2)])
        STb_r = Ring([kb.sb([128, H, 128], BF16, "STb") for _ in range(2)])
        xw_r = Ring([kb.sb([128, H, 64], BF16, "xw") for _ in range(2)])
        hst = kb.sb([64, H, 64], F32, "hst")
        hpb = kb.sb([64, H, 64], BF16, "hpb")
        ssrc = kb.sb([64, 512], F32, "ssrc")
        atb = kb.sb([128, H], F32, "atb")
        ysb = Ring([kb.sb([64, H, 128], F32, "ysb") for _ in range(2)])
        yfl = Ring([kb.sb([64, H, 128], F32, "yfl") for _ in range(2)])
        zt = Ring([kb.sb([64, H, 128], BF16, "zt") for _ in range(2)])
        yg = kb.sb([64, H, 128], F32, "yg")
        ysq = kb.sb([64, H, 128], BF16, "ysq")
        rs_ = kb.sb([64, 128], F32, "rs")
        yo = Ring([kb.sb([64, H, 128], BF16, "yo") for _ in range(2)])
        hflat = hst[:].rearrange("p h c -> p (h c)")
        for d in range(2):
            if loaders is not None and "state" in loaders:
                loaders["state"](d, hst, hflat, ssrc, atb)
            elif loaders is not None or ("Sch" + sfx) not in I:
                mset(kb, kb.dve, hst[:], 0.0, [hst])
            else:
                for k in range(3, -1, -1):
                    kb.dma(kb.sp, ssrc[:], I["Sch" + sfx].t[d, k], ssrc, I["Sch" + sfx])
                    if k == 3:
                        cp(kb, kb.dve, hflat, ssrc[:], [ssrc], [hst])
                    else:
                        kb.dma(kb.sp, atb[:], I["atch" + sfx].t[d, k:k + 1, :].broadcast_to([128, H]), atb, I["atch" + sfx])
                        act(kb, atb[:], atb[:], AF.Exp, [atb], [atb])
                        tt(kb, kb.dve, hst[:], hst[:], atb[0:64, :].unsqueeze(2).broadcast_to([64, H, 64]), ALU.mult, [hst, atb], [hst])
                        tt(kb, kb.dve, hflat, hflat, ssrc[:], ALU.add, [hst, ssrc], [hst])
            cp(kb, kb.dve, hpb[:], hst[:], [hst], [hpb])
            def stage1(ci, t_out, d=d):
                tsl = slice(ci * 128, (ci + 1) * 128)
                R_ = R__r.next(); csl = csl_r.next(); dec = dec_r.next(); wj = wj_r.next(); D1 = D1_r.next(); E_ = E__r.next(); Cx = Cx_r.next(); CexpT = CexpT_r.next(); STf = STf_r.next(); STb = STb_r.next(); xw = xw_r.next()
                a_d = a_t[:, ci, d * 8:(d + 1) * 8]
                mm(kb, pct[:, 0:8], tri[d][:], a_d, True, True, [tri[d], a_t], [pct])
                yield
                mm(kb, pct[:, 8:16], cst["ones_f"][:], a_d, True, True, [cst["ones_f"], a_t], [pct])
                yield
                tt(kb, kb.pool, R_[:], tri[d][:].unsqueeze(1).broadcast_to([128, H, 128]),
                   a_d.unsqueeze(2).broadcast_to([128, H, 128]), ALU.mult, [tri[d], a_t], [R_])
                yield
                for hh in range(2):
                    mm(kb, pB[:, hh * 4:(hh + 1) * 4, :].rearrange("p h i -> p (h i)"), cst["ones_f"][:],
                       R_[:, hh * 4:(hh + 1) * 4, :].rearrange("p h i -> p (h i)"), True, True, [cst["ones_f"], R_], [pB])
                    yield
                tt(kb, kb.dve, csl[:], pct[:, 0:8], ld_t[:, ci, d * 8:(d + 1) * 8], ALU.subtract, [pct, ld_t], [csl])
                yield
                act(kb, dec[:], pct[:, 8:16], AF.Exp, [pct], [dec])
                yield
                tt(kb, kb.dve, wj[:], pct[:, 8:16], csl[:], ALU.subtract, [pct, csl], [wj])
                yield
                act(kb, wj[:], wj[:], AF.Exp, [wj], [wj])
                yield
                tt(kb, kb.dve, D1[:], pB[:], csl[:].unsqueeze(2).broadcast_to([128, H, 128]), ALU.subtract, [pB, csl], [D1])
                yield
                tt(kb, kb.pool, D1[:], D1[:], maskneg[d][:].unsqueeze(1).broadcast_to([128, H, 128]), ALU.add,
                   [D1, maskneg[d]], [D1])
                yield
                act(kb, E_[:], D1[:], AF.Exp, [D1], [E_])
                yield
                act(kb, Cx[:], pB[:], AF.Exp, [pB], [Cx])
                yield
                tt(kb, kb.dve, CexpT[:].rearrange("p (g h) i -> p g h i", g=2), Cx[0:64].rearrange("p (g h) i -> p g h i", g=2),
                   bct[:, 1, :, tsl].unsqueeze(2).broadcast_to([64, 2, 4, 128]), ALU.mult, [Cx, bct], [CexpT])
                yield
                for g in range(2):
                    mm(kb, pG[:, g, :], bct[:, 0, g, tsl], bct[:, 1, g, tsl], True, True, [bct], [pG])
                    yield
                Ev = E_[:].rearrange("p (g h) i -> p g h i", g=2)
                Gv = pG[:].unsqueeze(2).broadcast_to([128, 2, 4, 128])
                if d == 0:
                    tt(kb, kb.dve, STf[:].rearrange("p (g h) i -> p g h i", g=2), Ev, Gv, ALU.mult, [E_, pG], [STf])
                    yield
                    tt(kb, kb.pool, STb[:], STf[:], DI[:], ALU.add, [STf, DI], [STb])
                    yield
                else:
                    tt(kb, kb.dve, STb[:].rearrange("p (g h) i -> p g h i", g=2), Ev, Gv, ALU.mult, [E_, pG], [STb])
                    yield
                t_out.update(tsl=tsl, wj=wj, dec=dec, CexpT=CexpT, STb=STb, xw=xw)

            def stage2(ci, t_, d=d):
                tsl = t_['tsl']; wj = t_['wj']; dec = t_['dec']; CexpT = t_['CexpT']; STb = t_['STb']; xw = t_['xw']
                for h in range(H):
                    g = h // 4
                    mm(kb, py[:, h, :], xtk[:, ci, h * 64:(h + 1) * 64], STb[:, h, :], True, False, [xtk, STb], [py], inc=False)
                    mm(kb, py[:, h, :], hpb[:, h, :], CexpT[:, h, :], False, True, [hpb, CexpT], [py], inc=True)
                    yield
                tt(kb, kb.pool, xw[:], xtk[:, ci, 0:512].rearrange("p (h c) -> p h c", h=H),
                   wj[:].unsqueeze(2).broadcast_to([128, H, 64]), ALU.mult, [xtk, wj], [xw])
                yield
                for g in range(2):
                    mm(kb, pSt[:, g * 256:(g + 1) * 256], xtk[:, ci, 512 + g * 64:512 + (g + 1) * 64],
                       xw[:, g * 4:(g + 1) * 4, :].rearrange("p h c -> p (h c)"), True, True, [xtk, xw], [pSt])
                    yield
                tt(kb, kb.dve, hst[:], hst[:], dec[0:64, :].unsqueeze(2).broadcast_to([64, H, 64]), ALU.mult, [hst, dec], [hst])
                yield
                tt(kb, kb.dve, hflat, hflat, pSt[:, :], ALU.add, [hst, pSt], [hst])
                yield
                cp(kb, kb.dve, hpb[:], hst[:], [hst], [hpb])
                yield
                if d == 0:
                    y_ = ysb.next()
                    cp(kb, kb.dve, y_[:], py[:], [py], [y_])
                    yield
                    kb.dma(kb.sp, yfw_d.t[ci].rearrange("p (h i) -> p h i", h=H), y_[:], yfw_d, y_)
                    yield
                else:
                    yf_ = yfl.next(); z_ = zt.next()
                    kb.dma(kb.sp, yf_[:], yfw_d.t[ci].rearrange("p (h i) -> p h i", h=H), yf_, yfw_d)
                    yield
                    kb.dma(kb.sp, z_[:], zs_v[:, :, tsl], z_, I["zs" + sfx])
                    yield
                    tt(kb, kb.dve, yg[:], py[:], yf_[:], ALU.add, [py, yf_], [yg])
                    yield
                    tt(kb, kb.pool, yg[:], yg[:], z_[:], ALU.mult, [yg, z_], [yg])
                    yield
                    act(kb, ysq[:], yg[:], AF.Square, [yg], [ysq])
                    yield
                    for h in range(H):
                        mm(kb, psq[:, :], cst["ones_b"][0:64, 0:64], ysq[:, h, :], h == 0, h == H - 1, [cst["ones_b"], ysq], [psq])
                        yield
                    act(kb, rs_[:], psq[:, :], AF.Sqrt, [psq, cst["eps"]], [rs_], bias=cst["eps"][0:64, 0:1], scale=1.0 / SSD_IN)
                    yield
                    kb.op(kb.dve, lambda e: e.reciprocal(out=rs_[:], in_=rs_[:]), reads=[rs_], writes=[rs_])
                    yield
                    tt(kb, kb.dve, yg[:], yg[:], rs_[:].unsqueeze(1).broadcast_to([64, H, 128]), ALU.mult, [yg, rs_], [yg])
                    yield
                    o_ = yo.next()
                    tt(kb, kb.pool, o_[:], yg[:], sng[:].unsqueeze(2).broadcast_to([64, H, 128]), ALU.mult, [yg, sng], [o_])
                    yield
                    kb.dma(kb.sp, mix_v[:, :, tsl], o_[:], mix_d, o_)
                    yield


            order = list(range(nch)) if d == 0 else list(range(nch - 1, -1, -1))

            def zip_run(gens):
                gens = [g for g in gens if g is not None]
                while gens:
                    for g in list(gens):
                        try:
                            next(g)
                        except StopIteration:
                            gens.remove(g)

            pend = None
            for ci in order:
                t_ = {}
                zip_run([stage1(ci, t_), stage2(*pend) if pend is not None else None])
                pend = (ci, t_)
            zip_run([stage2(*pend)])


def emit_tail(kb, cst, l, n, cidx, xin_d, mix_d, x1_d, h2_d, xout_d, I, mod, last):
    TBk = min(512, n)
    nblk = n // TBk
    g1c, sh2, g2c = mod[2], mod[3], mod[5]
    gT = kb.sb([NE, n], BF16, "gT")
    with kb.scope():
        wout = kb.sb([128, KC, D], BF16, "wout")
        for k in range(KC):
            kb.dma(kb.pool, wout[:, k, :], I["w_out"][l][k * 128:(k + 1) * 128, :], wout, I["w_out"])
        n2g = load_col(kb, I["norm2_g"], I["norm2_g"][l], KC, "n2g")
        gmod2 = kb.sb([128, KC], F32, "gmod2")
        ts(kb, kb.dve, gmod2[:], mod[4][:, :, cidx], 1.0, ALU.add, [mod[4]], [gmod2])
        tt(kb, kb.dve, gmod2[:], gmod2[:], n2g[:], ALU.mult, [gmod2, n2g], [gmod2])
        rw = kb.sb([128, KC, 36], F32, "rw")
        kb.dma(kb.sp, rw[:, :, 0:4], I["router_w1"][l].rearrange("(k p) c -> p k c", p=128), rw, I["router_w1"])
        kb.dma(kb.sp, rw[:, :, 4:36], I["router_w2"][l].rearrange("(k p) c -> p k c", p=128), rw, I["router_w2"])
        rb = kb.sb([128, 36], F32, "rb")
        kb.dma(kb.sp, rb[:, 0:4], I["router_b1"][l:l + 1, :].broadcast_to([128, 4]), rb, I["router_b1"])
        kb.dma(kb.sp, rb[:, 4:36], I["router_b2"][l:l + 1, :].broadcast_to([128, 32]), rb, I["router_b2"])
        mixb = Ring([kb.sb([128, KC, TBk], BF16, "mixb") for _ in range(2)])
        xt__rr = Ring([kb.sb([128, KC, TBk], F32, "xt") for _ in range(2)])
        x1_rr = Ring([kb.sb([128, KC, TBk], F32, "x1") for _ in range(2)])
        sq_rr = Ring([kb.sb([128, KC, TBk], BF16, "sq2") for _ in range(2)])
        rstd = kb.sb([128, TBk], F32, "rstd2")
        tmp = kb.sb([128, TBk], F32, "tmp2")
        h2f_rr = Ring([kb.sb([128, KC, TBk], F32, "h2f") for _ in range(2)])
        h2b_rr = Ring([kb.sb([128, KC, TBk], BF16, "h2b") for _ in range(2)])
        po = Ring([kb.ps([128, TBk], F32, "po") for _ in range(2)])
        psq = kb.ps([128, TBk], F32, "psq2")
        plg = kb.ps([128, 36], F32, "plg")
        pgt = kb.ps([NE, 128], F32, "pgt")
        lg = kb.sb([128, 36], F32, "lg")
        sm = kb.sb([128, 16], F32, "sm")
        e1 = kb.sb([128, 4], F32, "e1")
        ohg = kb.sb([128, 4], F32, "ohg")
        l2g = kb.sb([128, 4, 8], F32, "l2g")
        lsel = kb.sb([128, 8], F32, "lsel")
        e2 = kb.sb([128, 8], F32, "e2")
        mk1 = kb.sb([128, 8], F32, "mk1")
        mk2 = kb.sb([128, 8], F32, "mk2")
        lp = kb.sb([128, 8], F32, "lp")
        wi = kb.sb([128, 8], F32, "wi")
        gate = kb.sb([128, 4, 8], F32, "gate")
        for b in range(nblk):
            bs = slice(b * TBk, (b + 1) * TBk)
            mb = mixb.next()
            xt_ = xt__rr.next(); x1 = x1_rr.next(); sq = sq_rr.next(); h2f = h2f_rr.next(); h2b = h2b_rr.next()
            for k in range(KC):
                kb.dma(kb.sp, mb[:, k, :], mix_d.t[k, :, bs], mb, mix_d)
                kb.dma(kb.sp, xt_[:, k, :], xin_d.t[k, :, bs], xt_, xin_d)
            for dc in range(KC):
                p_ = po.next()
                for k in range(KC):
                    mm(kb, p_[:, :], wout[:, k, dc * 128:(dc + 1) * 128], mb[:, k, :], k == 0, k == KC - 1, [wout, mb], [p_])
                stt(kb, kb.dve, x1[:, dc, :], p_[:, :], g1c[:, dc, cidx:cidx + 1], xt_[:, dc, :], ALU.mult, ALU.add,
                    [p_, g1c, xt_], [x1])
            for k in range(KC):
                kb.dma(kb.sp, x1_d.t[k, :, bs], x1[:, k, :], x1_d, x1)
            act(kb, sq[:], x1[:], AF.Square, [x1], [sq])
            rms_bcast(kb, cst, [(sq[:, k, :], 128) for k in range(KC)], D, TBk, psq, rstd, [sq])
            for k in range(KC):
                stt(kb, kb.dve, tmp[:], x1[:, k, :], gmod2[:, k:k + 1], rstd[:], ALU.mult, ALU.mult, [x1, gmod2, rstd], [tmp])
                act(kb, h2f[:, k, :], tmp[:], AF.Identity, [tmp, sh2], [h2f], bias=sh2[:, k, cidx:cidx + 1])
            cp(kb, kb.pool, h2b[:], h2f[:], [h2f], [h2b])
            for k in range(KC):
                kb.dma(kb.sp, h2_d.t[k, :, bs], h2b[:, k, :], h2_d, h2b)
            for i in range(TBk // 128):
                for k in range(KC):
                    mm(kb, plg[:, :], h2f[:, k, i * 128:(i + 1) * 128], rw[:, k, :], k == 0, k == KC - 1, [h2f, rw], [plg])
                tt(kb, kb.dve, lg[:], plg[:, :], rb[:], ALU.add, [plg, rb], [lg])
                R = [lg]
                kb.op(kb.dve, lambda e: e.tensor_reduce(out=sm[:, 0:1], in_=lg[:, 0:4], axis=AX.X, op=ALU.max), reads=R, writes=[sm])
                ts(kb, kb.dve, sm[:, 1:2], sm[:, 0:1], -1.0, ALU.mult, [sm], [sm])
                act(kb, e1[:], lg[:, 0:4], AF.Exp, [lg, sm], [e1, sm], bias=sm[:, 1:2], accum_out=sm[:, 2:3])
                kb.op(kb.dve, lambda e: e.reciprocal(out=sm[:, 3:4], in_=sm[:, 2:3]), reads=[sm], writes=[sm])
                ts(kb, kb.dve, ohg[:], lg[:, 0:4], sm[:, 0:1], ALU.is_equal, [lg, sm], [ohg])
                tt(kb, kb.dve, l2g[:], lg[:, 4:36].rearrange("p (g e) -> p g e", g=4), ohg[:].unsqueeze(2).broadcast_to([128, 4, 8]),
                   ALU.mult, [lg, ohg], [l2g])
                kb.op(kb.dve, lambda e: e.tensor_reduce(out=lsel[:], in_=l2g[:].rearrange("p g e -> p e g"), axis=AX.X, op=ALU.add),
                      reads=[l2g], writes=[lsel])
                kb.op(kb.dve, lambda e: e.tensor_reduce(out=sm[:, 4:5], in_=lsel[:], axis=AX.X, op=ALU.max), reads=[lsel], writes=[sm])
                ts(kb, kb.dve, sm[:, 5:6], sm[:, 4:5], -1.0, ALU.mult, [sm], [sm])
                act(kb, e2[:], lsel[:], AF.Exp, [lsel, sm], [e2], bias=sm[:, 5:6])
                ts(kb, kb.dve, mk1[:], lsel[:], sm[:, 4:5], ALU.is_equal, [lsel, sm], [mk1])
                stt(kb, kb.dve, lp[:], mk1[:], -1.0e30, lsel[:], ALU.mult, ALU.add, [mk1, lsel], [lp])
                kb.op(kb.dve, lambda e: e.tensor_reduce(out=sm[:, 6:7], in_=lp[:], axis=AX.X, op=ALU.max), reads=[lp], writes=[sm])
                ts(kb, kb.dve, mk2[:], lp[:], sm[:, 6:7], ALU.is_equal, [lp, sm], [mk2])
                tt(kb, kb.dve, mk1[:], mk1[:], mk2[:], ALU.add, [mk1, mk2], [mk1])
                tt(kb, kb.dve, wi[:], e2[:], mk1[:], ALU.mult, [e2, mk1], [wi])
                kb.op(kb.dve, lambda e: e.tensor_reduce(out=sm[:, 7:8], in_=wi[:], axis=AX.X, op=ALU.add), reads=[wi], writes=[sm])
                kb.op(kb.dve, lambda e: e.reciprocal(out=sm[:, 8:9], in_=sm[:, 7:8]), reads=[sm], writes=[sm])
                tt(kb, kb.dve, sm[:, 9:10], sm[:, 8:9], sm[:, 3:4], ALU.mult, [sm], [sm])
                ts(kb, kb.dve, wi[:], wi[:], sm[:, 9:10], ALU.mult, [wi, sm], [wi])
                tt(kb, kb.dve, gate[:], ohg[:].unsqueeze(2).broadcast_to([128, 4, 8]), wi[:].unsqueeze(1).broadcast_to([128, 4, 8]),
                   ALU.mult, [ohg, wi], [gate])
                kb.op(kb.pe, lambda e: e.transpose(pgt[:, :], gate[:].rearrange("p g e -> p (g e)"), cst["ident_f"][:]),
                      reads=[gate, cst["ident_f"]], writes=[pgt])
                cp(kb, kb.dve, gT[:, b * TBk + i * 128:b * TBk + (i + 1) * 128], pgt[:, :], [pgt], [gT])
    TH = min(n, 2048)
    with kb.scope():
        sel = kb.sb([NE, NE * 128], BF16, "sel")
        kb.dma(kb.pool, sel[:], I["sel"][:, :], sel, I["sel"])
        yaccs = [kb.sb([128, KC, TBk], F32, "yacc") for _ in range(TH // TBk)]
        h2hs = [kb.sb([128, KC, TBk], BF16, "h2h") for _ in range(TH // TBk)]
        wg = Ring([kb.sb([128, KC, FF], BF16, "wg") for _ in range(2)])
        wu = Ring([kb.sb([128, KC, FF], BF16, "wu") for _ in range(2)])
        wd = Ring([kb.sb([128, 2, D], BF16, "wd") for _ in range(3)])
        pgb = kb.ps([128, TBk], F32, "pgb")
        pgu = Ring([kb.ps([128, TBk], F32, "pgu") for _ in range(4)])
        pyy = Ring([kb.ps([128, TBk], F32, "pyy") for _ in range(2)])
        gbc = Ring([kb.sb([128, TBk], BF16, "gbc") for _ in range(2)])
        sg = Ring([kb.sb([128, TBk], BF16, "sg") for _ in range(2)])
        tu = Ring([kb.sb([128, TBk], BF16, "tu") for _ in range(2)])
        A_ = Ring([kb.sb([128, 2, TBk], BF16, "A") for _ in range(3)])
        x1b_r = Ring([kb.sb([128, KC, TBk], F32, "x1b") for _ in range(2)])
        sqf = kb.sb([128, KC, TBk], BF16, "sqf") if last else None
        rsf = kb.sb([128, TBk], F32, "rsf") if last else None
        fg = load_col(kb, I["final_g"], I["final_g"].t, KC, "fg") if last else None
        def emit_down(e, d_, a_, bs):
            yacc = yaccs[bs.start // TBk]
            bs = slice(0, TBk)
            for dc in range(KC):
                py_ = pyy.next()
                for f in range(2):
                    mm(kb, py_[:, :], d_[:, f, dc * 128:(dc + 1) * 128], a_[:, f, :], f == 0, f == 1, [d_, a_], [py_])
                if e == 0:
                    act(kb, yacc[:, dc, bs], py_[:, :], AF.Copy, [py_], [yacc])
                else:
                    tt(kb, kb.dve, yacc[:, dc, bs], yacc[:, dc, bs], py_[:, :], ALU.add, [yacc, py_], [yacc])

        def load_w(e):
            g_ = wg.next(); u_ = wu.next(); d_ = wd.next()
            kb.dma(kb.pool, g_[:], I["w_gate"][l, e].rearrange("(k p) f -> p k f", p=128), g_, I["w_gate"])
            kb.dma(kb.pool, u_[:], I["w_up"][l, e].rearrange("(k p) f -> p k f", p=128), u_, I["w_up"])
            kb.dma(kb.pool, d_[:], I["w_down"][l, e].rearrange("(f p) c -> p f c", p=128), d_, I["w_down"])
            return g_, u_, d_

        wpre = [None]
        pend_down = None
        def load_h2h(hf, b):
            for k in range(KC):
                kb.dma(kb.sp, h2hs[b][:, k, :], h2_d.t[k, :, hf * TH + b * TBk:hf * TH + (b + 1) * TBk], h2hs[b], h2_d)

        for b in range(TH // TBk):
            load_h2h(0, b)
        for hf in range(n // TH):
            for e in range(NE):
                if wpre[0] is None:
                    wpre[0] = load_w(e)
                g_, u_, d_ = wpre[0]
                wpre[0] = load_w((e + 1) % NE) if (e + 1 < NE or hf + 1 < n // TH) else None
                for b in range(TH // TBk):
                    bs = slice(b * TBk, (b + 1) * TBk)
                    gs = slice(hf * TH + b * TBk, hf * TH + (b + 1) * TBk)
                    mm(kb, pgb[:, :], sel[:, e * 128:(e + 1) * 128], gT[:, gs], True, True, [sel, gT], [pgb])
                    gb_ = gbc.next()
                    act(kb, gb_[:], pgb[:, :], AF.Copy, [pgb], [gb_])
                    a_ = A_.next()
                    for f in range(2):
                        pg_ = pgu.next(); pu_ = pgu.next()
                        for k in range(KC):
                            mm(kb, pg_[:, :], g_[:, k, f * 128:(f + 1) * 128], h2hs[b][:, k, :], k == 0, k == KC - 1, [g_, h2hs[b]], [pg_])
                        for k in range(KC):
                            mm(kb, pu_[:, :], u_[:, k, f * 128:(f + 1) * 128], h2hs[b][:, k, :], k == 0, k == KC - 1, [u_, h2hs[b]], [pu_])
                        s_ = sg.next(); t_ = tu.next()
                        act(kb, s_[:], pg_[:, :], AF.Silu, [pg_], [s_])
                        tt(kb, kb.dve, t_[:], pu_[:, :], s_[:], ALU.mult, [pu_, s_], [t_])
                        tt(kb, kb.pool, a_[:, f, :], t_[:], gb_[:], ALU.mult, [t_, gb_], [a_])
                    if e == NE - 1 and hf + 1 < n // TH:
                        load_h2h(hf + 1, b)
                    if pend_down is not None:
                        emit_down(*pend_down)
                    pend_down = (e, d_, a_, bs)
            emit_down(*pend_down)
            pend_down = None
            for b in range(TH // TBk):
                bs = slice(b * TBk, (b + 1) * TBk)
                gs = slice(hf * TH + b * TBk, hf * TH + (b + 1) * TBk)
                x1b = x1b_r.next()
                for k in range(KC):
                    kb.dma(kb.act, x1b[:, k, :], x1_d.t[k, :, gs], x1b, x1_d)
                for k in range(KC):
                    stt(kb, kb.dve, x1b[:, k, :], yaccs[b][:, k, :], g2c[:, k, cidx:cidx + 1], x1b[:, k, :], ALU.mult, ALU.add,
                        [yaccs[b], g2c, x1b], [x1b])
                if last:
                    act(kb, sqf[:], x1b[:], AF.Square, [x1b], [sqf])
                    rms_bcast(kb, cst, [(sqf[:, k, :], 128) for k in range(KC)], D, TBk, pgb, rsf, [sqf])
                    for k in range(KC):
                        stt(kb, kb.dve, x1b[:, k, :], x1b[:, k, :], fg[:, k:k + 1], rsf[:], ALU.mult, ALU.mult, [x1b, fg, rsf], [x1b])
                for k in range(KC):
                    kb.dma(kb.act, xout_d.t[k, :, gs], x1b[:, k, :], xout_d, x1b)


def emit_phase_b(kb, I, T, l, last, cst, tri, maskneg, sel65, parts=("att", "ssd", "tail"), loaders=None):
    do_ctx = not last
    NK = CTX + 4 * T
    with kb.scope():
        mod = emit_mod(kb, I, l, cst, [2, 3, 4, 5])
        if "ssd" in parts:
            emit_ssd(kb, cst, l, T, "", I, I["mix"], I["yfw"], tri, maskneg, loaders)
            if do_ctx:
                emit_ssd(kb, cst, l, CTX, "c", I, I["mixc"], I["yfwc"], tri, maskneg, None)
        if "att" in parts:
            emit_attention(kb, cst, T, I["QT"], NK, I.get("KTall"), I.get("Vall"), I["mix"], sel65, loaders)
            if do_ctx:
                emit_attention(kb, cst, CTX, I["QTc"], CTX, I.get("KTall"), I.get("Vall"), I["mixc"], sel65, loaders)
        if "tail" in parts:
            emit_tail(kb, cst, l, T, 0, I["xT"], I["mix"], I["x1"], I["h2"], I["xout"], I, mod, last)
            if do_ctx:
                emit_tail(kb, cst, l, CTX, 1, I["xcT"], I["mixc"], I["x1c"], I["h2c"], I["xoutc"], I, mod, False)


def build_phase_b(T, l, last, parts=("att", "ssd", "tail"), debug=False):
    do_ctx = not last
    NK = CTX + 4 * T
    nc = bass.Bass("TRN2", target_bir_lowering=False)
    es = contextlib.ExitStack()
    with es:
        kb = KB(nc, es)
        I = {}

        def inp(name, shape, dtype=F32):
            I[name] = kb.dram(name, shape, dtype, "ExternalInput")

        def outp(name, shape, dtype=F32):
            I[name] = kb.dram(name, shape, dtype, "ExternalOutput")

        def scratch(name, shape, dtype=F32):
            I[name] = kb.dram(name, shape, dtype, "ExternalOutput" if debug else "Internal")

        inp("xT", [KC, 128, T]); inp("cT", [128, KC, 2]); inp("ident", [128, 128]); inp("tri", [2, 128, 128])
        inp("maskneg", [2, 128, 128]); inp("sel", [NE, NE * 128])
        inp("w_mod", [2, D, 6 * D]); inp("b_mod", [2, 6 * D]); inp("norm2_g", [2, D]); inp("w_out", [2, D, D])
        inp("router_w1", [2, D, 4]); inp("router_b1", [2, 4]); inp("router_w2", [2, D, NE]); inp("router_b2", [2, NE])
        inp("w_gate", [2, NE, D, FF]); inp("w_up", [2, NE, D, FF]); inp("w_down", [2, NE, FF, D])
        inp("d_skip", [2, H]); inp("ssd_norm_g", [2, SSD_IN]); inp("final_g", [D])
        inp("QT", [H, QK, T], BF16); inp("KTall", [H, QK, NK], BF16); inp("Vall", [NK, 512], BF16)
        groups = [("", T)] + ([("c", CTX)] if do_ctx else [])
        for sfx, n in groups:
            inp("zs" + sfx, [4, 128, n], BF16); inp("xtok" + sfx, [n, 640], BF16); inp("BCT" + sfx, [2, 128, n], BF16)
            inp("atok" + sfx, [n, 16]); inp("ldtok" + sfx, [n, 16])
            inp("Sch" + sfx, [2, 4, 64, 512]); inp("atch" + sfx, [2, 4, H])
            scratch("mix" + sfx, [KC, 128, n], BF16); scratch("yfw" + sfx, [n // 128, 64, H * 128])
            scratch("x1" + sfx, [KC, 128, n]); scratch("h2" + sfx, [KC, 128, n], BF16)
            outp("xout" + sfx, [KC, 128, n])
        if do_ctx:
            inp("xcT", [KC, 128, CTX]); inp("QTc", [H, QK, CTX], BF16)

        cst = load_consts(kb, I)
        tri = [kb.sb([128, 128], F32, "tri%d" % d) for d in range(2)]
        maskneg = [kb.sb([128, 128], F32, "mneg%d" % d) for d in range(2)]
        for d in range(2):
            kb.dma(kb.sp, tri[d][:], I["tri"][d], tri[d], I["tri"])
            kb.dma(kb.sp, maskneg[d][:], I["maskneg"][d], maskneg[d], I["maskneg"])
        sel65 = kb.sb([VD + 1, 64], F32, "sel65")
        mset(kb, kb.dve, sel65[:], 0.0, [sel65])
        mset(kb, kb.dve, sel65[64:65, :], 1.0, [sel65])
        emit_phase_b(kb, I, T, l, last, cst, tri, maskneg, sel65, parts)
        kb.finish()
    return nc


def pick_state(S_, d):
    out = np.zeros((64, 512), np.float32)
    for h in range(H):
        g = h // 4
        out[:, h * 64:(h + 1) * 64] = S_[g * 64:(g + 1) * 64, d * 512 + h * 64:d * 512 + (h + 1) * 64]
    return out


def host_inputs_b(W, l, last, T, s, xT_own, xcT, cT, QT, QTc, KTall, Vall, grp, Sch, atch):
    ident, tri = const_tables()
    maskneg = ((1.0 - tri) * NEG).astype(np.float32)
    sel = np.zeros((NE, NE, 128), np.float32)
    for e in range(NE):
        sel[e, e, :] = 1.0
    d = dict(xT=xT_own, cT=cT, ident=ident, tri=tri, maskneg=maskneg, sel=sel.reshape(NE, NE * 128),
             QT=QT, KTall=KTall, Vall=Vall)
    for k in ("w_mod", "b_mod", "norm2_g", "w_out", "router_w1", "router_b1", "router_w2", "router_b2",
              "w_gate", "w_up", "w_down", "d_skip", "ssd_norm_g", "final_g"):
        d[k] = np.ascontiguousarray(W[k], dtype=np.float32)
    for sfx in grp:
        for k, v in grp[sfx].items():
            d[k + sfx] = v
        d["Sch" + sfx] = Sch[sfx]
        d["atch" + sfx] = atch[sfx]
    if not last:
        d["xcT"] = xcT
        d["QTc"] = QTc
    return d


_NC_CACHE = {}


def _get_nc(kind, T, l, flag):
    key = (kind, T, l, flag)
    if key not in _NC_CACHE:
        _NC_CACHE[key] = build_phase_a(T, l, flag) if kind == "a" else build_phase_b(T, l, flag)
    return _NC_CACHE[key]


def _run(nc, in_maps):
    res = run_bass_kernel_spmd(nc, in_maps, core_ids=list(range(len(in_maps))))
    return res.results


def forward(W, x, c, ctx, c_ctx, ncores_per_batch=4):
    B, L, _ = x.shape
    R = ncores_per_batch
    T = L // R
    cos, sin = rope_tables_host(L)
    xl = [np.ascontiguousarray(x[b]) for b in range(B)]
    xc = [np.ascontiguousarray(ctx[b]) for b in range(B)]
    xT_own = {}
    for l in range(2):
        last = l == 1
        cores = [(b, s) for b in range(B) for s in range(R)]
        ins_a = [host_inputs_a(W, l, xl[b], xc[b], c[b], c_ctx, s, T, cos, sin) for (b, s) in cores]
        ra = _run(_get_nc("a", T, l, not last), ins_a)
        ins_b = []
        for ci, (b, s) in enumerate(cores):
            rb = [ra[b * R + r] for r in range(R)]
            me = ra[ci]
            KTall = np.concatenate([me["KTc"]] + [q["KT"] for q in rb], axis=2)
            Vall = np.concatenate([me["Vc"]] + [q["V"] for q in rb], axis=0)
            grp = {"": {k: me[k] for k in ("zs", "xtok", "BCT", "atok", "ldtok")}}
            Sch = {"": np.zeros((2, 4, 64, 512), np.float32)}
            atch = {"": np.zeros((2, 4, H), np.float32)}
            chains = ([rb[r] for r in range(s - 1, -1, -1)], [rb[r] for r in range(s + 1, R)])
            for d in range(2):
                srcs = [(q["S"], q["atot"]) for q in chains[d]] + [(me["Sc"], me["atotc"])]
                for k, (S_, at_) in enumerate(srcs):
                    Sch[""][d, k] = pick_state(S_, d)
                    atch[""][d, k] = at_[0, d * 8:(d + 1) * 8]
            if not last:
                grp["c"] = {k: me[k + "c"] for k in ("zs", "xtok", "BCT", "atok", "ldtok")}
                Sch["c"] = np.zeros((2, 4, 64, 512), np.float32)
                atch["c"] = np.zeros((2, 4, H), np.float32)
            ins_b.append(host_inputs_b(W, l, last, T, s, ins_a[ci]["xT"], ins_a[ci]["xcT"], ins_a[ci]["cT"], me["QT"],
                                       me["QTc"], KTall, Vall, grp, Sch, atch))
        rbo = _run(_get_nc("b", T, l, last), ins_b)
        for b in range(B):
            outs = [rbo[b * R + r]["xout"].reshape(D, T).T for r in range(R)]
            xl[b] = np.ascontiguousarray(np.concatenate(outs, axis=0))
            if not last:
                xc[b] = np.ascontiguousarray(rbo[b * R]["xoutc"].reshape(D, CTX).T)
    return np.stack(xl).astype(np.float32)


def kernel(x, c, ctx, c_ctx, w_mod, b_mod, norm1_g, norm2_g, w_in, q_norm_g, w_qb, kv_norm_g, w_kvb,
           conv_w, conv_b, a_log, dt_bias, d_skip, ssd_norm_g, w_out, router_w1, router_b1,
           router_w2, router_b2, w_gate, w_up, w_down, final_g):
    W = dict(w_mod=w_mod, b_mod=b_mod, norm1_g=norm1_g, norm2_g=norm2_g, w_in=w_in, q_norm_g=q_norm_g, w_qb=w_qb,
             kv_norm_g=kv_norm_g, w_kvb=w_kvb, conv_w=conv_w, conv_b=conv_b, a_log=a_log, dt_bias=dt_bias,
             d_skip=d_skip, ssd_norm_g=ssd_norm_g, w_out=w_out, router_w1=router_w1, router_b1=router_b1,
             router_w2=router_w2, router_b2=router_b2, w_gate=w_gate, w_up=w_up, w_down=w_down, final_g=final_g)
    W = {k: np.asarray(v, dtype=np.float32) for k, v in W.items()}
    return forward_fused(W, np.asarray(x, np.float32), np.asarray(c, np.float32), np.asarray(ctx, np.float32),
                         np.asarray(c_ctx, np.float32))


CC_GROUPS = [[0, 1, 2, 3], [4, 5, 6, 7]]


def build_fused(T, R=4):
    nc = bass.Bass("TRN2", target_bir_lowering=False)
    es = contextlib.ExitStack()
    with es:
        kb = KB(nc, es)
        I = {}

        def inp(name, shape, dtype=F32):
            I[name] = kb.dram(name, shape, dtype, "ExternalInput")

        inp("xT", [KC, 128, T]); inp("xhT", [KC, 128, 4]); inp("hmask", [128, 4]); inp("xcT", [KC, 128, CTX])
        inp("cT", [128, KC, 2]); inp("ident", [128, 128]); inp("tri", [2, 128, 128])
        inp("maskneg", [2, 128, 128]); inp("sel", [NE, NE * 128]); inp("chm", [128, 2, R]); inp("hsel", [128, 2, R])
        inp("ropeC", [QK, T]); inp("ropeS", [QK, T])
        inp("w_mod", [2, D, 6 * D]); inp("b_mod", [2, 6 * D]); inp("norm1_g", [2, D]); inp("norm2_g", [2, D])
        inp("w_in", [2, D, IN_COLS]); inp("q_norm_g", [2, QL]); inp("w_qb", [2, QL, H * QK])
        inp("kv_norm_g", [2, KVL]); inp("w_kvb", [2, KVL, H * 128])
        inp("conv_w", [2, 5, 768]); inp("conv_b", [2, 768]); inp("a_log", [2, 16]); inp("dt_bias", [2, 16])
        inp("w_out", [2, D, D])
        inp("router_w1", [2, D, 4]); inp("router_b1", [2, 4]); inp("router_w2", [2, D, NE]); inp("router_b2", [2, NE])
        inp("w_gate", [2, NE, D, FF]); inp("w_up", [2, NE, D, FF]); inp("w_down", [2, NE, FF, D])
        inp("d_skip", [2, H]); inp("ssd_norm_g", [2, SSD_IN]); inp("final_g", [D])
        out_d = kb.dram("out", [KC, 128, T], F32, "ExternalOutput")

        cst = load_consts(kb, I)
        tri = [kb.sb([128, 128], F32, "tri%d" % d) for d in range(2)]
        maskneg = [kb.sb([128, 128], F32, "mneg%d" % d) for d in range(2)]
        for d in range(2):
            kb.dma(kb.sp, tri[d][:], I["tri"][d], tri[d], I["tri"])
            kb.dma(kb.sp, maskneg[d][:], I["maskneg"][d], maskneg[d], I["maskneg"])
        sel65 = kb.sb([VD + 1, 64], F32, "sel65")
        mset(kb, kb.dve, sel65[:], 0.0, [sel65])
        mset(kb, kb.dve, sel65[64:65, :], 1.0, [sel65])
        chm = kb.sb([128, 2, R], F32, "chm")
        kb.dma(kb.sp, chm[:], I["chm"][:, :, :], chm, I["chm"])
        hsel = kb.sb([128, 2, R], F32, "hsel")
        kb.dma(kb.sp, hsel[:], I["hsel"][:, :, :], hsel, I["hsel"])

        x_cur, xh_cur, xc_cur = I["xT"], I["xhT"], I["xcT"]
        for l in range(2):
            last = l == 1
            Il = dict(I)
            Il["xT"], Il["xhT"], Il["xcT"] = x_cur, xh_cur, xc_cur

            def scr(name, shape, dtype=F32, kind="Internal"):
                Il[name] = kb.dram("%s_L%d" % (name, l), shape, dtype, kind)
                return Il[name]

            for sfx, n in (("", T), ("c", CTX)):
                scr("QT" + sfx, [H, QK, n], BF16); scr("KT" + sfx, [H, QK, n], BF16); scr("V" + sfx, [n, 512], BF16)
                scr("zs" + sfx, [4, 128, n], BF16); scr("xtok" + sfx, [n, 640], BF16); scr("BCT" + sfx, [2, 128, n], BF16)
                scr("atok" + sfx, [n, 16]); scr("ldtok" + sfx, [n, 16]); scr("S" + sfx, [128, 1024]); scr("atot" + sfx, [1, 16])
            emit_phase_a(kb, Il, T, l, not last, cst, tri)
            KTg = [scr("KTg%d" % h, [R * QK, T], BF16) for h in range(H)]
            VCH = min(T, 1024)
            Vg = [scr("Vg%d" % c, [R * VCH, 512], BF16) for c in range(T // VCH)]
            Sg = scr("Sg", [R * 128, 1024]); atg = scr("atg", [R, 16])
            kb.collective(Il["S"], Il["S"].t[:, :], Sg, Sg.t[:, :], CC_GROUPS)
            kb.collective(Il["atot"], Il["atot"].t[:, :], atg, atg.t[:, :], CC_GROUPS)
            for h in range(H):
                kb.collective(Il["KT"], Il["KT"].t[h], KTg[h], KTg[h].t[:, :], CC_GROUPS)
            for c in range(T // VCH):
                kb.collective(Il["V"], Il["V"].t[c * VCH:(c + 1) * VCH, :], Vg[c], Vg[c].t[:, :], CC_GROUPS)

            def kv_loader(h, k_, v_, nk, Il=Il, KTg=KTg, Vg=Vg, VCH=VCH):
                kb.dma(kb.sp, k_[:, 0:CTX], Il["KTc"].t[h, :, :], k_, Il["KTc"])
                kb.dma(kb.sp, v_[:, 0:CTX // 128, 0:VD],
                       Il["Vc"].t[:, h * VD:(h + 1) * VD].rearrange("(t p) c -> p t c", p=128), v_, Il["Vc"])
                if nk == CTX:
                    return
                ntl = T // 128
                ncl = VCH // 128
                for r in range(R):
                    kb.dma(kb.sp, k_[:, CTX + r * T:CTX + (r + 1) * T], KTg[h].t[r * QK:(r + 1) * QK, :], k_, KTg[h])
                    for c in range(T // VCH):
                        for t0 in range(0, ncl, 16):
                            t1_ = min(ncl, t0 + 16)
                            kb.dma(kb.sp, v_[:, 2 + r * ntl + c * ncl + t0:2 + r * ntl + c * ncl + t1_, 0:VD],
                                   Vg[c].t[r * VCH + t0 * 128:r * VCH + t1_ * 128, h * VD:(h + 1) * VD]
                                   .rearrange("(t p) c -> p t c", p=128), v_, Vg[c])

            def state_loader(d, hst, hflat, ssrc, atb, Il=Il, Sg=Sg, atg=atg):
                def load_S(src, row0):
                    for g in range(2):
                        kb.dma(kb.sp, ssrc[:, g * 256:(g + 1) * 256],
                               src.t[row0 + g * 64:row0 + (g + 1) * 64, d * 512 + g * 256:d * 512 + (g + 1) * 256], ssrc, src)
                load_S(Il["Sc"], 0)
                cp(kb, kb.dve, hflat, ssrc[:], [ssrc], [hst])
                for r in (range(R) if d == 0 else range(R - 1, -1, -1)):
                    mcol = chm[0:64, d, r:r + 1]
                    kb.dma(kb.sp, atb[:], atg.t[r:r + 1, d * 8:(d + 1) * 8].broadcast_to([128, H]), atb, atg)
                    ts(kb, kb.dve, atb[:], atb[:], chm[:, d, r:r + 1], ALU.mult, [atb, chm], [atb])
                    act(kb, atb[:], atb[:], AF.Exp, [atb], [atb])
                    load_S(Sg, r * 128)
                    tt(kb, kb.dve, hst[:], hst[:], atb[0:64, :].unsqueeze(2).broadcast_to([64, H, 64]), ALU.mult, [hst, atb], [hst])
                    stt(kb, kb.dve, hflat, ssrc[:], mcol, hflat, ALU.mult, ALU.add, [ssrc, chm, hst], [hst])

            for sfx, n in ([("", T)] + ([] if last else [("c", CTX)])):
                scr("mix" + sfx, [KC, 128, n], BF16); scr("yfw" + sfx, [n // 128, 64, H * 128])
                scr("x1" + sfx, [KC, 128, n]); scr("h2" + sfx, [KC, 128, n], BF16)
                if sfx == "" and last:
                    Il["xout"] = out_d
                else:
                    scr("xout" + sfx, [KC, 128, n])
            emit_phase_b(kb, Il, T, l, last, cst, tri, maskneg, sel65,
                         loaders={"kv": kv_loader, "state": state_loader})
            if not last:
                xe = scr("xe", [128, KC * 4]); xeg = scr("xeg", [R * 128, KC * 4]); xh1 = scr("xh1", [KC, 128, 4])
                with kb.scope():
                    et = kb.sb([128, KC, 4], F32, "et")
                    kb.dma(kb.sp, et[:, :, 0:2], Il["xout"].t[:, :, 0:2].rearrange("k p c -> p k c"), et, Il["xout"])
                    kb.dma(kb.sp, et[:, :, 2:4], Il["xout"].t[:, :, T - 2:T].rearrange("k p c -> p k c"), et, Il["xout"])
                    kb.dma(kb.sp, xe.t[:, :], et[:].rearrange("p k c -> p (k c)"), xe, et)
                    kb.collective(xe, xe.t[:, :], xeg, xeg.t[:, :], CC_GROUPS)
                    eg = kb.sb([128, R, KC, 4], F32, "eg")
                    kb.dma(kb.sp, eg[:], xeg.t.rearrange("(r p) (k c) -> p r k c", p=128, c=4), eg, xeg)
                    xh = kb.sb([128, KC, 4], F32, "xh")
                    mset(kb, kb.dve, xh[:], 0.0, [xh])
                    for r in range(R):
                        stt(kb, kb.dve, xh[:, :, 0:2], eg[:, r, :, 2:4], hsel[:, 0, r:r + 1], xh[:, :, 0:2], ALU.mult, ALU.add,
                            [eg, hsel, xh], [xh])
                        stt(kb, kb.dve, xh[:, :, 2:4], eg[:, r, :, 0:2], hsel[:, 1, r:r + 1], xh[:, :, 2:4], ALU.mult, ALU.add,
                            [eg, hsel, xh], [xh])
                    kb.dma(kb.sp, xh1.t.rearrange("k p c -> p k c"), xh[:], xh1, xh)
                x_cur, xh_cur, xc_cur = Il["xout"], xh1, Il["xoutc"]
        kb.finish()
    return nc


def host_inputs_fused(W, x_b, ctx_b, c_b, c_ctx, s, T, R, cos, sin):
    d = host_inputs_a(W, 0, x_b, ctx_b, c_b, c_ctx, s, T, cos, sin)
    ident, tri = const_tables()
    d["maskneg"] = ((1.0 - tri) * NEG).astype(np.float32)
    sel = np.zeros((NE, NE, 128), np.float32)
    for e in range(NE):
        sel[e, e, :] = 1.0
    d["sel"] = sel.reshape(NE, NE * 128)
    chm = np.zeros((128, 2, R), np.float32)
    hs = np.zeros((128, 2, R), np.float32)
    for r in range(R):
        chm[:, 0, r] = 1.0 if r < s else 0.0
        chm[:, 1, r] = 1.0 if r > s else 0.0
        hs[:, 0, r] = 1.0 if r == s - 1 else 0.0
        hs[:, 1, r] = 1.0 if r == s + 1 else 0.0
    d["chm"] = chm
    d["hsel"] = hs
    for k in ("norm2_g", "w_out", "router_w1", "router_b1", "router_w2", "router_b2", "w_gate", "w_up", "w_down",
              "d_skip", "ssd_norm_g", "final_g"):
        d[k] = np.ascontiguousarray(W[k], dtype=np.float32)
    return d


def forward_fused(W, x, c, ctx, c_ctx, R=4):
    B, L, _ = x.shape
    T = L // R
    cos, sin = rope_tables_host(L)
    cores = [(b, s) for b in range(B) for s in range(R)]
    ins = [host_inputs_fused(W, x[b], ctx[b], c[b], c_ctx, s, T, R, cos, sin) for (b, s) in cores]
    key = ("fused", T)
    if key not in _NC_CACHE:
        _NC_CACHE[key] = build_fused(T, R)
    res = _run(_NC_CACHE[key], ins)
    out = np.empty((B, L, D), np.float32)
    for ci, (b, s) in enumerate(cores):
        out[b, s * T:(s + 1) * T] = res[ci]["out"].reshape(D, T).T
    return out
```

```python
import contextlib
import numpy as np
import ml_dtypes
import concourse.bass as bass
import concourse.mybir as mybir
from concourse.bass_utils import run_bass_kernel_spmd

F32 = mybir.dt.float32
BF16 = mybir.dt.bfloat16
AF = mybir.ActivationFunctionType
ALU = mybir.AluOpType
AX = mybir.AxisListType
NPBF = ml_dtypes.bfloat16

D = 1024
KC = 8
CTX = 256
H = 8
QL, KVL, ROPE, NOPE, VD = 256, 128, 32, 64, 64
QK = NOPE + ROPE
SSD_IN = 512
NST = 64
IN_COLS = 1712
C_QA, C_KVA, C_KR, C_Z, C_XBC, C_DT = 0, 256, 384, 416, 928, 1696
NE, FF = 32, 256
EPS = 1e-6
GRID_W = 64
SCALE = float(QK) ** -0.5
NEG = -30000.0


class Sem:
    def __init__(self, kb, name):
        self.h = kb.es_top.enter_context(kb.nc.semaphore(name))
        self.n = 0


class Res:
    def __init__(self):
        self.w = {}
        self.r = {}


class Eng:
    def __init__(self, kb, name, be, is_pe=False):
        self.name = name
        self.be = be
        self.is_pe = is_pe
        self.sem = Sem(kb, "s_" + name)
        self.seen = {}


class TT:
    def __init__(self, kb, name, shape, dtype, space="sbuf", kind=None):
        self.kb = kb
        self.name = name
        self.res = Res()
        self.dsem = None
        self.space = space
        if kb.scope_tts and space != "dram":
            kb.scope_tts[-1].append(self)
        if space == "sbuf":
            self.t = kb.es.enter_context(kb.nc.sbuf_tensor(name, list(shape), dtype))
        elif space == "psum":
            self.t = kb.es.enter_context(kb.nc.psum_tensor(name, list(shape), dtype))
        else:
            self.t = kb.nc.dram_tensor(name, list(shape), dtype, kind=kind).ap()

    def __getitem__(self, idx):
        return self.t[idx]

    def get_dsem(self):
        if self.dsem is None:
            if self.kb.free_sems:
                self.dsem = self.kb.free_sems.pop()
            else:
                self.dsem = Sem(self.kb, "d_" + self.name)
        return self.dsem


class KB:
    def __init__(self, nc, es):
        self.nc = nc
        self.es = es
        self.es_top = es
        self.scope_tts = []
        self.free_sems = []
        self.ccsem = None
        self.pe = Eng(self, "pe", nc.tensor, True)
        self.act = Eng(self, "act", nc.scalar)
        self.dve = Eng(self, "dve", nc.vector)
        self.pool = Eng(self, "pool", nc.gpsimd)
        self.sp = Eng(self, "sp", nc.sync)
        self.uid = 0
        self.drams = []

    def name(self, p):
        self.uid += 1
        return "%s_%d" % (p, self.uid)

    def sb(self, shape, dtype, name="t"):
        return TT(self, self.name(name), shape, dtype, "sbuf")

    def ps(self, shape, dtype=F32, name="p"):
        return TT(self, self.name(name), shape, dtype, "psum")

    def dram(self, name, shape, dtype, kind):
        t = TT(self, name, shape, dtype, "dram", kind)
        self.drams.append(t)
        return t

    @contextlib.contextmanager
    def scope(self):
        outer = self.es
        inner = contextlib.ExitStack()
        self.es = inner
        self.scope_tts.append([])
        try:
            yield
        finally:
            tts = self.scope_tts.pop()
            self.barrier(tts)
            for t in tts:
                if t.dsem is not None:
                    self.free_sems.append(t.dsem)
                    t.dsem = None
            inner.close()
            self.es = outer

    def barrier(self, tts=()):
        engs = (self.pe, self.act, self.dve, self.pool, self.sp)
        sems = [e.sem for e in engs] + [t.dsem for t in tts if t.dsem is not None]
        for e in engs:
            for sm in sems:
                if sm is e.sem or sm.n <= 0 or e.seen.get(sm, 0) >= sm.n:
                    continue
                e.be.wait_ge(sm.h, sm.n)
                e.seen[sm] = sm.n

    def _waits(self, eng, reads, writes):
        need = {}
        for r in reads:
            for sm, v in r.res.w.items():
                need[sm] = max(need.get(sm, 0), v)
        for w in writes:
            for sm, v in list(w.res.w.items()) + list(w.res.r.items()):
                need[sm] = max(need.get(sm, 0), v)
        for sm, v in need.items():
            if sm is eng.sem:
                if eng.is_pe:
                    continue
                v = min(v, sm.n)
            if v <= 0 or eng.seen.get(sm, 0) >= v:
                continue
            eng.be.wait_ge(sm.h, v)
            eng.seen[sm] = v

    def op(self, eng, fn, reads=(), writes=(), inc=True):
        self._waits(eng, reads, writes)
        inst = fn(eng.be)
        if inc:
            eng.sem.n += 1
            inst.then_inc(eng.sem.h, 1)
            tick = eng.sem.n
        else:
            tick = eng.sem.n + 1
        for r in reads:
            r.res.r[eng.sem] = max(r.res.r.get(eng.sem, 0), tick)
        for w in writes:
            w.res.w[eng.sem] = max(w.res.w.get(eng.sem, 0), tick)
        return inst

    def dma(self, q, out, in_, dst, src, **kw):
        self._waits(q, [src], [dst])
        owner = dst if dst.space != "dram" else src
        ds = owner.get_dsem()
        inst = q.be.dma_start(out=out, in_=in_, **kw)
        ds.n += 16
        inst.then_inc(ds.h, 16)
        src.res.r[ds] = ds.n
        dst.res.w[ds] = ds.n
        return inst

    def collective(self, in_tt, in_ap, out_tt, out_ap, groups):
        if self.ccsem is None:
            self.ccsem = Sem(self, "ccsem")
        self._waits(self.pool, [in_tt], [out_tt])
        inst = self.pool.be.collective_compute("AllGather", ALU.bypass, replica_groups=groups, ins=[in_ap], outs=[out_ap])
        self.ccsem.n += 1
        inst.then_inc(self.ccsem.h)
        in_tt.res.r[self.ccsem] = self.ccsem.n
        out_tt.res.w[self.ccsem] = self.ccsem.n
        return inst

    def finish(self):
        need = {}
        for t in self.drams:
            for sm, v in t.res.w.items():
                need[sm] = max(need.get(sm, 0), v)
        for sm, v in need.items():
            self.sp.be.wait_ge(sm.h, v)
        for e in (self.pe, self.act, self.dve, self.pool):
            if e.sem.n > 0:
                self.sp.be.wait_ge(e.sem.h, e.sem.n)


def mm(kb, out, lhsT, rhs, start, stop, reads, writes, inc=None):
    if inc is None:
        inc = stop
    return kb.op(kb.pe, lambda e: e.matmul(out, lhsT=lhsT, rhs=rhs, start=start, stop=stop),
                 reads=reads, writes=writes, inc=inc)


def act(kb, out, in_, func, reads, writes, bias=None, scale=1.0, accum_out=None):
    kw = {}
    if bias is not None:
        kw["bias"] = bias
    if accum_out is not None:
        kw["accum_out"] = accum_out
    return kb.op(kb.act, lambda e: e.activation(out=out, in_=in_, func=func, scale=scale, **kw),
                 reads=reads, writes=writes)


def tt(kb, eng, out, in0, in1, op, reads, writes):
    return kb.op(eng, lambda e: e.tensor_tensor(out=out, in0=in0, in1=in1, op=op), reads=reads, writes=writes)


def ts(kb, eng, out, in0, s1, op0, reads, writes, s2=None, op1=None):
    if op1 is None:
        return kb.op(eng, lambda e: e.tensor_scalar(out=out, in0=in0, scalar1=s1, scalar2=None, op0=op0),
                     reads=reads, writes=writes)
    return kb.op(eng, lambda e: e.tensor_scalar(out=out, in0=in0, scalar1=s1, scalar2=s2, op0=op0, op1=op1),
                 reads=reads, writes=writes)


def stt(kb, eng, out, in0, scalar, in1, op0, op1, reads, writes):
    return kb.op(eng, lambda e: e.scalar_tensor_tensor(out=out, in0=in0, scalar=scalar, in1=in1, op0=op0, op1=op1),
                 reads=reads, writes=writes)


def cp(kb, eng, out, in_, reads, writes):
    return kb.op(eng, lambda e: e.tensor_copy(out=out, in_=in_), reads=reads, writes=writes)


def mset(kb, eng, ap, val, writes):
    return kb.op(eng, lambda e: e.memset(ap, val), reads=(), writes=writes)


class Ring:
    def __init__(self, items):
        self.items = items
        self.i = 0

    def next(self):
        t = self.items[self.i % len(self.items)]
        self.i += 1
        return t


def load_consts(kb, ins):
    c = {}
    c["ident_f"] = kb.sb([128, 128], F32, "identf")
    kb.dma(kb.sp, c["ident_f"][:], ins["ident"][:, :], c["ident_f"], ins["ident"])
    c["ident_b"] = kb.sb([128, 128], BF16, "identb")
    kb.dma(kb.pool, c["ident_b"][:], ins["ident"][:, :], c["ident_b"], ins["ident"])
    c["ones_f"] = kb.sb([128, 128], F32, "onesf")
    mset(kb, kb.dve, c["ones_f"][:], 1.0, [c["ones_f"]])
    c["ones_b"] = kb.sb([128, 128], BF16, "onesb")
    mset(kb, kb.dve, c["ones_b"][:], 1.0, [c["ones_b"]])
    c["eps"] = kb.sb([128, 1], F32, "eps")
    mset(kb, kb.dve, c["eps"][:], EPS, [c["eps"]])
    c["one1"] = kb.sb([128, 1], F32, "one1")
    mset(kb, kb.dve, c["one1"][:], 1.0, [c["one1"]])
    c["zero1"] = kb.sb([128, 1], F32, "zero1")
    mset(kb, kb.dve, c["zero1"][:], 0.0, [c["zero1"]])
    return c


def emit_mod(kb, ins, l, cst, sections):
    out = {sec: kb.sb([128, KC, 2], F32, "modT%d" % sec) for sec in sections}
    with kb.scope():
        cT = kb.sb([128, KC, 2], F32, "cT")
        kb.dma(kb.sp, cT[:], ins["cT"][:, :, :], cT, ins["cT"])
        cs = kb.sb([128, KC, 2], F32, "cs")
        act(kb, cs[:], cT[:], AF.Silu, [cT], [cs])
        bm = kb.sb([128, 6 * KC], F32, "bm")
        kb.dma(kb.sp, bm[:], ins["b_mod"][l].rearrange("(c p) -> p c", p=128), bm, ins["b_mod"],
               allow_slow_non_contiguous=True)
        wbufs = Ring([kb.sb([128, KC, 1024], F32, "wmod") for _ in range(2)])
        pm = kb.ps([128, KC, 2], F32, "pmod")
        for sec in sections:
            wt = wbufs.next()
            kb.dma(kb.sp, wt[:], ins["w_mod"][l][:, sec * 1024:(sec + 1) * 1024].rearrange("(k p) n -> p k n", p=128),
                   wt, ins["w_mod"])
            for cc in range(KC):
                for k in range(KC):
                    mm(kb, pm[:, cc, :], wt[:, k, cc * 128:(cc + 1) * 128], cs[:, k, :], k == 0, k == KC - 1,
                       [wt, cs], [pm])
            m = out[sec]
            tt(kb, kb.dve, m[:], pm[:], bm[:, sec * KC:(sec + 1) * KC].unsqueeze(2).broadcast_to([128, KC, 2]),
               ALU.add, [pm, bm], [m])
    return out


def load_col(kb, dram_tt, ap1d, n, name):
    t = kb.sb([128, n], F32, name)
    kb.dma(kb.sp, t[:], ap1d.rearrange("(c p) -> p c", p=128), t, dram_tt, allow_slow_non_contiguous=True)
    return t


def rms_bcast(kb, cst, src_sq_list, n_feat, ncols, psq, out_rstd, reads):
    n = len(src_sq_list)
    for i, (ap, kp) in enumerate(src_sq_list):
        mm(kb, psq[:, :ncols], cst["ones_b"][0:kp, :], ap, i == 0, i == n - 1, reads + [cst["ones_b"]], [psq])
    act(kb, out_rstd[:, :ncols], psq[:, :ncols], AF.Ln, [psq, cst["eps"]], [out_rstd],
        bias=cst["eps"][:, 0:1], scale=1.0 / n_feat)
    act(kb, out_rstd[:, :ncols], out_rstd[:, :ncols], AF.Exp, [out_rstd], [out_rstd], scale=-0.5)


def emit_phase_a(kb, I, T, l, with_ctx_q, cst, tri):
    with kb.scope():
        mod = emit_mod(kb, I, l, cst, [0, 1])
        g1 = load_col(kb, I["norm1_g"], I["norm1_g"][l], KC, "n1g")
        gmod = kb.sb([128, KC, 2], F32, "gmod")
        ts(kb, kb.dve, gmod[:], mod[1][:], 1.0, ALU.add, [mod[1]], [gmod])
        tt(kb, kb.dve, gmod[:], gmod[:], g1[:].unsqueeze(2).broadcast_to([128, KC, 2]), ALU.mult, [gmod, g1], [gmod])
        sh = mod[0]

        cw = kb.sb([128, 6, 5], F32, "cw")
        for k in range(5):
            kb.dma(kb.sp, cw[:, :, k], I["conv_w"][l][k].rearrange("(c p) -> p c", p=128), cw, I["conv_w"],
                   allow_slow_non_contiguous=True)
        cb = load_col(kb, I["conv_b"], I["conv_b"][l], 6, "cb")
        dtb = kb.sb([128, 16], F32, "dtb")
        kb.dma(kb.sp, dtb[:], I["dt_bias"][l:l + 1, :].broadcast_to([128, 16]), dtb, I["dt_bias"])
        Aneg = kb.sb([128, 16], F32, "Aneg")
        kb.dma(kb.sp, Aneg[:], I["a_log"][l:l + 1, :].broadcast_to([128, 16]), Aneg, I["a_log"])
        act(kb, Aneg[:], Aneg[:], AF.Exp, [Aneg], [Aneg])
        ts(kb, kb.dve, Aneg[:], Aneg[:], -1.0, ALU.mult, [Aneg], [Aneg])
        hmask = kb.sb([128, 4], F32, "hmask")
        kb.dma(kb.sp, hmask[:], I["hmask"][:, :], hmask, I["hmask"])

        xbc_l = kb.sb([128, 6, T + 4], BF16, "xbcpre_l")
        xbc_c = kb.sb([128, 6, CTX + 4], BF16, "xbcpre_c")
        mset(kb, kb.pool, xbc_c[:], 0.0, [xbc_c])
        sc_w = kb.scope(); sc_w.__enter__()
        win = kb.sb([128, KC, IN_COLS], BF16, "win")
        for k in range(KC):
            kb.dma(kb.pool, win[:, k, :], I["w_in"][l][k * 128:(k + 1) * 128, :], win, I["w_in"])
        wq = kb.sb([128, 2, H, QK], BF16, "wq")
        wqr = kb.sb([128, 2, H, QK], BF16, "wqr")
        wkn = kb.sb([128, H, NOPE], BF16, "wkn")
        wvv = kb.sb([128, H, VD], BF16, "wvv")
        sc_tmp = kb.scope(); sc_tmp.__enter__()
        wq_f = kb.sb([128, 2, H * QK], F32, "wqf")
        kb.dma(kb.sp, wq_f[:], I["w_qb"][l].rearrange("(k p) n -> p k n", p=128), wq_f, I["w_qb"])
        qg = load_col(kb, I["q_norm_g"], I["q_norm_g"][l], 2, "qg")
        mset(kb, kb.pool, wqr[:], 0.0, [wqr])
        for k in range(2):
            wv_ = wq_f[:, k, :].rearrange("p (h c) -> p h c", h=H)
            ts(kb, kb.dve, wq[:, k], wv_, qg[:, k:k + 1], ALU.mult, [wq_f, qg], [wq])
            ts(kb, kb.dve, wqr[:, k, :, 64:80], wv_[:, :, 80:96], qg[:, k:k + 1], ALU.mult, [wq_f, qg], [wqr], s2=-1.0, op1=ALU.mult)
            ts(kb, kb.dve, wqr[:, k, :, 80:96], wv_[:, :, 64:80], qg[:, k:k + 1], ALU.mult, [wq_f, qg], [wqr])
        wkv_f = kb.sb([128, H, 128], F32, "wkvf")
        kb.dma(kb.sp, wkv_f[:], I["w_kvb"][l].rearrange("p (h c) -> p h c", h=H), wkv_f, I["w_kvb"])
        kvg = load_col(kb, I["kv_norm_g"], I["kv_norm_g"][l], 1, "kvg")
        ts(kb, kb.dve, wkn[:], wkv_f[:, :, 0:NOPE], kvg[:, 0:1], ALU.mult, [wkv_f, kvg], [wkn])
        ts(kb, kb.dve, wvv[:], wkv_f[:, :, NOPE:128], kvg[:, 0:1], ALU.mult, [wkv_f, kvg], [wvv])
        sc_tmp.__exit__(None, None, None)
        wkr = kb.sb([128, KC, QK], BF16, "wkr")
        wkrr = kb.sb([128, KC, QK], BF16, "wkrr")
        mset(kb, kb.pool, wkr[:], 0.0, [wkr])
        mset(kb, kb.pool, wkrr[:], 0.0, [wkrr])
        cp(kb, kb.dve, wkr[:, :, 64:96], win[:, :, C_KR:C_KR + 32], [win], [wkr])
        ts(kb, kb.dve, wkrr[:, :, 64:80], win[:, :, C_KR + 16:C_KR + 32], -1.0, ALU.mult, [win], [wkrr])
        cp(kb, kb.dve, wkrr[:, :, 80:96], win[:, :, C_KR:C_KR + 16], [win], [wkrr])
        TB = 512
        xin = Ring([kb.sb([128, KC, TB], F32, "xin") for _ in range(2)])
        sq_r = Ring([kb.sb([128, KC, TB], BF16, "sq") for _ in range(1)])
        rstd_r = Ring([kb.sb([128, TB], F32, "rstd") for _ in range(2)])
        tmp_r = Ring([kb.sb([128, TB], F32, "tmp") for _ in range(2)])
        hT_r = Ring([kb.sb([128, KC, TB], BF16, "hT") for _ in range(2)])
        psq = kb.ps([128, TB], F32, "psq")
        pacc = Ring([kb.ps([128, TB], F32, "pacc") for _ in range(5)])
        pq2 = kb.ps([128, TB], F32, "pq2")
        qaT = kb.sb([128, 2, TB], BF16, "qaT")
        qsq = kb.sb([128, 2, TB], BF16, "qsq")
        rq = kb.sb([128, TB], F32, "rq")
        Crs = kb.sb([QK, TB], F32, "Crs")
        Srs = kb.sb([QK, TB], F32, "Srs")
        ropeC = kb.sb([QK, TB], F32, "ropeC")
        ropeS = kb.sb([QK, TB], F32, "ropeS")
        t1 = Ring([kb.sb([QK, TB], F32, "t1") for _ in range(1)])
        t2 = Ring([kb.sb([QK, TB], F32, "t2") for _ in range(1)])
        qst = Ring([kb.sb([QK, TB], BF16, "qst") for _ in range(4)])
        kst = Ring([kb.sb([NOPE, TB], BF16, "kst") for _ in range(6)])
        krp = Ring([kb.sb([QK, TB], BF16, "krp") for _ in range(2)])
        kvsq = kb.sb([128, TB], BF16, "kvsq")
        rkv = kb.sb([128, TB], F32, "rkv")
        kvn = kb.sb([128, TB], BF16, "kvn")
        vst = Ring([kb.sb([128, 4, 512], BF16, "vst") for _ in range(1)])
        zst = Ring([kb.sb([128, 4, TB], BF16, "zst") for _ in range(1)])
        dtr = kb.sb([128, 4, 16], F32, "dtr")
        dts = Ring([kb.sb([128, 4, 32], F32, "dts") for _ in range(2)])

        def run_group(n, xT_d, xoff, cidx, sfx, tabs, xbcpre, want_q, halo=None):
            nb = (n + TB - 1) // TB
            pre = [None]

            def load_x(bj):
                hl = bj == nb
                ww, oo = (4, 0) if hl else (min(TB, n - bj * TB), bj * TB)
                sr = halo if hl else xT_d
                xt_ = xin.next()
                for k in range(KC):
                    kb.dma(kb.sp, xt_[:, k, :ww], sr.t[k, :, (0 if hl else xoff + oo):(0 if hl else xoff + oo) + ww], xt_, sr)
                return xt_

            for bi in range(nb + (1 if halo is not None else 0)):
                is_halo = bi == nb
                if is_halo:
                    w_, o0 = 4, 0
                    src = halo
                else:
                    w_, o0 = min(TB, n - bi * TB), bi * TB
                    src = xT_d
                sq = sq_r.next(); rstd = rstd_r.next(); hT = hT_r.next()
                if bi == 0:
                    pre[0] = load_x(0)
                xt = pre[0]
                if bi + 1 < nb + (1 if halo is not None else 0):
                    pre[0] = load_x(bi + 1)
                act(kb, sq[:, :, :w_], xt[:, :, :w_], AF.Square, [xt], [sq])
                rms_bcast(kb, cst, [(sq[:, k, :w_], 128) for k in range(KC)], D, w_, psq, rstd, [sq])
                for k in range(KC):
                    tmp = tmp_r.next()
                    stt(kb, kb.dve, tmp[:, :w_], xt[:, k, :w_], gmod[:, k, cidx:cidx + 1], rstd[:, :w_], ALU.mult, ALU.mult,
                        [xt, gmod, rstd], [tmp])
                    act(kb, hT[:, k, :w_], tmp[:, :w_], AF.Identity, [tmp, sh], [hT], bias=sh[:, k, cidx:cidx + 1])

                def proj(pt, M, c0, wtile=None):
                    for k in range(KC):
                        lw = win[:, k, c0:c0 + M] if wtile is None else wtile[:, k, :]
                        mm(kb, pt[0:M, :w_], lw, hT[:, k, :w_], k == 0, k == KC - 1, [win if wtile is None else wtile, hT], [pt])

                for c in range(6):
                    pt = pacc.next()
                    proj(pt, 128, C_XBC + c * 128)
                    if is_halo:
                        tt(kb, kb.dve, xbcpre[:, c, 0:2], pt[:, 0:2], hmask[:, 0:2], ALU.mult, [pt, hmask], [xbcpre])
                        tt(kb, kb.dve, xbcpre[:, c, n + 2:n + 4], pt[:, 2:4], hmask[:, 2:4], ALU.mult, [pt, hmask], [xbcpre])
                    else:
                        act(kb, xbcpre[:, c, 2 + o0:2 + o0 + w_], pt[:, :w_], AF.Copy, [pt], [xbcpre])
                if is_halo:
                    continue
                zt = zst.next()
                for c in range(4):
                    pt = pacc.next()
                    proj(pt, 128, C_Z + c * 128)
                    act(kb, zt[:, c, :w_], pt[:, :w_], AF.Silu, [pt], [zt])
                for c in range(4):
                    kb.dma(kb.act, I["zs" + sfx].t[c, :, o0:o0 + w_], zt[:, c, :w_], I["zs" + sfx], zt)
                ntl = w_ // 128
                for i in range(ntl):
                    pt = pacc.next()
                    for k in range(KC):
                        mm(kb, pt[:, 0:16], hT[:, k, i * 128:(i + 1) * 128], win[:, k, C_DT:C_DT + 16], k == 0, k == KC - 1,
                           [hT, win], [pt])
                    tt(kb, kb.dve, dtr[:, i, :], pt[:, 0:16], dtb[:], ALU.add, [pt, dtb], [dtr])
                dt_ = dts.next()
                act(kb, dtr[:, :ntl, :], dtr[:, :ntl, :], AF.Exp, [dtr], [dtr])
                act(kb, dtr[:, :ntl, :], dtr[:, :ntl, :], AF.Ln, [dtr, cst["one1"]], [dtr], bias=cst["one1"][:, 0:1])
                tt(kb, kb.dve, dt_[:, :ntl, 0:16], dtr[:, :ntl, :], Aneg[:].unsqueeze(1).broadcast_to([128, ntl, 16]), ALU.mult,
                   [dtr, Aneg], [dt_])
                act(kb, dt_[:, :ntl, 16:32], dtr[:, :ntl, :], AF.Ln, [dtr], [dt_])
                kb.dma(kb.sp, I["atok" + sfx].t[o0:o0 + w_, :].rearrange("(i p) c -> p i c", p=128), dt_[:, :ntl, 0:16],
                       I["atok" + sfx], dt_)
                kb.dma(kb.sp, I["ldtok" + sfx].t[o0:o0 + w_, :].rearrange("(i p) c -> p i c", p=128), dt_[:, :ntl, 16:32],
                       I["ldtok" + sfx], dt_)
                if tabs is not None:
                    kb.dma(kb.sp, ropeC[:, :w_], I["ropeC"].t[:, o0:o0 + w_], ropeC, I["ropeC"])
                    kb.dma(kb.sp, ropeS[:, :w_], I["ropeS"].t[:, o0:o0 + w_], ropeS, I["ropeS"])
                else:
                    mset(kb, kb.pool, ropeC[:], 1.0, [ropeC])
                    mset(kb, kb.pool, ropeS[:], 0.0, [ropeS])
                pt = pacc.next()
                proj(pt, 128, C_KVA)
                act(kb, kvsq[:, :w_], pt[:, :w_], AF.Square, [pt], [kvsq])
                rms_bcast(kb, cst, [(kvsq[:, :w_], 128)], KVL, w_, psq, rkv, [kvsq])
                tt(kb, kb.dve, kvn[:, :w_], pt[:, :w_], rkv[:, :w_], ALU.mult, [pt, rkv], [kvn])
                pa = pacc.next()
                proj(pa, QK, 0, wkr)
                pb = pacc.next()
                proj(pb, QK, 0, wkrr)
                a1 = t1.next(); a2 = t2.next(); kr_ = krp.next()
                tt(kb, kb.dve, a1[64:96, :w_], pa[64:96, :w_], ropeC[64:96, :w_], ALU.mult, [pa, ropeC], [a1])
                tt(kb, kb.dve, a2[64:96, :w_], pb[64:96, :w_], ropeS[64:96, :w_], ALU.mult, [pb, ropeS], [a2])
                tt(kb, kb.pool, kr_[64:96, :w_], a1[64:96, :w_], a2[64:96, :w_], ALU.add, [a1, a2], [kr_])
                for h in range(H):
                    kb.dma(kb.pool, I["KT" + sfx].t[h, 64:96, o0:o0 + w_], kr_[64:96, :w_], I["KT" + sfx], kr_)
                for h in range(H):
                    pt = pacc.next()
                    mm(kb, pt[0:NOPE, :w_], wkn[:, h, :], kvn[:, :w_], True, True, [wkn, kvn], [pt])
                    ks_ = kst.next()
                    act(kb, ks_[:, :w_], pt[0:NOPE, :w_], AF.Copy, [pt], [ks_])
                    kb.dma(kb.act, I["KT" + sfx].t[h, 0:NOPE, o0:o0 + w_], ks_[:, :w_], I["KT" + sfx], ks_)
                vs_ = vst.next()
                for i in range(ntl):
                    pt = pacc.next()
                    mm(kb, pt[:, :], kvn[:, i * 128:(i + 1) * 128], wvv[:].rearrange("p h c -> p (h c)"), True, True,
                       [kvn, wvv], [pt])
                    act(kb, vs_[:, i, :], pt[:, :], AF.Copy, [pt], [vs_])
                kb.dma(kb.act, I["V" + sfx].t[o0:o0 + w_, :].rearrange("(i p) c -> p i c", p=128), vs_[:, :ntl, :],
                       I["V" + sfx], vs_)
                if want_q:
                    for c in range(2):
                        pt = pacc.next()
                        proj(pt, 128, C_QA + c * 128)
                        act(kb, qaT[:, c, :w_], pt[:, :w_], AF.Copy, [pt], [qaT])
                        act(kb, qsq[:, c, :w_], pt[:, :w_], AF.Square, [pt], [qsq])
                    rms_bcast(kb, cst, [(qsq[:, c, :w_], 128) for c in range(2)], QL, w_, psq, rq, [qsq])
                    tt(kb, kb.dve, Crs[:, :w_], ropeC[:, :w_], rq[0:QK, :w_], ALU.mult, [ropeC, rq], [Crs])
                    tt(kb, kb.dve, Srs[:, :w_], ropeS[:, :w_], rq[0:QK, :w_], ALU.mult, [ropeS, rq], [Srs])
                    for h in range(H):
                        pa = pacc.next()
                        for c in range(2):
                            mm(kb, pa[0:QK, :w_], wq[:, c, h, :], qaT[:, c, :w_], c == 0, c == 1, [wq, qaT], [pa])
                        for c in range(2):
                            mm(kb, pq2[0:QK, :w_], wqr[:, c, h, :], qaT[:, c, :w_], c == 0, c == 1, [wqr, qaT], [pq2])
                        a1 = t1.next(); a2 = t2.next(); q_ = qst.next()
                        tt(kb, kb.dve, a1[:, :w_], pa[0:QK, :w_], Crs[:, :w_], ALU.mult, [pa, Crs], [a1])
                        tt(kb, kb.dve, a2[:, :w_], pq2[0:QK, :w_], Srs[:, :w_], ALU.mult, [pq2, Srs], [a2])
                        tt(kb, kb.pool, q_[:, :w_], a1[:, :w_], a2[:, :w_], ALU.add, [a1, a2], [q_])
                        kb.dma(kb.pool, I["QT" + sfx].t[h, :, o0:o0 + w_], q_[:, :w_], I["QT" + sfx], q_)

        def ssd_prep(n, sfx, xbcpre):
            nch = n // 128
            cacc = kb.sb([128, n], F32, "cacc" + sfx)
            xbc = kb.sb([128, 6, n], BF16, "xbc" + sfx)
            for c in range(6):
                ts(kb, kb.dve, cacc[:], xbcpre[:, c, 0:n], cw[:, c, 0:1], ALU.mult, [xbcpre, cw], [cacc])
                for k in range(1, 5):
                    stt(kb, kb.dve, cacc[:], xbcpre[:, c, k:k + n], cw[:, c, k:k + 1], cacc[:], ALU.mult, ALU.add,
                        [xbcpre, cw, cacc], [cacc])
                act(kb, xbc[:, c, :], cacc[:], AF.Silu, [cacc, cb], [xbc], bias=cb[:, c:c + 1])
            for c in range(2):
                kb.dma(kb.sp, I["BCT" + sfx].t[c, :, :], xbc[:, 4 + c, :], I["BCT" + sfx], xbc)
            ptr = Ring([kb.ps([128, 640], BF16, "ptr" + sfx) for _ in range(2)])
            xtk = kb.sb([128, nch, 640], BF16, "xtk" + sfx)
            for ci in range(nch):
                p_ = ptr.next()
                for c in range(5):
                    kb.op(kb.pe, lambda e, c=c, p_=p_, ci=ci: e.transpose(p_[:, c * 128:(c + 1) * 128],
                                                                        xbc[:, c, ci * 128:(ci + 1) * 128], cst["ident_b"][:]),
                          reads=[xbc, cst["ident_b"]], writes=[p_], inc=(c == 4))
                cp(kb, kb.dve, xtk[:, ci, :], p_[:, :], [p_], [xtk])
            kb.dma(kb.sp, I["xtok" + sfx].t.rearrange("(i p) c -> p i c", p=128), xtk[:], I["xtok" + sfx], xtk)
            a_t = kb.sb([128, nch, 16], F32, "a_t" + sfx)
            ld_t = kb.sb([128, nch, 16], F32, "ld_t" + sfx)
            kb.dma(kb.sp, a_t[:], I["atok" + sfx].t.rearrange("(i p) c -> p i c", p=128), a_t, I["atok" + sfx])
            kb.dma(kb.sp, ld_t[:], I["ldtok" + sfx].t.rearrange("(i p) c -> p i c", p=128), ld_t, I["ldtok" + sfx])
            pcs = kb.ps([128, nch, 16], F32, "pcs" + sfx)
            ptot = kb.ps([128, nch, 16], F32, "ptot" + sfx)
            for ci in range(nch):
                for d in range(2):
                    mm(kb, pcs[:, ci, d * 8:(d + 1) * 8], tri[d][:], a_t[:, ci, d * 8:(d + 1) * 8], True, True, [tri[d], a_t], [pcs])
                mm(kb, ptot[:, ci, :], cst["ones_f"][:], a_t[:, ci, :], True, True, [cst["ones_f"], a_t], [ptot])
            tot = kb.sb([128, nch, 16], F32, "tot" + sfx)
            cp(kb, kb.dve, tot[:], ptot[:], [ptot], [tot])
            outer = kb.sb([128, nch, 16], F32, "outer" + sfx)
            mset(kb, kb.dve, outer[:], 0.0, [outer])
            for ci in range(nch - 2, -1, -1):
                tt(kb, kb.dve, outer[:, ci, 0:8], outer[:, ci + 1, 0:8], tot[:, ci + 1, 0:8], ALU.add, [outer, tot], [outer])
            for ci in range(1, nch):
                tt(kb, kb.dve, outer[:, ci, 8:16], outer[:, ci - 1, 8:16], tot[:, ci - 1, 8:16], ALU.add, [outer, tot], [outer])
            wexp = kb.sb([128, nch, 16], F32, "wexp" + sfx)
            tt(kb, kb.dve, wexp[:], tot[:], pcs[:], ALU.subtract, [tot, pcs], [wexp])
            tt(kb, kb.dve, wexp[:], wexp[:], outer[:], ALU.add, [wexp, outer], [wexp])
            tt(kb, kb.dve, wexp[:], wexp[:], ld_t[:], ALU.add, [wexp, ld_t], [wexp])
            act(kb, wexp[:], wexp[:], AF.Exp, [wexp], [wexp])
            pS = [kb.ps([128, 512], F32, "pS%d%s" % (d, sfx)) for d in range(2)]
            xw = Ring([kb.sb([128, 2, H, 64], BF16, "xw" + sfx) for _ in range(2)])
            for ci in range(nch):
                xw_ = xw.next()
                tt(kb, kb.dve, xw_[:], xtk[:, ci, 0:512].rearrange("p (h c) -> p h c", h=H).unsqueeze(1).broadcast_to([128, 2, H, 64]),
                   wexp[:, ci, :].rearrange("p (d h) -> p d h", d=2).unsqueeze(3).broadcast_to([128, 2, H, 64]), ALU.mult,
                   [xtk, wexp], [xw_])
                for d in range(2):
                    mm(kb, pS[d][:, :], xtk[:, ci, 512:640], xw_[:, d].rearrange("p h c -> p (h c)"), ci == 0, ci == nch - 1,
                       [xtk, xw_], [pS[d]], inc=True)
            Ssb = kb.sb([128, 2, 512], F32, "Ssb" + sfx)
            for d in range(2):
                cp(kb, kb.dve, Ssb[:, d, :], pS[d][:, :], [pS[d]], [Ssb])
            kb.dma(kb.sp, I["S" + sfx].t[:, :], Ssb[:].rearrange("p d c -> p (d c)"), I["S" + sfx], Ssb)
            at = kb.sb([128, 16], F32, "at" + sfx)
            tt(kb, kb.dve, at[:, 0:8], outer[:, 0, 0:8], tot[:, 0, 0:8], ALU.add, [outer, tot], [at])
            tt(kb, kb.dve, at[:, 8:16], outer[:, nch - 1, 8:16], tot[:, nch - 1, 8:16], ALU.add, [outer, tot], [at])
            kb.dma(kb.sp, I["atot" + sfx].t[0:1, :], at[0:1, :], I["atot" + sfx], at)

        run_group(CTX, I["xcT"], 0, 1, "c", None, xbc_c, with_ctx_q)
        run_group(T, I["xT"], 0, 0, "", True, xbc_l, True, halo=I["xhT"])
        sc_w.__exit__(None, None, None)
        with kb.scope():
            ssd_prep(CTX, "c", xbc_c)
        with kb.scope():
            ssd_prep(T, "", xbc_l)


def build_phase_a(T, l, with_ctx_q):
    nc = bass.Bass("TRN2", target_bir_lowering=False)
    es = contextlib.ExitStack()
    with es:
        kb = KB(nc, es)
        I = {}

        def inp(name, shape, dtype=F32):
            I[name] = kb.dram(name, shape, dtype, "ExternalInput")

        def outp(name, shape, dtype=F32):
            I[name] = kb.dram(name, shape, dtype, "ExternalOutput")

        inp("xT", [KC, 128, T]); inp("xhT", [KC, 128, 4]); inp("hmask", [128, 4]); inp("xcT", [KC, 128, CTX])
        inp("cT", [128, KC, 2]); inp("ident", [128, 128]); inp("tri", [2, 128, 128])
        inp("ropeC", [QK, T]); inp("ropeS", [QK, T])
        inp("w_mod", [2, D, 6 * D]); inp("b_mod", [2, 6 * D]); inp("norm1_g", [2, D])
        inp("w_in", [2, D, IN_COLS]); inp("q_norm_g", [2, QL]); inp("w_qb", [2, QL, H * QK])
        inp("kv_norm_g", [2, KVL]); inp("w_kvb", [2, KVL, H * 128])
        inp("conv_w", [2, 5, 768]); inp("conv_b", [2, 768]); inp("a_log", [2, 16]); inp("dt_bias", [2, 16])
        for sfx, n in (("", T), ("c", CTX)):
            outp("QT" + sfx, [H, QK, n], BF16); outp("KT" + sfx, [H, QK, n], BF16); outp("V" + sfx, [n, 512], BF16)
            outp("zs" + sfx, [4, 128, n], BF16); outp("xtok" + sfx, [n, 640], BF16); outp("BCT" + sfx, [2, 128, n], BF16)
            outp("atok" + sfx, [n, 16]); outp("ldtok" + sfx, [n, 16]); outp("S" + sfx, [128, 1024]); outp("atot" + sfx, [1, 16])

        cst = load_consts(kb, I)
        tri = [kb.sb([128, 128], F32, "tri%d" % d) for d in range(2)]
        for d in range(2):
            kb.dma(kb.sp, tri[d][:], I["tri"][d], tri[d], I["tri"])
        emit_phase_a(kb, I, T, l, with_ctx_q, cst, tri)
        kb.finish()
    return nc


def const_tables():
    j = np.arange(128)
    tri = np.stack([(j[:, None] <= j[None, :]), (j[:, None] >= j[None, :])]).astype(np.float32)
    return np.eye(128, dtype=np.float32), tri


def rope_tables_host(L):
    rows = L // GRID_W
    row = np.repeat(np.arange(rows), GRID_W)
    col = np.tile(np.arange(GRID_W), rows)
    inv = (10000.0 ** (-np.arange(8, dtype=np.float32) / 8)).astype(np.float32)
    ang = np.concatenate([row[:, None] * inv, col[:, None] * inv], -1).astype(np.float32)
    return np.cos(ang).astype(np.float32), np.sin(ang).astype(np.float32)


def fm(x2d):
    n = x2d.shape[0]
    return np.ascontiguousarray(x2d.T.reshape(KC, 128, n))


def host_inputs_a(W, l, x, ctx, c, cc, s, T, cos, sin):
    L = x.shape[0]
    ident, tri = const_tables()
    idx = [s * T - 2, s * T - 1, (s + 1) * T, (s + 1) * T + 1]
    halo = np.zeros((4, D), np.float32)
    hm = np.zeros((128, 4), np.float32)
    for i, t in enumerate(idx):
        if 0 <= t < L:
            halo[i] = x[t]
            hm[:, i] = 1.0
    ropeC = np.ones((QK, T), np.float32)
    ropeS = np.zeros((QK, T), np.float32)
    ropeC[64:80] = cos[s * T:(s + 1) * T].T
    ropeC[80:96] = cos[s * T:(s + 1) * T].T
    ropeS[64:80] = sin[s * T:(s + 1) * T].T
    ropeS[80:96] = sin[s * T:(s + 1) * T].T
    cT = np.stack([c.reshape(KC, 128).T, cc.reshape(KC, 128).T], -1).astype(np.float32)
    d = dict(xT=fm(x[s * T:(s + 1) * T]), xhT=fm(halo), hmask=hm, xcT=fm(ctx), cT=np.ascontiguousarray(cT),
             ident=ident, tri=tri, ropeC=ropeC, ropeS=ropeS)
    for k in ("w_mod", "b_mod", "norm1_g", "w_in", "q_norm_g", "w_qb", "kv_norm_g", "w_kvb", "conv_w", "conv_b"):
        d[k] = np.ascontiguousarray(W[k], dtype=np.float32)
    d["a_log"] = np.ascontiguousarray(W["a_log"], dtype=np.float32).reshape(2, 16)
    d["dt_bias"] = np.ascontiguousarray(W["dt_bias"], dtype=np.float32).reshape(2, 16)
    return d


def emit_attention(kb, cst, nq, QT_d, nk, KT_d, V_d, mix_d, sel65, loaders=None):
    NT = nk // 128
    QB = min(512, nq)
    with kb.scope():
        kT = Ring([kb.sb([QK, nk], BF16, "kT") for _ in range(2)])
        vA = Ring([kb.sb([128, NT, VD + 1], BF16, "vA") for _ in range(2)])
        for v_ in vA.items:
            mset(kb, kb.pool, v_[:], 1.0, [v_])
        qT = Ring([kb.sb([QK, nq], BF16, "qT") for _ in range(2)])
        pss = Ring([kb.ps([128, 1024], F32, "pss") for _ in range(2)])
        pso = Ring([kb.ps([128, 512], F32, "pso") for _ in range(2)])
        pden = kb.ps([64, 512], F32, "pden")
        pT = Ring([kb.sb([128, 1024], BF16, "pT") for _ in range(3)])
        osb = Ring([kb.sb([VD + 1, 512], F32, "osb") for _ in range(2)])
        rec = kb.sb([64, 512], F32, "rec")
        ost = Ring([kb.sb([64, 512], BF16, "ost") for _ in range(2)])
        def load_head(h):
            k_ = kT.next(); v_ = vA.next(); q_ = qT.next()
            if loaders is not None:
                loaders["kv"](h, k_, v_, nk)
            else:
                kb.dma(kb.sp, k_[:, :], KT_d.t[h, :, 0:nk], k_, KT_d)
                for t0 in range(0, NT, 16):
                    t1_ = min(NT, t0 + 16)
                    kb.dma(kb.sp, v_[:, t0:t1_, 0:VD],
                           V_d.t[t0 * 128:t1_ * 128, h * VD:(h + 1) * VD].rearrange("(t p) c -> p t c", p=128), v_, V_d)
            kb.dma(kb.sp, q_[:, :], QT_d.t[h, :, 0:nq], q_, QT_d)
            return k_, v_, q_

        nxt = load_head(0)
        for h in range(H):
            k_, v_, q_ = nxt
            if h + 1 < H:
                nxt = load_head(h + 1)
            for qb in range(nq // QB):
                qs = slice(qb * QB, (qb + 1) * QB)
                po = pso.next()
                npair = NT // 2
                pend = None

                def pv(pr, p_):
                    for j in range(2):
                        kt = 2 * pr + j
                        mm(kb, po[0:VD + 1, :QB], v_[:, kt, :], p_[:, j * 512:j * 512 + QB], kt == 0, kt == NT - 1,
                           [v_, p_], [po], inc=(j == 1))

                for pr in range(npair):
                    ps_ = pss.next()
                    for j in range(2):
                        kt = 2 * pr + j
                        mm(kb, ps_[:, j * 512:j * 512 + QB], k_[:, kt * 128:(kt + 1) * 128], q_[:, qs], True, True,
                           [k_, q_], [ps_], inc=(j == 1))
                    p_ = pT.next()
                    if QB == 512:
                        act(kb, p_[:, :], ps_[:, :], AF.Exp, [ps_], [p_], scale=SCALE)
                    else:
                        for j in range(2):
                            act(kb, p_[:, j * 512:j * 512 + QB], ps_[:, j * 512:j * 512 + QB], AF.Exp, [ps_], [p_], scale=SCALE)
                    if pend is not None:
                        pv(*pend)
                    pend = (pr, p_)
                pv(*pend)
                o_ = osb.next()
                cp(kb, kb.dve, o_[:, :QB], po[0:VD + 1, :QB], [po], [o_])
                mm(kb, pden[:, :QB], sel65[:, :], o_[:, :QB], True, True, [sel65, o_], [pden])
                kb.op(kb.dve, lambda e: e.reciprocal(out=rec[:, :QB], in_=pden[:, :QB]), reads=[pden], writes=[rec])
                s_ = ost.next()
                tt(kb, kb.dve, s_[:, :QB], o_[0:64, :QB], rec[:, :QB], ALU.mult, [o_, rec], [s_])
                kb.dma(kb.pool, mix_d.t[h // 2, (h % 2) * 64:(h % 2) * 64 + 64, qs], s_[:, :QB], mix_d, s_)


def emit_ssd(kb, cst, l, n, sfx, I, mix_d, yfw_d, tri, maskneg, loaders=None):
    nch = n // 128
    import os
    STOP = float(os.environ.get('SSD_STOP', '99'))
    with kb.scope():
        xtk = kb.sb([128, nch, 640], BF16, "xtk")
        kb.dma(kb.sp, xtk[:], I["xtok" + sfx].t.rearrange("(i p) c -> p i c", p=128), xtk, I["xtok" + sfx])
        bct = kb.sb([64, 2, 2, n], BF16, "bct")
        for c in range(2):
            kb.dma(kb.sp, bct[:, c, :, :], I["BCT" + sfx].t[c].rearrange("(g n) t -> n g t", g=2), bct, I["BCT" + sfx])
        a_t = kb.sb([128, nch, 16], F32, "a_t")
        ld_t = kb.sb([128, nch, 16], F32, "ld_t")
        kb.dma(kb.sp, a_t[:], I["atok" + sfx].t.rearrange("(i p) c -> p i c", p=128), a_t, I["atok" + sfx])
        kb.dma(kb.sp, ld_t[:], I["ldtok" + sfx].t.rearrange("(i p) c -> p i c", p=128), ld_t, I["ldtok" + sfx])
        dsk = kb.sb([128, H], F32, "dsk")
        kb.dma(kb.sp, dsk[:], I["d_skip"][l:l + 1, :].broadcast_to([128, H]), dsk, I["d_skip"])
        DI = kb.sb([128, H, 128], F32, "DI")
        tt(kb, kb.dve, DI[:], cst["ident_f"][:].unsqueeze(1).broadcast_to([128, H, 128]),
           dsk[:].unsqueeze(2).broadcast_to([128, H, 128]), ALU.mult, [cst["ident_f"], dsk], [DI])
        sng = kb.sb([64, H], F32, "sng")
        kb.dma(kb.sp, sng[:], I["ssd_norm_g"][l].rearrange("(h p) -> p h", p=64), sng, I["ssd_norm_g"],
               allow_slow_non_contiguous=True)
        zs_v = I["zs" + sfx].t.rearrange("c q t -> (c q) t").rearrange("(h p) t -> p h t", p=64)
        mix_v = mix_d.t[4:8].rearrange("c q t -> (c q) t").rearrange("(h p) t -> p h t", p=64)
        pct = kb.ps([128, 16], F32, "pct")
        pB = kb.ps([128, H, 128], F32, "pB")
        pG = kb.ps([128, 2, 128], F32, "pG")
        py = kb.ps([64, H, 128], F32, "py")
        pSt = kb.ps([64, 512], F32, "pSt")
        psq = kb.ps([64, 128], F32, "psqs")
        R__r = Ring([kb.sb([128, H, 128], F32, "R") for _ in range(2)])
        csl_r = Ring([kb.sb([128, H], F32, "csl") for _ in range(2)])
        dec_r = Ring([kb.sb([128, H], F32, "dec") for _ in range(2)])
        wj_r = Ring([kb.sb([128, H], F32, "wj") for _ in range(2)])
        D1_r = Ring([kb.sb([128, H, 128], F32, "D1") for _ in range(2)])
        E__r = Ring([kb.sb([128, H, 128], F32, "E") for _ in range(2)])
        Cx_r = Ring([kb.sb([128, H, 128], F32, "Cx") for _ in range(2)])
        CexpT_r = Ring([kb.sb([64, H, 128], BF16, "CexpT") for _ in range(2)])
        STf_r = Ring([kb.sb([128, H, 128], F32, "STf") for _ in range(2)])
        STb_r = Ring([kb.sb([128, H, 128], BF16, "STb") for _ in range(2)])
        xw_r = Ring([kb.sb([128, H, 64], BF16, "xw") for _ in range(2)])
        hst = kb.sb([64, H, 64], F32, "hst")
        hpb = kb.sb([64, H, 64], BF16, "hpb")
        ssrc = kb.sb([64, 512], F32, "ssrc")
        atb = kb.sb([128, H], F32, "atb")
        ysb = Ring([kb.sb([64, H, 128], F32, "ysb") for _ in range(2)])
        yfl = Ring([kb.sb([64, H, 128], F32, "yfl") for _ in range(2)])
        zt = Ring([kb.sb([64, H, 128], BF16, "zt") for _ in range(2)])
        yg = kb.sb([64, H, 128], F32, "yg")
        ysq = kb.sb([64, H, 128], BF16, "ysq")
        rs_ = kb.sb([64, 128], F32, "rs")
        yo = Ring([kb.sb([64, H, 128], BF16, "yo") for _ in range(2)])
        hflat = hst[:].rearrange("p h c -> p (h c)")
        for d in range(2):
            if loaders is not None and "state" in loaders:
                loaders["state"](d, hst, hflat, ssrc, atb)
            elif loaders is not None or ("Sch" + sfx) not in I:
                mset(kb, kb.dve, hst[:], 0.0, [hst])
            else:
                for k in range(3, -1, -1):
                    kb.dma(kb.sp, ssrc[:], I["Sch" + sfx].t[d, k], ssrc, I["Sch" + sfx])
                    if k == 3:
                        cp(kb, kb.dve, hflat, ssrc[:], [ssrc], [hst])
                    else:
                        kb.dma(kb.sp, atb[:], I["atch" + sfx].t[d, k:k + 1, :].broadcast_to([128, H]), atb, I["atch" + sfx])
                        act(kb, atb[:], atb[:], AF.Exp, [atb], [atb])
                        tt(kb, kb.dve, hst[:], hst[:], atb[0:64, :].unsqueeze(2).broadcast_to([64, H, 64]), ALU.mult, [hst, atb], [hst])
                        tt(kb, kb.dve, hflat, hflat, ssrc[:], ALU.add, [hst, ssrc], [hst])
            cp(kb, kb.dve, hpb[:], hst[:], [hst], [hpb])
            def stage1(ci, t_out, d=d):
                tsl = slice(ci * 128, (ci + 1) * 128)
                R_ = R__r.next(); csl = csl_r.next(); dec = dec_r.next(); wj = wj_r.next(); D1 = D1_r.next(); E_ = E__r.next(); Cx = Cx_r.next(); CexpT = CexpT_r.next(); STf = STf_r.next(); STb = STb_r.next(); xw = xw_r.next()
                a_d = a_t[:, ci, d * 8:(d + 1) * 8]
                mm(kb, pct[:, 0:8], tri[d][:], a_d, True, True, [tri[d], a_t], [pct])
                yield
                mm(kb, pct[:, 8:16], cst["ones_f"][:], a_d, True, True, [cst["ones_f"], a_t], [pct])
                yield
                tt(kb, kb.pool, R_[:], tri[d][:].unsqueeze(1).broadcast_to([128, H, 128]),
                   a_d.unsqueeze(2).broadcast_to([128, H, 128]), ALU.mult, [tri[d], a_t], [R_])
                yield
                for hh in range(2):
                    mm(kb, pB[:, hh * 4:(hh + 1) * 4, :].rearrange("p h i -> p (h i)"), cst["ones_f"][:],
                       R_[:, hh * 4:(hh + 1) * 4, :].rearrange("p h i -> p (h i)"), True, True, [cst["ones_f"], R_], [pB])
                    yield
                tt(kb, kb.dve, csl[:], pct[:, 0:8], ld_t[:, ci, d * 8:(d + 1) * 8], ALU.subtract, [pct, ld_t], [csl])
                yield
                act(kb, dec[:], pct[:, 8:16], AF.Exp, [pct], [dec])
                yield
                tt(kb, kb.dve, wj[:], pct[:, 8:16], csl[:], ALU.subtract, [pct, csl], [wj])
                yield
                act(kb, wj[:], wj[:], AF.Exp, [wj], [wj])
                yield
                tt(kb, kb.dve, D1[:], pB[:], csl[:].unsqueeze(2).broadcast_to([128, H, 128]), ALU.subtract, [pB, csl], [D1])
                yield
                tt(kb, kb.pool, D1[:], D1[:], maskneg[d][:].unsqueeze(1).broadcast_to([128, H, 128]), ALU.add,
                   [D1, maskneg[d]], [D1])
                yield
                act(kb, E_[:], D1[:], AF.Exp, [D1], [E_])
                yield
                act(kb, Cx[:], pB[:], AF.Exp, [pB], [Cx])
                yield
                tt(kb, kb.dve, CexpT[:].rearrange("p (g h) i -> p g h i", g=2), Cx[0:64].rearrange("p (g h) i -> p g h i", g=2),
                   bct[:, 1, :, tsl].unsqueeze(2).broadcast_to([64, 2, 4, 128]), ALU.mult, [Cx, bct], [CexpT])
                yield
                for g in range(2):
                    mm(kb, pG[:, g, :], bct[:, 0, g, tsl], bct[:, 1, g, tsl], True, True, [bct], [pG])
                    yield
                Ev = E_[:].rearrange("p (g h) i -> p g h i", g=2)
                Gv = pG[:].unsqueeze(2).broadcast_to([128, 2, 4, 128])
                if d == 0:
                    tt(kb, kb.dve, STf[:].rearrange("p (g h) i -> p g h i", g=2), Ev, Gv, ALU.mult, [E_, pG], [STf])
                    yield
                    tt(kb, kb.pool, STb[:], STf[:], DI[:], ALU.add, [STf, DI], [STb])
                    yield
                else:
                    tt(kb, kb.dve, STb[:].rearrange("p (g h) i -> p g h i", g=2), Ev, Gv, ALU.mult, [E_, pG], [STb])
                    yield
                t_out.update(tsl=tsl, wj=wj, dec=dec, CexpT=CexpT, STb=STb, xw=xw)

            def stage2(ci, t_, d=d):
                tsl = t_['tsl']; wj = t_['wj']; dec = t_['dec']; CexpT = t_['CexpT']; STb = t_['STb']; xw = t_['xw']
                for h in range(H):
                    g = h // 4
                    mm(kb, py[:, h, :], xtk[:, ci, h * 64:(h + 1) * 64], STb[:, h, :], True, False, [xtk, STb], [py], inc=False)
                    mm(kb, py[:, h, :], hpb[:, h, :], CexpT[:, h, :], False, True, [hpb, CexpT], [py], inc=True)
                    yield
                tt(kb, kb.pool, xw[:], xtk[:, ci, 0:512].rearrange("p (h c) -> p h c", h=H),
                   wj[:].unsqueeze(2).broadcast_to([128, H, 64]), ALU.mult, [xtk, wj], [xw])
                yield
                for g in range(2):
                    mm(kb, pSt[:, g * 256:(g + 1) * 256], xtk[:, ci, 512 + g * 64:512 + (g + 1) * 64],
                       xw[:, g * 4:(g + 1) * 4, :].rearrange("p h c -> p (h c)"), True, True, [xtk, xw], [pSt])
                    yield
                tt(kb, kb.dve, hst[:], hst[:], dec[0:64, :].unsqueeze(2).broadcast_to([64, H, 64]), ALU.mult, [hst, dec], [hst])
                yield
                tt(kb, kb.dve, hflat, hflat, pSt[:, :], ALU.add, [hst, pSt], [hst])
                yield
                cp(kb, kb.dve, hpb[:], hst[:], [hst], [hpb])
                yield
                if d == 0:
                    y_ = ysb.next()
                    cp(kb, kb.dve, y_[:], py[:], [py], [y_])
                    yield
                    kb.dma(kb.sp, yfw_d.t[ci].rearrange("p (h i) -> p h i", h=H), y_[:], yfw_d, y_)
                    yield
                else:
                    yf_ = yfl.next(); z_ = zt.next()
                    kb.dma(kb.sp, yf_[:], yfw_d.t[ci].rearrange("p (h i) -> p h i", h=H), yf_, yfw_d)
                    yield
                    kb.dma(kb.sp, z_[:], zs_v[:, :, tsl], z_, I["zs" + sfx])
                    yield
                    tt(kb, kb.dve, yg[:], py[:], yf_[:], ALU.add, [py, yf_], [yg])
                    yield
                    tt(kb, kb.pool, yg[:], yg[:], z_[:], ALU.mult, [yg, z_], [yg])
                    yield
                    act(kb, ysq[:], yg[:], AF.Square, [yg], [ysq])
                    yield
                    for h in range(H):
                        mm(kb, psq[:, :], cst["ones_b"][0:64, 0:64], ysq[:, h, :], h == 0, h == H - 1, [cst["ones_b"], ysq], [psq])
                        yield
                    act(kb, rs_[:], psq[:, :], AF.Ln, [psq, cst["eps"]], [rs_], bias=cst["eps"][0:64, 0:1], scale=1.0 / SSD_IN)
                    yield
                    act(kb, rs_[:], rs_[:], AF.Exp, [rs_], [rs_], scale=-0.5)
                    yield
                    tt(kb, kb.dve, yg[:], yg[:], rs_[:].unsqueeze(1).broadcast_to([64, H, 128]), ALU.mult, [yg, rs_], [yg])
                    yield
                    o_ = yo.next()
                    tt(kb, kb.pool, o_[:], yg[:], sng[:].unsqueeze(2).broadcast_to([64, H, 128]), ALU.mult, [yg, sng], [o_])
                    yield
                    kb.dma(kb.sp, mix_v[:, :, tsl], o_[:], mix_d, o_)
                    yield


            order = list(range(nch)) if d == 0 else list(range(nch - 1, -1, -1))

            def zip_run(gens):
                gens = [g for g in gens if g is not None]
                while gens:
                    for g in list(gens):
                        try:
                            next(g)
                        except StopIteration:
                            gens.remove(g)

            pend = None
            for ci in order:
                t_ = {}
                zip_run([stage1(ci, t_), stage2(*pend) if pend is not None else None])
                pend = (ci, t_)
            zip_run([stage2(*pend)])


def emit_tail(kb, cst, l, n, cidx, xin_d, mix_d, x1_d, h2_d, xout_d, I, mod, last):
    TBk = min(512, n)
    nblk = n // TBk
    g1c, sh2, g2c = mod[2], mod[3], mod[5]
    gT = kb.sb([NE, n], BF16, "gT")
    with kb.scope():
        wout = kb.sb([128, KC, D], BF16, "wout")
        for k in range(KC):
            kb.dma(kb.pool, wout[:, k, :], I["w_out"][l][k * 128:(k + 1) * 128, :], wout, I["w_out"])
        n2g = load_col(kb, I["norm2_g"], I["norm2_g"][l], KC, "n2g")
        gmod2 = kb.sb([128, KC], F32, "gmod2")
        ts(kb, kb.dve, gmod2[:], mod[4][:, :, cidx], 1.0, ALU.add, [mod[4]], [gmod2])
        tt(kb, kb.dve, gmod2[:], gmod2[:], n2g[:], ALU.mult, [gmod2, n2g], [gmod2])
        rw = kb.sb([128, KC, 36], F32, "rw")
        kb.dma(kb.sp, rw[:, :, 0:4], I["router_w1"][l].rearrange("(k p) c -> p k c", p=128), rw, I["router_w1"])
        kb.dma(kb.sp, rw[:, :, 4:36], I["router_w2"][l].rearrange("(k p) c -> p k c", p=128), rw, I["router_w2"])
        rb = kb.sb([128, 36], F32, "rb")
        kb.dma(kb.sp, rb[:, 0:4], I["router_b1"][l:l + 1, :].broadcast_to([128, 4]), rb, I["router_b1"])
        kb.dma(kb.sp, rb[:, 4:36], I["router_b2"][l:l + 1, :].broadcast_to([128, 32]), rb, I["router_b2"])
        mixb = Ring([kb.sb([128, KC, TBk], BF16, "mixb") for _ in range(2)])
        xt__rr = Ring([kb.sb([128, KC, TBk], F32, "xt") for _ in range(2)])
        x1_rr = Ring([kb.sb([128, KC, TBk], F32, "x1") for _ in range(2)])
        sq_rr = Ring([kb.sb([128, KC, TBk], BF16, "sq2") for _ in range(2)])
        rstd = kb.sb([128, TBk], F32, "rstd2")
        tmp = kb.sb([128, TBk], F32, "tmp2")
        h2f_rr = Ring([kb.sb([128, KC, TBk], F32, "h2f") for _ in range(2)])
        h2b_rr = Ring([kb.sb([128, KC, TBk], BF16, "h2b") for _ in range(2)])
        po = Ring([kb.ps([128, TBk], F32, "po") for _ in range(2)])
        psq = kb.ps([128, TBk], F32, "psq2")
        plg = kb.ps([128, 36], F32, "plg")
        pgt = kb.ps([NE, 128], F32, "pgt")
        lg = kb.sb([128, 36], F32, "lg")
        sm = kb.sb([128, 16], F32, "sm")
        e1 = kb.sb([128, 4], F32, "e1")
        ohg = kb.sb([128, 4], F32, "ohg")
        l2g = kb.sb([128, 4, 8], F32, "l2g")
        lsel = kb.sb([128, 8], F32, "lsel")
        e2 = kb.sb([128, 8], F32, "e2")
        mk1 = kb.sb([128, 8], F32, "mk1")
        mk2 = kb.sb([128, 8], F32, "mk2")
        lp = kb.sb([128, 8], F32, "lp")
        wi = kb.sb([128, 8], F32, "wi")
        gate = kb.sb([128, 4, 8], F32, "gate")
        for b in range(nblk):
            bs = slice(b * TBk, (b + 1) * TBk)
            mb = mixb.next()
            xt_ = xt__rr.next(); x1 = x1_rr.next(); sq = sq_rr.next(); h2f = h2f_rr.next(); h2b = h2b_rr.next()
            for k in range(KC):
                kb.dma(kb.sp, mb[:, k, :], mix_d.t[k, :, bs], mb, mix_d)
                kb.dma(kb.sp, xt_[:, k, :], xin_d.t[k, :, bs], xt_, xin_d)
            for dc in range(KC):
                p_ = po.next()
                for k in range(KC):
                    mm(kb, p_[:, :], wout[:, k, dc * 128:(dc + 1) * 128], mb[:, k, :], k == 0, k == KC - 1, [wout, mb], [p_])
                stt(kb, kb.dve, x1[:, dc, :], p_[:, :], g1c[:, dc, cidx:cidx + 1], xt_[:, dc, :], ALU.mult, ALU.add,
                    [p_, g1c, xt_], [x1])
            for k in range(KC):
                kb.dma(kb.sp, x1_d.t[k, :, bs], x1[:, k, :], x1_d, x1)
            act(kb, sq[:], x1[:], AF.Square, [x1], [sq])
            rms_bcast(kb, cst, [(sq[:, k, :], 128) for k in range(KC)], D, TBk, psq, rstd, [sq])
            for k in range(KC):
                stt(kb, kb.dve, tmp[:], x1[:, k, :], gmod2[:, k:k + 1], rstd[:], ALU.mult, ALU.mult, [x1, gmod2, rstd], [tmp])
                act(kb, h2f[:, k, :], tmp[:], AF.Identity, [tmp, sh2], [h2f], bias=sh2[:, k, cidx:cidx + 1])
            cp(kb, kb.pool, h2b[:], h2f[:], [h2f], [h2b])
            for k in range(KC):
                kb.dma(kb.sp, h2_d.t[k, :, bs], h2b[:, k, :], h2_d, h2b)
            for i in range(TBk // 128):
                for k in range(KC):
                    mm(kb, plg[:, :], h2f[:, k, i * 128:(i + 1) * 128], rw[:, k, :], k == 0, k == KC - 1, [h2f, rw], [plg])
                tt(kb, kb.dve, lg[:], plg[:, :], rb[:], ALU.add, [plg, rb], [lg])
                R = [lg]
                kb.op(kb.dve, lambda e: e.tensor_reduce(out=sm[:, 0:1], in_=lg[:, 0:4], axis=AX.X, op=ALU.max), reads=R, writes=[sm])
                ts(kb, kb.dve, sm[:, 1:2], sm[:, 0:1], -1.0, ALU.mult, [sm], [sm])
                act(kb, e1[:], lg[:, 0:4], AF.Exp, [lg, sm], [e1, sm], bias=sm[:, 1:2], accum_out=sm[:, 2:3])
                kb.op(kb.dve, lambda e: e.reciprocal(out=sm[:, 3:4], in_=sm[:, 2:3]), reads=[sm], writes=[sm])
                ts(kb, kb.dve, ohg[:], lg[:, 0:4], sm[:, 0:1], ALU.is_equal, [lg, sm], [ohg])
                tt(kb, kb.dve, l2g[:], lg[:, 4:36].rearrange("p (g e) -> p g e", g=4), ohg[:].unsqueeze(2).broadcast_to([128, 4, 8]),
                   ALU.mult, [lg, ohg], [l2g])
                kb.op(kb.dve, lambda e: e.tensor_reduce(out=lsel[:], in_=l2g[:].rearrange("p g e -> p e g"), axis=AX.X, op=ALU.add),
                      reads=[l2g], writes=[lsel])
                kb.op(kb.dve, lambda e: e.tensor_reduce(out=sm[:, 4:5], in_=lsel[:], axis=AX.X, op=ALU.max), reads=[lsel], writes=[sm])
                ts(kb, kb.dve, sm[:, 5:6], sm[:, 4:5], -1.0, ALU.mult, [sm], [sm])
                act(kb, e2[:], lsel[:], AF.Exp, [lsel, sm], [e2], bias=sm[:, 5:6])
                ts(kb, kb.dve, mk1[:], lsel[:], sm[:, 4:5], ALU.is_equal, [lsel, sm], [mk1])
                stt(kb, kb.dve, lp[:], mk1[:], -1.0e30, lsel[:], ALU.mult, ALU.add, [mk1, lsel], [lp])
                kb.op(kb.dve, lambda e: e.tensor_reduce(out=sm[:, 6:7], in_=lp[:], axis=AX.X, op=ALU.max), reads=[lp], writes=[sm])
                ts(kb, kb.dve, mk2[:], lp[:], sm[:, 6:7], ALU.is_equal, [lp, sm], [mk2])
                tt(kb, kb.dve, mk1[:], mk1[:], mk2[:], ALU.add, [mk1, mk2], [mk1])
                tt(kb, kb.dve, wi[:], e2[:], mk1[:], ALU.mult, [e2, mk1], [wi])
                kb.op(kb.dve, lambda e: e.tensor_reduce(out=sm[:, 7:8], in_=wi[:], axis=AX.X, op=ALU.add), reads=[wi], writes=[sm])
                kb.op(kb.dve, lambda e: e.reciprocal(out=sm[:, 8:9], in_=sm[:, 7:8]), reads=[sm], writes=[sm])
                tt(kb, kb.dve, sm[:, 9:10], sm[:, 8:9], sm[:, 3:4], ALU.mult, [sm], [sm])
                ts(kb, kb.dve, wi[:], wi[:], sm[:, 9:10], ALU.mult, [wi, sm], [wi])
                tt(kb, kb.dve, gate[:], ohg[:].unsqueeze(2).broadcast_to([128, 4, 8]), wi[:].unsqueeze(1).broadcast_to([128, 4, 8]),
                   ALU.mult, [ohg, wi], [gate])
                kb.op(kb.pe, lambda e: e.transpose(pgt[:, :], gate[:].rearrange("p g e -> p (g e)"), cst["ident_f"][:]),
                      reads=[gate, cst["ident_f"]], writes=[pgt])
                cp(kb, kb.dve, gT[:, b * TBk + i * 128:b * TBk + (i + 1) * 128], pgt[:, :], [pgt], [gT])
    TH = min(n, 2048)
    with kb.scope():
        sel = kb.sb([NE, NE * 128], BF16, "sel")
        kb.dma(kb.pool, sel[:], I["sel"][:, :], sel, I["sel"])
        yaccs = [kb.sb([128, KC, TBk], F32, "yacc") for _ in range(TH // TBk)]
        h2hs = [kb.sb([128, KC, TBk], BF16, "h2h") for _ in range(TH // TBk)]
        wg = Ring([kb.sb([128, KC, FF], BF16, "wg") for _ in range(2)])
        wu = Ring([kb.sb([128, KC, FF], BF16, "wu") for _ in range(2)])
        wd = Ring([kb.sb([128, 2, D], BF16, "wd") for _ in range(3)])
        pgb = kb.ps([128, TBk], F32, "pgb")
        pgu = Ring([kb.ps([128, TBk], F32, "pgu") for _ in range(4)])
        pyy = Ring([kb.ps([128, TBk], F32, "pyy") for _ in range(2)])
        gbc = Ring([kb.sb([128, TBk], BF16, "gbc") for _ in range(2)])
        sg = Ring([kb.sb([128, TBk], BF16, "sg") for _ in range(2)])
        tu = Ring([kb.sb([128, TBk], BF16, "tu") for _ in range(2)])
        A_ = Ring([kb.sb([128, 2, TBk], BF16, "A") for _ in range(3)])
        x1b_r = Ring([kb.sb([128, KC, TBk], F32, "x1b") for _ in range(2)])
        sqf = kb.sb([128, KC, TBk], BF16, "sqf") if last else None
        rsf = kb.sb([128, TBk], F32, "rsf") if last else None
        fg = load_col(kb, I["final_g"], I["final_g"].t, KC, "fg") if last else None
        def emit_down(e, d_, a_, bs):
            yacc = yaccs[bs.start // TBk]
            bs = slice(0, TBk)
            for dc in range(KC):
                py_ = pyy.next()
                for f in range(2):
                    mm(kb, py_[:, :], d_[:, f, dc * 128:(dc + 1) * 128], a_[:, f, :], f == 0, f == 1, [d_, a_], [py_])
                if e == 0:
                    act(kb, yacc[:, dc, bs], py_[:, :], AF.Copy, [py_], [yacc])
                else:
                    tt(kb, kb.dve, yacc[:, dc, bs], yacc[:, dc, bs], py_[:, :], ALU.add, [yacc, py_], [yacc])

        def load_w(e):
            g_ = wg.next(); u_ = wu.next(); d_ = wd.next()
            kb.dma(kb.pool, g_[:], I["w_gate"][l, e].rearrange("(k p) f -> p k f", p=128), g_, I["w_gate"])
            kb.dma(kb.pool, u_[:], I["w_up"][l, e].rearrange("(k p) f -> p k f", p=128), u_, I["w_up"])
            kb.dma(kb.pool, d_[:], I["w_down"][l, e].rearrange("(f p) c -> p f c", p=128), d_, I["w_down"])
            return g_, u_, d_

        wpre = [None]
        pend_down = None
        def load_h2h(hf, b):
            for k in range(KC):
                kb.dma(kb.sp, h2hs[b][:, k, :], h2_d.t[k, :, hf * TH + b * TBk:hf * TH + (b + 1) * TBk], h2hs[b], h2_d)

        for b in range(TH // TBk):
            load_h2h(0, b)
        for hf in range(n // TH):
            for e in range(NE):
                if wpre[0] is None:
                    wpre[0] = load_w(e)
                g_, u_, d_ = wpre[0]
                wpre[0] = load_w((e + 1) % NE) if (e + 1 < NE or hf + 1 < n // TH) else None
                for b in range(TH // TBk):
                    bs = slice(b * TBk, (b + 1) * TBk)
                    gs = slice(hf * TH + b * TBk, hf * TH + (b + 1) * TBk)
                    mm(kb, pgb[:, :], sel[:, e * 128:(e + 1) * 128], gT[:, gs], True, True, [sel, gT], [pgb])
                    gb_ = gbc.next()
                    act(kb, gb_[:], pgb[:, :], AF.Copy, [pgb], [gb_])
                    a_ = A_.next()
                    for f in range(2):
                        pg_ = pgu.next(); pu_ = pgu.next()
                        for k in range(KC):
                            mm(kb, pg_[:, :], g_[:, k, f * 128:(f + 1) * 128], h2hs[b][:, k, :], k == 0, k == KC - 1, [g_, h2hs[b]], [pg_])
                        for k in range(KC):
                            mm(kb, pu_[:, :], u_[:, k, f * 128:(f + 1) * 128], h2hs[b][:, k, :], k == 0, k == KC - 1, [u_, h2hs[b]], [pu_])
                        s_ = sg.next(); t_ = tu.next()
                        act(kb, s_[:], pg_[:, :], AF.Silu, [pg_], [s_])
                        tt(kb, kb.dve, t_[:], pu_[:, :], s_[:], ALU.mult, [pu_, s_], [t_])
                        tt(kb, kb.pool, a_[:, f, :], t_[:], gb_[:], ALU.mult, [t_, gb_], [a_])
                    if e == NE - 1 and hf + 1 < n // TH:
                        load_h2h(hf + 1, b)
                    if pend_down is not None:
                        emit_down(*pend_down)
                    pend_down = (e, d_, a_, bs)
            emit_down(*pend_down)
            pend_down = None
            for b in range(TH // TBk):
                bs = slice(b * TBk, (b + 1) * TBk)
                gs = slice(hf * TH + b * TBk, hf * TH + (b + 1) * TBk)
                x1b = x1b_r.next()
                for k in range(KC):
                    kb.dma(kb.act, x1b[:, k, :], x1_d.t[k, :, gs], x1b, x1_d)
                for k in range(KC):
                    stt(kb, kb.dve, x1b[:, k, :], yaccs[b][:, k, :], g2c[:, k, cidx:cidx + 1], x1b[:, k, :], ALU.mult, ALU.add,
                        [yaccs[b], g2c, x1b], [x1b])
                if last:
                    act(kb, sqf[:], x1b[:], AF.Square, [x1b], [sqf])
                    rms_bcast(kb, cst, [(sqf[:, k, :], 128) for k in range(KC)], D, TBk, pgb, rsf, [sqf])
                    for k in range(KC):
                        stt(kb, kb.dve, x1b[:, k, :], x1b[:, k, :], fg[:, k:k + 1], rsf[:], ALU.mult, ALU.mult, [x1b, fg, rsf], [x1b])
                for k in range(KC):
                    kb.dma(kb.act, xout_d.t[k, :, gs], x1b[:, k, :], xout_d, x1b)


def emit_phase_b(kb, I, T, l, last, cst, tri, maskneg, sel65, parts=("att", "ssd", "tail"), loaders=None):
    do_ctx = not last
    NK = CTX + 4 * T
    with kb.scope():
        mod = emit_mod(kb, I, l, cst, [2, 3, 4, 5])
        if "ssd" in parts:
            emit_ssd(kb, cst, l, T, "", I, I["mix"], I["yfw"], tri, maskneg, loaders)
            if do_ctx:
                emit_ssd(kb, cst, l, CTX, "c", I, I["mixc"], I["yfwc"], tri, maskneg, None)
        if "att" in parts:
            emit_attention(kb, cst, T, I["QT"], NK, I.get("KTall"), I.get("Vall"), I["mix"], sel65, loaders)
            if do_ctx:
                emit_attention(kb, cst, CTX, I["QTc"], CTX, I.get("KTall"), I.get("Vall"), I["mixc"], sel65, loaders)
        if "tail" in parts:
            emit_tail(kb, cst, l, T, 0, I["xT"], I["mix"], I["x1"], I["h2"], I["xout"], I, mod, last)
            if do_ctx:
                emit_tail(kb, cst, l, CTX, 1, I["xcT"], I["mixc"], I["x1c"], I["h2c"], I["xoutc"], I, mod, False)


def build_phase_b(T, l, last, parts=("att", "ssd", "tail"), debug=False):
    do_ctx = not last
    NK = CTX + 4 * T
    nc = bass.Bass("TRN2", target_bir_lowering=False)
    es = contextlib.ExitStack()
    with es:
        kb = KB(nc, es)
        I = {}

        def inp(name, shape, dtype=F32):
            I[name] = kb.dram(name, shape, dtype, "ExternalInput")

        def outp(name, shape, dtype=F32):
            I[name] = kb.dram(name, shape, dtype, "ExternalOutput")

        def scratch(name, shape, dtype=F32):
            I[name] = kb.dram(name, shape, dtype, "ExternalOutput" if debug else "Internal")

        inp("xT", [KC, 128, T]); inp("cT", [128, KC, 2]); inp("ident", [128, 128]); inp("tri", [2, 128, 128])
        inp("maskneg", [2, 128, 128]); inp("sel", [NE, NE * 128])
        inp("w_mod", [2, D, 6 * D]); inp("b_mod", [2, 6 * D]); inp("norm2_g", [2, D]); inp("w_out", [2, D, D])
        inp("router_w1", [2, D, 4]); inp("router_b1", [2, 4]); inp("router_w2", [2, D, NE]); inp("router_b2", [2, NE])
        inp("w_gate", [2, NE, D, FF]); inp("w_up", [2, NE, D, FF]); inp("w_down", [2, NE, FF, D])
        inp("d_skip", [2, H]); inp("ssd_norm_g", [2, SSD_IN]); inp("final_g", [D])
        inp("QT", [H, QK, T], BF16); inp("KTall", [H, QK, NK], BF16); inp("Vall", [NK, 512], BF16)
        groups = [("", T)] + ([("c", CTX)] if do_ctx else [])
        for sfx, n in groups:
            inp("zs" + sfx, [4, 128, n], BF16); inp("xtok" + sfx, [n, 640], BF16); inp("BCT" + sfx, [2, 128, n], BF16)
            inp("atok" + sfx, [n, 16]); inp("ldtok" + sfx, [n, 16])
            inp("Sch" + sfx, [2, 4, 64, 512]); inp("atch" + sfx, [2, 4, H])
            scratch("mix" + sfx, [KC, 128, n], BF16); scratch("yfw" + sfx, [n // 128, 64, H * 128])
            scratch("x1" + sfx, [KC, 128, n]); scratch("h2" + sfx, [KC, 128, n], BF16)
            outp("xout" + sfx, [KC, 128, n])
        if do_ctx:
            inp("xcT", [KC, 128, CTX]); inp("QTc", [H, QK, CTX], BF16)

        cst = load_consts(kb, I)
        tri = [kb.sb([128, 128], F32, "tri%d" % d) for d in range(2)]
        maskneg = [kb.sb([128, 128], F32, "mneg%d" % d) for d in range(2)]
        for d in range(2):
            kb.dma(kb.sp, tri[d][:], I["tri"][d], tri[d], I["tri"])
            kb.dma(kb.sp, maskneg[d][:], I["maskneg"][d], maskneg[d], I["maskneg"])
        sel65 = kb.sb([VD + 1, 64], F32, "sel65")
        mset(kb, kb.dve, sel65[:], 0.0, [sel65])
        mset(kb, kb.dve, sel65[64:65, :], 1.0, [sel65])
        emit_phase_b(kb, I, T, l, last, cst, tri, maskneg, sel65, parts)
        kb.finish()
    return nc


def pick_state(S_, d):
    out = np.zeros((64, 512), np.float32)
    for h in range(H):
        g = h // 4
        out[:, h * 64:(h + 1) * 64] = S_[g * 64:(g + 1) * 64, d * 512 + h * 64:d * 512 + (h + 1) * 64]
    return out


def host_inputs_b(W, l, last, T, s, xT_own, xcT, cT, QT, QTc, KTall, Vall, grp, Sch, atch):
    ident, tri = const_tables()
    maskneg = ((1.0 - tri) * NEG).astype(np.float32)
    sel = np.zeros((NE, NE, 128), np.float32)
    for e in range(NE):
        sel[e, e, :] = 1.0
    d = dict(xT=xT_own, cT=cT, ident=ident, tri=tri, maskneg=maskneg, sel=sel.reshape(NE, NE * 128),
             QT=QT, KTall=KTall, Vall=Vall)
    for k in ("w_mod", "b_mod", "norm2_g", "w_out", "router_w1", "router_b1", "router_w2", "router_b2",
              "w_gate", "w_up", "w_down", "d_skip", "ssd_norm_g", "final_g"):
        d[k] = np.ascontiguousarray(W[k], dtype=np.float32)
    for sfx in grp:
        for k, v in grp[sfx].items():
            d[k + sfx] = v
        d["Sch" + sfx] = Sch[sfx]
        d["atch" + sfx] = atch[sfx]
    if not last:
        d["xcT"] = xcT
        d["QTc"] = QTc
    return d


_NC_CACHE = {}


def _get_nc(kind, T, l, flag):
    key = (kind, T, l, flag)
    if key not in _NC_CACHE:
        _NC_CACHE[key] = build_phase_a(T, l, flag) if kind == "a" else build_phase_b(T, l, flag)
    return _NC_CACHE[key]


def _run(nc, in_maps):
    res = run_bass_kernel_spmd(nc, in_maps, core_ids=list(range(len(in_maps))))
    return res.results


def forward(W, x, c, ctx, c_ctx, ncores_per_batch=4):
    B, L, _ = x.shape
    R = ncores_per_batch
    T = L // R
    cos, sin = rope_tables_host(L)
    xl = [np.ascontiguousarray(x[b]) for b in range(B)]
    xc = [np.ascontiguousarray(ctx[b]) for b in range(B)]
    xT_own = {}
    for l in range(2):
        last = l == 1
        cores = [(b, s) for b in range(B) for s in range(R)]
        ins_a = [host_inputs_a(W, l, xl[b], xc[b], c[b], c_ctx, s, T, cos, sin) for (b, s) in cores]
        ra = _run(_get_nc("a", T, l, not last), ins_a)
        ins_b = []
        for ci, (b, s) in enumerate(cores):
            rb = [ra[b * R + r] for r in range(R)]
            me = ra[ci]
            KTall = np.concatenate([me["KTc"]] + [q["KT"] for q in rb], axis=2)
            Vall = np.concatenate([me["Vc"]] + [q["V"] for q in rb], axis=0)
            grp = {"": {k: me[k] for k in ("zs", "xtok", "BCT", "atok", "ldtok")}}
            Sch = {"": np.zeros((2, 4, 64, 512), np.float32)}
            atch = {"": np.zeros((2, 4, H), np.float32)}
            chains = ([rb[r] for r in range(s - 1, -1, -1)], [rb[r] for r in range(s + 1, R)])
            for d in range(2):
                srcs = [(q["S"], q["atot"]) for q in chains[d]] + [(me["Sc"], me["atotc"])]
                for k, (S_, at_) in enumerate(srcs):
                    Sch[""][d, k] = pick_state(S_, d)
                    atch[""][d, k] = at_[0, d * 8:(d + 1) * 8]
            if not last:
                grp["c"] = {k: me[k + "c"] for k in ("zs", "xtok", "BCT", "atok", "ldtok")}
                Sch["c"] = np.zeros((2, 4, 64, 512), np.float32)
                atch["c"] = np.zeros((2, 4, H), np.float32)
            ins_b.append(host_inputs_b(W, l, last, T, s, ins_a[ci]["xT"], ins_a[ci]["xcT"], ins_a[ci]["cT"], me["QT"],
                                       me["QTc"], KTall, Vall, grp, Sch, atch))
        rbo = _run(_get_nc("b", T, l, last), ins_b)
        for b in range(B):
            outs = [rbo[b * R + r]["xout"].reshape(D, T).T for r in range(R)]
            xl[b] = np.ascontiguousarray(np.concatenate(outs, axis=0))
            if not last:
                xc[b] = np.ascontiguousarray(rbo[b * R]["xoutc"].reshape(D, CTX).T)
    return np.stack(xl).astype(np.float32)


def kernel(x, c, ctx, c_ctx, w_mod, b_mod, norm1_g, norm2_g, w_in, q_norm_g, w_qb, kv_norm_g, w_kvb,
           conv_w, conv_b, a_log, dt_bias, d_skip, ssd_norm_g, w_out, router_w1, router_b1,
           router_w2, router_b2, w_gate, w_up, w_down, final_g):
    W = dict(w_mod=w_mod, b_mod=b_mod, norm1_g=norm1_g, norm2_g=norm2_g, w_in=w_in, q_norm_g=q_norm_g, w_qb=w_qb,
             kv_norm_g=kv_norm_g, w_kvb=w_kvb, conv_w=conv_w, conv_b=conv_b, a_log=a_log, dt_bias=dt_bias,
             d_skip=d_skip, ssd_norm_g=ssd_norm_g, w_out=w_out, router_w1=router_w1, router_b1=router_b1,
             router_w2=router_w2, router_b2=router_b2, w_gate=w_gate, w_up=w_up, w_down=w_down, final_g=final_g)
    W = {k: np.asarray(v, dtype=np.float32) for k, v in W.items()}
    return forward_fused(W, np.asarray(x, np.float32), np.asarray(c, np.float32), np.asarray(ctx, np.float32),
                         np.asarray(c_ctx, np.float32))


CC_GROUPS = [[0, 1, 2, 3], [4, 5, 6, 7]]


def build_fused(T, R=4):
    nc = bass.Bass("TRN2", target_bir_lowering=False)
    es = contextlib.ExitStack()
    with es:
        kb = KB(nc, es)
        I = {}

        def inp(name, shape, dtype=F32):
            I[name] = kb.dram(name, shape, dtype, "ExternalInput")

        inp("xT", [KC, 128, T]); inp("xhT", [KC, 128, 4]); inp("hmask", [128, 4]); inp("xcT", [KC, 128, CTX])
        inp("cT", [128, KC, 2]); inp("ident", [128, 128]); inp("tri", [2, 128, 128])
        inp("maskneg", [2, 128, 128]); inp("sel", [NE, NE * 128]); inp("chm", [128, 2, R]); inp("hsel", [128, 2, R])
        inp("ropeC", [QK, T]); inp("ropeS", [QK, T])
        inp("w_mod", [2, D, 6 * D]); inp("b_mod", [2, 6 * D]); inp("norm1_g", [2, D]); inp("norm2_g", [2, D])
        inp("w_in", [2, D, IN_COLS]); inp("q_norm_g", [2, QL]); inp("w_qb", [2, QL, H * QK])
        inp("kv_norm_g", [2, KVL]); inp("w_kvb", [2, KVL, H * 128])
        inp("conv_w", [2, 5, 768]); inp("conv_b", [2, 768]); inp("a_log", [2, 16]); inp("dt_bias", [2, 16])
        inp("w_out", [2, D, D])
        inp("router_w1", [2, D, 4]); inp("router_b1", [2, 4]); inp("router_w2", [2, D, NE]); inp("router_b2", [2, NE])
        inp("w_gate", [2, NE, D, FF]); inp("w_up", [2, NE, D, FF]); inp("w_down", [2, NE, FF, D])
        inp("d_skip", [2, H]); inp("ssd_norm_g", [2, SSD_IN]); inp("final_g", [D])
        out_d = kb.dram("out", [KC, 128, T], F32, "ExternalOutput")

        cst = load_consts(kb, I)
        tri = [kb.sb([128, 128], F32, "tri%d" % d) for d in range(2)]
        maskneg = [kb.sb([128, 128], F32, "mneg%d" % d) for d in range(2)]
        for d in range(2):
            kb.dma(kb.sp, tri[d][:], I["tri"][d], tri[d], I["tri"])
            kb.dma(kb.sp, maskneg[d][:], I["maskneg"][d], maskneg[d], I["maskneg"])
        sel65 = kb.sb([VD + 1, 64], F32, "sel65")
        mset(kb, kb.dve, sel65[:], 0.0, [sel65])
        mset(kb, kb.dve, sel65[64:65, :], 1.0, [sel65])
        chm = kb.sb([128, 2, R], F32, "chm")
        kb.dma(kb.sp, chm[:], I["chm"][:, :, :], chm, I["chm"])
        hsel = kb.sb([128, 2, R], F32, "hsel")
        kb.dma(kb.sp, hsel[:], I["hsel"][:, :, :], hsel, I["hsel"])

        x_cur, xh_cur, xc_cur = I["xT"], I["xhT"], I["xcT"]
        for l in range(2):
            last = l == 1
            Il = dict(I)
            Il["xT"], Il["xhT"], Il["xcT"] = x_cur, xh_cur, xc_cur

            def scr(name, shape, dtype=F32, kind="Internal"):
                Il[name] = kb.dram("%s_L%d" % (name, l), shape, dtype, kind)
                return Il[name]

            for sfx, n in (("", T), ("c", CTX)):
                scr("QT" + sfx, [H, QK, n], BF16); scr("KT" + sfx, [H, QK, n], BF16); scr("V" + sfx, [n, 512], BF16)
                scr("zs" + sfx, [4, 128, n], BF16); scr("xtok" + sfx, [n, 640], BF16); scr("BCT" + sfx, [2, 128, n], BF16)
                scr("atok" + sfx, [n, 16]); scr("ldtok" + sfx, [n, 16]); scr("S" + sfx, [128, 1024]); scr("atot" + sfx, [1, 16])
            emit_phase_a(kb, Il, T, l, not last, cst, tri)
            KTg = [scr("KTg%d" % h, [R * QK, T], BF16) for h in range(H)]
            VCH = min(T, 1024)
            Vg = [scr("Vg%d" % c, [R * VCH, 512], BF16) for c in range(T // VCH)]
            Sg = scr("Sg", [R * 128, 1024]); atg = scr("atg", [R, 16])
            kb.collective(Il["S"], Il["S"].t[:, :], Sg, Sg.t[:, :], CC_GROUPS)
            kb.collective(Il["atot"], Il["atot"].t[:, :], atg, atg.t[:, :], CC_GROUPS)
            for h in range(H):
                kb.collective(Il["KT"], Il["KT"].t[h], KTg[h], KTg[h].t[:, :], CC_GROUPS)
            for c in range(T // VCH):
                kb.collective(Il["V"], Il["V"].t[c * VCH:(c + 1) * VCH, :], Vg[c], Vg[c].t[:, :], CC_GROUPS)

            def kv_loader(h, k_, v_, nk, Il=Il, KTg=KTg, Vg=Vg, VCH=VCH):
                kb.dma(kb.sp, k_[:, 0:CTX], Il["KTc"].t[h, :, :], k_, Il["KTc"])
                kb.dma(kb.sp, v_[:, 0:CTX // 128, 0:VD],
                       Il["Vc"].t[:, h * VD:(h + 1) * VD].rearrange("(t p) c -> p t c", p=128), v_, Il["Vc"])
                if nk == CTX:
                    return
                ntl = T // 128
                ncl = VCH // 128
                for r in range(R):
                    kb.dma(kb.sp, k_[:, CTX + r * T:CTX + (r + 1) * T], KTg[h].t[r * QK:(r + 1) * QK, :], k_, KTg[h])
                    for c in range(T // VCH):
                        for t0 in range(0, ncl, 16):
                            t1_ = min(ncl, t0 + 16)
                            kb.dma(kb.sp, v_[:, 2 + r * ntl + c * ncl + t0:2 + r * ntl + c * ncl + t1_, 0:VD],
                                   Vg[c].t[r * VCH + t0 * 128:r * VCH + t1_ * 128, h * VD:(h + 1) * VD]
                                   .rearrange("(t p) c -> p t c", p=128), v_, Vg[c])

            def state_loader(d, hst, hflat, ssrc, atb, Il=Il, Sg=Sg, atg=atg):
                def load_S(src, row0):
                    for g in range(2):
                        kb.dma(kb.sp, ssrc[:, g * 256:(g + 1) * 256],
                               src.t[row0 + g * 64:row0 + (g + 1) * 64, d * 512 + g * 256:d * 512 + (g + 1) * 256], ssrc, src)
                load_S(Il["Sc"], 0)
                cp(kb, kb.dve, hflat, ssrc[:], [ssrc], [hst])
                for r in (range(R) if d == 0 else range(R - 1, -1, -1)):
                    mcol = chm[0:64, d, r:r + 1]
                    kb.dma(kb.sp, atb[:], atg.t[r:r + 1, d * 8:(d + 1) * 8].broadcast_to([128, H]), atb, atg)
                    ts(kb, kb.dve, atb[:], atb[:], chm[:, d, r:r + 1], ALU.mult, [atb, chm], [atb])
                    act(kb, atb[:], atb[:], AF.Exp, [atb], [atb])
                    load_S(Sg, r * 128)
                    tt(kb, kb.dve, hst[:], hst[:], atb[0:64, :].unsqueeze(2).broadcast_to([64, H, 64]), ALU.mult, [hst, atb], [hst])
                    stt(kb, kb.dve, hflat, ssrc[:], mcol, hflat, ALU.mult, ALU.add, [ssrc, chm, hst], [hst])

            for sfx, n in ([("", T)] + ([] if last else [("c", CTX)])):
                scr("mix" + sfx, [KC, 128, n], BF16); scr("yfw" + sfx, [n // 128, 64, H * 128])
                scr("x1" + sfx, [KC, 128, n]); scr("h2" + sfx, [KC, 128, n], BF16)
                if sfx == "" and last:
                    Il["xout"] = out_d
                else:
                    scr("xout" + sfx, [KC, 128, n])
            emit_phase_b(kb, Il, T, l, last, cst, tri, maskneg, sel65,
                         loaders={"kv": kv_loader, "state": state_loader})
            if not last:
                xe = scr("xe", [128, KC * 4]); xeg = scr("xeg", [R * 128, KC * 4]); xh1 = scr("xh1", [KC, 128, 4])
                with kb.scope():
                    et = kb.sb([128, KC, 4], F32, "et")
                    kb.dma(kb.sp, et[:, :, 0:2], Il["xout"].t[:, :, 0:2].rearrange("k p c -> p k c"), et, Il["xout"])
                    kb.dma(kb.sp, et[:, :, 2:4], Il["xout"].t[:, :, T - 2:T].rearrange("k p c -> p k c"), et, Il["xout"])
                    kb.dma(kb.sp, xe.t[:, :], et[:].rearrange("p k c -> p (k c)"), xe, et)
                    kb.collective(xe, xe.t[:, :], xeg, xeg.t[:, :], CC_GROUPS)
                    eg = kb.sb([128, R, KC, 4], F32, "eg")
                    kb.dma(kb.sp, eg[:], xeg.t.rearrange("(r p) (k c) -> p r k c", p=128, c=4), eg, xeg)
                    xh = kb.sb([128, KC, 4], F32, "xh")
                    mset(kb, kb.dve, xh[:], 0.0, [xh])
                    for r in range(R):
                        stt(kb, kb.dve, xh[:, :, 0:2], eg[:, r, :, 2:4], hsel[:, 0, r:r + 1], xh[:, :, 0:2], ALU.mult, ALU.add,
                            [eg, hsel, xh], [xh])
                        stt(kb, kb.dve, xh[:, :, 2:4], eg[:, r, :, 0:2], hsel[:, 1, r:r + 1], xh[:, :, 2:4], ALU.mult, ALU.add,
                            [eg, hsel, xh], [xh])
                    kb.dma(kb.sp, xh1.t.rearrange("k p c -> p k c"), xh[:], xh1, xh)
                x_cur, xh_cur, xc_cur = Il["xout"], xh1, Il["xoutc"]
        kb.finish()
    return nc


def host_inputs_fused(W, x_b, ctx_b, c_b, c_ctx, s, T, R, cos, sin):
    d = host_inputs_a(W, 0, x_b, ctx_b, c_b, c_ctx, s, T, cos, sin)
    ident, tri = const_tables()
    d["maskneg"] = ((1.0 - tri) * NEG).astype(np.float32)
    sel = np.zeros((NE, NE, 128), np.float32)
    for e in range(NE):
        sel[e, e, :] = 1.0
    d["sel"] = sel.reshape(NE, NE * 128)
    chm = np.zeros((128, 2, R), np.float32)
    hs = np.zeros((128, 2, R), np.float32)
    for r in range(R):
        chm[:, 0, r] = 1.0 if r < s else 0.0
        chm[:, 1, r] = 1.0 if r > s else 0.0
        hs[:, 0, r] = 1.0 if r == s - 1 else 0.0
        hs[:, 1, r] = 1.0 if r == s + 1 else 0.0
    d["chm"] = chm
    d["hsel"] = hs
    for k in ("norm2_g", "w_out", "router_w1", "router_b1", "router_w2", "router_b2", "w_gate", "w_up", "w_down",
              "d_skip", "ssd_norm_g", "final_g"):
        d[k] = np.ascontiguousarray(W[k], dtype=np.float32)
    return d


def forward_fused(W, x, c, ctx, c_ctx, R=4):
    B, L, _ = x.shape
    T = L // R
    cos, sin = rope_tables_host(L)
    cores = [(b, s) for b in range(B) for s in range(R)]
    ins = [host_inputs_fused(W, x[b], ctx[b], c[b], c_ctx, s, T, R, cos, sin) for (b, s) in cores]
    key = ("fused", T)
    if key not in _NC_CACHE:
        _NC_CACHE[key] = build_fused(T, R)
    res = _run(_NC_CACHE[key], ins)
    out = np.empty((B, L, D), np.float32)
    for ci, (b, s) in enumerate(cores):
        out[b, s * T:(s + 1) * T] = res[ci]["out"].reshape(D, T).T
    return out
```

```python
import contextlib
import numpy as np
import ml_dtypes
import concourse.bass as bass
import concourse.mybir as mybir
from concourse.bass_utils import run_bass_kernel_spmd

F32 = mybir.dt.float32
BF16 = mybir.dt.bfloat16
AF = mybir.ActivationFunctionType
ALU = mybir.AluOpType
AX = mybir.AxisListType
NPBF = ml_dtypes.bfloat16

D = 1024
KC = 8
CTX = 256
H = 8
QL, KVL, ROPE, NOPE, VD = 256, 128, 32, 64, 64
QK = NOPE + ROPE
SSD_IN = 512
NST = 64
IN_COLS = 1712
C_QA, C_KVA, C_KR, C_Z, C_XBC, C_DT = 0, 256, 384, 416, 928, 1696
NE, FF = 32, 256
EPS = 1e-6
GRID_W = 64
SCALE = float(QK) ** -0.5
NEG = -30000.0


class Sem:
    def __init__(self, kb, name):
        self.h = kb.es_top.enter_context(kb.nc.semaphore(name))
        self.n = 0


class Res:
    def __init__(self):
        self.w = {}
        self.r = {}


class Eng:
    def __init__(self, kb, name, be, is_pe=False):
        self.name = name
        self.be = be
        self.is_pe = is_pe
        self.sem = Sem(kb, "s_" + name)
        self.seen = {}


class TT:
    def __init__(self, kb, name, shape, dtype, space="sbuf", kind=None):
        self.kb = kb
        self.name = name
        self.res = Res()
        self.dsem = None
        self.space = space
        if kb.scope_tts and space != "dram":
            kb.scope_tts[-1].append(self)
        if space == "sbuf":
            self.t = kb.es.enter_context(kb.nc.sbuf_tensor(name, list(shape), dtype))
        elif space == "psum":
            self.t = kb.es.enter_context(kb.nc.psum_tensor(name, list(shape), dtype))
        else:
            self.t = kb.nc.dram_tensor(name, list(shape), dtype, kind=kind).ap()

    def __getitem__(self, idx):
        return self.t[idx]

    def get_dsem(self):
        if self.dsem is None:
            if self.kb.free_sems:
                self.dsem = self.kb.free_sems.pop()
            else:
                self.dsem = Sem(self.kb, "d_" + self.name)
        return self.dsem


class KB:
    def __init__(self, nc, es):
        self.nc = nc
        self.es = es
        self.es_top = es
        self.scope_tts = []
        self.free_sems = []
        self.ccsem = None
        self.pe = Eng(self, "pe", nc.tensor, True)
        self.act = Eng(self, "act", nc.scalar)
        self.dve = Eng(self, "dve", nc.vector)
        self.pool = Eng(self, "pool", nc.gpsimd)
        self.sp = Eng(self, "sp", nc.sync)
        self.uid = 0
        self.drams = []

    def name(self, p):
        self.uid += 1
        return "%s_%d" % (p, self.uid)

    def sb(self, shape, dtype, name="t"):
        return TT(self, self.name(name), shape, dtype, "sbuf")

    def ps(self, shape, dtype=F32, name="p"):
        return TT(self, self.name(name), shape, dtype, "psum")

    def dram(self, name, shape, dtype, kind):
        t = TT(self, name, shape, dtype, "dram", kind)
        self.drams.append(t)
        return t

    @contextlib.contextmanager
    def scope(self):
        outer = self.es
        inner = contextlib.ExitStack()
        self.es = inner
        self.scope_tts.append([])
        try:
            yield
        finally:
            tts = self.scope_tts.pop()
            self.barrier(tts)
            for t in tts:
                if t.dsem is not None:
                    self.free_sems.append(t.dsem)
                    t.dsem = None
            inner.close()
            self.es = outer

    def barrier(self, tts=()):
        engs = (self.pe, self.act, self.dve, self.pool, self.sp)
        sems = [e.sem for e in engs] + [t.dsem for t in tts if t.dsem is not None]
        for e in engs:
            for sm in sems:
                if sm is e.sem or sm.n <= 0 or e.seen.get(sm, 0) >= sm.n:
                    continue
                e.be.wait_ge(sm.h, sm.n)
                e.seen[sm] = sm.n

    def _waits(self, eng, reads, writes):
        need = {}
        for r in reads:
            for sm, v in r.res.w.items():
                need[sm] = max(need.get(sm, 0), v)
        for w in writes:
            for sm, v in list(w.res.w.items()) + list(w.res.r.items()):
                need[sm] = max(need.get(sm, 0), v)
        for sm, v in need.items():
            if sm is eng.sem:
                if eng.is_pe:
                    continue
                v = min(v, sm.n)
            if v <= 0 or eng.seen.get(sm, 0) >= v:
                continue
            eng.be.wait_ge(sm.h, v)
            eng.seen[sm] = v

    def op(self, eng, fn, reads=(), writes=(), inc=True):
        self._waits(eng, reads, writes)
        inst = fn(eng.be)
        if inc:
            eng.sem.n += 1
            inst.then_inc(eng.sem.h, 1)
            tick = eng.sem.n
        else:
            tick = eng.sem.n + 1
        for r in reads:
            r.res.r[eng.sem] = max(r.res.r.get(eng.sem, 0), tick)
        for w in writes:
            w.res.w[eng.sem] = max(w.res.w.get(eng.sem, 0), tick)
        return inst

    def dma(self, q, out, in_, dst, src, **kw):
        self._waits(q, [src], [dst])
        owner = dst if dst.space != "dram" else src
        ds = owner.get_dsem()
        inst = q.be.dma_start(out=out, in_=in_, **kw)
        ds.n += 16
        inst.then_inc(ds.h, 16)
        src.res.r[ds] = ds.n
        dst.res.w[ds] = ds.n
        return inst

    def collective(self, in_tt, in_ap, out_tt, out_ap, groups):
        if self.ccsem is None:
            self.ccsem = Sem(self, "ccsem")
        self._waits(self.pool, [in_tt], [out_tt])
        inst = self.pool.be.collective_compute("AllGather", ALU.bypass, replica_groups=groups, ins=[in_ap], outs=[out_ap])
        self.ccsem.n += 1
        inst.then_inc(self.ccsem.h)
        in_tt.res.r[self.ccsem] = self.ccsem.n
        out_tt.res.w[self.ccsem] = self.ccsem.n
        return inst

    def finish(self):
        need = {}
        for t in self.drams:
            for sm, v in t.res.w.items():
                need[sm] = max(need.get(sm, 0), v)
        for sm, v in need.items():
            self.sp.be.wait_ge(sm.h, v)
        for e in (self.pe, self.act, self.dve, self.pool):
            if e.sem.n > 0:
                self.sp.be.wait_ge(e.sem.h, e.sem.n)


def mm(kb, out, lhsT, rhs, start, stop, reads, writes, inc=None):
    if inc is None:
        inc = stop
    return kb.op(kb.pe, lambda e: e.matmul(out, lhsT=lhsT, rhs=rhs, start=start, stop=stop),
                 reads=reads, writes=writes, inc=inc)


def act(kb, out, in_, func, reads, writes, bias=None, scale=1.0, accum_out=None):
    kw = {}
    if bias is not None:
        kw["bias"] = bias
    if accum_out is not None:
        kw["accum_out"] = accum_out
    return kb.op(kb.act, lambda e: e.activation(out=out, in_=in_, func=func, scale=scale, **kw),
                 reads=reads, writes=writes)


def tt(kb, eng, out, in0, in1, op, reads, writes):
    return kb.op(eng, lambda e: e.tensor_tensor(out=out, in0=in0, in1=in1, op=op), reads=reads, writes=writes)


def ts(kb, eng, out, in0, s1, op0, reads, writes, s2=None, op1=None):
    if op1 is None:
        return kb.op(eng, lambda e: e.tensor_scalar(out=out, in0=in0, scalar1=s1, scalar2=None, op0=op0),
                     reads=reads, writes=writes)
    return kb.op(eng, lambda e: e.tensor_scalar(out=out, in0=in0, scalar1=s1, scalar2=s2, op0=op0, op1=op1),
                 reads=reads, writes=writes)


def stt(kb, eng, out, in0, scalar, in1, op0, op1, reads, writes):
    return kb.op(eng, lambda e: e.scalar_tensor_tensor(out=out, in0=in0, scalar=scalar, in1=in1, op0=op0, op1=op1),
                 reads=reads, writes=writes)


def cp(kb, eng, out, in_, reads, writes):
    return kb.op(eng, lambda e: e.tensor_copy(out=out, in_=in_), reads=reads, writes=writes)


def mset(kb, eng, ap, val, writes):
    return kb.op(eng, lambda e: e.memset(ap, val), reads=(), writes=writes)


class Ring:
    def __init__(self, items):
        self.items = items
        self.i = 0

    def next(self):
        t = self.items[self.i % len(self.items)]
        self.i += 1
        return t


def load_consts(kb, ins):
    c = {}
    c["ident_f"] = kb.sb([128, 128], F32, "identf")
    kb.dma(kb.sp, c["ident_f"][:], ins["ident"][:, :], c["ident_f"], ins["ident"])
    c["ident_b"] = kb.sb([128, 128], BF16, "identb")
    kb.dma(kb.pool, c["ident_b"][:], ins["ident"][:, :], c["ident_b"], ins["ident"])
    c["ones_f"] = kb.sb([128, 128], F32, "onesf")
    mset(kb, kb.dve, c["ones_f"][:], 1.0, [c["ones_f"]])
    c["ones_b"] = kb.sb([128, 128], BF16, "onesb")
    mset(kb, kb.dve, c["ones_b"][:], 1.0, [c["ones_b"]])
    c["eps"] = kb.sb([128, 1], F32, "eps")
    mset(kb, kb.dve, c["eps"][:], EPS, [c["eps"]])
    c["one1"] = kb.sb([128, 1], F32, "one1")
    mset(kb, kb.dve, c["one1"][:], 1.0, [c["one1"]])
    c["zero1"] = kb.sb([128, 1], F32, "zero1")
    mset(kb, kb.dve, c["zero1"][:], 0.0, [c["zero1"]])
    return c


def emit_mod(kb, ins, l, cst, sections):
    out = {sec: kb.sb([128, KC, 2], F32, "modT%d" % sec) for sec in sections}
    with kb.scope():
        cT = kb.sb([128, KC, 2], F32, "cT")
        kb.dma(kb.sp, cT[:], ins["cT"][:, :, :], cT, ins["cT"])
        cs = kb.sb([128, KC, 2], F32, "cs")
        act(kb, cs[:], cT[:], AF.Silu, [cT], [cs])
        bm = kb.sb([128, 6 * KC], F32, "bm")
        kb.dma(kb.sp, bm[:], ins["b_mod"][l].rearrange("(c p) -> p c", p=128), bm, ins["b_mod"],
               allow_slow_non_contiguous=True)
        wbufs = Ring([kb.sb([128, KC, 1024], F32, "wmod") for _ in range(2)])
        pm = kb.ps([128, KC, 2], F32, "pmod")
        for sec in sections:
            wt = wbufs.next()
            kb.dma(kb.sp, wt[:], ins["w_mod"][l][:, sec * 1024:(sec + 1) * 1024].rearrange("(k p) n -> p k n", p=128),
                   wt, ins["w_mod"])
            for cc in range(KC):
                for k in range(KC):
                    mm(kb, pm[:, cc, :], wt[:, k, cc * 128:(cc + 1) * 128], cs[:, k, :], k == 0, k == KC - 1,
                       [wt, cs], [pm])
            m = out[sec]
            tt(kb, kb.dve, m[:], pm[:], bm[:, sec * KC:(sec + 1) * KC].unsqueeze(2).broadcast_to([128, KC, 2]),
               ALU.add, [pm, bm], [m])
    return out


def load_col(kb, dram_tt, ap1d, n, name):
    t = kb.sb([128, n], F32, name)
    kb.dma(kb.sp, t[:], ap1d.rearrange("(c p) -> p c", p=128), t, dram_tt, allow_slow_non_contiguous=True)
    return t


def rms_bcast(kb, cst, src_sq_list, n_feat, ncols, psq, out_rstd, reads):
    n = len(src_sq_list)
    for i, (ap, kp) in enumerate(src_sq_list):
        mm(kb, psq[:, :ncols], cst["ones_b"][0:kp, :], ap, i == 0, i == n - 1, reads + [cst["ones_b"]], [psq])
    act(kb, out_rstd[:, :ncols], psq[:, :ncols], AF.Ln, [psq, cst["eps"]], [out_rstd],
        bias=cst["eps"][:, 0:1], scale=1.0 / n_feat)
    act(kb, out_rstd[:, :ncols], out_rstd[:, :ncols], AF.Exp, [out_rstd], [out_rstd], scale=-0.5)


def emit_phase_a(kb, I, T, l, with_ctx_q, cst, tri):
    with kb.scope():
        mod = emit_mod(kb, I, l, cst, [0, 1])
        g1 = load_col(kb, I["norm1_g"], I["norm1_g"][l], KC, "n1g")
        gmod = kb.sb([128, KC, 2], F32, "gmod")
        ts(kb, kb.dve, gmod[:], mod[1][:], 1.0, ALU.add, [mod[1]], [gmod])
        tt(kb, kb.dve, gmod[:], gmod[:], g1[:].unsqueeze(2).broadcast_to([128, KC, 2]), ALU.mult, [gmod, g1], [gmod])
        sh = mod[0]

        cw = kb.sb([128, 6, 5], F32, "cw")
        for k in range(5):
            kb.dma(kb.sp, cw[:, :, k], I["conv_w"][l][k].rearrange("(c p) -> p c", p=128), cw, I["conv_w"],
                   allow_slow_non_contiguous=True)
        cb = load_col(kb, I["conv_b"], I["conv_b"][l], 6, "cb")
        dtb = kb.sb([128, 16], F32, "dtb")
        kb.dma(kb.sp, dtb[:], I["dt_bias"][l:l + 1, :].broadcast_to([128, 16]), dtb, I["dt_bias"])
        Aneg = kb.sb([128, 16], F32, "Aneg")
        kb.dma(kb.sp, Aneg[:], I["a_log"][l:l + 1, :].broadcast_to([128, 16]), Aneg, I["a_log"])
        act(kb, Aneg[:], Aneg[:], AF.Exp, [Aneg], [Aneg])
        ts(kb, kb.dve, Aneg[:], Aneg[:], -1.0, ALU.mult, [Aneg], [Aneg])
        hmask = kb.sb([128, 4], F32, "hmask")
        kb.dma(kb.sp, hmask[:], I["hmask"][:, :], hmask, I["hmask"])

        xbc_l = kb.sb([128, 6, T + 4], BF16, "xbcpre_l")
        xbc_c = kb.sb([128, 6, CTX + 4], BF16, "xbcpre_c")
        mset(kb, kb.pool, xbc_c[:], 0.0, [xbc_c])
        sc_w = kb.scope(); sc_w.__enter__()
        win = kb.sb([128, KC, IN_COLS], BF16, "win")
        for k in range(KC):
            kb.dma(kb.pool, win[:, k, :], I["w_in"][l][k * 128:(k + 1) * 128, :], win, I["w_in"])
        wq = kb.sb([128, 2, H, QK], BF16, "wq")
        wqr = kb.sb([128, 2, H, QK], BF16, "wqr")
        wkn = kb.sb([128, H, NOPE], BF16, "wkn")
        wvv = kb.sb([128, H, VD], BF16, "wvv")
        sc_tmp = kb.scope(); sc_tmp.__enter__()
        wq_f = kb.sb([128, 2, H * QK], F32, "wqf")
        kb.dma(kb.sp, wq_f[:], I["w_qb"][l].rearrange("(k p) n -> p k n", p=128), wq_f, I["w_qb"])
        qg = load_col(kb, I["q_norm_g"], I["q_norm_g"][l], 2, "qg")
        mset(kb, kb.pool, wqr[:], 0.0, [wqr])
        for k in range(2):
            wv_ = wq_f[:, k, :].rearrange("p (h c) -> p h c", h=H)
            ts(kb, kb.dve, wq[:, k], wv_, qg[:, k:k + 1], ALU.mult, [wq_f, qg], [wq])
            ts(kb, kb.dve, wqr[:, k, :, 64:80], wv_[:, :, 80:96], qg[:, k:k + 1], ALU.mult, [wq_f, qg], [wqr], s2=-1.0, op1=ALU.mult)
            ts(kb, kb.dve, wqr[:, k, :, 80:96], wv_[:, :, 64:80], qg[:, k:k + 1], ALU.mult, [wq_f, qg], [wqr])
        wkv_f = kb.sb([128, H, 128], F32, "wkvf")
        kb.dma(kb.sp, wkv_f[:], I["w_kvb"][l].rearrange("p (h c) -> p h c", h=H), wkv_f, I["w_kvb"])
        kvg = load_col(kb, I["kv_norm_g"], I["kv_norm_g"][l], 1, "kvg")
        ts(kb, kb.dve, wkn[:], wkv_f[:, :, 0:NOPE], kvg[:, 0:1], ALU.mult, [wkv_f, kvg], [wkn])
        ts(kb, kb.dve, wvv[:], wkv_f[:, :, NOPE:128], kvg[:, 0:1], ALU.mult, [wkv_f, kvg], [wvv])
        sc_tmp.__exit__(None, None, None)
        wkr = kb.sb([128, KC, QK], BF16, "wkr")
        wkrr = kb.sb([128, KC, QK], BF16, "wkrr")
        mset(kb, kb.pool, wkr[:], 0.0, [wkr])
        mset(kb, kb.pool, wkrr[:], 0.0, [wkrr])
        cp(kb, kb.dve, wkr[:, :, 64:96], win[:, :, C_KR:C_KR + 32], [win], [wkr])
        ts(kb, kb.dve, wkrr[:, :, 64:80], win[:, :, C_KR + 16:C_KR + 32], -1.0, ALU.mult, [win], [wkrr])
        cp(kb, kb.dve, wkrr[:, :, 80:96], win[:, :, C_KR:C_KR + 16], [win], [wkrr])
        TB = 512
        xin = Ring([kb.sb([128, KC, TB], F32, "xin") for _ in range(2)])
        sq_r = Ring([kb.sb([128, KC, TB], BF16, "sq") for _ in range(1)])
        rstd_r = Ring([kb.sb([128, TB], F32, "rstd") for _ in range(2)])
        tmp_r = Ring([kb.sb([128, TB], F32, "tmp") for _ in range(2)])
        hT_r = Ring([kb.sb([128, KC, TB], BF16, "hT") for _ in range(2)])
        psq = kb.ps([128, TB], F32, "psq")
        pacc = Ring([kb.ps([128, TB], F32, "pacc") for _ in range(5)])
        pq2 = kb.ps([128, TB], F32, "pq2")
        qaT = kb.sb([128, 2, TB], BF16, "qaT")
        qsq = kb.sb([128, 2, TB], BF16, "qsq")
        rq = kb.sb([128, TB], F32, "rq")
        Crs = kb.sb([QK, TB], F32, "Crs")
        Srs = kb.sb([QK, TB], F32, "Srs")
        ropeC = kb.sb([QK, TB], F32, "ropeC")
        ropeS = kb.sb([QK, TB], F32, "ropeS")
        t1 = Ring([kb.sb([QK, TB], F32, "t1") for _ in range(1)])
        t2 = Ring([kb.sb([QK, TB], F32, "t2") for _ in range(1)])
        qst = Ring([kb.sb([QK, TB], BF16, "qst") for _ in range(4)])
        kst = Ring([kb.sb([NOPE, TB], BF16, "kst") for _ in range(6)])
        krp = Ring([kb.sb([QK, TB], BF16, "krp") for _ in range(2)])
        kvsq = kb.sb([128, TB], BF16, "kvsq")
        rkv = kb.sb([128, TB], F32, "rkv")
        kvn = kb.sb([128, TB], BF16, "kvn")
        vst = Ring([kb.sb([128, 4, 512], BF16, "vst") for _ in range(1)])
        zst = Ring([kb.sb([128, 4, TB], BF16, "zst") for _ in range(1)])
        dtr = kb.sb([128, 4, 16], F32, "dtr")
        dts = Ring([kb.sb([128, 4, 32], F32, "dts") for _ in range(2)])

        def run_group(n, xT_d, xoff, cidx, sfx, tabs, xbcpre, want_q, halo=None):
            nb = (n + TB - 1) // TB
            pre = [None]

            def load_x(bj):
                hl = bj == nb
                ww, oo = (4, 0) if hl else (min(TB, n - bj * TB), bj * TB)
                sr = halo if hl else xT_d
                xt_ = xin.next()
                for k in range(KC):
                    kb.dma(kb.sp, xt_[:, k, :ww], sr.t[k, :, (0 if hl else xoff + oo):(0 if hl else xoff + oo) + ww], xt_, sr)
                return xt_

            for bi in range(nb + (1 if halo is not None else 0)):
                is_halo = bi == nb
                if is_halo:
                    w_, o0 = 4, 0
                    src = halo
                else:
                    w_, o0 = min(TB, n - bi * TB), bi * TB
                    src = xT_d
                sq = sq_r.next(); rstd = rstd_r.next(); hT = hT_r.next()
                if bi == 0:
                    pre[0] = load_x(0)
                xt = pre[0]
                if bi + 1 < nb + (1 if halo is not None else 0):
                    pre[0] = load_x(bi + 1)
                act(kb, sq[:, :, :w_], xt[:, :, :w_], AF.Square, [xt], [sq])
                rms_bcast(kb, cst, [(sq[:, k, :w_], 128) for k in range(KC)], D, w_, psq, rstd, [sq])
                for k in range(KC):
                    tmp = tmp_r.next()
                    stt(kb, kb.dve, tmp[:, :w_], xt[:, k, :w_], gmod[:, k, cidx:cidx + 1], rstd[:, :w_], ALU.mult, ALU.mult,
                        [xt, gmod, rstd], [tmp])
                    act(kb, hT[:, k, :w_], tmp[:, :w_], AF.Identity, [tmp, sh], [hT], bias=sh[:, k, cidx:cidx + 1])

                def proj(pt, M, c0, wtile=None):
                    for k in range(KC):
                        lw = win[:, k, c0:c0 + M] if wtile is None else wtile[:, k, :]
                        mm(kb, pt[0:M, :w_], lw, hT[:, k, :w_], k == 0, k == KC - 1, [win if wtile is None else wtile, hT], [pt])

                for c in range(6):
                    pt = pacc.next()
                    proj(pt, 128, C_XBC + c * 128)
                    if is_halo:
                        tt(kb, kb.dve, xbcpre[:, c, 0:2], pt[:, 0:2], hmask[:, 0:2], ALU.mult, [pt, hmask], [xbcpre])
                        tt(kb, kb.dve, xbcpre[:, c, n + 2:n + 4], pt[:, 2:4], hmask[:, 2:4], ALU.mult, [pt, hmask], [xbcpre])
                    else:
                        act(kb, xbcpre[:, c, 2 + o0:2 + o0 + w_], pt[:, :w_], AF.Copy, [pt], [xbcpre])
                if is_halo:
                    continue
                zt = zst.next()
                for c in range(4):
                    pt = pacc.next()
                    proj(pt, 128, C_Z + c * 128)
                    act(kb, zt[:, c, :w_], pt[:, :w_], AF.Silu, [pt], [zt])
                for c in range(4):
                    kb.dma(kb.act, I["zs" + sfx].t[c, :, o0:o0 + w_], zt[:, c, :w_], I["zs" + sfx], zt)
                ntl = w_ // 128
                for i in range(ntl):
                    pt = pacc.next()
                    for k in range(KC):
                        mm(kb, pt[:, 0:16], hT[:, k, i * 128:(i + 1) * 128], win[:, k, C_DT:C_DT + 16], k == 0, k == KC - 1,
                           [hT, win], [pt])
                    tt(kb, kb.dve, dtr[:, i, :], pt[:, 0:16], dtb[:], ALU.add, [pt, dtb], [dtr])
                dt_ = dts.next()
                act(kb, dtr[:, :ntl, :], dtr[:, :ntl, :], AF.Exp, [dtr], [dtr])
                act(kb, dtr[:, :ntl, :], dtr[:, :ntl, :], AF.Ln, [dtr, cst["one1"]], [dtr], bias=cst["one1"][:, 0:1])
                tt(kb, kb.dve, dt_[:, :ntl, 0:16], dtr[:, :ntl, :], Aneg[:].unsqueeze(1).broadcast_to([128, ntl, 16]), ALU.mult,
                   [dtr, Aneg], [dt_])
                act(kb, dt_[:, :ntl, 16:32], dtr[:, :ntl, :], AF.Ln, [dtr], [dt_])
                kb.dma(kb.sp, I["atok" + sfx].t[o0:o0 + w_, :].rearrange("(i p) c -> p i c", p=128), dt_[:, :ntl, 0:16],
                       I["atok" + sfx], dt_)
                kb.dma(kb.sp, I["ldtok" + sfx].t[o0:o0 + w_, :].rearrange("(i p) c -> p i c", p=128), dt_[:, :ntl, 16:32],
                       I["ldtok" + sfx], dt_)
                if tabs is not None:
                    kb.dma(kb.sp, ropeC[:, :w_], I["ropeC"].t[:, o0:o0 + w_], ropeC, I["ropeC"])
                    kb.dma(kb.sp, ropeS[:, :w_], I["ropeS"].t[:, o0:o0 + w_], ropeS, I["ropeS"])
                else:
                    mset(kb, kb.pool, ropeC[:], 1.0, [ropeC])
                    mset(kb, kb.pool, ropeS[:], 0.0, [ropeS])
                pt = pacc.next()
                proj(pt, 128, C_KVA)
                act(kb, kvsq[:, :w_], pt[:, :w_], AF.Square, [pt], [kvsq])
                rms_bcast(kb, cst, [(kvsq[:, :w_], 128)], KVL, w_, psq, rkv, [kvsq])
                tt(kb, kb.dve, kvn[:, :w_], pt[:, :w_], rkv[:, :w_], ALU.mult, [pt, rkv], [kvn])
                pa = pacc.next()
                proj(pa, QK, 0, wkr)
                pb = pacc.next()
                proj(pb, QK, 0, wkrr)
                a1 = t1.next(); a2 = t2.next(); kr_ = krp.next()
                tt(kb, kb.dve, a1[64:96, :w_], pa[64:96, :w_], ropeC[64:96, :w_], ALU.mult, [pa, ropeC], [a1])
                tt(kb, kb.dve, a2[64:96, :w_], pb[64:96, :w_], ropeS[64:96, :w_], ALU.mult, [pb, ropeS], [a2])
                tt(kb, kb.pool, kr_[64:96, :w_], a1[64:96, :w_], a2[64:96, :w_], ALU.add, [a1, a2], [kr_])
                for h in range(H):
                    kb.dma(kb.pool, I["KT" + sfx].t[h, 64:96, o0:o0 + w_], kr_[64:96, :w_], I["KT" + sfx], kr_)
                for h in range(H):
                    pt = pacc.next()
                    mm(kb, pt[0:NOPE, :w_], wkn[:, h, :], kvn[:, :w_], True, True, [wkn, kvn], [pt])
                    ks_ = kst.next()
                    act(kb, ks_[:, :w_], pt[0:NOPE, :w_], AF.Copy, [pt], [ks_])
                    kb.dma(kb.act, I["KT" + sfx].t[h, 0:NOPE, o0:o0 + w_], ks_[:, :w_], I["KT" + sfx], ks_)
                vs_ = vst.next()
                for i in range(ntl):
                    pt = pacc.next()
                    mm(kb, pt[:, :], kvn[:, i * 128:(i + 1) * 128], wvv[:].rearrange("p h c -> p (h c)"), True, True,
                       [kvn, wvv], [pt])
                    act(kb, vs_[:, i, :], pt[:, :], AF.Copy, [pt], [vs_])
                kb.dma(kb.act, I["V" + sfx].t[o0:o0 + w_, :].rearrange("(i p) c -> p i c", p=128), vs_[:, :ntl, :],
                       I["V" + sfx], vs_)
                if want_q:
                    for c in range(2):
                        pt = pacc.next()
                        proj(pt, 128, C_QA + c * 128)
                        act(kb, qaT[:, c, :w_], pt[:, :w_], AF.Copy, [pt], [qaT])
                        act(kb, qsq[:, c, :w_], pt[:, :w_], AF.Square, [pt], [qsq])
                    rms_bcast(kb, cst, [(qsq[:, c, :w_], 128) for c in range(2)], QL, w_, psq, rq, [qsq])
                    tt(kb, kb.dve, Crs[:, :w_], ropeC[:, :w_], rq[0:QK, :w_], ALU.mult, [ropeC, rq], [Crs])
                    tt(kb, kb.dve, Srs[:, :w_], ropeS[:, :w_], rq[0:QK, :w_], ALU.mult, [ropeS, rq], [Srs])
                    for h in range(H):
                        pa = pacc.next()
                        for c in range(2):
                            mm(kb, pa[0:QK, :w_], wq[:, c, h, :], qaT[:, c, :w_], c == 0, c == 1, [wq, qaT], [pa])
                        for c in range(2):
                            mm(kb, pq2[0:QK, :w_], wqr[:, c, h, :], qaT[:, c, :w_], c == 0, c == 1, [wqr, qaT], [pq2])
                        a1 = t1.next(); a2 = t2.next(); q_ = qst.next()
                        tt(kb, kb.dve, a1[:, :w_], pa[0:QK, :w_], Crs[:, :w_], ALU.mult, [pa, Crs], [a1])
                        tt(kb, kb.dve, a2[:, :w_], pq2[0:QK, :w_], Srs[:, :w_], ALU.mult, [pq2, Srs], [a2])
                        tt(kb, kb.pool, q_[:, :w_], a1[:, :w_], a2[:, :w_], ALU.add, [a1, a2], [q_])
                        kb.dma(kb.pool, I["QT" + sfx].t[h, :, o0:o0 + w_], q_[:, :w_], I["QT" + sfx], q_)

        def ssd_prep(n, sfx, xbcpre):
            nch = n // 128
            cacc = kb.sb([128, n], F32, "cacc" + sfx)
            xbc = kb.sb([128, 6, n], BF16, "xbc" + sfx)
            for c in range(6):
                ts(kb, kb.dve, cacc[:], xbcpre[:, c, 0:n], cw[:, c, 0:1], ALU.mult, [xbcpre, cw], [cacc])
                for k in range(1, 5):
                    stt(kb, kb.dve, cacc[:], xbcpre[:, c, k:k + n], cw[:, c, k:k + 1], cacc[:], ALU.mult, ALU.add,
                        [xbcpre, cw, cacc], [cacc])
                act(kb, xbc[:, c, :], cacc[:], AF.Silu, [cacc, cb], [xbc], bias=cb[:, c:c + 1])
            for c in range(2):
                kb.dma(kb.sp, I["BCT" + sfx].t[c, :, :], xbc[:, 4 + c, :], I["BCT" + sfx], xbc)
            ptr = Ring([kb.ps([128, 640], BF16, "ptr" + sfx) for _ in range(2)])
            xtk = kb.sb([128, nch, 640], BF16, "xtk" + sfx)
            for ci in range(nch):
                p_ = ptr.next()
                for c in range(5):
                    kb.op(kb.pe, lambda e, c=c, p_=p_, ci=ci: e.transpose(p_[:, c * 128:(c + 1) * 128],
                                                                        xbc[:, c, ci * 128:(ci + 1) * 128], cst["ident_b"][:]),
                          reads=[xbc, cst["ident_b"]], writes=[p_], inc=(c == 4))
                cp(kb, kb.dve, xtk[:, ci, :], p_[:, :], [p_], [xtk])
            kb.dma(kb.sp, I["xtok" + sfx].t.rearrange("(i p) c -> p i c", p=128), xtk[:], I["xtok" + sfx], xtk)
            a_t = kb.sb([128, nch, 16], F32, "a_t" + sfx)
            ld_t = kb.sb([128, nch, 16], F32, "ld_t" + sfx)
            kb.dma(kb.sp, a_t[:], I["atok" + sfx].t.rearrange("(i p) c -> p i c", p=128), a_t, I["atok" + sfx])
            kb.dma(kb.sp, ld_t[:], I["ldtok" + sfx].t.rearrange("(i p) c -> p i c", p=128), ld_t, I["ldtok" + sfx])
            pcs = kb.ps([128, nch, 16], F32, "pcs" + sfx)
            ptot = kb.ps([128, nch, 16], F32, "ptot" + sfx)
            for ci in range(nch):
                for d in range(2):
                    mm(kb, pcs[:, ci, d * 8:(d + 1) * 8], tri[d][:], a_t[:, ci, d * 8:(d + 1) * 8], True, True, [tri[d], a_t], [pcs])
                mm(kb, ptot[:, ci, :], cst["ones_f"][:], a_t[:, ci, :], True, True, [cst["ones_f"], a_t], [ptot])
            tot = kb.sb([128, nch, 16], F32, "tot" + sfx)
            cp(kb, kb.dve, tot[:], ptot[:], [ptot], [tot])
            outer = kb.sb([128, nch, 16], F32, "outer" + sfx)
            mset(kb, kb.dve, outer[:], 0.0, [outer])
            for ci in range(nch - 2, -1, -1):
                tt(kb, kb.dve, outer[:, ci, 0:8], outer[:, ci + 1, 0:8], tot[:, ci + 1, 0:8], ALU.add, [outer, tot], [outer])
            for ci in range(1, nch):
                tt(kb, kb.dve, outer[:, ci, 8:16], outer[:, ci - 1, 8:16], tot[:, ci - 1, 8:16], ALU.add, [outer, tot], [outer])
            wexp = kb.sb([128, nch, 16], F32, "wexp" + sfx)
            tt(kb, kb.dve, wexp[:], tot[:], pcs[:], ALU.subtract, [tot, pcs], [wexp])
            tt(kb, kb.dve, wexp[:], wexp[:], outer[:], ALU.add, [wexp, outer], [wexp])
            tt(kb, kb.dve, wexp[:], wexp[:], ld_t[:], ALU.add, [wexp, ld_t], [wexp])
            act(kb, wexp[:], wexp[:], AF.Exp, [wexp], [wexp])
            pS = [kb.ps([128, 512], F32, "pS%d%s" % (d, sfx)) for d in range(2)]
            xw = Ring([kb.sb([128, 2, H, 64], BF16, "xw" + sfx) for _ in range(2)])
            for ci in range(nch):
                xw_ = xw.next()
                tt(kb, kb.dve, xw_[:], xtk[:, ci, 0:512].rearrange("p (h c) -> p h c", h=H).unsqueeze(1).broadcast_to([128, 2, H, 64]),
                   wexp[:, ci, :].rearrange("p (d h) -> p d h", d=2).unsqueeze(3).broadcast_to([128, 2, H, 64]), ALU.mult,
                   [xtk, wexp], [xw_])
                for d in range(2):
                    mm(kb, pS[d][:, :], xtk[:, ci, 512:640], xw_[:, d].rearrange("p h c -> p (h c)"), ci == 0, ci == nch - 1,
                       [xtk, xw_], [pS[d]], inc=True)
            Ssb = kb.sb([128, 2, 512], F32, "Ssb" + sfx)
            for d in range(2):
                cp(kb, kb.dve, Ssb[:, d, :], pS[d][:, :], [pS[d]], [Ssb])
            kb.dma(kb.sp, I["S" + sfx].t[:, :], Ssb[:].rearrange("p d c -> p (d c)"), I["S" + sfx], Ssb)
            at = kb.sb([128, 16], F32, "at" + sfx)
            tt(kb, kb.dve, at[:, 0:8], outer[:, 0, 0:8], tot[:, 0, 0:8], ALU.add, [outer, tot], [at])
            tt(kb, kb.dve, at[:, 8:16], outer[:, nch - 1, 8:16], tot[:, nch - 1, 8:16], ALU.add, [outer, tot], [at])
            kb.dma(kb.sp, I["atot" + sfx].t[0:1, :], at[0:1, :], I["atot" + sfx], at)

        run_group(CTX, I["xcT"], 0, 1, "c", None, xbc_c, with_ctx_q)
        run_group(T, I["xT"], 0, 0, "", True, xbc_l, True, halo=I["xhT"])
        sc_w.__exit__(None, None, None)
        with kb.scope():
            ssd_prep(CTX, "c", xbc_c)
        with kb.scope():
            ssd_prep(T, "", xbc_l)


def build_phase_a(T, l, with_ctx_q):
    nc = bass.Bass("TRN2", target_bir_lowering=False)
    es = contextlib.ExitStack()
    with es:
        kb = KB(nc, es)
        I = {}

        def inp(name, shape, dtype=F32):
            I[name] = kb.dram(name, shape, dtype, "ExternalInput")

        def outp(name, shape, dtype=F32):
            I[name] = kb.dram(name, shape, dtype, "ExternalOutput")

        inp("xT", [KC, 128, T]); inp("xhT", [KC, 128, 4]); inp("hmask", [128, 4]); inp("xcT", [KC, 128, CTX])
        inp("cT", [128, KC, 2]); inp("ident", [128, 128]); inp("tri", [2, 128, 128])
        inp("ropeC", [QK, T]); inp("ropeS", [QK, T])
        inp("w_mod", [2, D, 6 * D]); inp("b_mod", [2, 6 * D]); inp("norm1_g", [2, D])
        inp("w_in", [2, D, IN_COLS]); inp("q_norm_g", [2, QL]); inp("w_qb", [2, QL, H * QK])
        inp("kv_norm_g", [2, KVL]); inp("w_kvb", [2, KVL, H * 128])
        inp("conv_w", [2, 5, 768]); inp("conv_b", [2, 768]); inp("a_log", [2, 16]); inp("dt_bias", [2, 16])
        for sfx, n in (("", T), ("c", CTX)):
            outp("QT" + sfx, [H, QK, n], BF16); outp("KT" + sfx, [H, QK, n], BF16); outp("V" + sfx, [n, 512], BF16)
            outp("zs" + sfx, [4, 128, n], BF16); outp("xtok" + sfx, [n, 640], BF16); outp("BCT" + sfx, [2, 128, n], BF16)
            outp("atok" + sfx, [n, 16]); outp("ldtok" + sfx, [n, 16]); outp("S" + sfx, [128, 1024]); outp("atot" + sfx, [1, 16])

        cst = load_consts(kb, I)
        tri = [kb.sb([128, 128], F32, "tri%d" % d) for d in range(2)]
        for d in range(2):
            kb.dma(kb.sp, tri[d][:], I["tri"][d], tri[d], I["tri"])
        emit_phase_a(kb, I, T, l, with_ctx_q, cst, tri)
        kb.finish()
    return nc


def const_tables():
    j = np.arange(128)
    tri = np.stack([(j[:, None] <= j[None, :]), (j[:, None] >= j[None, :])]).astype(np.float32)
    return np.eye(128, dtype=np.float32), tri


def rope_tables_host(L):
    rows = L // GRID_W
    row = np.repeat(np.arange(rows), GRID_W)
    col = np.tile(np.arange(GRID_W), rows)
    inv = (10000.0 ** (-np.arange(8, dtype=np.float32) / 8)).astype(np.float32)
    ang = np.concatenate([row[:, None] * inv, col[:, None] * inv], -1).astype(np.float32)
    return np.cos(ang).astype(np.float32), np.sin(ang).astype(np.float32)


def fm(x2d):
    n = x2d.shape[0]
    return np.ascontiguousarray(x2d.T.reshape(KC, 128, n))


def host_inputs_a(W, l, x, ctx, c, cc, s, T, cos, sin):
    L = x.shape[0]
    ident, tri = const_tables()
    idx = [s * T - 2, s * T - 1, (s + 1) * T, (s + 1) * T + 1]
    halo = np.zeros((4, D), np.float32)
    hm = np.zeros((128, 4), np.float32)
    for i, t in enumerate(idx):
        if 0 <= t < L:
            halo[i] = x[t]
            hm[:, i] = 1.0
    ropeC = np.ones((QK, T), np.float32)
    ropeS = np.zeros((QK, T), np.float32)
    ropeC[64:80] = cos[s * T:(s + 1) * T].T
    ropeC[80:96] = cos[s * T:(s + 1) * T].T
    ropeS[64:80] = sin[s * T:(s + 1) * T].T
    ropeS[80:96] = sin[s * T:(s + 1) * T].T
    cT = np.stack([c.reshape(KC, 128).T, cc.reshape(KC, 128).T], -1).astype(np.float32)
    d = dict(xT=fm(x[s * T:(s + 1) * T]), xhT=fm(halo), hmask=hm, xcT=fm(ctx), cT=np.ascontiguousarray(cT),
             ident=ident, tri=tri, ropeC=ropeC, ropeS=ropeS)
    for k in ("w_mod", "b_mod", "norm1_g", "w_in", "q_norm_g", "w_qb", "kv_norm_g", "w_kvb", "conv_w", "conv_b"):
        d[k] = np.ascontiguousarray(W[k], dtype=np.float32)
    d["a_log"] = np.ascontiguousarray(W["a_log"], dtype=np.float32).reshape(2, 16)
    d["dt_bias"] = np.ascontiguousarray(W["dt_bias"], dtype=np.float32).reshape(2, 16)
    return d


def emit_attention(kb, cst, nq, QT_d, nk, KT_d, V_d, mix_d, sel65, loaders=None):
    NT = nk // 128
    QB = min(512, nq)
    with kb.scope():
        kT = Ring([kb.sb([QK, nk], BF16, "kT") for _ in range(2)])
        vA = Ring([kb.sb([128, NT, VD + 1], BF16, "vA") for _ in range(2)])
        for v_ in vA.items:
            mset(kb, kb.pool, v_[:], 1.0, [v_])
        qT = Ring([kb.sb([QK, nq], BF16, "qT") for _ in range(2)])
        pss = Ring([kb.ps([128, 1024], F32, "pss") for _ in range(2)])
        pso = Ring([kb.ps([128, 512], F32, "pso") for _ in range(2)])
        pden = kb.ps([64, 512], F32, "pden")
        pT = Ring([kb.sb([128, 1024], BF16, "pT") for _ in range(3)])
        osb = Ring([kb.sb([VD + 1, 512], F32, "osb") for _ in range(2)])
        rec = kb.sb([64, 512], F32, "rec")
        ost = Ring([kb.sb([64, 512], BF16, "ost") for _ in range(2)])
        def load_head(h):
            k_ = kT.next(); v_ = vA.next(); q_ = qT.next()
            if loaders is not None:
                loaders["kv"](h, k_, v_, nk)
            else:
                kb.dma(kb.sp, k_[:, :], KT_d.t[h, :, 0:nk], k_, KT_d)
                for t0 in range(0, NT, 16):
                    t1_ = min(NT, t0 + 16)
                    kb.dma(kb.sp, v_[:, t0:t1_, 0:VD],
                           V_d.t[t0 * 128:t1_ * 128, h * VD:(h + 1) * VD].rearrange("(t p) c -> p t c", p=128), v_, V_d)
            kb.dma(kb.sp, q_[:, :], QT_d.t[h, :, 0:nq], q_, QT_d)
            return k_, v_, q_

        fin_pend = [None]

        def finish_block(o_, h, qs):
            mm(kb, pden[:, :QB], sel65[:, :], o_[:, :QB], True, True, [sel65, o_], [pden])
            kb.op(kb.dve, lambda e: e.reciprocal(out=rec[:, :QB], in_=pden[:, :QB]), reads=[pden], writes=[rec])
            s_ = ost.next()
            tt(kb, kb.dve, s_[:, :QB], o_[0:64, :QB], rec[:, :QB], ALU.mult, [o_, rec], [s_])
            kb.dma(kb.pool, mix_d.t[h // 2, (h % 2) * 64:(h % 2) * 64 + 64, qs], s_[:, :QB], mix_d, s_)

        nxt = load_head(0)
        for h in range(H):
            k_, v_, q_ = nxt
            if h + 1 < H:
                nxt = load_head(h + 1)
            for qb in range(nq // QB):
                qs = slice(qb * QB, (qb + 1) * QB)
                po = pso.next()
                npair = NT // 2
                pend = None

                def pv(pr, p_):
                    for j in range(2):
                        kt = 2 * pr + j
                        mm(kb, po[0:VD + 1, :QB], v_[:, kt, :], p_[:, j * 512:j * 512 + QB], kt == 0, kt == NT - 1,
                           [v_, p_], [po], inc=(j == 1))

                for pr in range(npair):
                    ps_ = pss.next()
                    for j in range(2):
                        kt = 2 * pr + j
                        mm(kb, ps_[:, j * 512:j * 512 + QB], k_[:, kt * 128:(kt + 1) * 128], q_[:, qs], True, True,
                           [k_, q_], [ps_], inc=(j == 1))
                    p_ = pT.next()
                    if QB == 512:
                        act(kb, p_[:, :], ps_[:, :], AF.Exp, [ps_], [p_], scale=SCALE)
                    else:
                        for j in range(2):
                            act(kb, p_[:, j * 512:j * 512 + QB], ps_[:, j * 512:j * 512 + QB], AF.Exp, [ps_], [p_], scale=SCALE)
                    if pend is not None:
                        pv(*pend)
                    pend = (pr, p_)
                    if pr == 1 and fin_pend[0] is not None:
                        finish_block(*fin_pend[0])
                        fin_pend[0] = None
                pv(*pend)
                if fin_pend[0] is not None:
                    finish_block(*fin_pend[0])
                    fin_pend[0] = None
                o_ = osb.next()
                cp(kb, kb.dve, o_[:, :QB], po[0:VD + 1, :QB], [po], [o_])
                fin_pend[0] = (o_, h, qs)
        if fin_pend[0] is not None:
            finish_block(*fin_pend[0])


def emit_ssd(kb, cst, l, n, sfx, I, mix_d, yfw_d, tri, maskneg, loaders=None):
    nch = n // 128
    import os
    STOP = float(os.environ.get('SSD_STOP', '99'))
    with kb.scope():
        xtk = kb.sb([128, nch, 640], BF16, "xtk")
        kb.dma(kb.sp, xtk[:], I["xtok" + sfx].t.rearrange("(i p) c -> p i c", p=128), xtk, I["xtok" + sfx])
        bct = kb.sb([64, 2, 2, n], BF16, "bct")
        for c in range(2):
            kb.dma(kb.sp, bct[:, c, :, :], I["BCT" + sfx].t[c].rearrange("(g n) t -> n g t", g=2), bct, I["BCT" + sfx])
        a_t = kb.sb([128, nch, 16], F32, "a_t")
        ld_t = kb.sb([128, nch, 16], F32, "ld_t")
        kb.dma(kb.sp, a_t[:], I["atok" + sfx].t.rearrange("(i p) c -> p i c", p=128), a_t, I["atok" + sfx])
        kb.dma(kb.sp, ld_t[:], I["ldtok" + sfx].t.rearrange("(i p) c -> p i c", p=128), ld_t, I["ldtok" + sfx])
        dsk = kb.sb([128, H], F32, "dsk")
        kb.dma(kb.sp, dsk[:], I["d_skip"][l:l + 1, :].broadcast_to([128, H]), dsk, I["d_skip"])
        DI = kb.sb([128, H, 128], F32, "DI")
        tt(kb, kb.dve, DI[:], cst["ident_f"][:].unsqueeze(1).broadcast_to([128, H, 128]),
           dsk[:].unsqueeze(2).broadcast_to([128, H, 128]), ALU.mult, [cst["ident_f"], dsk], [DI])
        sng = kb.sb([64, H], F32, "sng")
        kb.dma(kb.sp, sng[:], I["ssd_norm_g"][l].rearrange("(h p) -> p h", p=64), sng, I["ssd_norm_g"],
               allow_slow_non_contiguous=True)
        zs_v = I["zs" + sfx].t.rearrange("c q t -> (c q) t").rearrange("(h p) t -> p h t", p=64)
        mix_v = mix_d.t[4:8].rearrange("c q t -> (c q) t").rearrange("(h p) t -> p h t", p=64)
        pct = kb.ps([128, 16], F32, "pct")
        pB = kb.ps([128, H, 128], F32, "pB")
        pG = kb.ps([128, 2, 128], F32, "pG")
        py = kb.ps([64, H, 128], F32, "py")
        pSt = kb.ps([64, 512], F32, "pSt")
        psq = kb.ps([64, 128], F32, "psqs")
        R__r = Ring([kb.sb([128, H, 128], F32, "R") for _ in range(2)])
        csl_r = Ring([kb.sb([128, H], F32, "csl") for _ in range(2)])
        dec_r = Ring([kb.sb([128, H], F32, "dec") for _ in range(2)])
        wj_r = Ring([kb.sb([128, H], F32, "wj") for _ in range(2)])
        D1_r = Ring([kb.sb([128, H, 128], F32, "D1") for _ in range(2)])
        E__r = Ring([kb.sb([128, H, 128], F32, "E") for _ in range(2)])
        Cx_r = Ring([kb.sb([128, H, 128], F32, "Cx") for _ in range(2)])
        CexpT_r = Ring([kb.sb([64, H, 128], BF16, "CexpT") for _ in range(2)])
        STf_r = Ring([kb.sb([128, H, 128], F32, "STf") for _ in range(2)])
        STb_r = Ring([kb.sb([128, H, 128], BF16, "STb") for _ in range(2)])
        xw_r = Ring([kb.sb([128, H, 64], BF16, "xw") for _ in range(2)])
        hst = kb.sb([64, H, 64], F32, "hst")
        hpb = kb.sb([64, H, 64], BF16, "hpb")
        ssrc = kb.sb([64, 512], F32, "ssrc")
        atb = kb.sb([128, H], F32, "atb")
        ysb = Ring([kb.sb([64, H, 128], F32, "ysb") for _ in range(2)])
        yfl = Ring([kb.sb([64, H, 128], F32, "yfl") for _ in range(2)])
        zt = Ring([kb.sb([64, H, 128], BF16, "zt") for _ in range(2)])
        yg = kb.sb([64, H, 128], F32, "yg")
        ysq = kb.sb([64, H, 128], BF16, "ysq")
        rs_ = kb.sb([64, 128], F32, "rs")
        yo = Ring([kb.sb([64, H, 128], BF16, "yo") for _ in range(2)])
        hflat = hst[:].rearrange("p h c -> p (h c)")
        for d in range(2):
            if loaders is not None and "state" in loaders:
                loaders["state"](d, hst, hflat, ssrc, atb)
            elif loaders is not None or ("Sch" + sfx) not in I:
                mset(kb, kb.dve, hst[:], 0.0, [hst])
            else:
                for k in range(3, -1, -1):
                    kb.dma(kb.sp, ssrc[:], I["Sch" + sfx].t[d, k], ssrc, I["Sch" + sfx])
                    if k == 3:
                        cp(kb, kb.dve, hflat, ssrc[:], [ssrc], [hst])
                    else:
                        kb.dma(kb.sp, atb[:], I["atch" + sfx].t[d, k:k + 1, :].broadcast_to([128, H]), atb, I["atch" + sfx])
                        act(kb, atb[:], atb[:], AF.Exp, [atb], [atb])
                        tt(kb, kb.dve, hst[:], hst[:], atb[0:64, :].unsqueeze(2).broadcast_to([64, H, 64]), ALU.mult, [hst, atb], [hst])
                        tt(kb, kb.dve, hflat, hflat, ssrc[:], ALU.add, [hst, ssrc], [hst])
            cp(kb, kb.dve, hpb[:], hst[:], [hst], [hpb])
            def stage1(ci, t_out, d=d):
                tsl = slice(ci * 128, (ci + 1) * 128)
                R_ = R__r.next(); csl = csl_r.next(); dec = dec_r.next(); wj = wj_r.next(); D1 = D1_r.next(); E_ = E__r.next(); Cx = Cx_r.next(); CexpT = CexpT_r.next(); STf = STf_r.next(); STb = STb_r.next(); xw = xw_r.next()
                a_d = a_t[:, ci, d * 8:(d + 1) * 8]
                mm(kb, pct[:, 0:8], tri[d][:], a_d, True, True, [tri[d], a_t], [pct])
                yield
                mm(kb, pct[:, 8:16], cst["ones_f"][:], a_d, True, True, [cst["ones_f"], a_t], [pct])
                yield
                tt(kb, kb.pool, R_[:], tri[d][:].unsqueeze(1).broadcast_to([128, H, 128]),
                   a_d.unsqueeze(2).broadcast_to([128, H, 128]), ALU.mult, [tri[d], a_t], [R_])
                yield
                for hh in range(2):
                    mm(kb, pB[:, hh * 4:(hh + 1) * 4, :].rearrange("p h i -> p (h i)"), cst["ones_f"][:],
                       R_[:, hh * 4:(hh + 1) * 4, :].rearrange("p h i -> p (h i)"), True, True, [cst["ones_f"], R_], [pB])
                    yield
                tt(kb, kb.dve, csl[:], pct[:, 0:8], ld_t[:, ci, d * 8:(d + 1) * 8], ALU.subtract, [pct, ld_t], [csl])
                yield
                act(kb, dec[:], pct[:, 8:16], AF.Exp, [pct], [dec])
                yield
                tt(kb, kb.dve, wj[:], pct[:, 8:16], csl[:], ALU.subtract, [pct, csl], [wj])
                yield
                act(kb, wj[:], wj[:], AF.Exp, [wj], [wj])
                yield
                tt(kb, kb.dve, D1[:], pB[:], csl[:].unsqueeze(2).broadcast_to([128, H, 128]), ALU.subtract, [pB, csl], [D1])
                yield
                tt(kb, kb.pool, D1[:], D1[:], maskneg[d][:].unsqueeze(1).broadcast_to([128, H, 128]), ALU.add,
                   [D1, maskneg[d]], [D1])
                yield
                act(kb, E_[:], D1[:], AF.Exp, [D1], [E_])
                yield
                act(kb, Cx[:], pB[:], AF.Exp, [pB], [Cx])
                yield
                tt(kb, kb.dve, CexpT[:].rearrange("p (g h) i -> p g h i", g=2), Cx[0:64].rearrange("p (g h) i -> p g h i", g=2),
                   bct[:, 1, :, tsl].unsqueeze(2).broadcast_to([64, 2, 4, 128]), ALU.mult, [Cx, bct], [CexpT])
                yield
                for g in range(2):
                    mm(kb, pG[:, g, :], bct[:, 0, g, tsl], bct[:, 1, g, tsl], True, True, [bct], [pG])
                    yield
                Ev = E_[:].rearrange("p (g h) i -> p g h i", g=2)
                Gv = pG[:].unsqueeze(2).broadcast_to([128, 2, 4, 128])
                if d == 0:
                    tt(kb, kb.dve, STf[:].rearrange("p (g h) i -> p g h i", g=2), Ev, Gv, ALU.mult, [E_, pG], [STf])
                    yield
                    tt(kb, kb.pool, STb[:], STf[:], DI[:], ALU.add, [STf, DI], [STb])
                    yield
                else:
                    tt(kb, kb.dve, STb[:].rearrange("p (g h) i -> p g h i", g=2), Ev, Gv, ALU.mult, [E_, pG], [STb])
                    yield
                t_out.update(tsl=tsl, wj=wj, dec=dec, CexpT=CexpT, STb=STb, xw=xw)

            def stage2(ci, t_, d=d):
                tsl = t_['tsl']; wj = t_['wj']; dec = t_['dec']; CexpT = t_['CexpT']; STb = t_['STb']; xw = t_['xw']
                for h in range(H):
                    g = h // 4
                    mm(kb, py[:, h, :], xtk[:, ci, h * 64:(h + 1) * 64], STb[:, h, :], True, False, [xtk, STb], [py], inc=False)
                    mm(kb, py[:, h, :], hpb[:, h, :], CexpT[:, h, :], False, True, [hpb, CexpT], [py], inc=True)
                    yield
                tt(kb, kb.pool, xw[:], xtk[:, ci, 0:512].rearrange("p (h c) -> p h c", h=H),
                   wj[:].unsqueeze(2).broadcast_to([128, H, 64]), ALU.mult, [xtk, wj], [xw])
                yield
                for g in range(2):
                    mm(kb, pSt[:, g * 256:(g + 1) * 256], xtk[:, ci, 512 + g * 64:512 + (g + 1) * 64],
                       xw[:, g * 4:(g + 1) * 4, :].rearrange("p h c -> p (h c)"), True, True, [xtk, xw], [pSt])
                    yield
                tt(kb, kb.dve, hst[:], hst[:], dec[0:64, :].unsqueeze(2).broadcast_to([64, H, 64]), ALU.mult, [hst, dec], [hst])
                yield
                tt(kb, kb.dve, hflat, hflat, pSt[:, :], ALU.add, [hst, pSt], [hst])
                yield
                cp(kb, kb.dve, hpb[:], hst[:], [hst], [hpb])
                yield
                if d == 0:
                    y_ = ysb.next()
                    cp(kb, kb.dve, y_[:], py[:], [py], [y_])
                    yield
                    kb.dma(kb.sp, yfw_d.t[ci].rearrange("p (h i) -> p h i", h=H), y_[:], yfw_d, y_)
                    yield
                else:
                    yf_ = yfl.next(); z_ = zt.next()
                    kb.dma(kb.sp, yf_[:], yfw_d.t[ci].rearrange("p (h i) -> p h i", h=H), yf_, yfw_d)
                    yield
                    kb.dma(kb.sp, z_[:], zs_v[:, :, tsl], z_, I["zs" + sfx])
                    yield
                    tt(kb, kb.dve, yg[:], py[:], yf_[:], ALU.add, [py, yf_], [yg])
                    yield
                    tt(kb, kb.pool, yg[:], yg[:], z_[:], ALU.mult, [yg, z_], [yg])
                    yield
                    act(kb, ysq[:], yg[:], AF.Square, [yg], [ysq])
                    yield
                    for h in range(H):
                        mm(kb, psq[:, :], cst["ones_b"][0:64, 0:64], ysq[:, h, :], h == 0, h == H - 1, [cst["ones_b"], ysq], [psq])
                        yield
                    act(kb, rs_[:], psq[:, :], AF.Ln, [psq, cst["eps"]], [rs_], bias=cst["eps"][0:64, 0:1], scale=1.0 / SSD_IN)
                    yield
                    act(kb, rs_[:], rs_[:], AF.Exp, [rs_], [rs_], scale=-0.5)
                    yield
                    tt(kb, kb.dve, yg[:], yg[:], rs_[:].unsqueeze(1).broadcast_to([64, H, 128]), ALU.mult, [yg, rs_], [yg])
                    yield
                    o_ = yo.next()
                    tt(kb, kb.pool, o_[:], yg[:], sng[:].unsqueeze(2).broadcast_to([64, H, 128]), ALU.mult, [yg, sng], [o_])
                    yield
                    kb.dma(kb.sp, mix_v[:, :, tsl], o_[:], mix_d, o_)
                    yield


            order = list(range(nch)) if d == 0 else list(range(nch - 1, -1, -1))

            def zip_run(gens):
                gens = [g for g in gens if g is not None]
                while gens:
                    for g in list(gens):
                        try:
                            next(g)
                        except StopIteration:
                            gens.remove(g)

            pend = None
            for ci in order:
                t_ = {}
                zip_run([stage1(ci, t_), stage2(*pend) if pend is not None else None])
                pend = (ci, t_)
            zip_run([stage2(*pend)])


def emit_tail(kb, cst, l, n, cidx, xin_d, mix_d, x1_d, h2_d, xout_d, I, mod, last):
    TBk = min(512, n)
    nblk = n // TBk
    g1c, sh2, g2c = mod[2], mod[3], mod[5]
    gT = kb.sb([NE, n], BF16, "gT")
    with kb.scope():
        wout = kb.sb([128, KC, D], BF16, "wout")
        for k in range(KC):
            kb.dma(kb.pool, wout[:, k, :], I["w_out"][l][k * 128:(k + 1) * 128, :], wout, I["w_out"])
        n2g = load_col(kb, I["norm2_g"], I["norm2_g"][l], KC, "n2g")
        gmod2 = kb.sb([128, KC], F32, "gmod2")
        ts(kb, kb.dve, gmod2[:], mod[4][:, :, cidx], 1.0, ALU.add, [mod[4]], [gmod2])
        tt(kb, kb.dve, gmod2[:], gmod2[:], n2g[:], ALU.mult, [gmod2, n2g], [gmod2])
        rw = kb.sb([128, KC, 36], F32, "rw")
        kb.dma(kb.sp, rw[:, :, 0:4], I["router_w1"][l].rearrange("(k p) c -> p k c", p=128), rw, I["router_w1"])
        kb.dma(kb.sp, rw[:, :, 4:36], I["router_w2"][l].rearrange("(k p) c -> p k c", p=128), rw, I["router_w2"])
        rb = kb.sb([128, 36], F32, "rb")
        kb.dma(kb.sp, rb[:, 0:4], I["router_b1"][l:l + 1, :].broadcast_to([128, 4]), rb, I["router_b1"])
        kb.dma(kb.sp, rb[:, 4:36], I["router_b2"][l:l + 1, :].broadcast_to([128, 32]), rb, I["router_b2"])
        mixb = Ring([kb.sb([128, KC, TBk], BF16, "mixb") for _ in range(2)])
        xt__rr = Ring([kb.sb([128, KC, TBk], F32, "xt") for _ in range(2)])
        x1_rr = Ring([kb.sb([128, KC, TBk], F32, "x1") for _ in range(2)])
        sq_rr = Ring([kb.sb([128, KC, TBk], BF16, "sq2") for _ in range(2)])
        rstd = kb.sb([128, TBk], F32, "rstd2")
        tmp = kb.sb([128, TBk], F32, "tmp2")
        h2f_rr = Ring([kb.sb([128, KC, TBk], F32, "h2f") for _ in range(2)])
        h2b_rr = Ring([kb.sb([128, KC, TBk], BF16, "h2b") for _ in range(2)])
        po = Ring([kb.ps([128, TBk], F32, "po") for _ in range(2)])
        psq = kb.ps([128, TBk], F32, "psq2")
        plg = kb.ps([128, 36], F32, "plg")
        pgt = kb.ps([NE, 128], F32, "pgt")
        lg = kb.sb([128, 36], F32, "lg")
        sm = kb.sb([128, 16], F32, "sm")
        e1 = kb.sb([128, 4], F32, "e1")
        ohg = kb.sb([128, 4], F32, "ohg")
        l2g = kb.sb([128, 4, 8], F32, "l2g")
        lsel = kb.sb([128, 8], F32, "lsel")
        e2 = kb.sb([128, 8], F32, "e2")
        mk1 = kb.sb([128, 8], F32, "mk1")
        mk2 = kb.sb([128, 8], F32, "mk2")
        lp = kb.sb([128, 8], F32, "lp")
        wi = kb.sb([128, 8], F32, "wi")
        gate = kb.sb([128, 4, 8], F32, "gate")
        for b in range(nblk):
            bs = slice(b * TBk, (b + 1) * TBk)
            mb = mixb.next()
            xt_ = xt__rr.next(); x1 = x1_rr.next(); sq = sq_rr.next(); h2f = h2f_rr.next(); h2b = h2b_rr.next()
            for k in range(KC):
                kb.dma(kb.sp, mb[:, k, :], mix_d.t[k, :, bs], mb, mix_d)
                kb.dma(kb.sp, xt_[:, k, :], xin_d.t[k, :, bs], xt_, xin_d)
            for dc in range(KC):
                p_ = po.next()
                for k in range(KC):
                    mm(kb, p_[:, :], wout[:, k, dc * 128:(dc + 1) * 128], mb[:, k, :], k == 0, k == KC - 1, [wout, mb], [p_])
                stt(kb, kb.dve, x1[:, dc, :], p_[:, :], g1c[:, dc, cidx:cidx + 1], xt_[:, dc, :], ALU.mult, ALU.add,
                    [p_, g1c, xt_], [x1])
            for k in range(KC):
                kb.dma(kb.sp, x1_d.t[k, :, bs], x1[:, k, :], x1_d, x1)
            act(kb, sq[:], x1[:], AF.Square, [x1], [sq])
            rms_bcast(kb, cst, [(sq[:, k, :], 128) for k in range(KC)], D, TBk, psq, rstd, [sq])
            for k in range(KC):
                stt(kb, kb.dve, tmp[:], x1[:, k, :], gmod2[:, k:k + 1], rstd[:], ALU.mult, ALU.mult, [x1, gmod2, rstd], [tmp])
                act(kb, h2f[:, k, :], tmp[:], AF.Identity, [tmp, sh2], [h2f], bias=sh2[:, k, cidx:cidx + 1])
            cp(kb, kb.pool, h2b[:], h2f[:], [h2f], [h2b])
            for k in range(KC):
                kb.dma(kb.sp, h2_d.t[k, :, bs], h2b[:, k, :], h2_d, h2b)
            for i in range(TBk // 128):
                for k in range(KC):
                    mm(kb, plg[:, :], h2f[:, k, i * 128:(i + 1) * 128], rw[:, k, :], k == 0, k == KC - 1, [h2f, rw], [plg])
                tt(kb, kb.dve, lg[:], plg[:, :], rb[:], ALU.add, [plg, rb], [lg])
                R = [lg]
                kb.op(kb.dve, lambda e: e.tensor_reduce(out=sm[:, 0:1], in_=lg[:, 0:4], axis=AX.X, op=ALU.max), reads=R, writes=[sm])
                ts(kb, kb.dve, sm[:, 1:2], sm[:, 0:1], -1.0, ALU.mult, [sm], [sm])
                act(kb, e1[:], lg[:, 0:4], AF.Exp, [lg, sm], [e1, sm], bias=sm[:, 1:2], accum_out=sm[:, 2:3])
                kb.op(kb.dve, lambda e: e.reciprocal(out=sm[:, 3:4], in_=sm[:, 2:3]), reads=[sm], writes=[sm])
                ts(kb, kb.dve, ohg[:], lg[:, 0:4], sm[:, 0:1], ALU.is_equal, [lg, sm], [ohg])
                tt(kb, kb.dve, l2g[:], lg[:, 4:36].rearrange("p (g e) -> p g e", g=4), ohg[:].unsqueeze(2).broadcast_to([128, 4, 8]),
                   ALU.mult, [lg, ohg], [l2g])
                kb.op(kb.dve, lambda e: e.tensor_reduce(out=lsel[:], in_=l2g[:].rearrange("p g e -> p e g"), axis=AX.X, op=ALU.add),
                      reads=[l2g], writes=[lsel])
                kb.op(kb.dve, lambda e: e.tensor_reduce(out=sm[:, 4:5], in_=lsel[:], axis=AX.X, op=ALU.max), reads=[lsel], writes=[sm])
                ts(kb, kb.dve, sm[:, 5:6], sm[:, 4:5], -1.0, ALU.mult, [sm], [sm])
                act(kb, e2[:], lsel[:], AF.Exp, [lsel, sm], [e2], bias=sm[:, 5:6])
                ts(kb, kb.dve, mk1[:], lsel[:], sm[:, 4:5], ALU.is_equal, [lsel, sm], [mk1])
                stt(kb, kb.dve, lp[:], mk1[:], -1.0e30, lsel[:], ALU.mult, ALU.add, [mk1, lsel], [lp])
                kb.op(kb.dve, lambda e: e.tensor_reduce(out=sm[:, 6:7], in_=lp[:], axis=AX.X, op=ALU.max), reads=[lp], writes=[sm])
                ts(kb, kb.dve, mk2[:], lp[:], sm[:, 6:7], ALU.is_equal, [lp, sm], [mk2])
                tt(kb, kb.dve, mk1[:], mk1[:], mk2[:], ALU.add, [mk1, mk2], [mk1])
                tt(kb, kb.dve, wi[:], e2[:], mk1[:], ALU.mult, [e2, mk1], [wi])
                kb.op(kb.dve, lambda e: e.tensor_reduce(out=sm[:, 7:8], in_=wi[:], axis=AX.X, op=ALU.add), reads=[wi], writes=[sm])
                kb.op(kb.dve, lambda e: e.reciprocal(out=sm[:, 8:9], in_=sm[:, 7:8]), reads=[sm], writes=[sm])
                tt(kb, kb.dve, sm[:, 9:10], sm[:, 8:9], sm[:, 3:4], ALU.mult, [sm], [sm])
                ts(kb, kb.dve, wi[:], wi[:], sm[:, 9:10], ALU.mult, [wi, sm], [wi])
                tt(kb, kb.dve, gate[:], ohg[:].unsqueeze(2).broadcast_to([128, 4, 8]), wi[:].unsqueeze(1).broadcast_to([128, 4, 8]),
                   ALU.mult, [ohg, wi], [gate])
                kb.op(kb.pe, lambda e: e.transpose(pgt[:, :], gate[:].rearrange("p g e -> p (g e)"), cst["ident_f"][:]),
                      reads=[gate, cst["ident_f"]], writes=[pgt])
                cp(kb, kb.dve, gT[:, b * TBk + i * 128:b * TBk + (i + 1) * 128], pgt[:, :], [pgt], [gT])
    TH = min(n, 2048)
    with kb.scope():
        sel = kb.sb([NE, NE * 128], BF16, "sel")
        kb.dma(kb.pool, sel[:], I["sel"][:, :], sel, I["sel"])
        yaccs = [kb.sb([128, KC, TBk], F32, "yacc") for _ in range(TH // TBk)]
        h2hs = [kb.sb([128, KC, TBk], BF16, "h2h") for _ in range(TH // TBk)]
        wg = Ring([kb.sb([128, KC, FF], BF16, "wg") for _ in range(2)])
        wu = Ring([kb.sb([128, KC, FF], BF16, "wu") for _ in range(2)])
        wd = Ring([kb.sb([128, 2, D], BF16, "wd") for _ in range(3)])
        pgb = kb.ps([128, TBk], F32, "pgb")
        pgu = Ring([kb.ps([128, TBk], F32, "pgu") for _ in range(4)])
        pyy = Ring([kb.ps([128, TBk], F32, "pyy") for _ in range(2)])
        gbc = Ring([kb.sb([128, TBk], BF16, "gbc") for _ in range(2)])
        sg = Ring([kb.sb([128, TBk], BF16, "sg") for _ in range(2)])
        tu = Ring([kb.sb([128, TBk], BF16, "tu") for _ in range(2)])
        A_ = Ring([kb.sb([128, 2, TBk], BF16, "A") for _ in range(3)])
        x1b_r = Ring([kb.sb([128, KC, TBk], F32, "x1b") for _ in range(2)])
        sqf = kb.sb([128, KC, TBk], BF16, "sqf") if last else None
        rsf = kb.sb([128, TBk], F32, "rsf") if last else None
        fg = load_col(kb, I["final_g"], I["final_g"].t, KC, "fg") if last else None
        def emit_down(e, d_, a_, bs):
            yacc = yaccs[bs.start // TBk]
            bs = slice(0, TBk)
            for dc in range(KC):
                py_ = pyy.next()
                for f in range(2):
                    mm(kb, py_[:, :], d_[:, f, dc * 128:(dc + 1) * 128], a_[:, f, :], f == 0, f == 1, [d_, a_], [py_])
                if e == 0:
                    act(kb, yacc[:, dc, bs], py_[:, :], AF.Copy, [py_], [yacc])
                else:
                    tt(kb, kb.dve, yacc[:, dc, bs], yacc[:, dc, bs], py_[:, :], ALU.add, [yacc, py_], [yacc])

        def load_w(e):
            g_ = wg.next(); u_ = wu.next(); d_ = wd.next()
            kb.dma(kb.pool, g_[:], I["w_gate"][l, e].rearrange("(k p) f -> p k f", p=128), g_, I["w_gate"])
            kb.dma(kb.pool, u_[:], I["w_up"][l, e].rearrange("(k p) f -> p k f", p=128), u_, I["w_up"])
            kb.dma(kb.pool, d_[:], I["w_down"][l, e].rearrange("(f p) c -> p f c", p=128), d_, I["w_down"])
            return g_, u_, d_

        wpre = [None]
        pend_down = None
        def load_h2h(hf, b):
            for k in range(KC):
                kb.dma(kb.sp, h2hs[b][:, k, :], h2_d.t[k, :, hf * TH + b * TBk:hf * TH + (b + 1) * TBk], h2hs[b], h2_d)

        for b in range(TH // TBk):
            load_h2h(0, b)
        for hf in range(n // TH):
            for e in range(NE):
                if wpre[0] is None:
                    wpre[0] = load_w(e)
                g_, u_, d_ = wpre[0]
                wpre[0] = load_w((e + 1) % NE) if (e + 1 < NE or hf + 1 < n // TH) else None
                for b in range(TH // TBk):
                    bs = slice(b * TBk, (b + 1) * TBk)
                    gs = slice(hf * TH + b * TBk, hf * TH + (b + 1) * TBk)
                    mm(kb, pgb[:, :], sel[:, e * 128:(e + 1) * 128], gT[:, gs], True, True, [sel, gT], [pgb])
                    gb_ = gbc.next()
                    act(kb, gb_[:], pgb[:, :], AF.Copy, [pgb], [gb_])
                    a_ = A_.next()
                    for f in range(2):
                        pg_ = pgu.next(); pu_ = pgu.next()
                        for k in range(KC):
                            mm(kb, pg_[:, :], g_[:, k, f * 128:(f + 1) * 128], h2hs[b][:, k, :], k == 0, k == KC - 1, [g_, h2hs[b]], [pg_])
                        for k in range(KC):
                            mm(kb, pu_[:, :], u_[:, k, f * 128:(f + 1) * 128], h2hs[b][:, k, :], k == 0, k == KC - 1, [u_, h2hs[b]], [pu_])
                        s_ = sg.next(); t_ = tu.next()
                        act(kb, s_[:], pg_[:, :], AF.Silu, [pg_], [s_])
                        tt(kb, kb.dve, t_[:], pu_[:, :], s_[:], ALU.mult, [pu_, s_], [t_])
                        tt(kb, kb.pool, a_[:, f, :], t_[:], gb_[:], ALU.mult, [t_, gb_], [a_])
                    if e == NE - 1 and hf + 1 < n // TH:
                        load_h2h(hf + 1, b)
                    if pend_down is not None:
                        emit_down(*pend_down)
                    pend_down = (e, d_, a_, bs)
            emit_down(*pend_down)
            pend_down = None
            for b in range(TH // TBk):
                bs = slice(b * TBk, (b + 1) * TBk)
                gs = slice(hf * TH + b * TBk, hf * TH + (b + 1) * TBk)
                x1b = x1b_r.next()
                for k in range(KC):
                    kb.dma(kb.act, x1b[:, k, :], x1_d.t[k, :, gs], x1b, x1_d)
                for k in range(KC):
                    stt(kb, kb.dve, x1b[:, k, :], yaccs[b][:, k, :], g2c[:, k, cidx:cidx + 1], x1b[:, k, :], ALU.mult, ALU.add,
                        [yaccs[b], g2c, x1b], [x1b])
                if last:
                    act(kb, sqf[:], x1b[:], AF.Square, [x1b], [sqf])
                    rms_bcast(kb, cst, [(sqf[:, k, :], 128) for k in range(KC)], D, TBk, pgb, rsf, [sqf])
                    for k in range(KC):
                        stt(kb, kb.dve, x1b[:, k, :], x1b[:, k, :], fg[:, k:k + 1], rsf[:], ALU.mult, ALU.mult, [x1b, fg, rsf], [x1b])
                for k in range(KC):
                    kb.dma(kb.act, xout_d.t[k, :, gs], x1b[:, k, :], xout_d, x1b)


def emit_phase_b(kb, I, T, l, last, cst, tri, maskneg, sel65, parts=("att", "ssd", "tail"), loaders=None):
    do_ctx = not last
    NK = CTX + 4 * T
    with kb.scope():
        mod = emit_mod(kb, I, l, cst, [2, 3, 4, 5])
        if "ssd" in parts:
            emit_ssd(kb, cst, l, T, "", I, I["mix"], I["yfw"], tri, maskneg, loaders)
            if do_ctx:
                emit_ssd(kb, cst, l, CTX, "c", I, I["mixc"], I["yfwc"], tri, maskneg, None)
        if "att" in parts:
            emit_attention(kb, cst, T, I["QT"], NK, I.get("KTall"), I.get("Vall"), I["mix"], sel65, loaders)
            if do_ctx:
                emit_attention(kb, cst, CTX, I["QTc"], CTX, I.get("KTall"), I.get("Vall"), I["mixc"], sel65, loaders)
        if "tail" in parts:
            emit_tail(kb, cst, l, T, 0, I["xT"], I["mix"], I["x1"], I["h2"], I["xout"], I, mod, last)
            if do_ctx:
                emit_tail(kb, cst, l, CTX, 1, I["xcT"], I["mixc"], I["x1c"], I["h2c"], I["xoutc"], I, mod, False)


def build_phase_b(T, l, last, parts=("att", "ssd", "tail"), debug=False):
    do_ctx = not last
    NK = CTX + 4 * T
    nc = bass.Bass("TRN2", target_bir_lowering=False)
    es = contextlib.ExitStack()
    with es:
        kb = KB(nc, es)
        I = {}

        def inp(name, shape, dtype=F32):
            I[name] = kb.dram(name, shape, dtype, "ExternalInput")

        def outp(name, shape, dtype=F32):
            I[name] = kb.dram(name, shape, dtype, "ExternalOutput")

        def scratch(name, shape, dtype=F32):
            I[name] = kb.dram(name, shape, dtype, "ExternalOutput" if debug else "Internal")

        inp("xT", [KC, 128, T]); inp("cT", [128, KC, 2]); inp("ident", [128, 128]); inp("tri", [2, 128, 128])
        inp("maskneg", [2, 128, 128]); inp("sel", [NE, NE * 128])
        inp("w_mod", [2, D, 6 * D]); inp("b_mod", [2, 6 * D]); inp("norm2_g", [2, D]); inp("w_out", [2, D, D])
        inp("router_w1", [2, D, 4]); inp("router_b1", [2, 4]); inp("router_w2", [2, D, NE]); inp("router_b2", [2, NE])
        inp("w_gate", [2, NE, D, FF]); inp("w_up", [2, NE, D, FF]); inp("w_down", [2, NE, FF, D])
        inp("d_skip", [2, H]); inp("ssd_norm_g", [2, SSD_IN]); inp("final_g", [D])
        inp("QT", [H, QK, T], BF16); inp("KTall", [H, QK, NK], BF16); inp("Vall", [NK, 512], BF16)
        groups = [("", T)] + ([("c", CTX)] if do_ctx else [])
        for sfx, n in groups:
            inp("zs" + sfx, [4, 128, n], BF16); inp("xtok" + sfx, [n, 640], BF16); inp("BCT" + sfx, [2, 128, n], BF16)
            inp("atok" + sfx, [n, 16]); inp("ldtok" + sfx, [n, 16])
            inp("Sch" + sfx, [2, 4, 64, 512]); inp("atch" + sfx, [2, 4, H])
            scratch("mix" + sfx, [KC, 128, n], BF16); scratch("yfw" + sfx, [n // 128, 64, H * 128])
            scratch("x1" + sfx, [KC, 128, n]); scratch("h2" + sfx, [KC, 128, n], BF16)
            outp("xout" + sfx, [KC, 128, n])
        if do_ctx:
            inp("xcT", [KC, 128, CTX]); inp("QTc", [H, QK, CTX], BF16)

        cst = load_consts(kb, I)
        tri = [kb.sb([128, 128], F32, "tri%d" % d) for d in range(2)]
        maskneg = [kb.sb([128, 128], F32, "mneg%d" % d) for d in range(2)]
        for d in range(2):
            kb.dma(kb.sp, tri[d][:], I["tri"][d], tri[d], I["tri"])
            kb.dma(kb.sp, maskneg[d][:], I["maskneg"][d], maskneg[d], I["maskneg"])
        sel65 = kb.sb([VD + 1, 64], F32, "sel65")
        mset(kb, kb.dve, sel65[:], 0.0, [sel65])
        mset(kb, kb.dve, sel65[64:65, :], 1.0, [sel65])
        emit_phase_b(kb, I, T, l, last, cst, tri, maskneg, sel65, parts)
        kb.finish()
    return nc


def pick_state(S_, d):
    out = np.zeros((64, 512), np.float32)
    for h in range(H):
        g = h // 4
        out[:, h * 64:(h + 1) * 64] = S_[g * 64:(g + 1) * 64, d * 512 + h * 64:d * 512 + (h + 1) * 64]
    return out


def host_inputs_b(W, l, last, T, s, xT_own, xcT, cT, QT, QTc, KTall, Vall, grp, Sch, atch):
    ident, tri = const_tables()
    maskneg = ((1.0 - tri) * NEG).astype(np.float32)
    sel = np.zeros((NE, NE, 128), np.float32)
    for e in range(NE):
        sel[e, e, :] = 1.0
    d = dict(xT=xT_own, cT=cT, ident=ident, tri=tri, maskneg=maskneg, sel=sel.reshape(NE, NE * 128),
             QT=QT, KTall=KTall, Vall=Vall)
    for k in ("w_mod", "b_mod", "norm2_g", "w_out", "router_w1", "router_b1", "router_w2", "router_b2",
              "w_gate", "w_up", "w_down", "d_skip", "ssd_norm_g", "final_g"):
        d[k] = np.ascontiguousarray(W[k], dtype=np.float32)
    for sfx in grp:
        for k, v in grp[sfx].items():
            d[k + sfx] = v
        d["Sch" + sfx] = Sch[sfx]
        d["atch" + sfx] = atch[sfx]
    if not last:
        d["xcT"] = xcT
        d["QTc"] = QTc
    return d


_NC_CACHE = {}


def _get_nc(kind, T, l, flag):
    key = (kind, T, l, flag)
    if key not in _NC_CACHE:
        _NC_CACHE[key] = build_phase_a(T, l, flag) if kind == "a" else build_phase_b(T, l, flag)
    return _NC_CACHE[key]


def _run(nc, in_maps):
    res = run_bass_kernel_spmd(nc, in_maps, core_ids=list(range(len(in_maps))))
    return res.results


def forward(W, x, c, ctx, c_ctx, ncores_per_batch=4):
    B, L, _ = x.shape
    R = ncores_per_batch
    T = L // R
    cos, sin = rope_tables_host(L)
    xl = [np.ascontiguousarray(x[b]) for b in range(B)]
    xc = [np.ascontiguousarray(ctx[b]) for b in range(B)]
    xT_own = {}
    for l in range(2):
        last = l == 1
        cores = [(b, s) for b in range(B) for s in range(R)]
        ins_a = [host_inputs_a(W, l, xl[b], xc[b], c[b], c_ctx, s, T, cos, sin) for (b, s) in cores]
        ra = _run(_get_nc("a", T, l, not last), ins_a)
        ins_b = []
        for ci, (b, s) in enumerate(cores):
            rb = [ra[b * R + r] for r in range(R)]
            me = ra[ci]
            KTall = np.concatenate([me["KTc"]] + [q["KT"] for q in rb], axis=2)
            Vall = np.concatenate([me["Vc"]] + [q["V"] for q in rb], axis=0)
            grp = {"": {k: me[k] for k in ("zs", "xtok", "BCT", "atok", "ldtok")}}
            Sch = {"": np.zeros((2, 4, 64, 512), np.float32)}
            atch = {"": np.zeros((2, 4, H), np.float32)}
            chains = ([rb[r] for r in range(s - 1, -1, -1)], [rb[r] for r in range(s + 1, R)])
            for d in range(2):
                srcs = [(q["S"], q["atot"]) for q in chains[d]] + [(me["Sc"], me["atotc"])]
                for k, (S_, at_) in enumerate(srcs):
                    Sch[""][d, k] = pick_state(S_, d)
                    atch[""][d, k] = at_[0, d * 8:(d + 1) * 8]
            if not last:
                grp["c"] = {k: me[k + "c"] for k in ("zs", "xtok", "BCT", "atok", "ldtok")}
                Sch["c"] = np.zeros((2, 4, 64, 512), np.float32)
                atch["c"] = np.zeros((2, 4, H), np.float32)
            ins_b.append(host_inputs_b(W, l, last, T, s, ins_a[ci]["xT"], ins_a[ci]["xcT"], ins_a[ci]["cT"], me["QT"],
                                       me["QTc"], KTall, Vall, grp, Sch, atch))
        rbo = _run(_get_nc("b", T, l, last), ins_b)
        for b in range(B):
            outs = [rbo[b * R + r]["xout"].reshape(D, T).T for r in range(R)]
            xl[b] = np.ascontiguousarray(np.concatenate(outs, axis=0))
            if not last:
                xc[b] = np.ascontiguousarray(rbo[b * R]["xoutc"].reshape(D, CTX).T)
    return np.stack(xl).astype(np.float32)


def kernel(x, c, ctx, c_ctx, w_mod, b_mod, norm1_g, norm2_g, w_in, q_norm_g, w_qb, kv_norm_g, w_kvb,
           conv_w, conv_b, a_log, dt_bias, d_skip, ssd_norm_g, w_out, router_w1, router_b1,
           router_w2, router_b2, w_gate, w_up, w_down, final_g):
    W = dict(w_mod=w_mod, b_mod=b_mod, norm1_g=norm1_g, norm2_g=norm2_g, w_in=w_in, q_norm_g=q_norm_g, w_qb=w_qb,
             kv_norm_g=kv_norm_g, w_kvb=w_kvb, conv_w=conv_w, conv_b=conv_b, a_log=a_log, dt_bias=dt_bias,
             d_skip=d_skip, ssd_norm_g=ssd_norm_g, w_out=w_out, router_w1=router_w1, router_b1=router_b1,
             router_w2=router_w2, router_b2=router_b2, w_gate=w_gate, w_up=w_up, w_down=w_down, final_g=final_g)
    W = {k: np.asarray(v, dtype=np.float32) for k, v in W.items()}
    return forward_fused(W, np.asarray(x, np.float32), np.asarray(c, np.float32), np.asarray(ctx, np.float32),
                         np.asarray(c_ctx, np.float32))


CC_GROUPS = [[0, 1, 2, 3], [4, 5, 6, 7]]


def build_fused(T, R=4):
    nc = bass.Bass("TRN2", target_bir_lowering=False)
    es = contextlib.ExitStack()
    with es:
        kb = KB(nc, es)
        I = {}

        def inp(name, shape, dtype=F32):
            I[name] = kb.dram(name, shape, dtype, "ExternalInput")

        inp("xT", [KC, 128, T]); inp("xhT", [KC, 128, 4]); inp("hmask", [128, 4]); inp("xcT", [KC, 128, CTX])
        inp("cT", [128, KC, 2]); inp("ident", [128, 128]); inp("tri", [2, 128, 128])
        inp("maskneg", [2, 128, 128]); inp("sel", [NE, NE * 128]); inp("chm", [128, 2, R]); inp("hsel", [128, 2, R])
        inp("ropeC", [QK, T]); inp("ropeS", [QK, T])
        inp("w_mod", [2, D, 6 * D]); inp("b_mod", [2, 6 * D]); inp("norm1_g", [2, D]); inp("norm2_g", [2, D])
        inp("w_in", [2, D, IN_COLS]); inp("q_norm_g", [2, QL]); inp("w_qb", [2, QL, H * QK])
        inp("kv_norm_g", [2, KVL]); inp("w_kvb", [2, KVL, H * 128])
        inp("conv_w", [2, 5, 768]); inp("conv_b", [2, 768]); inp("a_log", [2, 16]); inp("dt_bias", [2, 16])
        inp("w_out", [2, D, D])
        inp("router_w1", [2, D, 4]); inp("router_b1", [2, 4]); inp("router_w2", [2, D, NE]); inp("router_b2", [2, NE])
        inp("w_gate", [2, NE, D, FF]); inp("w_up", [2, NE, D, FF]); inp("w_down", [2, NE, FF, D])
        inp("d_skip", [2, H]); inp("ssd_norm_g", [2, SSD_IN]); inp("final_g", [D])
        out_d = kb.dram("out", [KC, 128, T], F32, "ExternalOutput")

        cst = load_consts(kb, I)
        tri = [kb.sb([128, 128], F32, "tri%d" % d) for d in range(2)]
        maskneg = [kb.sb([128, 128], F32, "mneg%d" % d) for d in range(2)]
        for d in range(2):
            kb.dma(kb.sp, tri[d][:], I["tri"][d], tri[d], I["tri"])
            kb.dma(kb.sp, maskneg[d][:], I["maskneg"][d], maskneg[d], I["maskneg"])
        sel65 = kb.sb([VD + 1, 64], F32, "sel65")
        mset(kb, kb.dve, sel65[:], 0.0, [sel65])
        mset(kb, kb.dve, sel65[64:65, :], 1.0, [sel65])
        chm = kb.sb([128, 2, R], F32, "chm")
        kb.dma(kb.sp, chm[:], I["chm"][:, :, :], chm, I["chm"])
        hsel = kb.sb([128, 2, R], F32, "hsel")
        kb.dma(kb.sp, hsel[:], I["hsel"][:, :, :], hsel, I["hsel"])

        x_cur, xh_cur, xc_cur = I["xT"], I["xhT"], I["xcT"]
        for l in range(2):
            last = l == 1
            Il = dict(I)
            Il["xT"], Il["xhT"], Il["xcT"] = x_cur, xh_cur, xc_cur

            def scr(name, shape, dtype=F32, kind="Internal"):
                Il[name] = kb.dram("%s_L%d" % (name, l), shape, dtype, kind)
                return Il[name]

            for sfx, n in (("", T), ("c", CTX)):
                scr("QT" + sfx, [H, QK, n], BF16); scr("KT" + sfx, [H, QK, n], BF16); scr("V" + sfx, [n, 512], BF16)
                scr("zs" + sfx, [4, 128, n], BF16); scr("xtok" + sfx, [n, 640], BF16); scr("BCT" + sfx, [2, 128, n], BF16)
                scr("atok" + sfx, [n, 16]); scr("ldtok" + sfx, [n, 16]); scr("S" + sfx, [128, 1024]); scr("atot" + sfx, [1, 16])
            emit_phase_a(kb, Il, T, l, not last, cst, tri)
            KTg = [scr("KTg%d" % h, [R * QK, T], BF16) for h in range(H)]
            VCH = min(T, 1024)
            Vg = [scr("Vg%d" % c, [R * VCH, 512], BF16) for c in range(T // VCH)]
            Sg = scr("Sg", [R * 128, 1024]); atg = scr("atg", [R, 16])
            kb.collective(Il["S"], Il["S"].t[:, :], Sg, Sg.t[:, :], CC_GROUPS)
            kb.collective(Il["atot"], Il["atot"].t[:, :], atg, atg.t[:, :], CC_GROUPS)
            for h in range(H):
                kb.collective(Il["KT"], Il["KT"].t[h], KTg[h], KTg[h].t[:, :], CC_GROUPS)
            for c in range(T // VCH):
                kb.collective(Il["V"], Il["V"].t[c * VCH:(c + 1) * VCH, :], Vg[c], Vg[c].t[:, :], CC_GROUPS)

            def kv_loader(h, k_, v_, nk, Il=Il, KTg=KTg, Vg=Vg, VCH=VCH):
                kb.dma(kb.sp, k_[:, 0:CTX], Il["KTc"].t[h, :, :], k_, Il["KTc"])
                kb.dma(kb.sp, v_[:, 0:CTX // 128, 0:VD],
                       Il["Vc"].t[:, h * VD:(h + 1) * VD].rearrange("(t p) c -> p t c", p=128), v_, Il["Vc"])
                if nk == CTX:
                    return
                ntl = T // 128
                ncl = VCH // 128
                for r in range(R):
                    kb.dma(kb.sp, k_[:, CTX + r * T:CTX + (r + 1) * T], KTg[h].t[r * QK:(r + 1) * QK, :], k_, KTg[h])
                    for c in range(T // VCH):
                        for t0 in range(0, ncl, 16):
                            t1_ = min(ncl, t0 + 16)
                            kb.dma(kb.sp, v_[:, 2 + r * ntl + c * ncl + t0:2 + r * ntl + c * ncl + t1_, 0:VD],
                                   Vg[c].t[r * VCH + t0 * 128:r * VCH + t1_ * 128, h * VD:(h + 1) * VD]
                                   .rearrange("(t p) c -> p t c", p=128), v_, Vg[c])

            def state_loader(d, hst, hflat, ssrc, atb, Il=Il, Sg=Sg, atg=atg):
                def load_S(src, row0):
                    for g in range(2):
                        kb.dma(kb.sp, ssrc[:, g * 256:(g + 1) * 256],
                               src.t[row0 + g * 64:row0 + (g + 1) * 64, d * 512 + g * 256:d * 512 + (g + 1) * 256], ssrc, src)
                load_S(Il["Sc"], 0)
                cp(kb, kb.dve, hflat, ssrc[:], [ssrc], [hst])
                for r in (range(R) if d == 0 else range(R - 1, -1, -1)):
                    mcol = chm[0:64, d, r:r + 1]
                    kb.dma(kb.sp, atb[:], atg.t[r:r + 1, d * 8:(d + 1) * 8].broadcast_to([128, H]), atb, atg)
                    ts(kb, kb.dve, atb[:], atb[:], chm[:, d, r:r + 1], ALU.mult, [atb, chm], [atb])
                    act(kb, atb[:], atb[:], AF.Exp, [atb], [atb])
                    load_S(Sg, r * 128)
                    tt(kb, kb.dve, hst[:], hst[:], atb[0:64, :].unsqueeze(2).broadcast_to([64, H, 64]), ALU.mult, [hst, atb], [hst])
                    stt(kb, kb.dve, hflat, ssrc[:], mcol, hflat, ALU.mult, ALU.add, [ssrc, chm, hst], [hst])

            for sfx, n in ([("", T)] + ([] if last else [("c", CTX)])):
                scr("mix" + sfx, [KC, 128, n], BF16); scr("yfw" + sfx, [n // 128, 64, H * 128])
                scr("x1" + sfx, [KC, 128, n]); scr("h2" + sfx, [KC, 128, n], BF16)
                if sfx == "" and last:
                    Il["xout"] = out_d
                else:
                    scr("xout" + sfx, [KC, 128, n])
            emit_phase_b(kb, Il, T, l, last, cst, tri, maskneg, sel65,
                         loaders={"kv": kv_loader, "state": state_loader})
            if not last:
                xe = scr("xe", [128, KC * 4]); xeg = scr("xeg", [R * 128, KC * 4]); xh1 = scr("xh1", [KC, 128, 4])
                with kb.scope():
                    et = kb.sb([128, KC, 4], F32, "et")
                    kb.dma(kb.sp, et[:, :, 0:2], Il["xout"].t[:, :, 0:2].rearrange("k p c -> p k c"), et, Il["xout"])
                    kb.dma(kb.sp, et[:, :, 2:4], Il["xout"].t[:, :, T - 2:T].rearrange("k p c -> p k c"), et, Il["xout"])
                    kb.dma(kb.sp, xe.t[:, :], et[:].rearrange("p k c -> p (k c)"), xe, et)
                    kb.collective(xe, xe.t[:, :], xeg, xeg.t[:, :], CC_GROUPS)
                    eg = kb.sb([128, R, KC, 4], F32, "eg")
                    kb.dma(kb.sp, eg[:], xeg.t.rearrange("(r p) (k c) -> p r k c", p=128, c=4), eg, xeg)
                    xh = kb.sb([128, KC, 4], F32, "xh")
                    mset(kb, kb.dve, xh[:], 0.0, [xh])
                    for r in range(R):
                        stt(kb, kb.dve, xh[:, :, 0:2], eg[:, r, :, 2:4], hsel[:, 0, r:r + 1], xh[:, :, 0:2], ALU.mult, ALU.add,
                            [eg, hsel, xh], [xh])
                        stt(kb, kb.dve, xh[:, :, 2:4], eg[:, r, :, 0:2], hsel[:, 1, r:r + 1], xh[:, :, 2:4], ALU.mult, ALU.add,
                            [eg, hsel, xh], [xh])
                    kb.dma(kb.sp, xh1.t.rearrange("k p c -> p k c"), xh[:], xh1, xh)
                x_cur, xh_cur, xc_cur = Il["xout"], xh1, Il["xoutc"]
        kb.finish()
    return nc


def host_inputs_fused(W, x_b, ctx_b, c_b, c_ctx, s, T, R, cos, sin):
    d = host_inputs_a(W, 0, x_b, ctx_b, c_b, c_ctx, s, T, cos, sin)
    ident, tri = const_tables()
    d["maskneg"] = ((1.0 - tri) * NEG).astype(np.float32)
    sel = np.zeros((NE, NE, 128), np.float32)
    for e in range(NE):
        sel[e, e, :] = 1.0
    d["sel"] = sel.reshape(NE, NE * 128)
    chm = np.zeros((128, 2, R), np.float32)
    hs = np.zeros((128, 2, R), np.float32)
    for r in range(R):
        chm[:, 0, r] = 1.0 if r < s else 0.0
        chm[:, 1, r] = 1.0 if r > s else 0.0
        hs[:, 0, r] = 1.0 if r == s - 1 else 0.0
        hs[:, 1, r] = 1.0 if r == s + 1 else 0.0
    d["chm"] = chm
    d["hsel"] = hs
    for k in ("norm2_g", "w_out", "router_w1", "router_b1", "router_w2", "router_b2", "w_gate", "w_up", "w_down",
              "d_skip", "ssd_norm_g", "final_g"):
        d[k] = np.ascontiguousarray(W[k], dtype=np.float32)
    return d


def forward_fused(W, x, c, ctx, c_ctx, R=4):
    B, L, _ = x.shape
    T = L // R
    cos, sin = rope_tables_host(L)
    cores = [(b, s) for b in range(B) for s in range(R)]
    ins = [host_inputs_fused(W, x[b], ctx[b], c[b], c_ctx, s, T, R, cos, sin) for (b, s) in cores]
    key = ("fused", T)
    if key not in _NC_CACHE:
        _NC_CACHE[key] = build_fused(T, R)
    res = _run(_NC_CACHE[key], ins)
    out = np.empty((B, L, D), np.float32)
    for ci, (b, s) in enumerate(cores):
        out[b, s * T:(s + 1) * T] = res[ci]["out"].reshape(D, T).T
    return out
```

```python
import contextlib
import numpy as np
import ml_dtypes
import concourse.bass as bass
import concourse.mybir as mybir
from concourse.bass_utils import run_bass_kernel_spmd

F32 = mybir.dt.float32
BF16 = mybir.dt.bfloat16
AF = mybir.ActivationFunctionType
ALU = mybir.AluOpType
AX = mybir.AxisListType
NPBF = ml_dtypes.bfloat16

D = 1024
KC = 8
CTX = 256
H = 8
QL, KVL, ROPE, NOPE, VD = 256, 128, 32, 64, 64
QK = NOPE + ROPE
SSD_IN = 512
NST = 64
IN_COLS = 1712
C_QA, C_KVA, C_KR, C_Z, C_XBC, C_DT = 0, 256, 384, 416, 928, 1696
NE, FF = 32, 256
EPS = 1e-6
GRID_W = 64
SCALE = float(QK) ** -0.5
NEG = -30000.0


class Sem:
    def __init__(self, kb, name):
        self.h = kb.es_top.enter_context(kb.nc.semaphore(name))
        self.n = 0


class Res:
    def __init__(self):
        self.w = {}
        self.r = {}


class Eng:
    def __init__(self, kb, name, be, is_pe=False):
        self.name = name
        self.be = be
        self.is_pe = is_pe
        self.sem = Sem(kb, "s_" + name)
        self.seen = {}


class TT:
    def __init__(self, kb, name, shape, dtype, space="sbuf", kind=None):
        self.kb = kb
        self.name = name
        self.res = Res()
        self.dsem = None
        self.space = space
        if kb.scope_tts and space != "dram":
            kb.scope_tts[-1].append(self)
        if space == "sbuf":
            self.t = kb.es.enter_context(kb.nc.sbuf_tensor(name, list(shape), dtype))
        elif space == "psum":
            self.t = kb.es.enter_context(kb.nc.psum_tensor(name, list(shape), dtype))
        else:
            self.t = kb.nc.dram_tensor(name, list(shape), dtype, kind=kind).ap()

    def __getitem__(self, idx):
        return self.t[idx]

    def get_dsem(self):
        if self.dsem is None:
            if self.kb.free_sems:
                self.dsem = self.kb.free_sems.pop()
            else:
                self.dsem = Sem(self.kb, "d_" + self.name)
        return self.dsem


class KB:
    def __init__(self, nc, es):
        self.nc = nc
        self.es = es
        self.es_top = es
        self.scope_tts = []
        self.free_sems = []
        self.ccsem = None
        self.pe = Eng(self, "pe", nc.tensor, True)
        self.act = Eng(self, "act", nc.scalar)
        self.dve = Eng(self, "dve", nc.vector)
        self.pool = Eng(self, "pool", nc.gpsimd)
        self.sp = Eng(self, "sp", nc.sync)
        self.uid = 0
        self.drams = []

    def name(self, p):
        self.uid += 1
        return "%s_%d" % (p, self.uid)

    def sb(self, shape, dtype, name="t"):
        return TT(self, self.name(name), shape, dtype, "sbuf")

    def ps(self, shape, dtype=F32, name="p"):
        return TT(self, self.name(name), shape, dtype, "psum")

    def dram(self, name, shape, dtype, kind):
        t = TT(self, name, shape, dtype, "dram", kind)
        self.drams.append(t)
        return t

    @contextlib.contextmanager
    def scope(self):
        outer = self.es
        inner = contextlib.ExitStack()
        self.es = inner
        self.scope_tts.append([])
        try:
            yield
        finally:
            tts = self.scope_tts.pop()
            self.barrier(tts)
            for t in tts:
                if t.dsem is not None:
                    self.free_sems.append(t.dsem)
                    t.dsem = None
            inner.close()
            self.es = outer

    def barrier(self, tts=()):
        engs = (self.pe, self.act, self.dve, self.pool, self.sp)
        sems = [e.sem for e in engs] + [t.dsem for t in tts if t.dsem is not None]
        for e in engs:
            for sm in sems:
                if sm is e.sem or sm.n <= 0 or e.seen.get(sm, 0) >= sm.n:
                    continue
                e.be.wait_ge(sm.h, sm.n)
                e.seen[sm] = sm.n

    def _waits(self, eng, reads, writes):
        need = {}
        for r in reads:
            for sm, v in r.res.w.items():
                need[sm] = max(need.get(sm, 0), v)
        for w in writes:
            for sm, v in list(w.res.w.items()) + list(w.res.r.items()):
                need[sm] = max(need.get(sm, 0), v)
        for sm, v in need.items():
            if sm is eng.sem:
                if eng.is_pe:
                    continue
                v = min(v, sm.n)
            if v <= 0 or eng.seen.get(sm, 0) >= v:
                continue
            eng.be.wait_ge(sm.h, v)
            eng.seen[sm] = v

    def op(self, eng, fn, reads=(), writes=(), inc=True):
        self._waits(eng, reads, writes)
        inst = fn(eng.be)
        if inc:
            eng.sem.n += 1
            inst.then_inc(eng.sem.h, 1)
            tick = eng.sem.n
        else:
            tick = eng.sem.n + 1
        for r in reads:
            r.res.r[eng.sem] = max(r.res.r.get(eng.sem, 0), tick)
        for w in writes:
            w.res.w[eng.sem] = max(w.res.w.get(eng.sem, 0), tick)
        return inst

    def dma(self, q, out, in_, dst, src, **kw):
        self._waits(q, [src], [dst])
        owner = dst if dst.space != "dram" else src
        ds = owner.get_dsem()
        inst = q.be.dma_start(out=out, in_=in_, **kw)
        ds.n += 16
        inst.then_inc(ds.h, 16)
        src.res.r[ds] = ds.n
        dst.res.w[ds] = ds.n
        return inst

    def collective(self, in_tt, in_ap, out_tt, out_ap, groups):
        if self.ccsem is None:
            self.ccsem = Sem(self, "ccsem")
        self._waits(self.pool, [in_tt], [out_tt])
        inst = self.pool.be.collective_compute("AllGather", ALU.bypass, replica_groups=groups, ins=[in_ap], outs=[out_ap])
        self.ccsem.n += 1
        inst.then_inc(self.ccsem.h)
        in_tt.res.r[self.ccsem] = self.ccsem.n
        out_tt.res.w[self.ccsem] = self.ccsem.n
        return inst

    def finish(self):
        need = {}
        for t in self.drams:
            for sm, v in t.res.w.items():
                need[sm] = max(need.get(sm, 0), v)
        for sm, v in need.items():
            self.sp.be.wait_ge(sm.h, v)
        for e in (self.pe, self.act, self.dve, self.pool):
            if e.sem.n > 0:
                self.sp.be.wait_ge(e.sem.h, e.sem.n)


def mm(kb, out, lhsT, rhs, start, stop, reads, writes, inc=None):
    if inc is None:
        inc = stop
    return kb.op(kb.pe, lambda e: e.matmul(out, lhsT=lhsT, rhs=rhs, start=start, stop=stop),
                 reads=reads, writes=writes, inc=inc)


def act(kb, out, in_, func, reads, writes, bias=None, scale=1.0, accum_out=None):
    kw = {}
    if bias is not None:
        kw["bias"] = bias
    if accum_out is not None:
        kw["accum_out"] = accum_out
    return kb.op(kb.act, lambda e: e.activation(out=out, in_=in_, func=func, scale=scale, **kw),
                 reads=reads, writes=writes)


def tt(kb, eng, out, in0, in1, op, reads, writes):
    return kb.op(eng, lambda e: e.tensor_tensor(out=out, in0=in0, in1=in1, op=op), reads=reads, writes=writes)


def ts(kb, eng, out, in0, s1, op0, reads, writes, s2=None, op1=None):
    if op1 is None:
        return kb.op(eng, lambda e: e.tensor_scalar(out=out, in0=in0, scalar1=s1, scalar2=None, op0=op0),
                     reads=reads, writes=writes)
    return kb.op(eng, lambda e: e.tensor_scalar(out=out, in0=in0, scalar1=s1, scalar2=s2, op0=op0, op1=op1),
                 reads=reads, writes=writes)


def stt(kb, eng, out, in0, scalar, in1, op0, op1, reads, writes):
    return kb.op(eng, lambda e: e.scalar_tensor_tensor(out=out, in0=in0, scalar=scalar, in1=in1, op0=op0, op1=op1),
                 reads=reads, writes=writes)


def cp(kb, eng, out, in_, reads, writes):
    return kb.op(eng, lambda e: e.tensor_copy(out=out, in_=in_), reads=reads, writes=writes)


def mset(kb, eng, ap, val, writes):
    return kb.op(eng, lambda e: e.memset(ap, val), reads=(), writes=writes)


class Ring:
    def __init__(self, items):
        self.items = items
        self.i = 0

    def next(self):
        t = self.items[self.i % len(self.items)]
        self.i += 1
        return t


def load_consts(kb, ins):
    c = {}
    c["ident_f"] = kb.sb([128, 128], F32, "identf")
    kb.dma(kb.sp, c["ident_f"][:], ins["ident"][:, :], c["ident_f"], ins["ident"])
    c["ident_b"] = kb.sb([128, 128], BF16, "identb")
    kb.dma(kb.pool, c["ident_b"][:], ins["ident"][:, :], c["ident_b"], ins["ident"])
    c["ones_f"] = kb.sb([128, 128], F32, "onesf")
    mset(kb, kb.dve, c["ones_f"][:], 1.0, [c["ones_f"]])
    c["ones_b"] = kb.sb([128, 128], BF16, "onesb")
    mset(kb, kb.dve, c["ones_b"][:], 1.0, [c["ones_b"]])
    c["eps"] = kb.sb([128, 1], F32, "eps")
    mset(kb, kb.dve, c["eps"][:], EPS, [c["eps"]])
    c["one1"] = kb.sb([128, 1], F32, "one1")
    mset(kb, kb.dve, c["one1"][:], 1.0, [c["one1"]])
    c["zero1"] = kb.sb([128, 1], F32, "zero1")
    mset(kb, kb.dve, c["zero1"][:], 0.0, [c["zero1"]])
    return c


def emit_mod(kb, ins, l, cst, sections):
    out = {sec: kb.sb([128, KC, 2], F32, "modT%d" % sec) for sec in sections}
    with kb.scope():
        cT = kb.sb([128, KC, 2], F32, "cT")
        kb.dma(kb.sp, cT[:], ins["cT"][:, :, :], cT, ins["cT"])
        cs = kb.sb([128, KC, 2], F32, "cs")
        act(kb, cs[:], cT[:], AF.Silu, [cT], [cs])
        bm = kb.sb([128, 6 * KC], F32, "bm")
        kb.dma(kb.sp, bm[:], ins["b_mod"][l].rearrange("(c p) -> p c", p=128), bm, ins["b_mod"],
               allow_slow_non_contiguous=True)
        wbufs = Ring([kb.sb([128, KC, 1024], F32, "wmod") for _ in range(2)])
        pm = kb.ps([128, KC, 2], F32, "pmod")
        for sec in sections:
            wt = wbufs.next()
            kb.dma(kb.sp, wt[:], ins["w_mod"][l][:, sec * 1024:(sec + 1) * 1024].rearrange("(k p) n -> p k n", p=128),
                   wt, ins["w_mod"])
            for cc in range(KC):
                for k in range(KC):
                    mm(kb, pm[:, cc, :], wt[:, k, cc * 128:(cc + 1) * 128], cs[:, k, :], k == 0, k == KC - 1,
                       [wt, cs], [pm])
            m = out[sec]
            tt(kb, kb.dve, m[:], pm[:], bm[:, sec * KC:(sec + 1) * KC].unsqueeze(2).broadcast_to([128, KC, 2]),
               ALU.add, [pm, bm], [m])
    return out


def load_col(kb, dram_tt, ap1d, n, name):
    t = kb.sb([128, n], F32, name)
    kb.dma(kb.sp, t[:], ap1d.rearrange("(c p) -> p c", p=128), t, dram_tt, allow_slow_non_contiguous=True)
    return t


def rms_bcast(kb, cst, src_sq_list, n_feat, ncols, psq, out_rstd, reads):
    n = len(src_sq_list)
    for i, (ap, kp) in enumerate(src_sq_list):
        mm(kb, psq[:, :ncols], cst["ones_b"][0:kp, :], ap, i == 0, i == n - 1, reads + [cst["ones_b"]], [psq])
    act(kb, out_rstd[:, :ncols], psq[:, :ncols], AF.Ln, [psq, cst["eps"]], [out_rstd],
        bias=cst["eps"][:, 0:1], scale=1.0 / n_feat)
    act(kb, out_rstd[:, :ncols], out_rstd[:, :ncols], AF.Exp, [out_rstd], [out_rstd], scale=-0.5)


def emit_phase_a(kb, I, T, l, with_ctx_q, cst, tri):
    with kb.scope():
        mod = emit_mod(kb, I, l, cst, [0, 1])
        g1 = load_col(kb, I["norm1_g"], I["norm1_g"][l], KC, "n1g")
        gmod = kb.sb([128, KC, 2], F32, "gmod")
        ts(kb, kb.dve, gmod[:], mod[1][:], 1.0, ALU.add, [mod[1]], [gmod])
        tt(kb, kb.dve, gmod[:], gmod[:], g1[:].unsqueeze(2).broadcast_to([128, KC, 2]), ALU.mult, [gmod, g1], [gmod])
        sh = mod[0]

        cw = kb.sb([128, 6, 5], F32, "cw")
        for k in range(5):
            kb.dma(kb.sp, cw[:, :, k], I["conv_w"][l][k].rearrange("(c p) -> p c", p=128), cw, I["conv_w"],
                   allow_slow_non_contiguous=True)
        cb = load_col(kb, I["conv_b"], I["conv_b"][l], 6, "cb")
        dtb = kb.sb([128, 16], F32, "dtb")
        kb.dma(kb.sp, dtb[:], I["dt_bias"][l:l + 1, :].broadcast_to([128, 16]), dtb, I["dt_bias"])
        Aneg = kb.sb([128, 16], F32, "Aneg")
        kb.dma(kb.sp, Aneg[:], I["a_log"][l:l + 1, :].broadcast_to([128, 16]), Aneg, I["a_log"])
        act(kb, Aneg[:], Aneg[:], AF.Exp, [Aneg], [Aneg])
        ts(kb, kb.dve, Aneg[:], Aneg[:], -1.0, ALU.mult, [Aneg], [Aneg])
        hmask = kb.sb([128, 4], F32, "hmask")
        kb.dma(kb.sp, hmask[:], I["hmask"][:, :], hmask, I["hmask"])

        xbc_l = kb.sb([128, 6, T + 4], BF16, "xbcpre_l")
        xbc_c = kb.sb([128, 6, CTX + 4], BF16, "xbcpre_c")
        mset(kb, kb.pool, xbc_c[:], 0.0, [xbc_c])
        sc_w = kb.scope(); sc_w.__enter__()
        win = kb.sb([128, KC, IN_COLS], BF16, "win")
        for k in range(KC):
            kb.dma(kb.pool, win[:, k, :], I["w_in"][l][k * 128:(k + 1) * 128, :], win, I["w_in"])
        wq = kb.sb([128, 2, H, QK], BF16, "wq")
        wqr = kb.sb([128, 2, H, QK], BF16, "wqr")
        wkn = kb.sb([128, H, NOPE], BF16, "wkn")
        wvv = kb.sb([128, H, VD], BF16, "wvv")
        sc_tmp = kb.scope(); sc_tmp.__enter__()
        wq_f = kb.sb([128, 2, H * QK], F32, "wqf")
        kb.dma(kb.sp, wq_f[:], I["w_qb"][l].rearrange("(k p) n -> p k n", p=128), wq_f, I["w_qb"])
        qg = load_col(kb, I["q_norm_g"], I["q_norm_g"][l], 2, "qg")
        mset(kb, kb.pool, wqr[:], 0.0, [wqr])
        for k in range(2):
            wv_ = wq_f[:, k, :].rearrange("p (h c) -> p h c", h=H)
            ts(kb, kb.dve, wq[:, k], wv_, qg[:, k:k + 1], ALU.mult, [wq_f, qg], [wq])
            ts(kb, kb.dve, wqr[:, k, :, 64:80], wv_[:, :, 80:96], qg[:, k:k + 1], ALU.mult, [wq_f, qg], [wqr], s2=-1.0, op1=ALU.mult)
            ts(kb, kb.dve, wqr[:, k, :, 80:96], wv_[:, :, 64:80], qg[:, k:k + 1], ALU.mult, [wq_f, qg], [wqr])
        wkv_f = kb.sb([128, H, 128], F32, "wkvf")
        kb.dma(kb.sp, wkv_f[:], I["w_kvb"][l].rearrange("p (h c) -> p h c", h=H), wkv_f, I["w_kvb"])
        kvg = load_col(kb, I["kv_norm_g"], I["kv_norm_g"][l], 1, "kvg")
        ts(kb, kb.dve, wkn[:], wkv_f[:, :, 0:NOPE], kvg[:, 0:1], ALU.mult, [wkv_f, kvg], [wkn])
        ts(kb, kb.dve, wvv[:], wkv_f[:, :, NOPE:128], kvg[:, 0:1], ALU.mult, [wkv_f, kvg], [wvv])
        sc_tmp.__exit__(None, None, None)
        wkr = kb.sb([128, KC, QK], BF16, "wkr")
        wkrr = kb.sb([128, KC, QK], BF16, "wkrr")
        mset(kb, kb.pool, wkr[:], 0.0, [wkr])
        mset(kb, kb.pool, wkrr[:], 0.0, [wkrr])
        cp(kb, kb.dve, wkr[:, :, 64:96], win[:, :, C_KR:C_KR + 32], [win], [wkr])
        ts(kb, kb.dve, wkrr[:, :, 64:80], win[:, :, C_KR + 16:C_KR + 32], -1.0, ALU.mult, [win], [wkrr])
        cp(kb, kb.dve, wkrr[:, :, 80:96], win[:, :, C_KR:C_KR + 16], [win], [wkrr])
        TB = 512
        xin = Ring([kb.sb([128, KC, TB], F32, "xin") for _ in range(2)])
        sq_r = Ring([kb.sb([128, KC, TB], BF16, "sq") for _ in range(1)])
        rstd_r = Ring([kb.sb([128, TB], F32, "rstd") for _ in range(2)])
        tmp_r = Ring([kb.sb([128, TB], F32, "tmp") for _ in range(2)])
        hT_r = Ring([kb.sb([128, KC, TB], BF16, "hT") for _ in range(2)])
        psq = kb.ps([128, TB], F32, "psq")
        pacc = Ring([kb.ps([128, TB], F32, "pacc") for _ in range(6)])
        pq2 = kb.ps([128, TB], F32, "pq2")
        qaT = kb.sb([128, 2, TB], BF16, "qaT")
        qsq = kb.sb([128, 2, TB], BF16, "qsq")
        rq = kb.sb([128, TB], F32, "rq")
        Crs = kb.sb([QK, TB], F32, "Crs")
        Srs = kb.sb([QK, TB], F32, "Srs")
        ropeC = kb.sb([QK, TB], F32, "ropeC")
        ropeS = kb.sb([QK, TB], F32, "ropeS")
        t1 = Ring([kb.sb([QK, TB], F32, "t1") for _ in range(1)])
        t2 = Ring([kb.sb([QK, TB], F32, "t2") for _ in range(1)])
        qst = Ring([kb.sb([QK, TB], BF16, "qst") for _ in range(4)])
        kst = Ring([kb.sb([NOPE, TB], BF16, "kst") for _ in range(6)])
        krp = Ring([kb.sb([QK, TB], BF16, "krp") for _ in range(2)])
        kvsq = kb.sb([128, TB], BF16, "kvsq")
        rkv = kb.sb([128, TB], F32, "rkv")
        kvn = kb.sb([128, TB], BF16, "kvn")
        vst = Ring([kb.sb([128, 4, 512], BF16, "vst") for _ in range(1)])
        zst = Ring([kb.sb([128, 4, TB], BF16, "zst") for _ in range(1)])
        dtr = kb.sb([128, 4, 16], F32, "dtr")
        dts = Ring([kb.sb([128, 4, 32], F32, "dts") for _ in range(2)])

        def run_group(n, xT_d, xoff, cidx, sfx, tabs, xbcpre, want_q, halo=None):
            nb = (n + TB - 1) // TB
            pre = [None]

            def load_x(bj):
                hl = bj == nb
                ww, oo = (4, 0) if hl else (min(TB, n - bj * TB), bj * TB)
                sr = halo if hl else xT_d
                xt_ = xin.next()
                for k in range(KC):
                    kb.dma(kb.sp, xt_[:, k, :ww], sr.t[k, :, (0 if hl else xoff + oo):(0 if hl else xoff + oo) + ww], xt_, sr)
                return xt_

            for bi in range(nb + (1 if halo is not None else 0)):
                is_halo = bi == nb
                if is_halo:
                    w_, o0 = 4, 0
                    src = halo
                else:
                    w_, o0 = min(TB, n - bi * TB), bi * TB
                    src = xT_d
                sq = sq_r.next(); rstd = rstd_r.next(); hT = hT_r.next()
                if bi == 0:
                    pre[0] = load_x(0)
                xt = pre[0]
                if bi + 1 < nb + (1 if halo is not None else 0):
                    pre[0] = load_x(bi + 1)
                act(kb, sq[:, :, :w_], xt[:, :, :w_], AF.Square, [xt], [sq])
                rms_bcast(kb, cst, [(sq[:, k, :w_], 128) for k in range(KC)], D, w_, psq, rstd, [sq])
                for k in range(KC):
                    tmp = tmp_r.next()
                    stt(kb, kb.dve, tmp[:, :w_], xt[:, k, :w_], gmod[:, k, cidx:cidx + 1], rstd[:, :w_], ALU.mult, ALU.mult,
                        [xt, gmod, rstd], [tmp])
                    act(kb, hT[:, k, :w_], tmp[:, :w_], AF.Identity, [tmp, sh], [hT], bias=sh[:, k, cidx:cidx + 1])

                def proj(pt, M, c0, wtile=None):
                    for k in range(KC):
                        lw = win[:, k, c0:c0 + M] if wtile is None else wtile[:, k, :]
                        mm(kb, pt[0:M, :w_], lw, hT[:, k, :w_], k == 0, k == KC - 1, [win if wtile is None else wtile, hT], [pt])

                for c in range(6):
                    pt = pacc.next()
                    proj(pt, 128, C_XBC + c * 128)
                    if is_halo:
                        tt(kb, kb.dve, xbcpre[:, c, 0:2], pt[:, 0:2], hmask[:, 0:2], ALU.mult, [pt, hmask], [xbcpre])
                        tt(kb, kb.dve, xbcpre[:, c, n + 2:n + 4], pt[:, 2:4], hmask[:, 2:4], ALU.mult, [pt, hmask], [xbcpre])
                    else:
                        act(kb, xbcpre[:, c, 2 + o0:2 + o0 + w_], pt[:, :w_], AF.Copy, [pt], [xbcpre])
                if is_halo:
                    continue
                zt = zst.next()
                for c in range(4):
                    pt = pacc.next()
                    proj(pt, 128, C_Z + c * 128)
                    act(kb, zt[:, c, :w_], pt[:, :w_], AF.Silu, [pt], [zt])
                for c in range(4):
                    kb.dma(kb.act, I["zs" + sfx].t[c, :, o0:o0 + w_], zt[:, c, :w_], I["zs" + sfx], zt)
                ntl = w_ // 128
                for i in range(ntl):
                    pt = pacc.next()
                    for k in range(KC):
                        mm(kb, pt[:, 0:16], hT[:, k, i * 128:(i + 1) * 128], win[:, k, C_DT:C_DT + 16], k == 0, k == KC - 1,
                           [hT, win], [pt])
                    tt(kb, kb.dve, dtr[:, i, :], pt[:, 0:16], dtb[:], ALU.add, [pt, dtb], [dtr])
                dt_ = dts.next()
                act(kb, dtr[:, :ntl, :], dtr[:, :ntl, :], AF.Exp, [dtr], [dtr])
                act(kb, dtr[:, :ntl, :], dtr[:, :ntl, :], AF.Ln, [dtr, cst["one1"]], [dtr], bias=cst["one1"][:, 0:1])
                tt(kb, kb.dve, dt_[:, :ntl, 0:16], dtr[:, :ntl, :], Aneg[:].unsqueeze(1).broadcast_to([128, ntl, 16]), ALU.mult,
                   [dtr, Aneg], [dt_])
                act(kb, dt_[:, :ntl, 16:32], dtr[:, :ntl, :], AF.Ln, [dtr], [dt_])
                kb.dma(kb.sp, I["atok" + sfx].t[o0:o0 + w_, :].rearrange("(i p) c -> p i c", p=128), dt_[:, :ntl, 0:16],
                       I["atok" + sfx], dt_)
                kb.dma(kb.sp, I["ldtok" + sfx].t[o0:o0 + w_, :].rearrange("(i p) c -> p i c", p=128), dt_[:, :ntl, 16:32],
                       I["ldtok" + sfx], dt_)
                if tabs is not None:
                    kb.dma(kb.sp, ropeC[:, :w_], I["ropeC"].t[:, o0:o0 + w_], ropeC, I["ropeC"])
                    kb.dma(kb.sp, ropeS[:, :w_], I["ropeS"].t[:, o0:o0 + w_], ropeS, I["ropeS"])
                else:
                    mset(kb, kb.pool, ropeC[:], 1.0, [ropeC])
                    mset(kb, kb.pool, ropeS[:], 0.0, [ropeS])
                pt = pacc.next()
                proj(pt, 128, C_KVA)
                act(kb, kvsq[:, :w_], pt[:, :w_], AF.Square, [pt], [kvsq])
                rms_bcast(kb, cst, [(kvsq[:, :w_], 128)], KVL, w_, psq, rkv, [kvsq])
                tt(kb, kb.dve, kvn[:, :w_], pt[:, :w_], rkv[:, :w_], ALU.mult, [pt, rkv], [kvn])
                pa = pacc.next()
                proj(pa, QK, 0, wkr)
                pb = pacc.next()
                proj(pb, QK, 0, wkrr)
                a1 = t1.next(); a2 = t2.next(); kr_ = krp.next()
                tt(kb, kb.dve, a1[64:96, :w_], pa[64:96, :w_], ropeC[64:96, :w_], ALU.mult, [pa, ropeC], [a1])
                tt(kb, kb.dve, a2[64:96, :w_], pb[64:96, :w_], ropeS[64:96, :w_], ALU.mult, [pb, ropeS], [a2])
                tt(kb, kb.pool, kr_[64:96, :w_], a1[64:96, :w_], a2[64:96, :w_], ALU.add, [a1, a2], [kr_])
                for h in range(H):
                    kb.dma(kb.pool, I["KT" + sfx].t[h, 64:96, o0:o0 + w_], kr_[64:96, :w_], I["KT" + sfx], kr_)
                for h in range(H):
                    pt = pacc.next()
                    mm(kb, pt[0:NOPE, :w_], wkn[:, h, :], kvn[:, :w_], True, True, [wkn, kvn], [pt])
                    ks_ = kst.next()
                    act(kb, ks_[:, :w_], pt[0:NOPE, :w_], AF.Copy, [pt], [ks_])
                    kb.dma(kb.act, I["KT" + sfx].t[h, 0:NOPE, o0:o0 + w_], ks_[:, :w_], I["KT" + sfx], ks_)
                vs_ = vst.next()
                for i in range(ntl):
                    pt = pacc.next()
                    mm(kb, pt[:, :], kvn[:, i * 128:(i + 1) * 128], wvv[:].rearrange("p h c -> p (h c)"), True, True,
                       [kvn, wvv], [pt])
                    act(kb, vs_[:, i, :], pt[:, :], AF.Copy, [pt], [vs_])
                kb.dma(kb.act, I["V" + sfx].t[o0:o0 + w_, :].rearrange("(i p) c -> p i c", p=128), vs_[:, :ntl, :],
                       I["V" + sfx], vs_)
                if want_q:
                    for c in range(2):
                        pt = pacc.next()
                        proj(pt, 128, C_QA + c * 128)
                        act(kb, qaT[:, c, :w_], pt[:, :w_], AF.Copy, [pt], [qaT])
                        act(kb, qsq[:, c, :w_], pt[:, :w_], AF.Square, [pt], [qsq])
                    rms_bcast(kb, cst, [(qsq[:, c, :w_], 128) for c in range(2)], QL, w_, psq, rq, [qsq])
                    tt(kb, kb.dve, Crs[:, :w_], ropeC[:, :w_], rq[0:QK, :w_], ALU.mult, [ropeC, rq], [Crs])
                    tt(kb, kb.dve, Srs[:, :w_], ropeS[:, :w_], rq[0:QK, :w_], ALU.mult, [ropeS, rq], [Srs])
                    for h in range(H):
                        pa = pacc.next()
                        for c in range(2):
                            mm(kb, pa[0:QK, :w_], wq[:, c, h, :], qaT[:, c, :w_], c == 0, c == 1, [wq, qaT], [pa])
                        for c in range(2):
                            mm(kb, pq2[0:QK, :w_], wqr[:, c, h, :], qaT[:, c, :w_], c == 0, c == 1, [wqr, qaT], [pq2])
                        a1 = t1.next(); a2 = t2.next(); q_ = qst.next()
                        tt(kb, kb.dve, a1[:, :w_], pa[0:QK, :w_], Crs[:, :w_], ALU.mult, [pa, Crs], [a1])
                        tt(kb, kb.dve, a2[:, :w_], pq2[0:QK, :w_], Srs[:, :w_], ALU.mult, [pq2, Srs], [a2])
                        tt(kb, kb.pool, q_[:, :w_], a1[:, :w_], a2[:, :w_], ALU.add, [a1, a2], [q_])
                        kb.dma(kb.pool, I["QT" + sfx].t[h, :, o0:o0 + w_], q_[:, :w_], I["QT" + sfx], q_)

        def ssd_prep(n, sfx, xbcpre):
            nch = n // 128
            cacc = kb.sb([128, n], F32, "cacc" + sfx)
            xbc = kb.sb([128, 6, n], BF16, "xbc" + sfx)
            for c in range(6):
                ts(kb, kb.dve, cacc[:], xbcpre[:, c, 0:n], cw[:, c, 0:1], ALU.mult, [xbcpre, cw], [cacc])
                for k in range(1, 5):
                    stt(kb, kb.dve, cacc[:], xbcpre[:, c, k:k + n], cw[:, c, k:k + 1], cacc[:], ALU.mult, ALU.add,
                        [xbcpre, cw, cacc], [cacc])
                act(kb, xbc[:, c, :], cacc[:], AF.Silu, [cacc, cb], [xbc], bias=cb[:, c:c + 1])
            for c in range(2):
                kb.dma(kb.sp, I["BCT" + sfx].t[c, :, :], xbc[:, 4 + c, :], I["BCT" + sfx], xbc)
            ptr = Ring([kb.ps([128, 640], BF16, "ptr" + sfx) for _ in range(2)])
            xtk = kb.sb([128, nch, 640], BF16, "xtk" + sfx)
            for ci in range(nch):
                p_ = ptr.next()
                for c in range(5):
                    kb.op(kb.pe, lambda e, c=c, p_=p_, ci=ci: e.transpose(p_[:, c * 128:(c + 1) * 128],
                                                                        xbc[:, c, ci * 128:(ci + 1) * 128], cst["ident_b"][:]),
                          reads=[xbc, cst["ident_b"]], writes=[p_], inc=(c == 4))
                cp(kb, kb.dve, xtk[:, ci, :], p_[:, :], [p_], [xtk])
            kb.dma(kb.sp, I["xtok" + sfx].t.rearrange("(i p) c -> p i c", p=128), xtk[:], I["xtok" + sfx], xtk)
            a_t = kb.sb([128, nch, 16], F32, "a_t" + sfx)
            ld_t = kb.sb([128, nch, 16], F32, "ld_t" + sfx)
            kb.dma(kb.sp, a_t[:], I["atok" + sfx].t.rearrange("(i p) c -> p i c", p=128), a_t, I["atok" + sfx])
            kb.dma(kb.sp, ld_t[:], I["ldtok" + sfx].t.rearrange("(i p) c -> p i c", p=128), ld_t, I["ldtok" + sfx])
            pcs = kb.ps([128, nch, 16], F32, "pcs" + sfx)
            ptot = kb.ps([128, nch, 16], F32, "ptot" + sfx)
            for ci in range(nch):
                for d in range(2):
                    mm(kb, pcs[:, ci, d * 8:(d + 1) * 8], tri[d][:], a_t[:, ci, d * 8:(d + 1) * 8], True, True, [tri[d], a_t], [pcs])
                mm(kb, ptot[:, ci, :], cst["ones_f"][:], a_t[:, ci, :], True, True, [cst["ones_f"], a_t], [ptot])
            tot = kb.sb([128, nch, 16], F32, "tot" + sfx)
            cp(kb, kb.dve, tot[:], ptot[:], [ptot], [tot])
            outer = kb.sb([128, nch, 16], F32, "outer" + sfx)
            mset(kb, kb.dve, outer[:], 0.0, [outer])
            for ci in range(nch - 2, -1, -1):
                tt(kb, kb.dve, outer[:, ci, 0:8], outer[:, ci + 1, 0:8], tot[:, ci + 1, 0:8], ALU.add, [outer, tot], [outer])
            for ci in range(1, nch):
                tt(kb, kb.dve, outer[:, ci, 8:16], outer[:, ci - 1, 8:16], tot[:, ci - 1, 8:16], ALU.add, [outer, tot], [outer])
            wexp = kb.sb([128, nch, 16], F32, "wexp" + sfx)
            tt(kb, kb.dve, wexp[:], tot[:], pcs[:], ALU.subtract, [tot, pcs], [wexp])
            tt(kb, kb.dve, wexp[:], wexp[:], outer[:], ALU.add, [wexp, outer], [wexp])
            tt(kb, kb.dve, wexp[:], wexp[:], ld_t[:], ALU.add, [wexp, ld_t], [wexp])
            act(kb, wexp[:], wexp[:], AF.Exp, [wexp], [wexp])
            pS = [kb.ps([128, 512], F32, "pS%d%s" % (d, sfx)) for d in range(2)]
            xw = Ring([kb.sb([128, 2, H, 64], BF16, "xw" + sfx) for _ in range(2)])
            for ci in range(nch):
                xw_ = xw.next()
                tt(kb, kb.dve, xw_[:], xtk[:, ci, 0:512].rearrange("p (h c) -> p h c", h=H).unsqueeze(1).broadcast_to([128, 2, H, 64]),
                   wexp[:, ci, :].rearrange("p (d h) -> p d h", d=2).unsqueeze(3).broadcast_to([128, 2, H, 64]), ALU.mult,
                   [xtk, wexp], [xw_])
                for d in range(2):
                    mm(kb, pS[d][:, :], xtk[:, ci, 512:640], xw_[:, d].rearrange("p h c -> p (h c)"), ci == 0, ci == nch - 1,
                       [xtk, xw_], [pS[d]], inc=True)
            Ssb = kb.sb([128, 2, 512], F32, "Ssb" + sfx)
            for d in range(2):
                cp(kb, kb.dve, Ssb[:, d, :], pS[d][:, :], [pS[d]], [Ssb])
            kb.dma(kb.sp, I["S" + sfx].t[:, :], Ssb[:].rearrange("p d c -> p (d c)"), I["S" + sfx], Ssb)
            at = kb.sb([128, 16], F32, "at" + sfx)
            tt(kb, kb.dve, at[:, 0:8], outer[:, 0, 0:8], tot[:, 0, 0:8], ALU.add, [outer, tot], [at])
            tt(kb, kb.dve, at[:, 8:16], outer[:, nch - 1, 8:16], tot[:, nch - 1, 8:16], ALU.add, [outer, tot], [at])
            kb.dma(kb.sp, I["atot" + sfx].t[0:1, :], at[0:1, :], I["atot" + sfx], at)

        run_group(CTX, I["xcT"], 0, 1, "c", None, xbc_c, with_ctx_q)
        run_group(T, I["xT"], 0, 0, "", True, xbc_l, True, halo=I["xhT"])
        sc_w.__exit__(None, None, None)
        with kb.scope():
            ssd_prep(CTX, "c", xbc_c)
        with kb.scope():
            ssd_prep(T, "", xbc_l)


def build_phase_a(T, l, with_ctx_q):
    nc = bass.Bass("TRN2", target_bir_lowering=False)
    es = contextlib.ExitStack()
    with es:
        kb = KB(nc, es)
        I = {}

        def inp(name, shape, dtype=F32):
            I[name] = kb.dram(name, shape, dtype, "ExternalInput")

        def outp(name, shape, dtype=F32):
            I[name] = kb.dram(name, shape, dtype, "ExternalOutput")

        inp("xT", [KC, 128, T]); inp("xhT", [KC, 128, 4]); inp("hmask", [128, 4]); inp("xcT", [KC, 128, CTX])
        inp("cT", [128, KC, 2]); inp("ident", [128, 128]); inp("tri", [2, 128, 128])
        inp("ropeC", [QK, T]); inp("ropeS", [QK, T])
        inp("w_mod", [2, D, 6 * D]); inp("b_mod", [2, 6 * D]); inp("norm1_g", [2, D])
        inp("w_in", [2, D, IN_COLS]); inp("q_norm_g", [2, QL]); inp("w_qb", [2, QL, H * QK])
        inp("kv_norm_g", [2, KVL]); inp("w_kvb", [2, KVL, H * 128])
        inp("conv_w", [2, 5, 768]); inp("conv_b", [2, 768]); inp("a_log", [2, 16]); inp("dt_bias", [2, 16])
        for sfx, n in (("", T), ("c", CTX)):
            outp("QT" + sfx, [H, QK, n], BF16); outp("KT" + sfx, [H, QK, n], BF16); outp("V" + sfx, [n, 512], BF16)
            outp("zs" + sfx, [4, 128, n], BF16); outp("xtok" + sfx, [n, 640], BF16); outp("BCT" + sfx, [2, 128, n], BF16)
            outp("atok" + sfx, [n, 16]); outp("ldtok" + sfx, [n, 16]); outp("S" + sfx, [128, 1024]); outp("atot" + sfx, [1, 16])

        cst = load_consts(kb, I)
        tri = [kb.sb([128, 128], F32, "tri%d" % d) for d in range(2)]
        for d in range(2):
            kb.dma(kb.sp, tri[d][:], I["tri"][d], tri[d], I["tri"])
        emit_phase_a(kb, I, T, l, with_ctx_q, cst, tri)
        kb.finish()
    return nc


def const_tables():
    j = np.arange(128)
    tri = np.stack([(j[:, None] <= j[None, :]), (j[:, None] >= j[None, :])]).astype(np.float32)
    return np.eye(128, dtype=np.float32), tri


def rope_tables_host(L):
    rows = L // GRID_W
    row = np.repeat(np.arange(rows), GRID_W)
    col = np.tile(np.arange(GRID_W), rows)
    inv = (10000.0 ** (-np.arange(8, dtype=np.float32) / 8)).astype(np.float32)
    ang = np.concatenate([row[:, None] * inv, col[:, None] * inv], -1).astype(np.float32)
    return np.cos(ang).astype(np.float32), np.sin(ang).astype(np.float32)


def fm(x2d):
    n = x2d.shape[0]
    return np.ascontiguousarray(x2d.T.reshape(KC, 128, n))


def host_inputs_a(W, l, x, ctx, c, cc, s, T, cos, sin):
    L = x.shape[0]
    ident, tri = const_tables()
    idx = [s * T - 2, s * T - 1, (s + 1) * T, (s + 1) * T + 1]
    halo = np.zeros((4, D), np.float32)
    hm = np.zeros((128, 4), np.float32)
    for i, t in enumerate(idx):
        if 0 <= t < L:
            halo[i] = x[t]
            hm[:, i] = 1.0
    ropeC = np.ones((QK, T), np.float32)
    ropeS = np.zeros((QK, T), np.float32)
    ropeC[64:80] = cos[s * T:(s + 1) * T].T
    ropeC[80:96] = cos[s * T:(s + 1) * T].T
    ropeS[64:80] = sin[s * T:(s + 1) * T].T
    ropeS[80:96] = sin[s * T:(s + 1) * T].T
    cT = np.stack([c.reshape(KC, 128).T, cc.reshape(KC, 128).T], -1).astype(np.float32)
    d = dict(xT=fm(x[s * T:(s + 1) * T]), xhT=fm(halo), hmask=hm, xcT=fm(ctx), cT=np.ascontiguousarray(cT),
             ident=ident, tri=tri, ropeC=ropeC, ropeS=ropeS)
    for k in ("w_mod", "b_mod", "norm1_g", "w_in", "q_norm_g", "w_qb", "kv_norm_g", "w_kvb", "conv_w", "conv_b"):
        d[k] = np.ascontiguousarray(W[k], dtype=np.float32)
    d["a_log"] = np.ascontiguousarray(W["a_log"], dtype=np.float32).reshape(2, 16)
    d["dt_bias"] = np.ascontiguousarray(W["dt_bias"], dtype=np.float32).reshape(2, 16)
    return d


def emit_attention(kb, cst, nq, QT_d, nk, KT_d, V_d, mix_d, sel65, loaders=None):
    NT = nk // 128
    QB = min(512, nq)
    with kb.scope():
        kT = Ring([kb.sb([QK, nk], BF16, "kT") for _ in range(2)])
        vA = Ring([kb.sb([128, NT, VD + 1], BF16, "vA") for _ in range(2)])
        for v_ in vA.items:
            mset(kb, kb.pool, v_[:], 1.0, [v_])
        qT = Ring([kb.sb([QK, nq], BF16, "qT") for _ in range(2)])
        pss = Ring([kb.ps([128, 1024], F32, "pss") for _ in range(2)])
        pso = Ring([kb.ps([128, 512], F32, "pso") for _ in range(2)])
        pden = kb.ps([64, 512], F32, "pden")
        pT = Ring([kb.sb([128, 1024], BF16, "pT") for _ in range(3)])
        osb = Ring([kb.sb([VD + 1, 512], F32, "osb") for _ in range(2)])
        rec = kb.sb([64, 512], F32, "rec")
        ost = Ring([kb.sb([64, 512], BF16, "ost") for _ in range(2)])
        def load_head(h):
            k_ = kT.next(); v_ = vA.next(); q_ = qT.next()
            if loaders is not None:
                loaders["kv"](h, k_, v_, nk)
            else:
                kb.dma(kb.sp, k_[:, :], KT_d.t[h, :, 0:nk], k_, KT_d)
                for t0 in range(0, NT, 16):
                    t1_ = min(NT, t0 + 16)
                    kb.dma(kb.sp, v_[:, t0:t1_, 0:VD],
                           V_d.t[t0 * 128:t1_ * 128, h * VD:(h + 1) * VD].rearrange("(t p) c -> p t c", p=128), v_, V_d)
            kb.dma(kb.sp, q_[:, :], QT_d.t[h, :, 0:nq], q_, QT_d)
            return k_, v_, q_

        fin_pend = [None]

        def finish_block(o_, h, qs):
            mm(kb, pden[:, :QB], sel65[:, :], o_[:, :QB], True, True, [sel65, o_], [pden])
            kb.op(kb.dve, lambda e: e.reciprocal(out=rec[:, :QB], in_=pden[:, :QB]), reads=[pden], writes=[rec])
            s_ = ost.next()
            tt(kb, kb.dve, s_[:, :QB], o_[0:64, :QB], rec[:, :QB], ALU.mult, [o_, rec], [s_])
            kb.dma(kb.pool, mix_d.t[h // 2, (h % 2) * 64:(h % 2) * 64 + 64, qs], s_[:, :QB], mix_d, s_)

        nxt = load_head(0)
        for h in range(H):
            k_, v_, q_ = nxt
            if h + 1 < H:
                nxt = load_head(h + 1)
            for qb in range(nq // QB):
                qs = slice(qb * QB, (qb + 1) * QB)
                po = pso.next()
                npair = NT // 2
                pend = None

                def pv(pr, p_):
                    for j in range(2):
                        kt = 2 * pr + j
                        mm(kb, po[0:VD + 1, :QB], v_[:, kt, :], p_[:, j * 512:j * 512 + QB], kt == 0, kt == NT - 1,
                           [v_, p_], [po], inc=(j == 1))

                for pr in range(npair):
                    ps_ = pss.next()
                    for j in range(2):
                        kt = 2 * pr + j
                        mm(kb, ps_[:, j * 512:j * 512 + QB], k_[:, kt * 128:(kt + 1) * 128], q_[:, qs], True, True,
                           [k_, q_], [ps_], inc=(j == 1))
                    p_ = pT.next()
                    if QB == 512:
                        act(kb, p_[:, :], ps_[:, :], AF.Exp, [ps_], [p_], scale=SCALE)
                    else:
                        for j in range(2):
                            act(kb, p_[:, j * 512:j * 512 + QB], ps_[:, j * 512:j * 512 + QB], AF.Exp, [ps_], [p_], scale=SCALE)
                    if pend is not None:
                        pv(*pend)
                    pend = (pr, p_)
                    if pr == 1 and fin_pend[0] is not None:
                        finish_block(*fin_pend[0])
                        fin_pend[0] = None
                pv(*pend)
                if fin_pend[0] is not None:
                    finish_block(*fin_pend[0])
                    fin_pend[0] = None
                o_ = osb.next()
                cp(kb, kb.dve, o_[:, :QB], po[0:VD + 1, :QB], [po], [o_])
                fin_pend[0] = (o_, h, qs)
        if fin_pend[0] is not None:
            finish_block(*fin_pend[0])


def emit_ssd(kb, cst, l, n, sfx, I, mix_d, yfw_d, tri, maskneg, loaders=None):
    nch = n // 128
    import os
    STOP = float(os.environ.get('SSD_STOP', '99'))
    with kb.scope():
        xtk = kb.sb([128, nch, 640], BF16, "xtk")
        kb.dma(kb.sp, xtk[:], I["xtok" + sfx].t.rearrange("(i p) c -> p i c", p=128), xtk, I["xtok" + sfx])
        bct = kb.sb([64, 2, 2, n], BF16, "bct")
        for c in range(2):
            kb.dma(kb.sp, bct[:, c, :, :], I["BCT" + sfx].t[c].rearrange("(g n) t -> n g t", g=2), bct, I["BCT" + sfx])
        a_t = kb.sb([128, nch, 16], F32, "a_t")
        ld_t = kb.sb([128, nch, 16], F32, "ld_t")
        kb.dma(kb.sp, a_t[:], I["atok" + sfx].t.rearrange("(i p) c -> p i c", p=128), a_t, I["atok" + sfx])
        kb.dma(kb.sp, ld_t[:], I["ldtok" + sfx].t.rearrange("(i p) c -> p i c", p=128), ld_t, I["ldtok" + sfx])
        dsk = kb.sb([128, H], F32, "dsk")
        kb.dma(kb.sp, dsk[:], I["d_skip"][l:l + 1, :].broadcast_to([128, H]), dsk, I["d_skip"])
        DI = kb.sb([128, H, 128], F32, "DI")
        tt(kb, kb.dve, DI[:], cst["ident_f"][:].unsqueeze(1).broadcast_to([128, H, 128]),
           dsk[:].unsqueeze(2).broadcast_to([128, H, 128]), ALU.mult, [cst["ident_f"], dsk], [DI])
        sng = kb.sb([64, H], F32, "sng")
        kb.dma(kb.sp, sng[:], I["ssd_norm_g"][l].rearrange("(h p) -> p h", p=64), sng, I["ssd_norm_g"],
               allow_slow_non_contiguous=True)
        zs_v = I["zs" + sfx].t.rearrange("c q t -> (c q) t").rearrange("(h p) t -> p h t", p=64)
        mix_v = mix_d.t[4:8].rearrange("c q t -> (c q) t").rearrange("(h p) t -> p h t", p=64)
        pct = kb.ps([128, 16], F32, "pct")
        pB = kb.ps([128, H, 128], F32, "pB")
        pG = kb.ps([128, 2, 128], F32, "pG")
        py = kb.ps([64, H, 128], F32, "py")
        pSt = kb.ps([64, 512], F32, "pSt")
        psq = kb.ps([64, 128], F32, "psqs")
        R__r = Ring([kb.sb([128, H, 128], F32, "R") for _ in range(2)])
        csl_r = Ring([kb.sb([128, H], F32, "csl") for _ in range(2)])
        dec_r = Ring([kb.sb([128, H], F32, "dec") for _ in range(2)])
        wj_r = Ring([kb.sb([128, H], F32, "wj") for _ in range(2)])
        D1_r = Ring([kb.sb([128, H, 128], F32, "D1") for _ in range(2)])
        E__r = Ring([kb.sb([128, H, 128], F32, "E") for _ in range(2)])
        Cx_r = Ring([kb.sb([128, H, 128], F32, "Cx") for _ in range(2)])
        CexpT_r = Ring([kb.sb([64, H, 128], BF16, "CexpT") for _ in range(2)])
        STf_r = Ring([kb.sb([128, H, 128], F32, "STf") for _ in range(2)])
        STb_r = Ring([kb.sb([128, H, 128], BF16, "STb") for _ in range(2)])
        xw_r = Ring([kb.sb([128, H, 64], BF16, "xw") for _ in range(2)])
        hst = kb.sb([64, H, 64], F32, "hst")
        hpb = kb.sb([64, H, 64], BF16, "hpb")
        ssrc = kb.sb([64, 512], F32, "ssrc")
        atb = kb.sb([128, H], F32, "atb")
        ysb = Ring([kb.sb([64, H, 128], F32, "ysb") for _ in range(2)])
        yfl = Ring([kb.sb([64, H, 128], F32, "yfl") for _ in range(2)])
        zt = Ring([kb.sb([64, H, 128], BF16, "zt") for _ in range(2)])
        yg = kb.sb([64, H, 128], F32, "yg")
        ysq = kb.sb([64, H, 128], BF16, "ysq")
        rs_ = kb.sb([64, 128], F32, "rs")
        yo = Ring([kb.sb([64, H, 128], BF16, "yo") for _ in range(2)])
        hflat = hst[:].rearrange("p h c -> p (h c)")
        for d in range(2):
            if loaders is not None and "state" in loaders:
                loaders["state"](d, hst, hflat, ssrc, atb)
            elif loaders is not None or ("Sch" + sfx) not in I:
                mset(kb, kb.dve, hst[:], 0.0, [hst])
            else:
                for k in range(3, -1, -1):
                    kb.dma(kb.sp, ssrc[:], I["Sch" + sfx].t[d, k], ssrc, I["Sch" + sfx])
                    if k == 3:
                        cp(kb, kb.dve, hflat, ssrc[:], [ssrc], [hst])
                    else:
                        kb.dma(kb.sp, atb[:], I["atch" + sfx].t[d, k:k + 1, :].broadcast_to([128, H]), atb, I["atch" + sfx])
                        act(kb, atb[:], atb[:], AF.Exp, [atb], [atb])
                        tt(kb, kb.dve, hst[:], hst[:], atb[0:64, :].unsqueeze(2).broadcast_to([64, H, 64]), ALU.mult, [hst, atb], [hst])
                        tt(kb, kb.dve, hflat, hflat, ssrc[:], ALU.add, [hst, ssrc], [hst])
            cp(kb, kb.dve, hpb[:], hst[:], [hst], [hpb])
            def stage1(ci, t_out, d=d):
                tsl = slice(ci * 128, (ci + 1) * 128)
                R_ = R__r.next(); csl = csl_r.next(); dec = dec_r.next(); wj = wj_r.next(); D1 = D1_r.next(); E_ = E__r.next(); Cx = Cx_r.next(); CexpT = CexpT_r.next(); STf = STf_r.next(); STb = STb_r.next(); xw = xw_r.next()
                a_d = a_t[:, ci, d * 8:(d + 1) * 8]
                mm(kb, pct[:, 0:8], tri[d][:], a_d, True, True, [tri[d], a_t], [pct])
                yield
                mm(kb, pct[:, 8:16], cst["ones_f"][:], a_d, True, True, [cst["ones_f"], a_t], [pct])
                yield
                tt(kb, kb.pool, R_[:], tri[d][:].unsqueeze(1).broadcast_to([128, H, 128]),
                   a_d.unsqueeze(2).broadcast_to([128, H, 128]), ALU.mult, [tri[d], a_t], [R_])
                yield
                for hh in range(2):
                    mm(kb, pB[:, hh * 4:(hh + 1) * 4, :].rearrange("p h i -> p (h i)"), cst["ones_f"][:],
                       R_[:, hh * 4:(hh + 1) * 4, :].rearrange("p h i -> p (h i)"), True, True, [cst["ones_f"], R_], [pB])
                    yield
                tt(kb, kb.dve, csl[:], pct[:, 0:8], ld_t[:, ci, d * 8:(d + 1) * 8], ALU.subtract, [pct, ld_t], [csl])
                yield
                act(kb, dec[:], pct[:, 8:16], AF.Exp, [pct], [dec])
                yield
                tt(kb, kb.dve, wj[:], pct[:, 8:16], csl[:], ALU.subtract, [pct, csl], [wj])
                yield
                act(kb, wj[:], wj[:], AF.Exp, [wj], [wj])
                yield
                tt(kb, kb.dve, D1[:], pB[:], csl[:].unsqueeze(2).broadcast_to([128, H, 128]), ALU.subtract, [pB, csl], [D1])
                yield
                tt(kb, kb.pool, D1[:], D1[:], maskneg[d][:].unsqueeze(1).broadcast_to([128, H, 128]), ALU.add,
                   [D1, maskneg[d]], [D1])
                yield
                act(kb, E_[:], D1[:], AF.Exp, [D1], [E_])
                yield
                act(kb, Cx[:], pB[:], AF.Exp, [pB], [Cx])
                yield
                tt(kb, kb.dve, CexpT[:].rearrange("p (g h) i -> p g h i", g=2), Cx[0:64].rearrange("p (g h) i -> p g h i", g=2),
                   bct[:, 1, :, tsl].unsqueeze(2).broadcast_to([64, 2, 4, 128]), ALU.mult, [Cx, bct], [CexpT])
                yield
                for g in range(2):
                    mm(kb, pG[:, g, :], bct[:, 0, g, tsl], bct[:, 1, g, tsl], True, True, [bct], [pG])
                    yield
                Ev = E_[:].rearrange("p (g h) i -> p g h i", g=2)
                Gv = pG[:].unsqueeze(2).broadcast_to([128, 2, 4, 128])
                if d == 0:
                    tt(kb, kb.dve, STf[:].rearrange("p (g h) i -> p g h i", g=2), Ev, Gv, ALU.mult, [E_, pG], [STf])
                    yield
                    tt(kb, kb.pool, STb[:], STf[:], DI[:], ALU.add, [STf, DI], [STb])
                    yield
                else:
                    tt(kb, kb.dve, STb[:].rearrange("p (g h) i -> p g h i", g=2), Ev, Gv, ALU.mult, [E_, pG], [STb])
                    yield
                t_out.update(tsl=tsl, wj=wj, dec=dec, CexpT=CexpT, STb=STb, xw=xw)

            def stage2(ci, t_, d=d):
                tsl = t_['tsl']; wj = t_['wj']; dec = t_['dec']; CexpT = t_['CexpT']; STb = t_['STb']; xw = t_['xw']
                for h in range(H):
                    g = h // 4
                    mm(kb, py[:, h, :], xtk[:, ci, h * 64:(h + 1) * 64], STb[:, h, :], True, False, [xtk, STb], [py], inc=False)
                    mm(kb, py[:, h, :], hpb[:, h, :], CexpT[:, h, :], False, True, [hpb, CexpT], [py], inc=True)
                    yield
                tt(kb, kb.pool, xw[:], xtk[:, ci, 0:512].rearrange("p (h c) -> p h c", h=H),
                   wj[:].unsqueeze(2).broadcast_to([128, H, 64]), ALU.mult, [xtk, wj], [xw])
                yield
                for g in range(2):
                    mm(kb, pSt[:, g * 256:(g + 1) * 256], xtk[:, ci, 512 + g * 64:512 + (g + 1) * 64],
                       xw[:, g * 4:(g + 1) * 4, :].rearrange("p h c -> p (h c)"), True, True, [xtk, xw], [pSt])
                    yield
                tt(kb, kb.dve, hst[:], hst[:], dec[0:64, :].unsqueeze(2).broadcast_to([64, H, 64]), ALU.mult, [hst, dec], [hst])
                yield
                tt(kb, kb.dve, hflat, hflat, pSt[:, :], ALU.add, [hst, pSt], [hst])
                yield
                cp(kb, kb.dve, hpb[:], hst[:], [hst], [hpb])
                yield
                if d == 0:
                    y_ = ysb.next()
                    cp(kb, kb.dve, y_[:], py[:], [py], [y_])
                    yield
                    kb.dma(kb.sp, yfw_d.t[ci].rearrange("p (h i) -> p h i", h=H), y_[:], yfw_d, y_)
                    yield
                else:
                    yf_ = yfl.next(); z_ = zt.next()
                    kb.dma(kb.sp, yf_[:], yfw_d.t[ci].rearrange("p (h i) -> p h i", h=H), yf_, yfw_d)
                    yield
                    kb.dma(kb.sp, z_[:], zs_v[:, :, tsl], z_, I["zs" + sfx])
                    yield
                    tt(kb, kb.dve, yg[:], py[:], yf_[:], ALU.add, [py, yf_], [yg])
                    yield
                    tt(kb, kb.pool, yg[:], yg[:], z_[:], ALU.mult, [yg, z_], [yg])
                    yield
                    act(kb, ysq[:], yg[:], AF.Square, [yg], [ysq])
                    yield
                    for h in range(H):
                        mm(kb, psq[:, :], cst["ones_b"][0:64, 0:64], ysq[:, h, :], h == 0, h == H - 1, [cst["ones_b"], ysq], [psq])
                        yield
                    act(kb, rs_[:], psq[:, :], AF.Ln, [psq, cst["eps"]], [rs_], bias=cst["eps"][0:64, 0:1], scale=1.0 / SSD_IN)
                    yield
                    act(kb, rs_[:], rs_[:], AF.Exp, [rs_], [rs_], scale=-0.5)
                    yield
                    tt(kb, kb.dve, yg[:], yg[:], rs_[:].unsqueeze(1).broadcast_to([64, H, 128]), ALU.mult, [yg, rs_], [yg])
                    yield
                    o_ = yo.next()
                    tt(kb, kb.pool, o_[:], yg[:], sng[:].unsqueeze(2).broadcast_to([64, H, 128]), ALU.mult, [yg, sng], [o_])
                    yield
                    kb.dma(kb.sp, mix_v[:, :, tsl], o_[:], mix_d, o_)
                    yield


            order = list(range(nch)) if d == 0 else list(range(nch - 1, -1, -1))

            def zip_run(gens):
                gens = [g for g in gens if g is not None]
                while gens:
                    for g in list(gens):
                        try:
                            next(g)
                        except StopIteration:
                            gens.remove(g)

            pend = None
            for ci in order:
                t_ = {}
                zip_run([stage1(ci, t_), stage2(*pend) if pend is not None else None])
                pend = (ci, t_)
            zip_run([stage2(*pend)])


def emit_tail(kb, cst, l, n, cidx, xin_d, mix_d, x1_d, h2_d, xout_d, I, mod, last):
    TBk = min(512, n)
    nblk = n // TBk
    g1c, sh2, g2c = mod[2], mod[3], mod[5]
    gT = kb.sb([NE, n], BF16, "gT")
    with kb.scope():
        wout = kb.sb([128, KC, D], BF16, "wout")
        for k in range(KC):
            kb.dma(kb.pool, wout[:, k, :], I["w_out"][l][k * 128:(k + 1) * 128, :], wout, I["w_out"])
        n2g = load_col(kb, I["norm2_g"], I["norm2_g"][l], KC, "n2g")
        gmod2 = kb.sb([128, KC], F32, "gmod2")
        ts(kb, kb.dve, gmod2[:], mod[4][:, :, cidx], 1.0, ALU.add, [mod[4]], [gmod2])
        tt(kb, kb.dve, gmod2[:], gmod2[:], n2g[:], ALU.mult, [gmod2, n2g], [gmod2])
        rw = kb.sb([128, KC, 36], F32, "rw")
        kb.dma(kb.sp, rw[:, :, 0:4], I["router_w1"][l].rearrange("(k p) c -> p k c", p=128), rw, I["router_w1"])
        kb.dma(kb.sp, rw[:, :, 4:36], I["router_w2"][l].rearrange("(k p) c -> p k c", p=128), rw, I["router_w2"])
        rb = kb.sb([128, 36], F32, "rb")
        kb.dma(kb.sp, rb[:, 0:4], I["router_b1"][l:l + 1, :].broadcast_to([128, 4]), rb, I["router_b1"])
        kb.dma(kb.sp, rb[:, 4:36], I["router_b2"][l:l + 1, :].broadcast_to([128, 32]), rb, I["router_b2"])
        mixb = Ring([kb.sb([128, KC, TBk], BF16, "mixb") for _ in range(2)])
        xt__rr = Ring([kb.sb([128, KC, TBk], F32, "xt") for _ in range(2)])
        x1_rr = Ring([kb.sb([128, KC, TBk], F32, "x1") for _ in range(2)])
        sq_rr = Ring([kb.sb([128, KC, TBk], BF16, "sq2") for _ in range(2)])
        rstd = kb.sb([128, TBk], F32, "rstd2")
        tmp = kb.sb([128, TBk], F32, "tmp2")
        h2f_rr = Ring([kb.sb([128, KC, TBk], F32, "h2f") for _ in range(2)])
        h2b_rr = Ring([kb.sb([128, KC, TBk], BF16, "h2b") for _ in range(2)])
        po = Ring([kb.ps([128, TBk], F32, "po") for _ in range(4)])
        psq = kb.ps([128, TBk], F32, "psq2")
        plg = kb.ps([128, 36], F32, "plg")
        pgt = kb.ps([NE, 128], F32, "pgt")
        lg = kb.sb([128, 36], F32, "lg")
        sm = kb.sb([128, 16], F32, "sm")
        e1 = kb.sb([128, 4], F32, "e1")
        ohg = kb.sb([128, 4], F32, "ohg")
        l2g = kb.sb([128, 4, 8], F32, "l2g")
        lsel = kb.sb([128, 8], F32, "lsel")
        e2 = kb.sb([128, 8], F32, "e2")
        mk1 = kb.sb([128, 8], F32, "mk1")
        mk2 = kb.sb([128, 8], F32, "mk2")
        lp = kb.sb([128, 8], F32, "lp")
        wi = kb.sb([128, 8], F32, "wi")
        gate = kb.sb([128, 4, 8], F32, "gate")
        for b in range(nblk):
            bs = slice(b * TBk, (b + 1) * TBk)
            mb = mixb.next()
            xt_ = xt__rr.next(); x1 = x1_rr.next(); sq = sq_rr.next(); h2f = h2f_rr.next(); h2b = h2b_rr.next()
            for k in range(KC):
                kb.dma(kb.sp, mb[:, k, :], mix_d.t[k, :, bs], mb, mix_d)
                kb.dma(kb.sp, xt_[:, k, :], xin_d.t[k, :, bs], xt_, xin_d)
            for dc in range(KC):
                p_ = po.next()
                for k in range(KC):
                    mm(kb, p_[:, :], wout[:, k, dc * 128:(dc + 1) * 128], mb[:, k, :], k == 0, k == KC - 1, [wout, mb], [p_])
                stt(kb, kb.dve, x1[:, dc, :], p_[:, :], g1c[:, dc, cidx:cidx + 1], xt_[:, dc, :], ALU.mult, ALU.add,
                    [p_, g1c, xt_], [x1])
            for k in range(KC):
                kb.dma(kb.sp, x1_d.t[k, :, bs], x1[:, k, :], x1_d, x1)
            act(kb, sq[:], x1[:], AF.Square, [x1], [sq])
            rms_bcast(kb, cst, [(sq[:, k, :], 128) for k in range(KC)], D, TBk, psq, rstd, [sq])
            for k in range(KC):
                stt(kb, kb.dve, tmp[:], x1[:, k, :], gmod2[:, k:k + 1], rstd[:], ALU.mult, ALU.mult, [x1, gmod2, rstd], [tmp])
                act(kb, h2f[:, k, :], tmp[:], AF.Identity, [tmp, sh2], [h2f], bias=sh2[:, k, cidx:cidx + 1])
            cp(kb, kb.pool, h2b[:], h2f[:], [h2f], [h2b])
            for k in range(KC):
                kb.dma(kb.sp, h2_d.t[k, :, bs], h2b[:, k, :], h2_d, h2b)
            for i in range(TBk // 128):
                for k in range(KC):
                    mm(kb, plg[:, :], h2f[:, k, i * 128:(i + 1) * 128], rw[:, k, :], k == 0, k == KC - 1, [h2f, rw], [plg])
                tt(kb, kb.dve, lg[:], plg[:, :], rb[:], ALU.add, [plg, rb], [lg])
                R = [lg]
                kb.op(kb.dve, lambda e: e.tensor_reduce(out=sm[:, 0:1], in_=lg[:, 0:4], axis=AX.X, op=ALU.max), reads=R, writes=[sm])
                ts(kb, kb.dve, sm[:, 1:2], sm[:, 0:1], -1.0, ALU.mult, [sm], [sm])
                act(kb, e1[:], lg[:, 0:4], AF.Exp, [lg, sm], [e1, sm], bias=sm[:, 1:2], accum_out=sm[:, 2:3])
                kb.op(kb.dve, lambda e: e.reciprocal(out=sm[:, 3:4], in_=sm[:, 2:3]), reads=[sm], writes=[sm])
                ts(kb, kb.dve, ohg[:], lg[:, 0:4], sm[:, 0:1], ALU.is_equal, [lg, sm], [ohg])
                tt(kb, kb.dve, l2g[:], lg[:, 4:36].rearrange("p (g e) -> p g e", g=4), ohg[:].unsqueeze(2).broadcast_to([128, 4, 8]),
                   ALU.mult, [lg, ohg], [l2g])
                kb.op(kb.dve, lambda e: e.tensor_reduce(out=lsel[:], in_=l2g[:].rearrange("p g e -> p e g"), axis=AX.X, op=ALU.add),
                      reads=[l2g], writes=[lsel])
                kb.op(kb.dve, lambda e: e.tensor_reduce(out=sm[:, 4:5], in_=lsel[:], axis=AX.X, op=ALU.max), reads=[lsel], writes=[sm])
                ts(kb, kb.dve, sm[:, 5:6], sm[:, 4:5], -1.0, ALU.mult, [sm], [sm])
                act(kb, e2[:], lsel[:], AF.Exp, [lsel, sm], [e2], bias=sm[:, 5:6])
                ts(kb, kb.dve, mk1[:], lsel[:], sm[:, 4:5], ALU.is_equal, [lsel, sm], [mk1])
                stt(kb, kb.dve, lp[:], mk1[:], -1.0e30, lsel[:], ALU.mult, ALU.add, [mk1, lsel], [lp])
                kb.op(kb.dve, lambda e: e.tensor_reduce(out=sm[:, 6:7], in_=lp[:], axis=AX.X, op=ALU.max), reads=[lp], writes=[sm])
                ts(kb, kb.dve, mk2[:], lp[:], sm[:, 6:7], ALU.is_equal, [lp, sm], [mk2])
                tt(kb, kb.dve, mk1[:], mk1[:], mk2[:], ALU.add, [mk1, mk2], [mk1])
                tt(kb, kb.dve, wi[:], e2[:], mk1[:], ALU.mult, [e2, mk1], [wi])
                kb.op(kb.dve, lambda e: e.tensor_reduce(out=sm[:, 7:8], in_=wi[:], axis=AX.X, op=ALU.add), reads=[wi], writes=[sm])
                kb.op(kb.dve, lambda e: e.reciprocal(out=sm[:, 8:9], in_=sm[:, 7:8]), reads=[sm], writes=[sm])
                tt(kb, kb.dve, sm[:, 9:10], sm[:, 8:9], sm[:, 3:4], ALU.mult, [sm], [sm])
                ts(kb, kb.dve, wi[:], wi[:], sm[:, 9:10], ALU.mult, [wi, sm], [wi])
                tt(kb, kb.dve, gate[:], ohg[:].unsqueeze(2).broadcast_to([128, 4, 8]), wi[:].unsqueeze(1).broadcast_to([128, 4, 8]),
                   ALU.mult, [ohg, wi], [gate])
                kb.op(kb.pe, lambda e: e.transpose(pgt[:, :], gate[:].rearrange("p g e -> p (g e)"), cst["ident_f"][:]),
                      reads=[gate, cst["ident_f"]], writes=[pgt])
                cp(kb, kb.dve, gT[:, b * TBk + i * 128:b * TBk + (i + 1) * 128], pgt[:, :], [pgt], [gT])
    TH = min(n, 2048)
    with kb.scope():
        sel = kb.sb([NE, NE * 128], BF16, "sel")
        kb.dma(kb.pool, sel[:], I["sel"][:, :], sel, I["sel"])
        yaccs = [kb.sb([128, KC, TBk], F32, "yacc") for _ in range(TH // TBk)]
        h2hs = [kb.sb([128, KC, TBk], BF16, "h2h") for _ in range(TH // TBk)]
        wg = Ring([kb.sb([128, KC, FF], BF16, "wg") for _ in range(2)])
        wu = Ring([kb.sb([128, KC, FF], BF16, "wu") for _ in range(2)])
        wd = Ring([kb.sb([128, 2, D], BF16, "wd") for _ in range(3)])
        pgb = kb.ps([128, TBk], F32, "pgb")
        pgu = Ring([kb.ps([128, TBk], F32, "pgu") for _ in range(4)])
        pyy = Ring([kb.ps([128, TBk], F32, "pyy") for _ in range(3)])
        gbc = Ring([kb.sb([128, TBk], BF16, "gbc") for _ in range(2)])
        sg = Ring([kb.sb([128, TBk], BF16, "sg") for _ in range(2)])
        tu = Ring([kb.sb([128, TBk], BF16, "tu") for _ in range(2)])
        A_ = Ring([kb.sb([128, 2, TBk], BF16, "A") for _ in range(3)])
        x1b_r = Ring([kb.sb([128, KC, TBk], F32, "x1b") for _ in range(2)])
        sqf = kb.sb([128, KC, TBk], BF16, "sqf") if last else None
        rsf = kb.sb([128, TBk], F32, "rsf") if last else None
        fg = load_col(kb, I["final_g"], I["final_g"].t, KC, "fg") if last else None
        def emit_down(e, d_, a_, bs):
            yacc = yaccs[bs.start // TBk]
            bs = slice(0, TBk)
            for dc in range(KC):
                py_ = pyy.next()
                for f in range(2):
                    mm(kb, py_[:, :], d_[:, f, dc * 128:(dc + 1) * 128], a_[:, f, :], f == 0, f == 1, [d_, a_], [py_])
                if e == 0:
                    act(kb, yacc[:, dc, bs], py_[:, :], AF.Copy, [py_], [yacc])
                else:
                    tt(kb, kb.dve, yacc[:, dc, bs], yacc[:, dc, bs], py_[:, :], ALU.add, [yacc, py_], [yacc])

        def load_w(e):
            g_ = wg.next(); u_ = wu.next(); d_ = wd.next()
            kb.dma(kb.pool, g_[:], I["w_gate"][l, e].rearrange("(k p) f -> p k f", p=128), g_, I["w_gate"])
            kb.dma(kb.pool, u_[:], I["w_up"][l, e].rearrange("(k p) f -> p k f", p=128), u_, I["w_up"])
            kb.dma(kb.pool, d_[:], I["w_down"][l, e].rearrange("(f p) c -> p f c", p=128), d_, I["w_down"])
            return g_, u_, d_

        wpre = [None]
        pend_down = None
        def load_h2h(hf, b):
            for k in range(KC):
                kb.dma(kb.sp, h2hs[b][:, k, :], h2_d.t[k, :, hf * TH + b * TBk:hf * TH + (b + 1) * TBk], h2hs[b], h2_d)

        for b in range(TH // TBk):
            load_h2h(0, b)
        for hf in range(n // TH):
            for e in range(NE):
                if wpre[0] is None:
                    wpre[0] = load_w(e)
                g_, u_, d_ = wpre[0]
                wpre[0] = load_w((e + 1) % NE) if (e + 1 < NE or hf + 1 < n // TH) else None
                for b in range(TH // TBk):
                    bs = slice(b * TBk, (b + 1) * TBk)
                    gs = slice(hf * TH + b * TBk, hf * TH + (b + 1) * TBk)
                    mm(kb, pgb[:, :], sel[:, e * 128:(e + 1) * 128], gT[:, gs], True, True, [sel, gT], [pgb])
                    gb_ = gbc.next()
                    act(kb, gb_[:], pgb[:, :], AF.Copy, [pgb], [gb_])
                    a_ = A_.next()
                    for f in range(2):
                        pg_ = pgu.next(); pu_ = pgu.next()
                        for k in range(KC):
                            mm(kb, pg_[:, :], g_[:, k, f * 128:(f + 1) * 128], h2hs[b][:, k, :], k == 0, k == KC - 1, [g_, h2hs[b]], [pg_])
                        for k in range(KC):
                            mm(kb, pu_[:, :], u_[:, k, f * 128:(f + 1) * 128], h2hs[b][:, k, :], k == 0, k == KC - 1, [u_, h2hs[b]], [pu_])
                        s_ = sg.next(); t_ = tu.next()
                        act(kb, s_[:], pg_[:, :], AF.Silu, [pg_], [s_])
                        tt(kb, kb.dve, t_[:], pu_[:, :], s_[:], ALU.mult, [pu_, s_], [t_])
                        tt(kb, kb.pool, a_[:, f, :], t_[:], gb_[:], ALU.mult, [t_, gb_], [a_])
                    if e == NE - 1 and hf + 1 < n // TH:
                        load_h2h(hf + 1, b)
                    if pend_down is not None:
                        emit_down(*pend_down)
                    pend_down = (e, d_, a_, bs)
            emit_down(*pend_down)
            pend_down = None
            for b in range(TH // TBk):
                bs = slice(b * TBk, (b + 1) * TBk)
                gs = slice(hf * TH + b * TBk, hf * TH + (b + 1) * TBk)
                x1b = x1b_r.next()
                for k in range(KC):
                    kb.dma(kb.act, x1b[:, k, :], x1_d.t[k, :, gs], x1b, x1_d)
                for k in range(KC):
                    stt(kb, kb.dve, x1b[:, k, :], yaccs[b][:, k, :], g2c[:, k, cidx:cidx + 1], x1b[:, k, :], ALU.mult, ALU.add,
                        [yaccs[b], g2c, x1b], [x1b])
                if last:
                    act(kb, sqf[:], x1b[:], AF.Square, [x1b], [sqf])
                    rms_bcast(kb, cst, [(sqf[:, k, :], 128) for k in range(KC)], D, TBk, pgb, rsf, [sqf])
                    for k in range(KC):
                        stt(kb, kb.dve, x1b[:, k, :], x1b[:, k, :], fg[:, k:k + 1], rsf[:], ALU.mult, ALU.mult, [x1b, fg, rsf], [x1b])
                for k in range(KC):
                    kb.dma(kb.act, xout_d.t[k, :, gs], x1b[:, k, :], xout_d, x1b)


def emit_phase_b(kb, I, T, l, last, cst, tri, maskneg, sel65, parts=("att", "ssd", "tail"), loaders=None):
    do_ctx = not last
    NK = CTX + 4 * T
    with kb.scope():
        mod = emit_mod(kb, I, l, cst, [2, 3, 4, 5])
        if "ssd" in parts:
            emit_ssd(kb, cst, l, T, "", I, I["mix"], I["yfw"], tri, maskneg, loaders)
            if do_ctx:
                emit_ssd(kb, cst, l, CTX, "c", I, I["mixc"], I["yfwc"], tri, maskneg, None)
        if "att" in parts:
            emit_attention(kb, cst, T, I["QT"], NK, I.get("KTall"), I.get("Vall"), I["mix"], sel65, loaders)
            if do_ctx:
                emit_attention(kb, cst, CTX, I["QTc"], CTX, I.get("KTall"), I.get("Vall"), I["mixc"], sel65, loaders)
        if "tail" in parts:
            emit_tail(kb, cst, l, T, 0, I["xT"], I["mix"], I["x1"], I["h2"], I["xout"], I, mod, last)
            if do_ctx:
                emit_tail(kb, cst, l, CTX, 1, I["xcT"], I["mixc"], I["x1c"], I["h2c"], I["xoutc"], I, mod, False)


def build_phase_b(T, l, last, parts=("att", "ssd", "tail"), debug=False):
    do_ctx = not last
    NK = CTX + 4 * T
    nc = bass.Bass("TRN2", target_bir_lowering=False)
    es = contextlib.ExitStack()
    with es:
        kb = KB(nc, es)
        I = {}

        def inp(name, shape, dtype=F32):
            I[name] = kb.dram(name, shape, dtype, "ExternalInput")

        def outp(name, shape, dtype=F32):
            I[name] = kb.dram(name, shape, dtype, "ExternalOutput")

        def scratch(name, shape, dtype=F32):
            I[name] = kb.dram(name, shape, dtype, "ExternalOutput" if debug else "Internal")

        inp("xT", [KC, 128, T]); inp("cT", [128, KC, 2]); inp("ident", [128, 128]); inp("tri", [2, 128, 128])
        inp("maskneg", [2, 128, 128]); inp("sel", [NE, NE * 128])
        inp("w_mod", [2, D, 6 * D]); inp("b_mod", [2, 6 * D]); inp("norm2_g", [2, D]); inp("w_out", [2, D, D])
        inp("router_w1", [2, D, 4]); inp("router_b1", [2, 4]); inp("router_w2", [2, D, NE]); inp("router_b2", [2, NE])
        inp("w_gate", [2, NE, D, FF]); inp("w_up", [2, NE, D, FF]); inp("w_down", [2, NE, FF, D])
        inp("d_skip", [2, H]); inp("ssd_norm_g", [2, SSD_IN]); inp("final_g", [D])
        inp("QT", [H, QK, T], BF16); inp("KTall", [H, QK, NK], BF16); inp("Vall", [NK, 512], BF16)
        groups = [("", T)] + ([("c", CTX)] if do_ctx else [])
        for sfx, n in groups:
            inp("zs" + sfx, [4, 128, n], BF16); inp("xtok" + sfx, [n, 640], BF16); inp("BCT" + sfx, [2, 128, n], BF16)
            inp("atok" + sfx, [n, 16]); inp("ldtok" + sfx, [n, 16])
            inp("Sch" + sfx, [2, 4, 64, 512]); inp("atch" + sfx, [2, 4, H])
            scratch("mix" + sfx, [KC, 128, n], BF16); scratch("yfw" + sfx, [n // 128, 64, H * 128])
            scratch("x1" + sfx, [KC, 128, n]); scratch("h2" + sfx, [KC, 128, n], BF16)
            outp("xout" + sfx, [KC, 128, n])
        if do_ctx:
            inp("xcT", [KC, 128, CTX]); inp("QTc", [H, QK, CTX], BF16)

        cst = load_consts(kb, I)
        tri = [kb.sb([128, 128], F32, "tri%d" % d) for d in range(2)]
        maskneg = [kb.sb([128, 128], F32, "mneg%d" % d) for d in range(2)]
        for d in range(2):
            kb.dma(kb.sp, tri[d][:], I["tri"][d], tri[d], I["tri"])
            kb.dma(kb.sp, maskneg[d][:], I["maskneg"][d], maskneg[d], I["maskneg"])
        sel65 = kb.sb([VD + 1, 64], F32, "sel65")
        mset(kb, kb.dve, sel65[:], 0.0, [sel65])
        mset(kb, kb.dve, sel65[64:65, :], 1.0, [sel65])
        emit_phase_b(kb, I, T, l, last, cst, tri, maskneg, sel65, parts)
        kb.finish()
    return nc


def pick_state(S_, d):
    out = np.zeros((64, 512), np.float32)
    for h in range(H):
        g = h // 4
        out[:, h * 64:(h + 1) * 64] = S_[g * 64:(g + 1) * 64, d * 512 + h * 64:d * 512 + (h + 1) * 64]
    return out


def host_inputs_b(W, l, last, T, s, xT_own, xcT, cT, QT, QTc, KTall, Vall, grp, Sch, atch):
    ident, tri = const_tables()
    maskneg = ((1.0 - tri) * NEG).astype(np.float32)
    sel = np.zeros((NE, NE, 128), np.float32)
    for e in range(NE):
        sel[e, e, :] = 1.0
    d = dict(xT=xT_own, cT=cT, ident=ident, tri=tri, maskneg=maskneg, sel=sel.reshape(NE, NE * 128),
             QT=QT, KTall=KTall, Vall=Vall)
    for k in ("w_mod", "b_mod", "norm2_g", "w_out", "router_w1", "router_b1", "router_w2", "router_b2",
              "w_gate", "w_up", "w_down", "d_skip", "ssd_norm_g", "final_g"):
        d[k] = np.ascontiguousarray(W[k], dtype=np.float32)
    for sfx in grp:
        for k, v in grp[sfx].items():
            d[k + sfx] = v
        d["Sch" + sfx] = Sch[sfx]
        d["atch" + sfx] = atch[sfx]
    if not last:
        d["xcT"] = xcT
        d["QTc"] = QTc
    return d


_NC_CACHE = {}


def _get_nc(kind, T, l, flag):
    key = (kind, T, l, flag)
    if key not in _NC_CACHE:
        _NC_CACHE[key] = build_phase_a(T, l, flag) if kind == "a" else build_phase_b(T, l, flag)
    return _NC_CACHE[key]


def _run(nc, in_maps):
    res = run_bass_kernel_spmd(nc, in_maps, core_ids=list(range(len(in_maps))))
    return res.results


def forward(W, x, c, ctx, c_ctx, ncores_per_batch=4):
    B, L, _ = x.shape
    R = ncores_per_batch
    T = L // R
    cos, sin = rope_tables_host(L)
    xl = [np.ascontiguousarray(x[b]) for b in range(B)]
    xc = [np.ascontiguousarray(ctx[b]) for b in range(B)]
    xT_own = {}
    for l in range(2):
        last = l == 1
        cores = [(b, s) for b in range(B) for s in range(R)]
        ins_a = [host_inputs_a(W, l, xl[b], xc[b], c[b], c_ctx, s, T, cos, sin) for (b, s) in cores]
        ra = _run(_get_nc("a", T, l, not last), ins_a)
        ins_b = []
        for ci, (b, s) in enumerate(cores):
            rb = [ra[b * R + r] for r in range(R)]
            me = ra[ci]
            KTall = np.concatenate([me["KTc"]] + [q["KT"] for q in rb], axis=2)
            Vall = np.concatenate([me["Vc"]] + [q["V"] for q in rb], axis=0)
            grp = {"": {k: me[k] for k in ("zs", "xtok", "BCT", "atok", "ldtok")}}
            Sch = {"": np.zeros((2, 4, 64, 512), np.float32)}
            atch = {"": np.zeros((2, 4, H), np.float32)}
            chains = ([rb[r] for r in range(s - 1, -1, -1)], [rb[r] for r in range(s + 1, R)])
            for d in range(2):
                srcs = [(q["S"], q["atot"]) for q in chains[d]] + [(me["Sc"], me["atotc"])]
                for k, (S_, at_) in enumerate(srcs):
                    Sch[""][d, k] = pick_state(S_, d)
                    atch[""][d, k] = at_[0, d * 8:(d + 1) * 8]
            if not last:
                grp["c"] = {k: me[k + "c"] for k in ("zs", "xtok", "BCT", "atok", "ldtok")}
                Sch["c"] = np.zeros((2, 4, 64, 512), np.float32)
                atch["c"] = np.zeros((2, 4, H), np.float32)
            ins_b.append(host_inputs_b(W, l, last, T, s, ins_a[ci]["xT"], ins_a[ci]["xcT"], ins_a[ci]["cT"], me["QT"],
                                       me["QTc"], KTall, Vall, grp, Sch, atch))
        rbo = _run(_get_nc("b", T, l, last), ins_b)
        for b in range(B):
            outs = [rbo[b * R + r]["xout"].reshape(D, T).T for r in range(R)]
            xl[b] = np.ascontiguousarray(np.concatenate(outs, axis=0))
            if not last:
                xc[b] = np.ascontiguousarray(rbo[b * R]["xoutc"].reshape(D, CTX).T)
    return np.stack(xl).astype(np.float32)


def kernel(x, c, ctx, c_ctx, w_mod, b_mod, norm1_g, norm2_g, w_in, q_norm_g, w_qb, kv_norm_g, w_kvb,
           conv_w, conv_b, a_log, dt_bias, d_skip, ssd_norm_g, w_out, router_w1, router_b1,
           router_w2, router_b2, w_gate, w_up, w_down, final_g):
    W = dict(w_mod=w_mod, b_mod=b_mod, norm1_g=norm1_g, norm2_g=norm2_g, w_in=w_in, q_norm_g=q_norm_g, w_qb=w_qb,
             kv_norm_g=kv_norm_g, w_kvb=w_kvb, conv_w=conv_w, conv_b=conv_b, a_log=a_log, dt_bias=dt_bias,
             d_skip=d_skip, ssd_norm_g=ssd_norm_g, w_out=w_out, router_w1=router_w1, router_b1=router_b1,
             router_w2=router_w2, router_b2=router_b2, w_gate=w_gate, w_up=w_up, w_down=w_down, final_g=final_g)
    W = {k: np.asarray(v, dtype=np.float32) for k, v in W.items()}
    return forward_fused(W, np.asarray(x, np.float32), np.asarray(c, np.float32), np.asarray(ctx, np.float32),
                         np.asarray(c_ctx, np.float32))


CC_GROUPS = [[0, 1, 2, 3], [4, 5, 6, 7]]


def build_fused(T, R=4):
    nc = bass.Bass("TRN2", target_bir_lowering=False)
    es = contextlib.ExitStack()
    with es:
        kb = KB(nc, es)
        I = {}

        def inp(name, shape, dtype=F32):
            I[name] = kb.dram(name, shape, dtype, "ExternalInput")

        inp("xT", [KC, 128, T]); inp("xhT", [KC, 128, 4]); inp("hmask", [128, 4]); inp("xcT", [KC, 128, CTX])
        inp("cT", [128, KC, 2]); inp("ident", [128, 128]); inp("tri", [2, 128, 128])
        inp("maskneg", [2, 128, 128]); inp("sel", [NE, NE * 128]); inp("chm", [128, 2, R]); inp("hsel", [128, 2, R])
        inp("ropeC", [QK, T]); inp("ropeS", [QK, T])
        inp("w_mod", [2, D, 6 * D]); inp("b_mod", [2, 6 * D]); inp("norm1_g", [2, D]); inp("norm2_g", [2, D])
        inp("w_in", [2, D, IN_COLS]); inp("q_norm_g", [2, QL]); inp("w_qb", [2, QL, H * QK])
        inp("kv_norm_g", [2, KVL]); inp("w_kvb", [2, KVL, H * 128])
        inp("conv_w", [2, 5, 768]); inp("conv_b", [2, 768]); inp("a_log", [2, 16]); inp("dt_bias", [2, 16])
        inp("w_out", [2, D, D])
        inp("router_w1", [2, D, 4]); inp("router_b1", [2, 4]); inp("router_w2", [2, D, NE]); inp("router_b2", [2, NE])
        inp("w_gate", [2, NE, D, FF]); inp("w_up", [2, NE, D, FF]); inp("w_down", [2, NE, FF, D])
        inp("d_skip", [2, H]); inp("ssd_norm_g", [2, SSD_IN]); inp("final_g", [D])
        out_d = kb.dram("out", [KC, 128, T], F32, "ExternalOutput")

        cst = load_consts(kb, I)
        tri = [kb.sb([128, 128], F32, "tri%d" % d) for d in range(2)]
        maskneg = [kb.sb([128, 128], F32, "mneg%d" % d) for d in range(2)]
        for d in range(2):
            kb.dma(kb.sp, tri[d][:], I["tri"][d], tri[d], I["tri"])
            kb.dma(kb.sp, maskneg[d][:], I["maskneg"][d], maskneg[d], I["maskneg"])
        sel65 = kb.sb([VD + 1, 64], F32, "sel65")
        mset(kb, kb.dve, sel65[:], 0.0, [sel65])
        mset(kb, kb.dve, sel65[64:65, :], 1.0, [sel65])
        chm = kb.sb([128, 2, R], F32, "chm")
        kb.dma(kb.sp, chm[:], I["chm"][:, :, :], chm, I["chm"])
        hsel = kb.sb([128, 2, R], F32, "hsel")
        kb.dma(kb.sp, hsel[:], I["hsel"][:, :, :], hsel, I["hsel"])

        x_cur, xh_cur, xc_cur = I["xT"], I["xhT"], I["xcT"]
        for l in range(2):
            last = l == 1
            Il = dict(I)
            Il["xT"], Il["xhT"], Il["xcT"] = x_cur, xh_cur, xc_cur

            def scr(name, shape, dtype=F32, kind="Internal"):
                Il[name] = kb.dram("%s_L%d" % (name, l), shape, dtype, kind)
                return Il[name]

            for sfx, n in (("", T), ("c", CTX)):
                scr("QT" + sfx, [H, QK, n], BF16); scr("KT" + sfx, [H, QK, n], BF16); scr("V" + sfx, [n, 512], BF16)
                scr("zs" + sfx, [4, 128, n], BF16); scr("xtok" + sfx, [n, 640], BF16); scr("BCT" + sfx, [2, 128, n], BF16)
                scr("atok" + sfx, [n, 16]); scr("ldtok" + sfx, [n, 16]); scr("S" + sfx, [128, 1024]); scr("atot" + sfx, [1, 16])
            emit_phase_a(kb, Il, T, l, not last, cst, tri)
            KTg = [scr("KTg%d" % h, [R * QK, T], BF16) for h in range(H)]
            VCH = min(T, 1024)
            Vg = [scr("Vg%d" % c, [R * VCH, 512], BF16) for c in range(T // VCH)]
            Sg = scr("Sg", [R * 128, 1024]); atg = scr("atg", [R, 16])
            kb.collective(Il["S"], Il["S"].t[:, :], Sg, Sg.t[:, :], CC_GROUPS)
            kb.collective(Il["atot"], Il["atot"].t[:, :], atg, atg.t[:, :], CC_GROUPS)
            for h in range(H):
                kb.collective(Il["KT"], Il["KT"].t[h], KTg[h], KTg[h].t[:, :], CC_GROUPS)
            for c in range(T // VCH):
                kb.collective(Il["V"], Il["V"].t[c * VCH:(c + 1) * VCH, :], Vg[c], Vg[c].t[:, :], CC_GROUPS)

            def kv_loader(h, k_, v_, nk, Il=Il, KTg=KTg, Vg=Vg, VCH=VCH):
                kb.dma(kb.sp, k_[:, 0:CTX], Il["KTc"].t[h, :, :], k_, Il["KTc"])
                kb.dma(kb.sp, v_[:, 0:CTX // 128, 0:VD],
                       Il["Vc"].t[:, h * VD:(h + 1) * VD].rearrange("(t p) c -> p t c", p=128), v_, Il["Vc"])
                if nk == CTX:
                    return
                ntl = T // 128
                ncl = VCH // 128
                for r in range(R):
                    kb.dma(kb.sp, k_[:, CTX + r * T:CTX + (r + 1) * T], KTg[h].t[r * QK:(r + 1) * QK, :], k_, KTg[h])
                    for c in range(T // VCH):
                        for t0 in range(0, ncl, 16):
                            t1_ = min(ncl, t0 + 16)
                            kb.dma(kb.sp, v_[:, 2 + r * ntl + c * ncl + t0:2 + r * ntl + c * ncl + t1_, 0:VD],
                                   Vg[c].t[r * VCH + t0 * 128:r * VCH + t1_ * 128, h * VD:(h + 1) * VD]
                                   .rearrange("(t p) c -> p t c", p=128), v_, Vg[c])

            def state_loader(d, hst, hflat, ssrc, atb, Il=Il, Sg=Sg, atg=atg):
                def load_S(src, row0):
                    for g in range(2):
                        kb.dma(kb.sp, ssrc[:, g * 256:(g + 1) * 256],
                               src.t[row0 + g * 64:row0 + (g + 1) * 64, d * 512 + g * 256:d * 512 + (g + 1) * 256], ssrc, src)
                load_S(Il["Sc"], 0)
                cp(kb, kb.dve, hflat, ssrc[:], [ssrc], [hst])
                for r in (range(R) if d == 0 else range(R - 1, -1, -1)):
                    mcol = chm[0:64, d, r:r + 1]
                    kb.dma(kb.sp, atb[:], atg.t[r:r + 1, d * 8:(d + 1) * 8].broadcast_to([128, H]), atb, atg)
                    ts(kb, kb.dve, atb[:], atb[:], chm[:, d, r:r + 1], ALU.mult, [atb, chm], [atb])
                    act(kb, atb[:], atb[:], AF.Exp, [atb], [atb])
                    load_S(Sg, r * 128)
                    tt(kb, kb.dve, hst[:], hst[:], atb[0:64, :].unsqueeze(2).broadcast_to([64, H, 64]), ALU.mult, [hst, atb], [hst])
                    stt(kb, kb.dve, hflat, ssrc[:], mcol, hflat, ALU.mult, ALU.add, [ssrc, chm, hst], [hst])

            for sfx, n in ([("", T)] + ([] if last else [("c", CTX)])):
                scr("mix" + sfx, [KC, 128, n], BF16); scr("yfw" + sfx, [n // 128, 64, H * 128])
                scr("x1" + sfx, [KC, 128, n]); scr("h2" + sfx, [KC, 128, n], BF16)
                if sfx == "" and last:
                    Il["xout"] = out_d
                else:
                    scr("xout" + sfx, [KC, 128, n])
            emit_phase_b(kb, Il, T, l, last, cst, tri, maskneg, sel65,
                         loaders={"kv": kv_loader, "state": state_loader})
            if not last:
                xe = scr("xe", [128, KC * 4]); xeg = scr("xeg", [R * 128, KC * 4]); xh1 = scr("xh1", [KC, 128, 4])
                with kb.scope():
                    et = kb.sb([128, KC, 4], F32, "et")
                    kb.dma(kb.sp, et[:, :, 0:2], Il["xout"].t[:, :, 0:2].rearrange("k p c -> p k c"), et, Il["xout"])
                    kb.dma(kb.sp, et[:, :, 2:4], Il["xout"].t[:, :, T - 2:T].rearrange("k p c -> p k c"), et, Il["xout"])
                    kb.dma(kb.sp, xe.t[:, :], et[:].rearrange("p k c -> p (k c)"), xe, et)
                    kb.collective(xe, xe.t[:, :], xeg, xeg.t[:, :], CC_GROUPS)
                    eg = kb.sb([128, R, KC, 4], F32, "eg")
                    kb.dma(kb.sp, eg[:], xeg.t.rearrange("(r p) (k c) -> p r k c", p=128, c=4), eg, xeg)
                    xh = kb.sb([128, KC, 4], F32, "xh")
                    mset(kb, kb.dve, xh[:], 0.0, [xh])
                    for r in range(R):
                        stt(kb, kb.dve, xh[:, :, 0:2], eg[:, r, :, 2:4], hsel[:, 0, r:r + 1], xh[:, :, 0:2], ALU.mult, ALU.add,
                            [eg, hsel, xh], [xh])
                        stt(kb, kb.dve, xh[:, :, 2:4], eg[:, r, :, 0:2], hsel[:, 1, r:r + 1], xh[:, :, 2:4], ALU.mult, ALU.add,
                            [eg, hsel, xh], [xh])
                    kb.dma(kb.sp, xh1.t.rearrange("k p c -> p k c"), xh[:], xh1, xh)
                x_cur, xh_cur, xc_cur = Il["xout"], xh1, Il["xoutc"]
        kb.finish()
    return nc


def host_inputs_fused(W, x_b, ctx_b, c_b, c_ctx, s, T, R, cos, sin):
    d = host_inputs_a(W, 0, x_b, ctx_b, c_b, c_ctx, s, T, cos, sin)
    ident, tri = const_tables()
    d["maskneg"] = ((1.0 - tri) * NEG).astype(np.float32)
    sel = np.zeros((NE, NE, 128), np.float32)
    for e in range(NE):
        sel[e, e, :] = 1.0
    d["sel"] = sel.reshape(NE, NE * 128)
    chm = np.zeros((128, 2, R), np.float32)
    hs = np.zeros((128, 2, R), np.float32)
    for r in range(R):
        chm[:, 0, r] = 1.0 if r < s else 0.0
        chm[:, 1, r] = 1.0 if r > s else 0.0
        hs[:, 0, r] = 1.0 if r == s - 1 else 0.0
        hs[:, 1, r] = 1.0 if r == s + 1 else 0.0
    d["chm"] = chm
    d["hsel"] = hs
    for k in ("norm2_g", "w_out", "router_w1", "router_b1", "router_w2", "router_b2", "w_gate", "w_up", "w_down",
              "d_skip", "ssd_norm_g", "final_g"):
        d[k] = np.ascontiguousarray(W[k], dtype=np.float32)
    return d


def forward_fused(W, x, c, ctx, c_ctx, R=4):
    B, L, _ = x.shape
    T = L // R
    cos, sin = rope_tables_host(L)
    cores = [(b, s) for b in range(B) for s in range(R)]
    ins = [host_inputs_fused(W, x[b], ctx[b], c[b], c_ctx, s, T, R, cos, sin) for (b, s) in cores]
    key = ("fused", T)
    if key not in _NC_CACHE:
        _NC_CACHE[key] = build_fused(T, R)
    res = _run(_NC_CACHE[key], ins)
    out = np.empty((B, L, D), np.float32)
    for ci, (b, s) in enumerate(cores):
        out[b, s * T:(s + 1) * T] = res[ci]["out"].reshape(D, T).T
    return out
```
